# Optimizing a Trainium2 kernel written in Bass

```python
import jax, jax.numpy as jnp
from jax import lax
import numpy as np

D_MODEL = 1024
BATCH = 8
SEQ = 4096
DEPTH = 4

CHUNK = 64
MEM_LEN = 256
Q_BLOCK = 128
N_MIXERS = 2
DA_HEADS = 6
DA_HEAD_DIM = 64
DA_WIDTH = DA_HEADS * 2 * DA_HEAD_DIM
HG_HEADS = 6
HG_KEY_DIM = 128
HG_VAL_DIM = 128
HG_KEY_WIDTH = HG_HEADS * HG_KEY_DIM
HG_VAL_WIDTH = HG_HEADS * HG_VAL_DIM
MEM_HEADS = 4
MEM_HEAD_DIM = 64
MEM_WIDTH = MEM_HEADS * MEM_HEAD_DIM
MIX_WIDTH = DA_WIDTH + MEM_WIDTH
D_FF_DENSE = 2816
N_EXPERTS = 8
TOP_K = 2
D_FF_EXPERT = 3584
EPS = 1e-6
MASK_VALUE = -1e30
N_A = (DEPTH + 1) // 2
N_B = DEPTH // 2

kernel_name = "hybrid_diffattn_hgrn2_moe_trunk"

F32 = jnp.float32


def rms_norm(x, g):
    xf = x.astype(F32)
    y = xf * lax.rsqrt(jnp.mean(xf * xf, axis=-1, keepdims=True) + EPS)
    return (y * g.astype(F32)).astype(x.dtype)


def alibi_slopes(n):
    return jnp.asarray(2.0 ** (-8.0 * np.arange(1, n + 1) / n), F32)


def memory_attention(q_mem, mem, mem_norm_g, w_mem_kv):
    B, S, _ = q_mem.shape
    kv = rms_norm(mem, mem_norm_g) @ w_mem_kv
    k, v = jnp.split(kv, 2, axis=-1)
    q = q_mem.reshape(B, S, MEM_HEADS, MEM_HEAD_DIM)
    k = k.reshape(B, -1, MEM_HEADS, MEM_HEAD_DIM)
    v = v.reshape(B, -1, MEM_HEADS, MEM_HEAD_DIM)
    s = jnp.einsum('bshd,bmhd->bhsm', q, k).astype(F32) * (MEM_HEAD_DIM ** -0.5)
    p = jax.nn.softmax(s, axis=-1).astype(v.dtype)
    o = jnp.einsum('bhsm,bmhd->bshd', p, v)
    return o.reshape(B, S, MEM_WIDTH)


def diff_attention(q, k, v, lam, lam_init, subln_g):
    B, S = q.shape[:2]
    nb = S // Q_BLOCK
    scale = DA_HEAD_DIM ** -0.5
    slopes = alibi_slopes(DA_HEADS)
    kpos = jnp.arange(S)
    kchunk = kpos // CHUNK
    qb = q.reshape(B, nb, Q_BLOCK, DA_HEADS, 2, DA_HEAD_DIM).transpose(1, 0, 2, 3, 4, 5)
    starts = jnp.arange(nb) * Q_BLOCK

    def block(args):
        q_blk, start = args
        qpos = start + jnp.arange(Q_BLOCK)
        dist = jnp.abs(qpos[:, None] - kpos[None, :]).astype(F32)
        allowed = kchunk[None, :] <= (qpos // CHUNK)[:, None]
        bias = jnp.where(allowed[None], -slopes[:, None, None] * dist[None], MASK_VALUE)
        s = jnp.einsum('bqhmd,bkhmd->bhmqk', q_blk, k).astype(F32) * scale + bias[None, :, None]
        p = jax.nn.softmax(s, axis=-1)
        a = (p[:, :, 0] - lam * p[:, :, 1]).astype(v.dtype)
        return jnp.einsum('bhqk,bkhe->bqhe', a, v)

    o = lax.map(block, (qb, starts))
    o = o.transpose(1, 0, 2, 3, 4).reshape(B, S, DA_HEADS, 2 * DA_HEAD_DIM)
    o = rms_norm(o, subln_g) * (1.0 - lam_init)
    return o.reshape(B, S, DA_WIDTH)


def diff_attention_mixer(h, mem, w_in, lq1, lk1, lq2, lk2, subln_g, mem_norm_g, w_mem_kv, w_out, lam_init):
    B, S, _ = h.shape
    proj = h @ w_in
    q, k, v, qm = jnp.split(proj, [DA_WIDTH, 2 * DA_WIDTH, 3 * DA_WIDTH], axis=-1)
    q = q.reshape(B, S, DA_HEADS, 2, DA_HEAD_DIM)
    k = k.reshape(B, S, DA_HEADS, 2, DA_HEAD_DIM)
    v = v.reshape(B, S, DA_HEADS, 2 * DA_HEAD_DIM)
    lam = (jnp.exp(jnp.sum(lq1.astype(F32) * lk1.astype(F32)))
           - jnp.exp(jnp.sum(lq2.astype(F32) * lk2.astype(F32))) + lam_init)
    o = diff_attention(q, k, v, lam, lam_init, subln_g).astype(h.dtype)
    om = memory_attention(qm, mem, mem_norm_g, w_mem_kv).astype(h.dtype)
    return jnp.concatenate([o, om], axis=-1) @ w_out


def hgrn2_chunkwise(q, k, v, log_f):
    B, S, H, dk = q.shape
    dv = v.shape[-1]
    nc = S // CHUNK

    def to_chunks(t):
        return t.astype(F32).reshape(B, nc, CHUNK, H, t.shape[-1]).transpose(1, 0, 3, 2, 4)

    xs = (to_chunks(q), to_chunks(k), to_chunks(v), to_chunks(log_f))
    causal = jnp.tril(jnp.ones((CHUNK, CHUNK), bool))[None, None, :, :, None]

    def step(state, inp):
        q_, k_, v_, g_ = inp
        b = jnp.cumsum(g_, axis=2)
        b_last = b[:, :, -1:, :]
        diff = jnp.where(causal, b[:, :, :, None, :] - b[:, :, None, :, :], 0.0)
        decay = jnp.where(causal, jnp.exp(diff), 0.0)
        attn = jnp.einsum('bhtsk,bhsk->bhts', q_[:, :, :, None, :] * decay, k_)
        o = (jnp.einsum('bhts,bhsv->bhtv', attn, v_)
             + jnp.einsum('bhtk,bhkv->bhtv', q_ * jnp.exp(b), state))
        state = (jnp.exp(b_last)[:, :, 0, :, None] * state
                 + jnp.einsum('bhsk,bhsv->bhkv', k_ * jnp.exp(b_last - b), v_))
        return state, o

    state0 = jnp.zeros((B, H, dk, dv), F32)
    _, o = lax.scan(step, state0, xs)
    return o.transpose(1, 0, 3, 2, 4).reshape(B, S, H, dv)


def hgrn2_mixer(h, mem, w_in, lb, out_norm_g, mem_norm_g, w_mem_kv, w_out):
    B, S, _ = h.shape
    proj = h @ w_in
    q, fz, i, g, qm = jnp.split(
        proj, [HG_KEY_WIDTH, 2 * HG_KEY_WIDTH, 2 * HG_KEY_WIDTH + HG_VAL_WIDTH,
               2 * HG_KEY_WIDTH + 2 * HG_VAL_WIDTH], axis=-1)
    fz32 = fz.astype(F32)
    f = lb + (1.0 - lb) * jax.nn.sigmoid(fz32)
    log_f = jnp.log(f)
    k = (1.0 - lb) * jax.nn.sigmoid(-fz32)
    heads = lambda t: t.reshape(B, S, HG_HEADS, -1)
    o = hgrn2_chunkwise(heads(q), heads(k), heads(i), heads(log_f))
    o = rms_norm(o, out_norm_g) * jax.nn.silu(heads(g).astype(F32))
    o = o.reshape(B, S, HG_VAL_WIDTH).astype(h.dtype)
    om = memory_attention(qm, mem, mem_norm_g, w_mem_kv).astype(h.dtype)
    return jnp.concatenate([o, om], axis=-1) @ w_out


def dense_swiglu(h, w_gate_up, w_down):
    gt, up = jnp.split(h @ w_gate_up, 2, axis=-1)
    return (jax.nn.silu(gt) * up) @ w_down


def moe_swiglu(h, w_router, w_gate_up, w_down):
    B, S, D = h.shape
    t = h.reshape(-1, D)
    logits = (t @ w_router).astype(F32)
    top_val, top_idx = lax.top_k(logits, TOP_K)
    top_w = jax.nn.softmax(top_val, axis=-1)
    combine = jnp.sum(jax.nn.one_hot(top_idx, N_EXPERTS, dtype=F32) * top_w[..., None], axis=1)
    out = jnp.zeros_like(t)
    for e in range(N_EXPERTS):
        gt, up = jnp.split(t @ w_gate_up[e], 2, axis=-1)
        y = (jax.nn.silu(gt) * up) @ w_down[e]
        out = out + combine[:, e:e + 1].astype(t.dtype) * y
    return out.reshape(B, S, D)


def setup_inputs(seed: int = 0) -> dict:
    key = jax.random.key(seed)
    ks = iter(jax.random.split(key, 40))
    D = D_MODEL
    res = (2.0 * DEPTH) ** -0.5

    def nrm(shape, scale):
        return jax.random.normal(next(ks), shape, F32) * scale

    def gain(shape):
        return 1.0 + 0.02 * jax.random.normal(next(ks), shape, F32)

    return {
        "x": nrm((BATCH, SEQ, D), 1.0),
        "mem": nrm((BATCH, MEM_LEN, D), 1.0),
        "a_norm_mix": gain((N_A, D)),
        "a_w_in": nrm((N_A, D, 3 * DA_WIDTH + MEM_WIDTH), D ** -0.5),
        "a_lam_q1": nrm((N_A, DA_HEAD_DIM), 0.1),
        "a_lam_k1": nrm((N_A, DA_HEAD_DIM), 0.1),
        "a_lam_q2": nrm((N_A, DA_HEAD_DIM), 0.1),
        "a_lam_k2": nrm((N_A, DA_HEAD_DIM), 0.1),
        "a_subln": gain((N_A, 2 * DA_HEAD_DIM)),
        "a_mem_norm": gain((N_A, D)),
        "a_w_mem_kv": nrm((N_A, D, 2 * MEM_WIDTH), D ** -0.5),
        "a_w_out": nrm((N_A, MIX_WIDTH, D), MIX_WIDTH ** -0.5 * res),
        "b_norm_mix": gain((N_B, D)),
        "b_w_in": nrm((N_B, D, 2 * HG_KEY_WIDTH + 2 * HG_VAL_WIDTH + MEM_WIDTH), D ** -0.5),
        "b_lb_logits": nrm((N_B, HG_KEY_WIDTH), 0.5),
        "b_out_norm": gain((N_B, HG_VAL_DIM)),
        "b_mem_norm": gain((N_B, D)),
        "b_w_mem_kv": nrm((N_B, D, 2 * MEM_WIDTH), D ** -0.5),
        "b_w_out": nrm((N_B, HG_VAL_WIDTH + MEM_WIDTH, D), (HG_VAL_WIDTH + MEM_WIDTH) ** -0.5 * res),
        "dense_norm": gain((N_A, D)),
        "dense_w_gate_up": nrm((N_A, D, 2 * D_FF_DENSE), D ** -0.5),
        "dense_w_down": nrm((N_A, D_FF_DENSE, D), D_FF_DENSE ** -0.5 * res),
        "moe_norm": gain((N_B, D)),
        "moe_router": nrm((N_B, D, N_EXPERTS), D ** -0.5),
        "moe_w_gate_up": nrm((N_B, N_EXPERTS, D, 2 * D_FF_EXPERT), D ** -0.5),
        "moe_w_down": nrm((N_B, N_EXPERTS, D_FF_EXPERT, D), D_FF_EXPERT ** -0.5 * res),
        "final_norm": gain((D,)),
    }


def reference(x, mem,
              a_norm_mix, a_w_in, a_lam_q1, a_lam_k1, a_lam_q2, a_lam_k2, a_subln, a_mem_norm, a_w_mem_kv, a_w_out,
              b_norm_mix, b_w_in, b_lb_logits, b_out_norm, b_mem_norm, b_w_mem_kv, b_w_out,
              dense_norm, dense_w_gate_up, dense_w_down,
              moe_norm, moe_router, moe_w_gate_up, moe_w_down,
              final_norm):
    lb_p = jax.nn.softmax(b_lb_logits.astype(F32), axis=0)
    lb_all = jnp.cumsum(lb_p, axis=0) - lb_p[0:1]
    for i in range(DEPTH):
        if i % N_MIXERS == 0:
            j = i // N_MIXERS
            lam_init = 0.8 - 0.6 * float(np.exp(-0.3 * i))
            h = rms_norm(x, a_norm_mix[j])
            x = x + diff_attention_mixer(h, mem, a_w_in[j], a_lam_q1[j], a_lam_k1[j], a_lam_q2[j], a_lam_k2[j],
                                         a_subln[j], a_mem_norm[j], a_w_mem_kv[j], a_w_out[j], lam_init)
        else:
            j = i // N_MIXERS
            h = rms_norm(x, b_norm_mix[j])
            x = x + hgrn2_mixer(h, mem, b_w_in[j], lb_all[j], b_out_norm[j], b_mem_norm[j],
                                b_w_mem_kv[j], b_w_out[j])
        if i % 2 == 0:
            j = i // 2
            x = x + dense_swiglu(rms_norm(x, dense_norm[j]), dense_w_gate_up[j], dense_w_down[j])
        else:
            j = i // 2
            x = x + moe_swiglu(rms_norm(x, moe_norm[j]), moe_router[j], moe_w_gate_up[j], moe_w_down[j])
    return rms_norm(x, final_norm)
```

```python
import numpy as np
import concourse.bass as bass
import concourse.mybir as mybir
from concourse.bass_utils import run_bass_kernel_spmd
from contextlib import ExitStack

F32 = mybir.dt.float32
BF16 = mybir.dt.bfloat16
ALU = mybir.AluOpType
AF = mybir.ActivationFunctionType
AX = mybir.AxisListType

S = 4096
D = 1024
NT = 32
EPS = 1e-6
DEPTH = 4
DA_W = 768
HG_W = 768
MEM_W = 256
DFF_D = 2816
DFF_E = 3584
NEXP = 8
MEM_LEN = 256

ENGS = ['pe', 'act', 'dve', 'pool', 'sp']
DMA_POOL = {'sp': 24, 'pool': 24, 'act': 4}


class Op:
    __slots__ = ('eng', 'fn', 'deps', 'need', 'sig', 'is_dma', 'dsem', 'dval', 'uid')


class Prog:
    def __init__(self, nc, stack, strict=True):
        self.nc = nc
        self.stack = stack
        self.strict = strict
        self.ops = {e: [] for e in ENGS}
        self.lastw = {}
        self.reads = {}
        self.uid = 0
        self.esem = {e: stack.enter_context(nc.semaphore('s_' + e)) for e in ENGS}
        self.sigc = {e: 0 for e in ENGS}
        self.dsems = {}
        self.dcur = {}
        self.dlast = {}
        self.dfence = {}
        for q, n in DMA_POOL.items():
            self.dsems[q] = [stack.enter_context(nc.semaphore('d_%s%d' % (q, i))) for i in range(n)]
            self.dcur[q] = 0
            self.dlast[q] = [None] * n
            self.dfence[q] = [0] * n
        self.efence = {e: 0 for e in ENGS}
        self.ntile = 0
        self.nflush = 0

    def sb(self, st, shape, dtype, name=None):
        self.ntile += 1
        name = (name or 't') + '_%d' % self.ntile
        return st.enter_context(self.nc.sbuf_tensor(name, list(shape), dtype))

    def ps(self, st, shape, dtype=F32, name=None):
        self.ntile += 1
        name = (name or 'p') + '_%d' % self.ntile
        return st.enter_context(self.nc.psum_tensor(name, list(shape), dtype))

    def add(self, eng, fn, reads=(), writes=(), dma=False):
        op = Op()
        op.eng = eng
        op.fn = fn
        op.need = False
        op.sig = None
        op.is_dma = dma
        op.uid = self.uid
        self.uid += 1
        deps = []
        for k in reads:
            w = self.lastw.get(k)
            if w is not None:
                deps.append((w, 'raw'))
        for k in writes:
            w = self.lastw.get(k)
            if w is not None:
                deps.append((w, 'waw'))
            rd = self.reads.get(k)
            if rd:
                for r in rd.values():
                    deps.append((r, 'war'))
        fdeps = []
        seen = set()
        for d, kind in deps:
            if d.uid in seen:
                continue
            if (not d.is_dma) and (not dma) and d.eng == eng:
                if eng == 'pe' or kind == 'war' or not self.strict:
                    continue
            seen.add(d.uid)
            fdeps.append(d)
        if dma:
            q = eng
            i = self.dcur[q]
            self.dcur[q] = (i + 1) % len(self.dsems[q])
            prev = self.dlast[q][i]
            op.dsem = self.dsems[q][i]
            op.dval = (prev.dval if prev is not None else 0) + 16
            if prev is not None and prev.uid not in seen and prev.dval > self.dfence[q][i]:
                fdeps.append(prev)
                seen.add(prev.uid)
            self.dlast[q][i] = op
        for d in fdeps:
            d.need = True
        op.deps = fdeps
        rk = ('dma', op.uid) if dma else eng
        for k in writes:
            self.lastw[k] = op
            self.reads[k] = {}
        for k in reads:
            self.reads.setdefault(k, {})[rk] = op
        self.ops[eng].append(op)
        return op

    def flush(self):
        for e in ENGS:
            comp = [op for op in self.ops[e] if not op.is_dma and op.fn is not None]
            if comp:
                comp[-1].need = True
            c = self.sigc[e]
            for op in self.ops[e]:
                if op.need and not op.is_dma:
                    c += 1
                    op.sig = c
            self.sigc[e] = c
        efence = dict(self.efence)
        dfence = {q: list(v) for q, v in self.dfence.items()}

        def run(e, engobj):
            waited = {}
            for e2 in ENGS:
                if efence[e2] > 0:
                    engobj.wait_ge(self.esem[e2], efence[e2])
                    waited[id(self.esem[e2])] = efence[e2]
            for q in dfence:
                for i, v in enumerate(dfence[q]):
                    if v > 0:
                        engobj.wait_ge(self.dsems[q][i], v)
                        waited[id(self.dsems[q][i])] = v
            for op in self.ops[e]:
                for d in op.deps:
                    if d.is_dma:
                        sem, val = d.dsem, d.dval
                    else:
                        sem, val = self.esem[d.eng], d.sig
                    key = id(sem)
                    if waited.get(key, 0) < val:
                        engobj.wait_ge(sem, val)
                        waited[key] = val
                ins = op.fn(engobj)
                if op.is_dma:
                    ins.then_inc(op.dsem, 16)
                elif op.need:
                    ins.then_inc(self.esem[e], 1)

        with self.nc.Block() as block:
            @block.tensor
            def _(t):
                run('pe', t)

            @block.scalar
            def _(t):
                run('act', t)

            @block.vector
            def _(t):
                run('dve', t)

            @block.gpsimd
            def _(t):
                run('pool', t)

            @block.sync
            def _(t):
                run('sp', t)

        for e in ENGS:
            self.efence[e] = self.sigc[e]
            self.ops[e] = []
        for q in self.dlast:
            for i, op in enumerate(self.dlast[q]):
                if op is not None:
                    self.dfence[q][i] = op.dval
        self.lastw = {}
        self.reads = {}
        self.nflush += 1

    def dma(self, q, out, in_, reads=(), writes=(), cast=False, slow=False):
        if slow:
            fn = lambda e, o=out, i=in_: e.dma_start(out=o, in_=i, allow_slow_non_contiguous=True)
        elif cast:
            fn = lambda e, o=out, i=in_: e.dma_start(out=o, in_=i, max_dma_last_dim=4096)
        else:
            fn = lambda e, o=out, i=in_: e.dma_start(out=o, in_=i)
        return self.add(q, fn, reads, writes, dma=True)

    def mmg(self, mms, reads, writes):
        mms = list(mms)

        def fn(e, mms=mms):
            ins = None
            for (o, l, r, s0, s1) in mms:
                ins = e.matmul(o, l, r, start=s0, stop=s1)
            return ins
        return self.add('pe', fn, reads, writes)

    def trg(self, trs, reads, writes):
        trs = list(trs)

        def fn(e, trs=trs):
            ins = None
            for (o, i, idn) in trs:
                ins = e.transpose(o, i, idn)
            return ins
        return self.add('pe', fn, reads, writes)

    def act(self, out, in_, func, reads, writes, bias=None, scale=None, accum=None):
        kw = {}
        if bias is not None:
            kw['bias'] = bias
        if scale is not None:
            kw['scale'] = scale
        if accum is not None:
            kw['accum_out'] = accum
        return self.add('act', lambda e, o=out, i=in_, f=func, kw=kw: e.activation(out=o, in_=i, func=f, **kw), reads, writes)

    def tt(self, eng, out, in0, in1, op, reads, writes):
        return self.add(eng, lambda e, o=out, a=in0, b=in1, p=op: e.tensor_tensor(out=o, in0=a, in1=b, op=p), reads, writes)

    def ts(self, eng, out, in0, s1, s2, op0, op1, reads, writes):
        if op1 is None:
            return self.add(eng, lambda e, o=out, a=in0, s1=s1, p0=op0: e.tensor_scalar(out=o, in0=a, scalar1=s1, scalar2=None, op0=p0), reads, writes)
        return self.add(eng, lambda e, o=out, a=in0, s1=s1, s2=s2, p0=op0, p1=op1: e.tensor_scalar(out=o, in0=a, scalar1=s1, scalar2=s2, op0=p0, op1=p1), reads, writes)

    def stt(self, out, in0, scalar, in1, op0, op1, reads, writes):
        return self.add('dve', lambda e, o=out, a=in0, s=scalar, b=in1, p0=op0, p1=op1: e.scalar_tensor_tensor(out=o, in0=a, scalar=s, in1=b, op0=p0, op1=p1), reads, writes)

    def copy(self, eng, out, in_, reads, writes):
        if eng == 'act':
            return self.add('act', lambda e, o=out, i=in_: e.copy(out=o, in_=i), reads, writes)
        return self.add(eng, lambda e, o=out, i=in_: e.tensor_copy(out=o, in_=i), reads, writes)

    def memset(self, eng, ap, val, writes):
        return self.add(eng, lambda e, a=ap, v=val: e.memset(a, v), (), writes)

    def recip(self, out, in_, reads, writes):
        return self.add('dve', lambda e, o=out, i=in_: e.reciprocal(out=o, in_=i), reads, writes)

    def reduce(self, out, in_, op, reads, writes):
        return self.add('dve', lambda e, o=out, i=in_, p=op: e.tensor_reduce(out=o, in_=i, axis=AX.X, op=p), reads, writes)

    def scan(self, out, d0, d1, reads, writes):
        return self.add('dve', lambda e, o=out, a=d0, b=d1: e.tensor_tensor_scan(out=o, data0=a, data1=b, initial=0.0, op0=ALU.mult, op1=ALU.add), reads, writes)


class K:
    pass


def wview(ap2d):
    return ap2d.rearrange("(c p) n -> p c n", p=128)


def norm_rows(P, k, st, x_ap, gbc, hb_out, tag, want_f32=None):
    t = k.nt[tag]
    P.act(t['junk'][:], x_ap, AF.Square, reads=[tag + 'x'], writes=[tag + 'junk', tag + 'ssq'], accum=t['ssq'][:])
    P.ts('dve', t['ms'][:], t['ssq'][:], 1.0 / D, EPS, ALU.mult, ALU.add, reads=[tag + 'ssq'], writes=[tag + 'ms'])
    P.tt('pool', t['rstd'][:], t['ms'][:], k.neghalf[:], ALU.pow, reads=[tag + 'ms'], writes=[tag + 'rstd'])
    if want_f32 is not None:
        P.stt(want_f32, x_ap, t['rstd'][:, 0:1], gbc, ALU.mult, ALU.mult, reads=[tag + 'x', tag + 'rstd', 'gbc'], writes=[tag + 'hf'])
        P.copy('act', hb_out, want_f32, reads=[tag + 'hf'], writes=[tag + 'hb'])
    else:
        P.stt(hb_out, x_ap, t['rstd'][:, 0:1], gbc, ALU.mult, ALU.mult, reads=[tag + 'x', tag + 'rstd', 'gbc'], writes=[tag + 'hb'])


def alloc_norm_tmps(P, k, st, tags):
    k.nt = {}
    for tag in tags:
        k.nt[tag] = {
            'junk': P.sb(st, [128, D], F32, 'junk'),
            'ssq': P.sb(st, [128, 1], F32, 'ssq'),
            'ms': P.sb(st, [128, 1], F32, 'ms'),
            'rstd': P.sb(st, [128, 1], F32, 'rstd'),
        }


def phase_hT(P, k, x_src, gain_row, hT):
    with ExitStack() as st:
        gbc = P.sb(st, [128, D], F32, 'gbc')
        xt = [P.sb(st, [128, D], F32, 'xt') for _ in range(2)]
        hb = [P.sb(st, [128, D], BF16, 'hb') for _ in range(2)]
        pt = [P.ps(st, [128, 8, 128], BF16, 'pt') for _ in range(2)]
        alloc_norm_tmps(P, k, st, ['n0', 'n1'])
        P.dma('sp', gbc[:], gain_row.partition_broadcast(128), writes=['gbc'])
        for tt in range(NT):
            b = tt % 2
            tag = 'n%d' % b
            P.dma('sp', xt[b][:], x_src[tt * 128:(tt + 1) * 128, :], writes=[tag + 'x'])
            norm_rows(P, k, st, xt[b][:], gbc[:], hb[b][:], tag)
            P.trg([(pt[b][:, c, :], hb[b][:, c * 128:(c + 1) * 128], k.ident[:]) for c in range(8)],
                  reads=[tag + 'hb'], writes=[tag + 'pt'])
            P.copy('act' if tt % 2 else 'dve', hT[:, :, tt * 128:(tt + 1) * 128], pt[b][:], reads=[tag + 'pt'], writes=[('hT', tt)])
        P.flush()


def phase_memkv(P, k, mem_ap, gain_row, wkv_ap, kmT, vm):
    with ExitStack() as st:
        gbc = P.sb(st, [128, D], F32, 'gbc')
        xt = [P.sb(st, [128, D], F32, 'xt') for _ in range(2)]
        hb = [P.sb(st, [128, D], BF16, 'hb') for _ in range(2)]
        pt = [P.ps(st, [128, 8, 128], BF16, 'pt') for _ in range(2)]
        memT = P.sb(st, [128, 8, MEM_LEN], BF16, 'memT')
        wkv = P.sb(st, [128, 8, 512], BF16, 'wkv')
        pk = P.ps(st, [128, 256], F32, 'pk')
        alloc_norm_tmps(P, k, st, ['n0', 'n1'])
        P.dma('sp', gbc[:], gain_row.partition_broadcast(128), writes=['gbc'])
        P.dma('pool', wkv[:], wview(wkv_ap), writes=['wkv'], cast=True)
        P.memset('pool', vm[:], 1.0, writes=['vm'])
        for mt in range(2):
            tag = 'n%d' % mt
            P.dma('sp', xt[mt][:], mem_ap[mt * 128:(mt + 1) * 128, :], writes=[tag + 'x'])
            norm_rows(P, k, st, xt[mt][:], gbc[:], hb[mt][:], tag)
            P.trg([(pt[mt][:, c, :], hb[mt][:, c * 128:(c + 1) * 128], k.ident[:]) for c in range(8)],
                  reads=[tag + 'hb'], writes=[tag + 'pt'])
            P.copy('dve', memT[:, :, mt * 128:(mt + 1) * 128], pt[mt][:], reads=[tag + 'pt'], writes=[('memT', mt)])
        for p in range(2):
            P.mmg([(pk[:], wkv[:, dc, p * 128:(p + 1) * 128], memT[:, dc, :], dc == 0, dc == 7) for dc in range(8)],
                  reads=['wkv', ('memT', 0), ('memT', 1)], writes=['pk'])
            P.copy('dve', kmT[:, p, :], pk[:], reads=['pk'], writes=[('kmT', p)])
        for mt in range(2):
            P.mmg([(pk[:], memT[:, dc, mt * 128:(mt + 1) * 128], wkv[:, dc, 256:512], dc == 0, dc == 7) for dc in range(8)],
                  reads=['wkv', ('memT', 0), ('memT', 1)], writes=['pk'])
            P.copy('dve', vm[:, mt, :, 0:64], pk[:].rearrange("p (h d) -> p h d", h=4), reads=['pk', 'vm'], writes=[('vm', mt)])
        P.flush()


def phase_memattn(P, k, hT, w_in_ap, col0, kmT, vm, o_s):
    with ExitStack() as st:
        wqm = P.sb(st, [128, 8, 256], BF16, 'wqm')
        qmT = P.sb(st, [128, 2, S], BF16, 'qmT')
        pq = [P.ps(st, [128, 512], F32, 'pq') for _ in range(2)]
        sc = [P.ps(st, [128, 512], F32, 'sc') for _ in range(2)]
        pacc = [P.ps(st, [128, 512], F32, 'pacc') for _ in range(4)]
        pT = [P.sb(st, [128, 512], BF16, 'pT') for _ in range(2)]
        rr = [P.sb(st, [128, 1], F32, 'rr') for _ in range(4)]
        omb = [P.sb(st, [128, 4, 256], BF16, 'omb') for _ in range(2)]
        P.dma('pool', wqm[:], wview(w_in_ap)[:, :, col0:col0 + 256], writes=['wqm'], cast=True)
        for tb in range(8):
            for p in range(2):
                b = (tb * 2 + p) % 2
                P.mmg([(pq[b][:], wqm[:, dc, p * 128:(p + 1) * 128], hT[:, dc, tb * 512:(tb + 1) * 512], dc == 0, dc == 7) for dc in range(8)],
                      reads=['wqm'], writes=[('pq', b)])
                P.act(qmT[:, p, tb * 512:(tb + 1) * 512], pq[b][:], AF.Identity, reads=[('pq', b)], writes=[('qmT', tb, p)], scale=0.125)
        cnt = 0
        for tb in range(8):
            ob = omb[tb % 2]
            for hm in range(4):
                p = hm // 2
                base = (hm % 2) * 64
                for mt in range(2):
                    b = cnt % 2
                    cnt += 1
                    P.mmg([(sc[b][:], kmT[base:base + 64, p, mt * 128:(mt + 1) * 128], qmT[base:base + 64, p, tb * 512:(tb + 1) * 512], True, True)],
                          reads=[('qmT', tb, p)], writes=[('sc', b)])
                    P.act(pT[b][:], sc[b][:], AF.Exp, reads=[('sc', b)], writes=[('pT', b)])
                    P.mmg([(pacc[qs][:, 0:65], pT[b][:, qs * 128:(qs + 1) * 128], vm[:, mt, hm, :], mt == 0, mt == 1) for qs in range(4)],
                          reads=[('pT', b)], writes=[('pacc', qs) for qs in range(4)])
                for qs in range(4):
                    P.recip(rr[qs][:], pacc[qs][:, 64:65], reads=[('pacc', qs)], writes=[('rr', qs)])
                    P.act(ob[:, qs, hm * 64:(hm + 1) * 64], pacc[qs][:, 0:64], AF.Identity, reads=[('pacc', qs), ('rr', qs)],
                          writes=[('omb', tb % 2, qs)], scale=rr[qs][:, 0:1])
            for qs in range(4):
                tt = tb * 4 + qs
                P.dma('sp', o_s[tt * 128:(tt + 1) * 128, 768:1024], ob[:, qs, :], reads=[('omb', tb % 2, qs)], writes=[('o_s', tt, 'm')])
        P.flush()


def phase_outproj(P, k, o_s, w_out_ap, x_src, x_dst):
    with ExitStack() as st:
        wo = P.sb(st, [128, 8, D], BF16, 'wo')
        ot = [P.sb(st, [128, D], BF16, 'ot') for _ in range(2)]
        oT = [P.sb(st, [128, 8, 128], BF16, 'oT') for _ in range(2)]
        xt = [P.sb(st, [128, D], F32, 'xt') for _ in range(2)]
        xn = [P.sb(st, [128, D], F32, 'xn') for _ in range(2)]
        pt = [P.ps(st, [128, 8, 128], BF16, 'pt') for _ in range(2)]
        po = [P.ps(st, [128, 512], F32, 'po') for _ in range(4)]
        P.dma('pool', wo[:], wview(w_out_ap), writes=['wo'], cast=True)
        for tt in range(NT):
            b = tt % 2
            P.dma('sp', ot[b][:], o_s[tt * 128:(tt + 1) * 128, :], writes=[('ot', b)])
            P.dma('sp', xt[b][:], x_src[tt * 128:(tt + 1) * 128, :], writes=[('xt', b)])
            P.trg([(pt[b][:, c, :], ot[b][:, c * 128:(c + 1) * 128], k.ident[:]) for c in range(8)],
                  reads=[('ot', b)], writes=[('pt', b)])
            P.copy('act', oT[b][:], pt[b][:], reads=[('pt', b)], writes=[('oT', b)])
            for hh in range(2):
                pb = b * 2 + hh
                P.mmg([(po[pb][:], oT[b][:, fc, :], wo[:, fc, hh * 512:(hh + 1) * 512], fc == 0, fc == 7) for fc in range(8)],
                      reads=[('oT', b), 'wo'], writes=[('po', pb)])
                P.tt('dve', xn[b][:, hh * 512:(hh + 1) * 512], po[pb][:], xt[b][:, hh * 512:(hh + 1) * 512], ALU.add,
                     reads=[('po', pb), ('xt', b)], writes=[('xn', b, hh)])
            P.dma('sp', x_dst[tt * 128:(tt + 1) * 128, :], xn[b][:], reads=[('xn', b, 0), ('xn', b, 1)], writes=[('xd', tt)])
        P.flush()


def phase_diffattn(P, k, hT, j, lam_init, o_s):
    inp = k.inp
    w_in = wview(inp['a_w_in'][j])
    with ExitStack() as st:
        wqkv = [P.sb(st, [128, 8, 384], BF16, 'wqkv') for _ in range(2)]
        qT = [P.sb(st, [128, 2, S], BF16, 'qT') for _ in range(2)]
        kT = [P.sb(st, [128, 2, S], BF16, 'kT') for _ in range(2)]
        va = [P.sb(st, [128, NT, 129], BF16, 'va') for _ in range(2)]
        dmask = P.sb(st, [128, 6, 128], BF16, 'dmask')
        cbias = P.sb(st, [128, 6 * 35], F32, 'cbias')
        gsub = P.sb(st, [128, 128], F32, 'gsub')
        lv = [P.sb(st, [128, 64], F32, 'lv') for _ in range(4)]
        lt = P.sb(st, [128, 64], F32, 'lt')
        ls = [P.sb(st, [128, 1], F32, 'ls') for _ in range(2)]
        neglam = P.sb(st, [128, 1], F32, 'neglam')
        clam = P.sb(st, [128, 2], F32, 'clam')
        pT = [P.sb(st, [128, 512], BF16, 'pT') for _ in range(3)]
        o0 = P.sb(st, [128, 4, 128], F32, 'o0')
        oo = [P.sb(st, [128, 128], F32, 'oo') for _ in range(2)]
        ob = [P.sb(st, [128, 128], BF16, 'ob') for _ in range(2)]
        junk = P.sb(st, [128, 128], F32, 'junk')
        sm = {n: [P.sb(st, [128, 1], F32, n) for _ in range(2)] for n in ('r0', 'r1', 'ssq', 'ms', 'lnm', 'rstd')}
        pp = [P.ps(st, [128, 512], F32, 'pp') for _ in range(2)]
        sc = [P.ps(st, [128, 512], F32, 'sc') for _ in range(2)]
        pacc = [P.ps(st, [128, 512], F32, 'pacc') for _ in range(4)]

        P.dma('pool', dmask[:], inp['c_dmask'].rearrange("h k q -> k h q"), writes=['dmask'], cast=True)
        P.dma('sp', cbias[:], inp['c_bias'], writes=['cbias'])
        P.dma('sp', gsub[:], inp['a_subln'][j:j + 1, :].partition_broadcast(128), writes=['gsub'])
        P.dma('sp', clam[:], inp['c_lam'], writes=['clam'])
        P.ts('dve', gsub[:], gsub[:], clam[:, 0:1], None, ALU.mult, None, reads=['gsub', 'clam'], writes=['gsub'])
        for i, nm in enumerate(['a_lam_q1', 'a_lam_k1', 'a_lam_q2', 'a_lam_k2']):
            P.dma('sp', lv[i][:], inp[nm][j:j + 1, :].partition_broadcast(128), writes=[('lv', i)])
        for i in range(2):
            P.tt('dve', lt[:], lv[2 * i][:], lv[2 * i + 1][:], ALU.mult, reads=[('lv', 2 * i), ('lv', 2 * i + 1)], writes=['lt'])
            P.reduce(ls[i][:], lt[:], ALU.add, reads=['lt'], writes=[('ls', i)])
            P.act(ls[i][:], ls[i][:], AF.Exp, reads=[('ls', i)], writes=[('ls', i)])
        P.tt('dve', neglam[:], ls[1][:], ls[0][:], ALU.subtract, reads=[('ls', 0), ('ls', 1)], writes=['neglam'])
        P.ts('dve', neglam[:], neglam[:], clam[:, 1:2], None, ALU.add, None, reads=['neglam', 'clam'], writes=['neglam'])
        for b in range(2):
            P.memset('pool', va[b][:, :, 128:129], 1.0, writes=[('va1', b)])

        def project(h):
            b = h % 2
            W = wqkv[b]
            for i, c0 in enumerate([h * 128, DA_W + h * 128, 2 * DA_W + h * 128]):
                P.dma('pool', W[:, :, i * 128:(i + 1) * 128], w_in[:, :, c0:c0 + 128], writes=[('w', b, i)], cast=True)
            for m in range(2):
                P.dma('pool', qT[b][64:70, m, :], inp['c_qaug'][h], writes=[('qa', b, m)], cast=True)
                P.dma('pool', kT[b][64:70, m, :], inp['c_kaug'][h], writes=[('ka', b, m)], cast=True)
            n = 0
            for tb in range(8):
                for m in range(2):
                    for isk in range(2):
                        pb = n % 2
                        n += 1
                        c0 = isk * 128 + m * 64
                        P.mmg([(pp[pb][0:64, :], W[:, dc, c0:c0 + 64], hT[:, dc, tb * 512:(tb + 1) * 512], dc == 0, dc == 7) for dc in range(8)],
                              reads=[('w', b, isk)], writes=[('pp', pb)])
                        if isk == 0:
                            P.act(qT[b][0:64, m, tb * 512:(tb + 1) * 512], pp[pb][0:64, :], AF.Identity, reads=[('pp', pb)], writes=[('q', b, m, tb)], scale=0.125)
                        else:
                            P.copy('dve', kT[b][0:64, m, tb * 512:(tb + 1) * 512], pp[pb][0:64, :], reads=[('pp', pb)], writes=[('k', b, m, tb)])
            for tt in range(NT):
                pb = n % 2
                n += 1
                P.mmg([(pp[pb][:, 0:128], hT[:, dc, tt * 128:(tt + 1) * 128], W[:, dc, 256:384], dc == 0, dc == 7) for dc in range(8)],
                      reads=[('w', b, 2)], writes=[('pp', pb)])
                P.copy('dve' if tt % 2 else 'act', va[b][:, tt, 0:128], pp[pb][:, 0:128], reads=[('pp', pb), ('va1', b)], writes=[('v', b, tt)])

        def attend(h):
            b = h % 2
            cnt = 0
            ev = 0
            for jq in range(8):
                for m in range(2):
                    nkt = 4 * jq + 4
                    for kt in range(nkt):
                        r = kt - 4 * jq
                        off = max(r, 0) * 128
                        N = 512 - off
                        sb_ = cnt % 2
                        pb = cnt % 3
                        cnt += 1
                        P.mmg([(sc[sb_][:, 0:N], kT[b][0:70, m, kt * 128:(kt + 1) * 128], qT[b][0:70, m, jq * 512 + off:(jq + 1) * 512], True, True)],
                              reads=[('q', b, m, jq), ('qa', b, m), ('ka', b, m)] + [('k', b, m, kt // 4)], writes=[('sc', sb_)])
                        bi = h * 35 + (4 * jq - kt + 3)
                        P.act(pT[pb][:, off:512], sc[sb_][:, 0:N], AF.Exp, reads=[('sc', sb_), 'cbias'], writes=[('pT', pb)], bias=cbias[:, bi:bi + 1])
                        if r >= 0:
                            P.tt('dve', pT[pb][:, off:off + 128], pT[pb][:, off:off + 128], dmask[:, h, :], ALU.mult,
                                 reads=[('pT', pb), 'dmask'], writes=[('pT', pb)])
                        qs0 = max(r, 0)
                        P.mmg([(pacc[qs][:, 0:129], pT[pb][:, qs * 128:(qs + 1) * 128], va[b][:, kt, :], kt == 0, kt == 4 * jq + qs) for qs in range(qs0, 4)],
                              reads=[('pT', pb), ('v', b, kt), ('va1', b)], writes=[('pacc', qs) for qs in range(qs0, 4)])
                    for qs in range(4):
                        e = ev % 2
                        if m == 0:
                            P.recip(sm['r0'][e][:], pacc[qs][:, 128:129], reads=[('pacc', qs)], writes=[('r0', e)])
                            P.act(o0[:, qs, :], pacc[qs][:, 0:128], AF.Identity, reads=[('pacc', qs), ('r0', e)], writes=[('o0', qs)], scale=sm['r0'][e][:, 0:1])
                        else:
                            P.recip(sm['r1'][e][:], pacc[qs][:, 128:129], reads=[('pacc', qs)], writes=[('r1', e)])
                            P.tt('dve', sm['r1'][e][:], sm['r1'][e][:], neglam[:], ALU.mult, reads=[('r1', e), 'neglam'], writes=[('r1', e)])
                            P.stt(oo[e][:], pacc[qs][:, 0:128], sm['r1'][e][:, 0:1], o0[:, qs, :], ALU.mult, ALU.add,
                                  reads=[('pacc', qs), ('r1', e), ('o0', qs)], writes=[('oo', e)])
                            P.act(junk[:], oo[e][:], AF.Square, reads=[('oo', e)], writes=['junk', ('ssq', e)], accum=sm['ssq'][e][:])
                            P.ts('dve', sm['ms'][e][:], sm['ssq'][e][:], 1.0 / 128, EPS, ALU.mult, ALU.add, reads=[('ssq', e)], writes=[('ms', e)])
                            P.act(sm['lnm'][e][:], sm['ms'][e][:], AF.Ln, reads=[('ms', e)], writes=[('lnm', e)])
                            P.act(sm['rstd'][e][:], sm['lnm'][e][:], AF.Exp, reads=[('lnm', e)], writes=[('rstd', e)], scale=-0.5)
                            P.stt(ob[e][:], oo[e][:], sm['rstd'][e][:, 0:1], gsub[:], ALU.mult, ALU.mult,
                                  reads=[('oo', e), ('rstd', e), 'gsub'], writes=[('ob', e)])
                            tt = jq * 4 + qs
                            P.dma('sp', o_s[tt * 128:(tt + 1) * 128, h * 128:(h + 1) * 128], ob[e][:], reads=[('ob', e)], writes=[('o_s', tt, h)])
                        ev += 1

        project(0)
        for h in range(6):
            if h + 1 < 6:
                project(h + 1)
            attend(h)
        P.flush()


def phase_hgrn(P, k, hT, j, o_s):
    inp = k.inp
    w_in = wview(inp['b_w_in'][j])
    with ExitStack() as st:
        W = [P.sb(st, [128, 8, 512], BF16, 'W') for _ in range(1)]
        qeT = [P.sb(st, [128, S], BF16, 'qeT') for _ in range(1)]
        keT = [P.sb(st, [128, S], BF16, 'keT') for _ in range(1)]
        ketok = [P.sb(st, [128, NT, 128], BF16, 'ketok') for _ in range(1)]
        vtok = [P.sb(st, [128, NT, 128], BF16, 'vtok') for _ in range(1)]
        sgtok = [P.sb(st, [128, NT, 128], BF16, 'sgtok') for _ in range(1)]
        decay = [P.sb(st, [128, 64], F32, 'decay') for _ in range(1)]
        lbl = P.sb(st, [128, 2, 6], F32, 'lbl')
        oml = P.sb(st, [128, 6], F32, 'oml')
        gn = P.sb(st, [128, 128], F32, 'gn')
        cmask = P.sb(st, [128, 128], BF16, 'cmask')
        smask = P.sb(st, [128, 512], F32, 'smask')
        et = P.sb(st, [128, 512], F32, 'et')
        dt_ = P.sb(st, [128, 512], F32, 'dt')
        kk = P.sb(st, [128, 512], F32, 'kk')
        gt = P.sb(st, [128, 512], F32, 'gt')
        bt = P.sb(st, [128, 512], F32, 'bt')
        eb = P.sb(st, [128, 512], F32, 'eb')
        enb = P.sb(st, [128, 512], F32, 'enb')
        Sst = P.sb(st, [128, 128], F32, 'Sst')
        Sbf = P.sb(st, [128, 128], BF16, 'Sbf')
        at = [P.sb(st, [128, 128], BF16, 'at') for _ in range(2)]
        o1 = [P.sb(st, [128, 128], F32, 'o1') for _ in range(2)]
        ob = [P.sb(st, [128, 128], BF16, 'ob') for _ in range(2)]
        junk = P.sb(st, [128, 128], F32, 'junk')
        sm = {n: [P.sb(st, [128, 1], F32, n) for _ in range(2)] for n in ('ssq', 'ms', 'lnm', 'rstd')}
        pq = P.ps(st, [128, 512], F32, 'pq')
        qraw = P.sb(st, [128, 512], F32, 'qraw')
        ptr = P.ps(st, [128, 4, 128], BF16, 'ptr')
        pvg2 = [P.ps(st, [128, 128], F32, 'pvg') for _ in range(2)]
        pat = P.ps(st, [128, 128], F32, 'pat')
        pkv2 = [P.ps(st, [128, 128], F32, 'pkv') for _ in range(2)]
        po = [P.ps(st, [128, 128], F32, 'po') for _ in range(1)]

        P.dma('pool', cmask[:], inp['c_cmask'], writes=['cmask'], cast=True)
        P.dma('sp', smask[:], inp['c_smask'], writes=['smask'])
        P.dma('sp', gn[:], inp['b_out_norm'][j:j + 1, :].partition_broadcast(128), writes=['gn'])
        lbf = P.sb(st, [128, 1], F32, 'lbf')
        P.dma('sp', lbf[:], inp['c_lbflag'], writes=['lbf'])
        lbl2 = P.sb(st, [2, 768], F32, 'lbl2')
        lbT = P.sb(st, [128, 6, 2], F32, 'lbT')
        P.dma('sp', lbl2[:], inp['b_lb_logits'], writes=['lbl2'])
        P.trg([(pvg2[0][:, 2 * h:2 * h + 2], lbl2[0:2, h * 128:(h + 1) * 128], k.identf[0:2, 0:2]) for h in range(6)], reads=['lbl2'], writes=[('pvg', 0)])
        P.copy('dve', lbT[:], pvg2[0][:, 0:12].rearrange("p (h l) -> p h l", l=2), reads=[('pvg', 0)], writes=['lbl'])
        P.tt('dve', oml[:], lbT[:, :, 0], lbT[:, :, 1], ALU.subtract, reads=['lbl'], writes=['oml'])
        P.act(oml[:], oml[:], AF.Exp, reads=['oml'], writes=['oml'])
        P.ts('dve', oml[:], oml[:], 1.0, None, ALU.add, None, reads=['oml'], writes=['oml'])
        P.recip(oml[:], oml[:], reads=['oml'], writes=['oml'])
        P.ts('dve', oml[:], oml[:], lbf[:, 0:1], 1.0, ALU.mult, ALU.add, reads=['oml', 'lbf'], writes=['oml'])

        def project(h):
            b = 0
            for i, c0 in enumerate([h * 128, HG_W + h * 128, 2 * HG_W + h * 128, 3 * HG_W + h * 128]):
                P.dma('pool', W[b][:, :, i * 128:(i + 1) * 128], w_in[:, :, c0:c0 + 128], writes=[('w', b, i)], cast=True)
            for tb in range(8):
                blk = slice(tb * 512, (tb + 1) * 512)
                P.mmg([(pq[:], W[b][:, dc, 0:128], hT[:, dc, blk], dc == 0, dc == 7) for dc in range(8)], reads=[('w', b, 0)], writes=['pq'])
                P.copy('act', qraw[:], pq[:], reads=['pq'], writes=['qraw'])
                P.mmg([(pq[:], W[b][:, dc, 128:256], hT[:, dc, blk], dc == 0, dc == 7) for dc in range(8)], reads=[('w', b, 1)], writes=['pq'])
                P.act(et[:], pq[:], AF.Exp, reads=['pq'], writes=['et'], scale=-1.0)
                P.ts('dve', dt_[:], et[:], 1.0, None, ALU.add, None, reads=['et'], writes=['dt'])
                P.recip(dt_[:], dt_[:], reads=['dt'], writes=['dt'])
                P.stt(kk[:], et[:], oml[:, h:h + 1], dt_[:], ALU.mult, ALU.mult, reads=['et', 'dt', 'oml'], writes=['kk'])
                P.act(gt[:], kk[:], AF.Ln, reads=['kk'], writes=['gt'], scale=-1.0, bias=1.0)
                P.scan(bt[:], smask[:], gt[:], reads=['smask', 'gt'], writes=['bt'])
                P.act(eb[:], bt[:], AF.Exp, reads=['bt'], writes=['eb'])
                P.act(enb[:], bt[:], AF.Exp, reads=['bt'], writes=['enb'], scale=-1.0)
                P.tt('dve', qeT[b][:, blk], qraw[:], eb[:], ALU.mult, reads=['qraw', 'eb'], writes=[('qe', b, tb)])
                P.tt('dve', keT[b][:, blk], kk[:], enb[:], ALU.mult, reads=['kk', 'enb'], writes=[('ke', b, tb)])
                P.copy('act', decay[b][:, tb * 8:(tb + 1) * 8], eb[:].rearrange("p (c t) -> p c t", t=64)[:, :, 63], reads=['eb'], writes=[('dec', b, tb)])
                P.trg([(ptr[:, i, :], keT[b][:, tb * 512 + i * 128: tb * 512 + (i + 1) * 128], k.ident[:]) for i in range(4)],
                      reads=[('ke', b, tb)], writes=['ptr'])
                P.copy('act', ketok[b][:, tb * 4:(tb + 1) * 4, :], ptr[:], reads=['ptr'], writes=[('ketok', b, tb)])
                for tq in range(4):
                    tt = tb * 4 + tq
                    tok = slice(tt * 128, (tt + 1) * 128)
                    P.mmg([(pvg2[0][:], hT[:, dc, tok], W[b][:, dc, 256:384], dc == 0, dc == 7) for dc in range(8)], reads=[('w', b, 2)], writes=[('pvg', 0)])
                    P.copy('dve', vtok[b][:, tt, :], pvg2[0][:], reads=[('pvg', 0)], writes=[('vtok', b, tt)])
                    P.mmg([(pvg2[1][:], hT[:, dc, tok], W[b][:, dc, 384:512], dc == 0, dc == 7) for dc in range(8)], reads=[('w', b, 3)], writes=[('pvg', 1)])
                    P.act(sgtok[b][:, tt, :], pvg2[1][:], AF.Silu, reads=[('pvg', 1)], writes=[('sgtok', b, tt)])

        def recur(h):
            b = 0
            P.memset('pool', Sst[:], 0.0, writes=['Sst'])
            P.memset('pool', Sbf[:], 0.0, writes=['Sbf'])
            for tt in range(NT):
                e = 0
                tb = tt // 4
                tok = slice(tt * 128, (tt + 1) * 128)
                P.mmg([(pat[:], keT[b][:, tok], qeT[b][:, tok], True, True)], reads=[('ke', b, tb), ('qe', b, tb)], writes=['pat'])
                P.tt('dve', at[e][:], pat[:], cmask[:], ALU.mult, reads=['pat', 'cmask'], writes=[('at', e)])
                P.mmg([(pkv2[ci][:], ketok[b][ci * 64:(ci + 1) * 64, tt, :], vtok[b][ci * 64:(ci + 1) * 64, tt, :], True, True) for ci in range(2)],
                      reads=[('ketok', b, tb), ('vtok', b, tt)], writes=['pkv'])
                P.mmg([(po[e][:], at[e][:], vtok[b][:, tt, :], True, False),
                       (po[e][0:64, :], qeT[b][:, tt * 128:tt * 128 + 64], Sbf[:], False, False)],
                      reads=[('at', e), ('vtok', b, tt), ('qe', b, tb), 'Sbf'], writes=[('po', e)])
                for ci in range(2):
                    c = 2 * tt + ci
                    P.tt('dve', Sst[:], pkv2[ci][:], Sst[:], ALU.add, reads=['pkv', 'Sst'], writes=['Sst'])
                    P.ts('dve', Sst[:], Sst[:], decay[b][:, c:c + 1], None, ALU.mult, None, reads=['Sst', ('dec', b, tb)], writes=['Sst'])
                    P.copy('act', Sbf[:], Sst[:], reads=['Sst'], writes=['Sbf'])
                    if ci == 0:
                        P.mmg([(po[e][64:128, :], qeT[b][:, tt * 128 + 64:(tt + 1) * 128], Sbf[:], False, True)],
                              reads=[('qe', b, tb), 'Sbf'], writes=[('po', e)])
                P.act(junk[:], po[e][:], AF.Square, reads=[('po', e)], writes=['junk', ('ssq', e)], accum=sm['ssq'][e][:])
                P.ts('dve', sm['ms'][e][:], sm['ssq'][e][:], 1.0 / 128, EPS, ALU.mult, ALU.add, reads=[('ssq', e)], writes=[('ms', e)])
                P.act(sm['lnm'][e][:], sm['ms'][e][:], AF.Ln, reads=[('ms', e)], writes=[('lnm', e)])
                P.act(sm['rstd'][e][:], sm['lnm'][e][:], AF.Exp, reads=[('lnm', e)], writes=[('rstd', e)], scale=-0.5)
                P.stt(o1[e][:], po[e][:], sm['rstd'][e][:, 0:1], gn[:], ALU.mult, ALU.mult, reads=[('po', e), ('rstd', e), 'gn'], writes=[('o1', e)])
                P.tt('dve', ob[e][:], o1[e][:], sgtok[b][:, tt, :], ALU.mult, reads=[('o1', e), ('sgtok', b, tt)], writes=[('ob', e)])
                P.dma('sp', o_s[tt * 128:(tt + 1) * 128, h * 128:(h + 1) * 128], ob[e][:], reads=[('ob', e)], writes=[('o_s', tt, h)])

        import os
        dbgm = int(os.environ.get('HG_DBG', '0'))
        for h in range(6):
            if dbgm == 1:
                break
            project(h)
            if dbgm == 2:
                continue
            recur(h)
        P.flush()


def phase_ffn(P, k, x_src, xacc_src, x_dst, gain_row, w_gu_ap, w_dn_ap, dff, router_ap=None, esel_ap=None):
    HT = S // 2
    NTH = NT // 2
    ngrp = (dff + 511) // 512
    moe = router_ap is not None
    gu = wview(w_gu_ap)
    for half in range(2):
        with ExitStack() as st0:
            xacc = P.sb(st0, [128, NTH, D], F32, 'xacc')
            hTh = P.sb(st0, [128, 8, HT], BF16, 'hTh')
            csel = P.sb(st0, [128, NTH], F32, 'csel')
            with ExitStack() as st:
                gbc = P.sb(st, [128, D], F32, 'gbc')
                hb = [P.sb(st, [128, D], BF16, 'hb') for _ in range(2)]
                pt = [P.ps(st, [128, 8, 128], BF16, 'pt') for _ in range(2)]
                alloc_norm_tmps(P, k, st, ['n0', 'n1'])
                P.dma('sp', gbc[:], gain_row.partition_broadcast(128), writes=['gbc'])
                if moe:
                    xp = [P.sb(st, [128, D], F32, 'xp') for _ in range(2)]
                    hf2 = [P.sb(st, [128, D], F32, 'hf') for _ in range(2)]
                    hTf = P.sb(st, [128, 8, 128], F32, 'hTf')
                    wr = P.sb(st, [128, 8, 8], F32, 'wr')
                    esel = P.sb(st, [128, 8], F32, 'esel')
                    ptf = [P.ps(st, [128, 4, 128], F32, 'ptf') for _ in range(2)]
                    plg = P.ps(st, [128, 8], F32, 'plg')
                    lg = P.sb(st, [128, 8], F32, 'lg')
                    lg2 = P.sb(st, [128, 8], F32, 'lg2')
                    eq1 = P.sb(st, [128, 8], F32, 'eq1')
                    eq2 = P.sb(st, [128, 8], F32, 'eq2')
                    m1 = P.sb(st, [128, 1], F32, 'm1')
                    m2 = P.sb(st, [128, 1], F32, 'm2')
                    w1 = P.sb(st, [128, 1], F32, 'w1')
                    w2 = P.sb(st, [128, 1], F32, 'w2')
                    P.dma('sp', wr[:], wview(router_ap), writes=['wr'])
                    P.dma('sp', esel[:], esel_ap, writes=['esel'])
                for tl in range(NTH):
                    tt = half * NTH + tl
                    b = tl % 2
                    tag = 'n%d' % b
                    rows = slice(tt * 128, (tt + 1) * 128)
                    if moe:
                        P.dma('sp', xacc[:, tl, :], xacc_src[rows, :], writes=[('xacc', tl)])
                        P.dma('sp', xp[b][:], x_src[rows, :], writes=[tag + 'x'])
                        hf = hf2[b]
                        norm_rows(P, k, st, xp[b][:], gbc[:], hb[b][:], tag, want_f32=hf[:])
                        for q4 in range(2):
                            P.trg([(ptf[q4][:, c, :], hf[:, (q4 * 4 + c) * 128:(q4 * 4 + c + 1) * 128], k.identf[:]) for c in range(4)],
                                  reads=[tag + 'hf'], writes=[('ptf', q4)])
                            P.copy('dve', hTf[:, q4 * 4:(q4 + 1) * 4, :], ptf[q4][:], reads=[('ptf', q4)], writes=[('hTf', q4)])
                        P.mmg([(plg[:], hTf[:, dc, :], wr[:, dc, :], dc == 0, dc == 7) for dc in range(8)],
                              reads=[('hTf', 0), ('hTf', 1), 'wr'], writes=['plg'])
                        P.copy('dve', lg[:], plg[:], reads=['plg'], writes=['lg'])
                        P.reduce(m1[:], lg[:], ALU.max, reads=['lg'], writes=['m1'])
                        P.ts('dve', eq1[:], lg[:], m1[:, 0:1], None, ALU.is_equal, None, reads=['lg', 'm1'], writes=['eq1'])
                        P.stt(lg2[:], eq1[:], -1e30, lg[:], ALU.mult, ALU.add, reads=['eq1', 'lg'], writes=['lg2'])
                        P.reduce(m2[:], lg2[:], ALU.max, reads=['lg2'], writes=['m2'])
                        P.ts('dve', eq2[:], lg2[:], m2[:, 0:1], None, ALU.is_equal, None, reads=['lg2', 'm2'], writes=['eq2'])
                        P.tt('dve', w2[:], m2[:], m1[:], ALU.subtract, reads=['m1', 'm2'], writes=['w2'])
                        P.act(w2[:], w2[:], AF.Exp, reads=['w2'], writes=['w2'])
                        P.ts('dve', w1[:], w2[:], 1.0, None, ALU.add, None, reads=['w2'], writes=['w1'])
                        P.recip(w1[:], w1[:], reads=['w1'], writes=['w1'])
                        P.tt('dve', w2[:], w2[:], w1[:], ALU.mult, reads=['w1', 'w2'], writes=['w2'])
                        P.ts('dve', eq1[:], eq1[:], w1[:, 0:1], None, ALU.mult, None, reads=['eq1', 'w1'], writes=['eq1'])
                        P.stt(eq2[:], eq2[:], w2[:, 0:1], eq1[:], ALU.mult, ALU.add, reads=['eq2', 'w2', 'eq1'], writes=['eq2'])
                        P.tt('dve', eq2[:], eq2[:], esel[:], ALU.mult, reads=['eq2', 'esel'], writes=['eq2'])
                        P.reduce(csel[:, tl:tl + 1], eq2[:], ALU.add, reads=['eq2'], writes=[('csel', tl)])
                    else:
                        P.dma('sp', xacc[:, tl, :], x_src[rows, :], writes=[tag + 'x', ('xacc', tl)])
                        norm_rows(P, k, st, xacc[:, tl, :], gbc[:], hb[b][:], tag)
                    P.trg([(pt[b][:, c, :], hb[b][:, c * 128:(c + 1) * 128], k.ident[:]) for c in range(8)],
                          reads=[tag + 'hb'], writes=[tag + 'pt'])
                    P.copy('act' if tl % 2 else 'dve', hTh[:, :, tl * 128:(tl + 1) * 128], pt[b][:], reads=[tag + 'pt'], writes=[('hT', tl)])
                P.flush()
            with ExitStack() as st:
                wg = [P.sb(st, [128, 8, 512], BF16, 'wg') for _ in range(2)]
                wu = [P.sb(st, [128, 8, 512], BF16, 'wu') for _ in range(2)]
                wd = [P.sb(st, [128, 4, D], BF16, 'wd') for _ in range(2)]
                sg = [P.sb(st, [128, 512], F32, 'sg') for _ in range(2)]
                aT = [P.sb(st, [128, 4, 512], BF16, 'aT') for _ in range(2)]
                pg = [P.ps(st, [128, 512], F32, 'pg') for _ in range(2)]
                pu = [P.ps(st, [128, 512], F32, 'pu') for _ in range(2)]
                po = [P.ps(st, [128, 512], F32, 'po') for _ in range(4)]
                it = 0
                gi = 0
                oi = 0
                ai = 0
                for fg in range(ngrp):
                    F = min(512, dff - fg * 512)
                    nfc = F // 128
                    wb = it % 2
                    it += 1
                    P.dma('pool', wg[wb][:, :, 0:F], gu[:, :, fg * 512:fg * 512 + F], writes=[('wg', wb)], cast=True)
                    P.dma('pool', wu[wb][:, :, 0:F], gu[:, :, dff + fg * 512:dff + fg * 512 + F], writes=[('wu', wb)], cast=True)
                    P.dma('pool', wd[wb][:, 0:nfc, :], wview(w_dn_ap[fg * 512:fg * 512 + F, :]), writes=[('wd', wb)], cast=True)
                    for tb in range(HT // 512):
                        blk = slice(tb * 512, (tb + 1) * 512)
                        ab = ai % 2
                        ai += 1
                        for fc in range(nfc):
                            g = gi % 2
                            gi += 1
                            P.mmg([(pg[g][:], wg[wb][:, dc, fc * 128:(fc + 1) * 128], hTh[:, dc, blk], dc == 0, dc == 7) for dc in range(8)],
                                  reads=[('wg', wb)], writes=[('pg', g)])
                            P.mmg([(pu[g][:], wu[wb][:, dc, fc * 128:(fc + 1) * 128], hTh[:, dc, blk], dc == 0, dc == 7) for dc in range(8)],
                                  reads=[('wu', wb)], writes=[('pu', g)])
                            P.act(sg[g][:], pg[g][:], AF.Silu, reads=[('pg', g)], writes=[('sg', g)])
                            P.tt('dve', aT[ab][:, fc, :], pu[g][:], sg[g][:], ALU.mult, reads=[('pu', g), ('sg', g)], writes=[('aT', ab, fc)])
                        for tq in range(4):
                            tl = tb * 4 + tq
                            for hh in range(2):
                                o = oi % 4
                                oi += 1
                                P.mmg([(po[o][:], aT[ab][:, fc, tq * 128:(tq + 1) * 128], wd[wb][:, fc, hh * 512:(hh + 1) * 512], fc == 0, fc == nfc - 1) for fc in range(nfc)],
                                      reads=[('aT', ab, fc) for fc in range(nfc)] + [('wd', wb)], writes=[('po', o)])
                                sc_ = csel[:, tl:tl + 1] if moe else 1.0
                                P.stt(xacc[:, tl, hh * 512:(hh + 1) * 512], po[o][:], sc_, xacc[:, tl, hh * 512:(hh + 1) * 512], ALU.mult, ALU.add,
                                      reads=[('po', o), ('xacc', tl, hh)], writes=[('xacc', tl, hh)])
                for tl in range(NTH):
                    tt = half * NTH + tl
                    P.dma('sp', x_dst[tt * 128:(tt + 1) * 128, :], xacc[:, tl, :], reads=[('xacc', tl, 0), ('xacc', tl, 1)], writes=[('xd', tt)])
                P.flush()


def phase_fnorm(P, k, x_src, gain_row, out_ap):
    with ExitStack() as st:
        gbc = P.sb(st, [128, D], F32, 'gbc')
        xt = [P.sb(st, [128, D], F32, 'xt') for _ in range(2)]
        yo = [P.sb(st, [128, D], F32, 'yo') for _ in range(2)]
        alloc_norm_tmps(P, k, st, ['n0', 'n1'])
        P.dma('sp', gbc[:], gain_row.partition_broadcast(128), writes=['gbc'])
        for tt in range(NT):
            b = tt % 2
            tag = 'n%d' % b
            t = k.nt[tag]
            P.dma('sp', xt[b][:], x_src[tt * 128:(tt + 1) * 128, :], writes=[tag + 'x'])
            P.act(t['junk'][:], xt[b][:], AF.Square, reads=[tag + 'x'], writes=[tag + 'junk', tag + 'ssq'], accum=t['ssq'][:])
            P.ts('dve', t['ms'][:], t['ssq'][:], 1.0 / D, EPS, ALU.mult, ALU.add, reads=[tag + 'ssq'], writes=[tag + 'ms'])
            P.tt('pool', t['rstd'][:], t['ms'][:], k.neghalf[:], ALU.pow, reads=[tag + 'ms'], writes=[tag + 'rstd'])
            P.stt(yo[b][:], xt[b][:], t['rstd'][:, 0:1], gbc[:], ALU.mult, ALU.mult, reads=[tag + 'x', tag + 'rstd', 'gbc'], writes=[('yo', b)])
            P.dma('sp', out_ap[tt * 128:(tt + 1) * 128, :], yo[b][:], reads=[('yo', b)], writes=[('out', tt)])
        P.flush()


CONST_SHAPES = {"c_ident": [128, 128], "c_kaug": [6, 6, S], "c_qaug": [6, 6, S], "c_dmask": [6, 128, 128], "c_bias": [128, 6 * 35],
                "c_cmask": [128, 128], "c_smask": [128, 512]}
STEP_INPUTS = {
    'attn': {"x": [S, D], "mem": [MEM_LEN, D], "a_norm_mix": [1, D], "a_w_in": [1, D, 2560], "a_lam_q1": [1, 64], "a_lam_k1": [1, 64],
             "a_lam_q2": [1, 64], "a_lam_k2": [1, 64], "a_subln": [1, 128], "a_mem_norm": [1, D], "a_w_mem_kv": [1, D, 512],
             "a_w_out": [1, D, D], "c_lam": [128, 2], "c_ident": 0, "c_kaug": 0, "c_qaug": 0, "c_dmask": 0, "c_bias": 0},
    'hgrn': {"x": [S, D], "mem": [MEM_LEN, D], "b_norm_mix": [1, D], "b_w_in": [1, D, 3328], "b_lb_logits": [2, 768], "b_out_norm": [1, 128],
             "b_mem_norm": [1, D], "b_w_mem_kv": [1, D, 512], "b_w_out": [1, D, D], "c_lbflag": [128, 1], "c_ident": 0, "c_cmask": 0, "c_smask": 0},
    'dense': {"x": [S, D], "norm": [1, D], "w_gu": [D, 2 * DFF_D], "w_dn": [DFF_D, D], "c_ident": 0},
    'moe1': {"x": [S, D], "xacc": [S, D], "norm": [1, D], "router": [D, 8], "w_gu": [D, 2 * DFF_E], "w_dn": [DFF_E, D], "esel": [128, 8], "c_ident": 0},
    'fnorm': {"x": [S, D], "gain": [1, D], "c_ident": 0},
}


def build_step(kind):
    nc = bass.Bass("TRN2", target_bir_lowering=False)
    inp = {}
    for n, shp in STEP_INPUTS[kind].items():
        if shp == 0:
            shp = CONST_SHAPES[n]
        inp[n] = nc.dram_tensor(n, shp, F32, kind="ExternalInput").ap()
    out = nc.dram_tensor("out", [S, D], F32, kind="ExternalOutput").ap()
    o_s = nc.dram_tensor("o_s", [S, D], BF16, kind="Internal").ap()
    with ExitStack() as st:
        P = Prog(nc, st)
        k = K()
        k.inp = inp
        k.ident = P.sb(st, [128, 128], BF16, 'ident')
        k.identf = P.sb(st, [128, 128], F32, 'identf')
        k.neghalf = P.sb(st, [128, 1], F32, 'neghalf')
        P.dma('pool', k.ident[:], inp['c_ident'], writes=['ident'], cast=True)
        P.dma('sp', k.identf[:], inp['c_ident'], writes=['identf'])
        P.memset('pool', k.neghalf[:], -0.5, writes=['neghalf'])
        P.flush()
        if kind in ('attn', 'hgrn'):
            pre = 'a_' if kind == 'attn' else 'b_'
            with ExitStack() as stm:
                hT = P.sb(stm, [128, 8, S], BF16, 'hT')
                kmT = P.sb(stm, [128, 2, MEM_LEN], BF16, 'kmT')
                vm = P.sb(stm, [128, 2, 4, 65], BF16, 'vm')
                phase_hT(P, k, inp['x'], inp[pre + 'norm_mix'][0:1, :], hT)
                phase_memkv(P, k, inp['mem'], inp[pre + 'mem_norm'][0:1, :], inp[pre + 'w_mem_kv'][0], kmT, vm)
                if kind == 'attn':
                    phase_diffattn(P, k, hT, 0, None, o_s)
                    phase_memattn(P, k, hT, inp['a_w_in'][0], 3 * DA_W, kmT, vm, o_s)
                else:
                    phase_hgrn(P, k, hT, 0, o_s)
                    phase_memattn(P, k, hT, inp['b_w_in'][0], 4 * HG_W, kmT, vm, o_s)
            phase_outproj(P, k, o_s, inp[pre + 'w_out'][0], inp['x'], out)
        elif kind == 'dense':
            phase_ffn(P, k, inp['x'], inp['x'], out, inp['norm'][0:1, :], inp['w_gu'], inp['w_dn'], DFF_D)
        elif kind == 'moe1':
            phase_ffn(P, k, inp['x'], inp['xacc'], out, inp['norm'][0:1, :], inp['w_gu'], inp['w_dn'], DFF_E,
                      router_ap=inp['router'], esel_ap=inp['esel'])
        elif kind == 'fnorm':
            phase_fnorm(P, k, inp['x'], inp['gain'][0:1, :], out)
        P.add('sp', lambda e: e.nop(), reads=[], writes=[])
        P.flush()
    return nc


def make_consts():
    slopes = 2.0 ** (-8.0 * np.arange(1, 7) / 6.0)
    import ml_dtypes
    bf = ml_dtypes.bfloat16

    def hi_lo(v):
        hi = np.float32(np.float32(v).astype(bf).astype(np.float32))
        lo = np.float32(np.float32(v - hi).astype(bf).astype(np.float32))
        return hi, lo
    c = {}
    c['c_ident'] = np.eye(128, dtype=np.float32)
    pos = np.arange(S)
    krel = (pos % 128).astype(np.float32)
    qrel = pos % 512
    qhi = (qrel & ~3).astype(np.float32)
    qlo = (qrel & 3).astype(np.float32)
    kaug = np.zeros((6, 6, S), np.float32)
    qaug = np.zeros((6, 6, S), np.float32)
    for h in range(6):
        hi, lo = hi_lo(slopes[h])
        kaug[h, 0] = krel
        kaug[h, 1] = krel
        kaug[h, 2] = hi
        kaug[h, 3] = lo
        kaug[h, 4] = hi
        kaug[h, 5] = lo
        qaug[h, 0] = hi
        qaug[h, 1] = lo
        qaug[h, 2] = -qhi
        qaug[h, 3] = -qhi
        qaug[h, 4] = -qlo
        qaug[h, 5] = -qlo
    c['c_kaug'] = kaug
    c['c_qaug'] = qaug
    kk = np.arange(128)[:, None]
    qq = np.arange(128)[None, :]
    dm = np.zeros((6, 128, 128), np.float32)
    for h in range(6):
        allowed = (kk // 64) <= (qq // 64)
        val = np.where(kk <= qq, 1.0, np.exp(-2.0 * slopes[h] * (kk - qq)))
        dm[h] = np.where(allowed, val, 0.0)
    c['c_dmask'] = dm
    cb = np.zeros((128, 6 * 35), np.float32)
    for h in range(6):
        for idx in range(35):
            d = idx - 3
            cb[:, h * 35 + idx] = -slopes[h] * 128.0 * d
    c['c_bias'] = cb
    s_ = np.arange(128)[:, None]
    t_ = np.arange(128)[None, :]
    c['c_cmask'] = ((s_ <= t_) & ((s_ // 64) == (t_ // 64))).astype(np.float32)
    sm = np.ones((128, 512), np.float32)
    sm[:, ::64] = 0.0
    c['c_smask'] = sm
    return c


_CACHE = {}
_CONSTS = {}


def launch(kind, per_core, shared, n_cores):
    if kind not in _CACHE:
        _CACHE[kind] = build_step(kind)
    if not _CONSTS:
        _CONSTS.update(make_consts())
    nc = _CACHE[kind]
    sh = {}
    for n, shp in STEP_INPUTS[kind].items():
        if n in per_core:
            continue
        if shp == 0:
            sh[n] = _CONSTS[n]
        else:
            sh[n] = np.ascontiguousarray(np.asarray(shared[n], dtype=np.float32)).reshape(shp)
    in_maps = []
    for c in range(n_cores):
        m = dict(sh)
        for n, a in per_core.items():
            m[n] = np.ascontiguousarray(a[c])
        in_maps.append(m)
    res = run_bass_kernel_spmd(nc, in_maps, core_ids=list(range(n_cores)))
    return np.stack([np.asarray(r['out'], dtype=np.float32).reshape(S, D) for r in res.results], axis=0)


def run_step(i, which, x, inputs, n_cores):
    f = lambda n: np.asarray(inputs[n], dtype=np.float32)
    j = i // 2
    mem = f('mem')[:n_cores]
    if which == 'mix' and i % 2 == 0:
        lam_init = 0.8 - 0.6 * float(np.exp(-0.3 * i))
        clam = np.zeros((128, 2), np.float32)
        clam[:, 0] = 1.0 - lam_init
        clam[:, 1] = -lam_init
        sh = {n: f(n)[j:j + 1] for n in ['a_norm_mix', 'a_w_in', 'a_lam_q1', 'a_lam_k1', 'a_lam_q2', 'a_lam_k2', 'a_subln', 'a_mem_norm', 'a_w_mem_kv', 'a_w_out']}
        sh['c_lam'] = clam
        return launch('attn', {'x': x, 'mem': mem}, sh, n_cores)
    if which == 'mix':
        sh = {n: f(n)[j:j + 1] for n in ['b_norm_mix', 'b_w_in', 'b_out_norm', 'b_mem_norm', 'b_w_mem_kv', 'b_w_out']}
        sh['b_lb_logits'] = f('b_lb_logits')
        sh['c_lbflag'] = np.full((128, 1), -float(j), np.float32)
        return launch('hgrn', {'x': x, 'mem': mem}, sh, n_cores)
    if i % 2 == 0:
        sh = {'norm': f('dense_norm')[j:j + 1], 'w_gu': f('dense_w_gate_up')[j], 'w_dn': f('dense_w_down')[j]}
        return launch('dense', {'x': x}, sh, n_cores)
    xacc = x
    for e in range(NEXP):
        esel = np.zeros((128, 8), np.float32)
        esel[:, e] = 1.0
        sh = {'norm': f('moe_norm')[j:j + 1], 'router': f('moe_router')[j], 'w_gu': f('moe_w_gate_up')[j, e], 'w_dn': f('moe_w_down')[j, e], 'esel': esel}
        xacc = launch('moe1', {'x': x, 'xacc': xacc}, sh, n_cores)
    return xacc


def kernel(**inputs):
    n_cores = 8
    x = np.asarray(inputs['x'], dtype=np.float32)
    for i in range(DEPTH):
        x = run_step(i, 'mix', x, inputs, n_cores)
        x = run_step(i, 'ffn', x, inputs, n_cores)
    return launch('fnorm', {'x': x}, {'gain': np.asarray(inputs['final_norm'], dtype=np.float32).reshape(1, D)}, n_cores)
```

```python
import numpy as np
import concourse.bass as bass
import concourse.mybir as mybir
from concourse.bass_utils import run_bass_kernel_spmd
from contextlib import ExitStack

F32 = mybir.dt.float32
BF16 = mybir.dt.bfloat16
ALU = mybir.AluOpType
AF = mybir.ActivationFunctionType
AX = mybir.AxisListType

S = 4096
D = 1024
NT = 32
EPS = 1e-6
DEPTH = 4
DA_W = 768
HG_W = 768
MEM_W = 256
DFF_D = 2816
DFF_E = 3584
NEXP = 8
MEM_LEN = 256

ENGS = ['pe', 'act', 'dve', 'pool', 'sp']
DMA_POOL = {'sp': 24, 'pool': 24, 'act': 4}


class Op:
    __slots__ = ('eng', 'fn', 'deps', 'need', 'sig', 'is_dma', 'dsem', 'dval', 'uid')


class Prog:
    def __init__(self, nc, stack, strict=True):
        self.nc = nc
        self.stack = stack
        self.strict = strict
        self.ops = {e: [] for e in ENGS}
        self.lastw = {}
        self.reads = {}
        self.uid = 0
        self.esem = {e: stack.enter_context(nc.semaphore('s_' + e)) for e in ENGS}
        self.sigc = {e: 0 for e in ENGS}
        self.dsems = {}
        self.dcur = {}
        self.dlast = {}
        self.dfence = {}
        for q, n in DMA_POOL.items():
            self.dsems[q] = [stack.enter_context(nc.semaphore('d_%s%d' % (q, i))) for i in range(n)]
            self.dcur[q] = 0
            self.dlast[q] = [None] * n
            self.dfence[q] = [0] * n
        self.efence = {e: 0 for e in ENGS}
        self.ntile = 0
        self.nflush = 0

    def sb(self, st, shape, dtype, name=None):
        self.ntile += 1
        name = (name or 't') + '_%d' % self.ntile
        return st.enter_context(self.nc.sbuf_tensor(name, list(shape), dtype))

    def ps(self, st, shape, dtype=F32, name=None):
        self.ntile += 1
        name = (name or 'p') + '_%d' % self.ntile
        return st.enter_context(self.nc.psum_tensor(name, list(shape), dtype))

    def add(self, eng, fn, reads=(), writes=(), dma=False):
        op = Op()
        op.eng = eng
        op.fn = fn
        op.need = False
        op.sig = None
        op.is_dma = dma
        op.uid = self.uid
        self.uid += 1
        deps = []
        for k in reads:
            w = self.lastw.get(k)
            if w is not None:
                deps.append((w, 'raw'))
        for k in writes:
            w = self.lastw.get(k)
            if w is not None:
                deps.append((w, 'waw'))
            rd = self.reads.get(k)
            if rd:
                for r in rd.values():
                    deps.append((r, 'war'))
        fdeps = []
        seen = set()
        for d, kind in deps:
            if d.uid in seen:
                continue
            if (not d.is_dma) and (not dma) and d.eng == eng:
                if eng == 'pe' or kind == 'war' or not self.strict:
                    continue
            seen.add(d.uid)
            fdeps.append(d)
        if dma:
            q = eng
            i = self.dcur[q]
            self.dcur[q] = (i + 1) % len(self.dsems[q])
            prev = self.dlast[q][i]
            op.dsem = self.dsems[q][i]
            op.dval = (prev.dval if prev is not None else 0) + 16
            if prev is not None and prev.uid not in seen and prev.dval > self.dfence[q][i]:
                fdeps.append(prev)
                seen.add(prev.uid)
            self.dlast[q][i] = op
        for d in fdeps:
            d.need = True
        op.deps = fdeps
        rk = ('dma', op.uid) if dma else eng
        for k in writes:
            self.lastw[k] = op
            self.reads[k] = {}
        for k in reads:
            self.reads.setdefault(k, {})[rk] = op
        self.ops[eng].append(op)
        return op

    def flush(self):
        for e in ENGS:
            comp = [op for op in self.ops[e] if not op.is_dma and op.fn is not None]
            if comp:
                comp[-1].need = True
            c = self.sigc[e]
            for op in self.ops[e]:
                if op.need and not op.is_dma:
                    c += 1
                    op.sig = c
            self.sigc[e] = c
        efence = dict(self.efence)
        dfence = {q: list(v) for q, v in self.dfence.items()}

        def run(e, engobj):
            waited = {}
            for e2 in ENGS:
                if efence[e2] > 0:
                    engobj.wait_ge(self.esem[e2], efence[e2])
                    waited[id(self.esem[e2])] = efence[e2]
            for q in dfence:
                for i, v in enumerate(dfence[q]):
                    if v > 0:
                        engobj.wait_ge(self.dsems[q][i], v)
                        waited[id(self.dsems[q][i])] = v
            for op in self.ops[e]:
                for d in op.deps:
                    if d.is_dma:
                        sem, val = d.dsem, d.dval
                    else:
                        sem, val = self.esem[d.eng], d.sig
                    key = id(sem)
                    if waited.get(key, 0) < val:
                        engobj.wait_ge(sem, val)
                        waited[key] = val
                ins = op.fn(engobj)
                if op.is_dma:
                    ins.then_inc(op.dsem, 16)
                elif op.need:
                    ins.then_inc(self.esem[e], 1)

        with self.nc.Block() as block:
            @block.tensor
            def _(t):
                run('pe', t)

            @block.scalar
            def _(t):
                run('act', t)

            @block.vector
            def _(t):
                run('dve', t)

            @block.gpsimd
            def _(t):
                run('pool', t)

            @block.sync
            def _(t):
                run('sp', t)

        for e in ENGS:
            self.efence[e] = self.sigc[e]
            self.ops[e] = []
        for q in self.dlast:
            for i, op in enumerate(self.dlast[q]):
                if op is not None:
                    self.dfence[q][i] = op.dval
        self.lastw = {}
        self.reads = {}
        self.nflush += 1

    def dma(self, q, out, in_, reads=(), writes=(), cast=False, slow=False):
        if slow:
            fn = lambda e, o=out, i=in_: e.dma_start(out=o, in_=i, allow_slow_non_contiguous=True)
        elif cast:
            fn = lambda e, o=out, i=in_: e.dma_start(out=o, in_=i, max_dma_last_dim=4096)
        else:
            fn = lambda e, o=out, i=in_: e.dma_start(out=o, in_=i)
        return self.add(q, fn, reads, writes, dma=True)

    def mmg(self, mms, reads, writes):
        mms = list(mms)

        def fn(e, mms=mms):
            ins = None
            for (o, l, r, s0, s1) in mms:
                ins = e.matmul(o, l, r, start=s0, stop=s1)
            return ins
        return self.add('pe', fn, reads, writes)

    def trg(self, trs, reads, writes):
        trs = list(trs)

        def fn(e, trs=trs):
            ins = None
            for (o, i, idn) in trs:
                ins = e.transpose(o, i, idn)
            return ins
        return self.add('pe', fn, reads, writes)

    def act(self, out, in_, func, reads, writes, bias=None, scale=None, accum=None):
        kw = {}
        if bias is not None:
            kw['bias'] = bias
        if scale is not None:
            kw['scale'] = scale
        if accum is not None:
            kw['accum_out'] = accum
        return self.add('act', lambda e, o=out, i=in_, f=func, kw=kw: e.activation(out=o, in_=i, func=f, **kw), reads, writes)

    def tt(self, eng, out, in0, in1, op, reads, writes):
        return self.add(eng, lambda e, o=out, a=in0, b=in1, p=op: e.tensor_tensor(out=o, in0=a, in1=b, op=p), reads, writes)

    def ts(self, eng, out, in0, s1, s2, op0, op1, reads, writes):
        if op1 is None:
            return self.add(eng, lambda e, o=out, a=in0, s1=s1, p0=op0: e.tensor_scalar(out=o, in0=a, scalar1=s1, scalar2=None, op0=p0), reads, writes)
        return self.add(eng, lambda e, o=out, a=in0, s1=s1, s2=s2, p0=op0, p1=op1: e.tensor_scalar(out=o, in0=a, scalar1=s1, scalar2=s2, op0=p0, op1=p1), reads, writes)

    def stt(self, out, in0, scalar, in1, op0, op1, reads, writes):
        return self.add('dve', lambda e, o=out, a=in0, s=scalar, b=in1, p0=op0, p1=op1: e.scalar_tensor_tensor(out=o, in0=a, scalar=s, in1=b, op0=p0, op1=p1), reads, writes)

    def copy(self, eng, out, in_, reads, writes):
        if eng == 'act':
            return self.add('act', lambda e, o=out, i=in_: e.copy(out=o, in_=i), reads, writes)
        return self.add(eng, lambda e, o=out, i=in_: e.tensor_copy(out=o, in_=i), reads, writes)

    def memset(self, eng, ap, val, writes):
        return self.add(eng, lambda e, a=ap, v=val: e.memset(a, v), (), writes)

    def recip(self, out, in_, reads, writes):
        return self.add('dve', lambda e, o=out, i=in_: e.reciprocal(out=o, in_=i), reads, writes)

    def reduce(self, out, in_, op, reads, writes):
        return self.add('dve', lambda e, o=out, i=in_, p=op: e.tensor_reduce(out=o, in_=i, axis=AX.X, op=p), reads, writes)

    def scan(self, out, d0, d1, reads, writes):
        return self.add('dve', lambda e, o=out, a=d0, b=d1: e.tensor_tensor_scan(out=o, data0=a, data1=b, initial=0.0, op0=ALU.mult, op1=ALU.add), reads, writes)


class K:
    pass


def wview(ap2d):
    return ap2d.rearrange("(c p) n -> p c n", p=128)


def norm_rows(P, k, st, x_ap, gbc, hb_out, tag, want_f32=None):
    t = k.nt[tag]
    P.act(t['junk'][:], x_ap, AF.Square, reads=[tag + 'x'], writes=[tag + 'junk', tag + 'ssq'], accum=t['ssq'][:])
    P.ts('dve', t['ms'][:], t['ssq'][:], 1.0 / D, EPS, ALU.mult, ALU.add, reads=[tag + 'ssq'], writes=[tag + 'ms'])
    P.tt('pool', t['rstd'][:], t['ms'][:], k.neghalf[:], ALU.pow, reads=[tag + 'ms'], writes=[tag + 'rstd'])
    if want_f32 is not None:
        P.stt(want_f32, x_ap, t['rstd'][:, 0:1], gbc, ALU.mult, ALU.mult, reads=[tag + 'x', tag + 'rstd', 'gbc'], writes=[tag + 'hf'])
        P.copy('act', hb_out, want_f32, reads=[tag + 'hf'], writes=[tag + 'hb'])
    else:
        P.stt(hb_out, x_ap, t['rstd'][:, 0:1], gbc, ALU.mult, ALU.mult, reads=[tag + 'x', tag + 'rstd', 'gbc'], writes=[tag + 'hb'])


def alloc_norm_tmps(P, k, st, tags):
    k.nt = {}
    for tag in tags:
        k.nt[tag] = {
            'junk': P.sb(st, [128, D], F32, 'junk'),
            'ssq': P.sb(st, [128, 1], F32, 'ssq'),
            'ms': P.sb(st, [128, 1], F32, 'ms'),
            'rstd': P.sb(st, [128, 1], F32, 'rstd'),
        }


def phase_hT(P, k, x_src, gain_row, hT):
    with ExitStack() as st:
        gbc = P.sb(st, [128, D], F32, 'gbc')
        xt = [P.sb(st, [128, D], F32, 'xt') for _ in range(2)]
        hb = [P.sb(st, [128, D], BF16, 'hb') for _ in range(2)]
        pt = [P.ps(st, [128, 8, 128], BF16, 'pt') for _ in range(2)]
        alloc_norm_tmps(P, k, st, ['n0', 'n1'])
        P.dma('sp', gbc[:], gain_row.partition_broadcast(128), writes=['gbc'])
        for tt in range(NT):
            b = tt % 2
            tag = 'n%d' % b
            P.dma('sp', xt[b][:], x_src[tt * 128:(tt + 1) * 128, :], writes=[tag + 'x'])
            norm_rows(P, k, st, xt[b][:], gbc[:], hb[b][:], tag)
            P.trg([(pt[b][:, c, :], hb[b][:, c * 128:(c + 1) * 128], k.ident[:]) for c in range(8)],
                  reads=[tag + 'hb'], writes=[tag + 'pt'])
            P.copy('act' if tt % 2 else 'dve', hT[:, :, tt * 128:(tt + 1) * 128], pt[b][:], reads=[tag + 'pt'], writes=[('hT', tt)])
        P.flush()


def phase_memkv(P, k, mem_ap, gain_row, wkv_ap, kmT, vm):
    with ExitStack() as st:
        gbc = P.sb(st, [128, D], F32, 'gbc')
        xt = [P.sb(st, [128, D], F32, 'xt') for _ in range(2)]
        hb = [P.sb(st, [128, D], BF16, 'hb') for _ in range(2)]
        pt = [P.ps(st, [128, 8, 128], BF16, 'pt') for _ in range(2)]
        memT = P.sb(st, [128, 8, MEM_LEN], BF16, 'memT')
        wkv = P.sb(st, [128, 8, 512], BF16, 'wkv')
        pk = P.ps(st, [128, 256], F32, 'pk')
        alloc_norm_tmps(P, k, st, ['n0', 'n1'])
        P.dma('sp', gbc[:], gain_row.partition_broadcast(128), writes=['gbc'])
        P.dma('pool', wkv[:], wview(wkv_ap), writes=['wkv'], cast=True)
        P.memset('pool', vm[:], 1.0, writes=['vm'])
        for mt in range(2):
            tag = 'n%d' % mt
            P.dma('sp', xt[mt][:], mem_ap[mt * 128:(mt + 1) * 128, :], writes=[tag + 'x'])
            norm_rows(P, k, st, xt[mt][:], gbc[:], hb[mt][:], tag)
            P.trg([(pt[mt][:, c, :], hb[mt][:, c * 128:(c + 1) * 128], k.ident[:]) for c in range(8)],
                  reads=[tag + 'hb'], writes=[tag + 'pt'])
            P.copy('dve', memT[:, :, mt * 128:(mt + 1) * 128], pt[mt][:], reads=[tag + 'pt'], writes=[('memT', mt)])
        for p in range(2):
            P.mmg([(pk[:], wkv[:, dc, p * 128:(p + 1) * 128], memT[:, dc, :], dc == 0, dc == 7) for dc in range(8)],
                  reads=['wkv', ('memT', 0), ('memT', 1)], writes=['pk'])
            P.copy('dve', kmT[:, p, :], pk[:], reads=['pk'], writes=[('kmT', p)])
        for mt in range(2):
            P.mmg([(pk[:], memT[:, dc, mt * 128:(mt + 1) * 128], wkv[:, dc, 256:512], dc == 0, dc == 7) for dc in range(8)],
                  reads=['wkv', ('memT', 0), ('memT', 1)], writes=['pk'])
            P.copy('dve', vm[:, mt, :, 0:64], pk[:].rearrange("p (h d) -> p h d", h=4), reads=['pk', 'vm'], writes=[('vm', mt)])
        P.flush()


def phase_memattn(P, k, hT, w_in_ap, col0, kmT, vm, o_s):
    with ExitStack() as st:
        wqm = P.sb(st, [128, 8, 256], BF16, 'wqm')
        qmT = P.sb(st, [128, 2, S], BF16, 'qmT')
        pq = [P.ps(st, [128, 512], F32, 'pq') for _ in range(2)]
        sc = [P.ps(st, [128, 512], F32, 'sc') for _ in range(2)]
        pacc = [P.ps(st, [128, 512], F32, 'pacc') for _ in range(4)]
        pT = [P.sb(st, [128, 512], BF16, 'pT') for _ in range(2)]
        rr = [P.sb(st, [128, 1], F32, 'rr') for _ in range(4)]
        omb = [P.sb(st, [128, 4, 256], BF16, 'omb') for _ in range(2)]
        P.dma('pool', wqm[:], wview(w_in_ap)[:, :, col0:col0 + 256], writes=['wqm'], cast=True)
        for tb in range(8):
            for p in range(2):
                b = (tb * 2 + p) % 2
                P.mmg([(pq[b][:], wqm[:, dc, p * 128:(p + 1) * 128], hT[:, dc, tb * 512:(tb + 1) * 512], dc == 0, dc == 7) for dc in range(8)],
                      reads=['wqm'], writes=[('pq', b)])
                P.act(qmT[:, p, tb * 512:(tb + 1) * 512], pq[b][:], AF.Identity, reads=[('pq', b)], writes=[('qmT', tb, p)], scale=0.125)
        cnt = 0
        for tb in range(8):
            ob = omb[tb % 2]
            for hm in range(4):
                p = hm // 2
                base = (hm % 2) * 64
                for mt in range(2):
                    b = cnt % 2
                    cnt += 1
                    P.mmg([(sc[b][:], kmT[base:base + 64, p, mt * 128:(mt + 1) * 128], qmT[base:base + 64, p, tb * 512:(tb + 1) * 512], True, True)],
                          reads=[('qmT', tb, p)], writes=[('sc', b)])
                    P.act(pT[b][:], sc[b][:], AF.Exp, reads=[('sc', b)], writes=[('pT', b)])
                    P.mmg([(pacc[qs][:, 0:65], pT[b][:, qs * 128:(qs + 1) * 128], vm[:, mt, hm, :], mt == 0, mt == 1) for qs in range(4)],
                          reads=[('pT', b)], writes=[('pacc', qs) for qs in range(4)])
                for qs in range(4):
                    P.recip(rr[qs][:], pacc[qs][:, 64:65], reads=[('pacc', qs)], writes=[('rr', qs)])
                    P.act(ob[:, qs, hm * 64:(hm + 1) * 64], pacc[qs][:, 0:64], AF.Identity, reads=[('pacc', qs), ('rr', qs)],
                          writes=[('omb', tb % 2, qs)], scale=rr[qs][:, 0:1])
            for qs in range(4):
                tt = tb * 4 + qs
                P.dma('sp', o_s[tt * 128:(tt + 1) * 128, 768:1024], ob[:, qs, :], reads=[('omb', tb % 2, qs)], writes=[('o_s', tt, 'm')])
        P.flush()


def phase_outproj(P, k, o_s, w_out_ap, x_src, x_dst):
    with ExitStack() as st:
        wo = P.sb(st, [128, 8, D], BF16, 'wo')
        ot = [P.sb(st, [128, D], BF16, 'ot') for _ in range(2)]
        oT = [P.sb(st, [128, 8, 128], BF16, 'oT') for _ in range(2)]
        xt = [P.sb(st, [128, D], F32, 'xt') for _ in range(2)]
        xn = [P.sb(st, [128, D], F32, 'xn') for _ in range(2)]
        pt = [P.ps(st, [128, 8, 128], BF16, 'pt') for _ in range(2)]
        po = [P.ps(st, [128, 512], F32, 'po') for _ in range(4)]
        P.dma('pool', wo[:], wview(w_out_ap), writes=['wo'], cast=True)
        for tt in range(NT):
            b = tt % 2
            P.dma('sp', ot[b][:], o_s[tt * 128:(tt + 1) * 128, :], writes=[('ot', b)])
            P.dma('sp', xt[b][:], x_src[tt * 128:(tt + 1) * 128, :], writes=[('xt', b)])
            P.trg([(pt[b][:, c, :], ot[b][:, c * 128:(c + 1) * 128], k.ident[:]) for c in range(8)],
                  reads=[('ot', b)], writes=[('pt', b)])
            P.copy('act', oT[b][:], pt[b][:], reads=[('pt', b)], writes=[('oT', b)])
            for hh in range(2):
                pb = b * 2 + hh
                P.mmg([(po[pb][:], oT[b][:, fc, :], wo[:, fc, hh * 512:(hh + 1) * 512], fc == 0, fc == 7) for fc in range(8)],
                      reads=[('oT', b), 'wo'], writes=[('po', pb)])
                P.tt('dve', xn[b][:, hh * 512:(hh + 1) * 512], po[pb][:], xt[b][:, hh * 512:(hh + 1) * 512], ALU.add,
                     reads=[('po', pb), ('xt', b)], writes=[('xn', b, hh)])
            P.dma('sp', x_dst[tt * 128:(tt + 1) * 128, :], xn[b][:], reads=[('xn', b, 0), ('xn', b, 1)], writes=[('xd', tt)])
        P.flush()


def phase_diffattn(P, k, hT, j, lam_init, o_s):
    inp = k.inp
    w_in = wview(inp['a_w_in'][j])
    with ExitStack() as st:
        wqkv = [P.sb(st, [128, 8, 384], BF16, 'wqkv') for _ in range(2)]
        qT = [P.sb(st, [128, 2, S], BF16, 'qT') for _ in range(2)]
        kT = [P.sb(st, [128, 2, S], BF16, 'kT') for _ in range(2)]
        va = [P.sb(st, [128, NT, 129], BF16, 'va') for _ in range(2)]
        dmask = P.sb(st, [128, 6, 128], BF16, 'dmask')
        cbias = P.sb(st, [128, 6 * 35], F32, 'cbias')
        gsub = P.sb(st, [128, 128], F32, 'gsub')
        lv = [P.sb(st, [128, 64], F32, 'lv') for _ in range(4)]
        lt = P.sb(st, [128, 64], F32, 'lt')
        ls = [P.sb(st, [128, 1], F32, 'ls') for _ in range(2)]
        neglam = P.sb(st, [128, 1], F32, 'neglam')
        clam = P.sb(st, [128, 2], F32, 'clam')
        pT = [P.sb(st, [128, 512], BF16, 'pT') for _ in range(3)]
        o0 = P.sb(st, [128, 4, 128], F32, 'o0')
        oo = [P.sb(st, [128, 128], F32, 'oo') for _ in range(2)]
        ob = [P.sb(st, [128, 128], BF16, 'ob') for _ in range(2)]
        junk = P.sb(st, [128, 128], F32, 'junk')
        sm = {n: [P.sb(st, [128, 1], F32, n) for _ in range(2)] for n in ('r0', 'r1', 'ssq', 'ms', 'lnm', 'rstd')}
        pp = [P.ps(st, [128, 512], F32, 'pp') for _ in range(2)]
        sc = [P.ps(st, [128, 512], F32, 'sc') for _ in range(2)]
        pacc = [P.ps(st, [128, 512], F32, 'pacc') for _ in range(4)]

        P.dma('pool', dmask[:], inp['c_dmask'].rearrange("h k q -> k h q"), writes=['dmask'], cast=True)
        P.dma('sp', cbias[:], inp['c_bias'], writes=['cbias'])
        P.dma('sp', gsub[:], inp['a_subln'][j:j + 1, :].partition_broadcast(128), writes=['gsub'])
        P.dma('sp', clam[:], inp['c_lam'][j], writes=['clam'])
        P.ts('dve', gsub[:], gsub[:], clam[:, 0:1], None, ALU.mult, None, reads=['gsub', 'clam'], writes=['gsub'])
        for i, nm in enumerate(['a_lam_q1', 'a_lam_k1', 'a_lam_q2', 'a_lam_k2']):
            P.dma('sp', lv[i][:], inp[nm][j:j + 1, :].partition_broadcast(128), writes=[('lv', i)])
        for i in range(2):
            P.tt('dve', lt[:], lv[2 * i][:], lv[2 * i + 1][:], ALU.mult, reads=[('lv', 2 * i), ('lv', 2 * i + 1)], writes=['lt'])
            P.reduce(ls[i][:], lt[:], ALU.add, reads=['lt'], writes=[('ls', i)])
            P.act(ls[i][:], ls[i][:], AF.Exp, reads=[('ls', i)], writes=[('ls', i)])
        P.tt('dve', neglam[:], ls[1][:], ls[0][:], ALU.subtract, reads=[('ls', 0), ('ls', 1)], writes=['neglam'])
        P.ts('dve', neglam[:], neglam[:], clam[:, 1:2], None, ALU.add, None, reads=['neglam', 'clam'], writes=['neglam'])
        for b in range(2):
            P.memset('pool', va[b][:, :, 128:129], 1.0, writes=[('va1', b)])

        def project(h):
            b = h % 2
            W = wqkv[b]
            for i, c0 in enumerate([h * 128, DA_W + h * 128, 2 * DA_W + h * 128]):
                P.dma('pool', W[:, :, i * 128:(i + 1) * 128], w_in[:, :, c0:c0 + 128], writes=[('w', b, i)], cast=True)
            for m in range(2):
                P.dma('pool', qT[b][64:70, m, :], inp['c_qaug'][h], writes=[('qa', b, m)], cast=True)
                P.dma('pool', kT[b][64:70, m, :], inp['c_kaug'][h], writes=[('ka', b, m)], cast=True)
            n = 0
            for tb in range(8):
                for m in range(2):
                    for isk in range(2):
                        pb = n % 2
                        n += 1
                        c0 = isk * 128 + m * 64
                        P.mmg([(pp[pb][0:64, :], W[:, dc, c0:c0 + 64], hT[:, dc, tb * 512:(tb + 1) * 512], dc == 0, dc == 7) for dc in range(8)],
                              reads=[('w', b, isk)], writes=[('pp', pb)])
                        if isk == 0:
                            P.act(qT[b][0:64, m, tb * 512:(tb + 1) * 512], pp[pb][0:64, :], AF.Identity, reads=[('pp', pb)], writes=[('q', b, m, tb)], scale=0.125)
                        else:
                            P.copy('dve', kT[b][0:64, m, tb * 512:(tb + 1) * 512], pp[pb][0:64, :], reads=[('pp', pb)], writes=[('k', b, m, tb)])
            for tt in range(NT):
                pb = n % 2
                n += 1
                P.mmg([(pp[pb][:, 0:128], hT[:, dc, tt * 128:(tt + 1) * 128], W[:, dc, 256:384], dc == 0, dc == 7) for dc in range(8)],
                      reads=[('w', b, 2)], writes=[('pp', pb)])
                P.copy('dve' if tt % 2 else 'act', va[b][:, tt, 0:128], pp[pb][:, 0:128], reads=[('pp', pb), ('va1', b)], writes=[('v', b, tt)])

        def attend(h):
            b = h % 2
            cnt = 0
            ev = 0
            for jq in range(8):
                for m in range(2):
                    nkt = 4 * jq + 4
                    for kt in range(nkt):
                        r = kt - 4 * jq
                        off = max(r, 0) * 128
                        N = 512 - off
                        sb_ = cnt % 2
                        pb = cnt % 3
                        cnt += 1
                        P.mmg([(sc[sb_][:, 0:N], kT[b][0:70, m, kt * 128:(kt + 1) * 128], qT[b][0:70, m, jq * 512 + off:(jq + 1) * 512], True, True)],
                              reads=[('q', b, m, jq), ('qa', b, m), ('ka', b, m)] + [('k', b, m, kt // 4)], writes=[('sc', sb_)])
                        bi = h * 35 + (4 * jq - kt + 3)
                        P.act(pT[pb][:, off:512], sc[sb_][:, 0:N], AF.Exp, reads=[('sc', sb_), 'cbias'], writes=[('pT', pb)], bias=cbias[:, bi:bi + 1])
                        if r >= 0:
                            P.tt('dve', pT[pb][:, off:off + 128], pT[pb][:, off:off + 128], dmask[:, h, :], ALU.mult,
                                 reads=[('pT', pb), 'dmask'], writes=[('pT', pb)])
                        qs0 = max(r, 0)
                        P.mmg([(pacc[qs][:, 0:129], pT[pb][:, qs * 128:(qs + 1) * 128], va[b][:, kt, :], kt == 0, kt == 4 * jq + qs) for qs in range(qs0, 4)],
                              reads=[('pT', pb), ('v', b, kt), ('va1', b)], writes=[('pacc', qs) for qs in range(qs0, 4)])
                    for qs in range(4):
                        e = ev % 2
                        if m == 0:
                            P.recip(sm['r0'][e][:], pacc[qs][:, 128:129], reads=[('pacc', qs)], writes=[('r0', e)])
                            P.act(o0[:, qs, :], pacc[qs][:, 0:128], AF.Identity, reads=[('pacc', qs), ('r0', e)], writes=[('o0', qs)], scale=sm['r0'][e][:, 0:1])
                        else:
                            P.recip(sm['r1'][e][:], pacc[qs][:, 128:129], reads=[('pacc', qs)], writes=[('r1', e)])
                            P.tt('dve', sm['r1'][e][:], sm['r1'][e][:], neglam[:], ALU.mult, reads=[('r1', e), 'neglam'], writes=[('r1', e)])
                            P.stt(oo[e][:], pacc[qs][:, 0:128], sm['r1'][e][:, 0:1], o0[:, qs, :], ALU.mult, ALU.add,
                                  reads=[('pacc', qs), ('r1', e), ('o0', qs)], writes=[('oo', e)])
                            P.act(junk[:], oo[e][:], AF.Square, reads=[('oo', e)], writes=['junk', ('ssq', e)], accum=sm['ssq'][e][:])
                            P.ts('dve', sm['ms'][e][:], sm['ssq'][e][:], 1.0 / 128, EPS, ALU.mult, ALU.add, reads=[('ssq', e)], writes=[('ms', e)])
                            P.act(sm['lnm'][e][:], sm['ms'][e][:], AF.Ln, reads=[('ms', e)], writes=[('lnm', e)])
                            P.act(sm['rstd'][e][:], sm['lnm'][e][:], AF.Exp, reads=[('lnm', e)], writes=[('rstd', e)], scale=-0.5)
                            P.stt(ob[e][:], oo[e][:], sm['rstd'][e][:, 0:1], gsub[:], ALU.mult, ALU.mult,
                                  reads=[('oo', e), ('rstd', e), 'gsub'], writes=[('ob', e)])
                            tt = jq * 4 + qs
                            P.dma('sp', o_s[tt * 128:(tt + 1) * 128, h * 128:(h + 1) * 128], ob[e][:], reads=[('ob', e)], writes=[('o_s', tt, h)])
                        ev += 1

        project(0)
        for h in range(6):
            if h + 1 < 6:
                project(h + 1)
            attend(h)
        P.flush()


def phase_hgrn(P, k, hT, j, o_s):
    inp = k.inp
    w_in = wview(inp['b_w_in'][j])
    with ExitStack() as st:
        W = [P.sb(st, [128, 8, 512], BF16, 'W') for _ in range(1)]
        qeT = [P.sb(st, [128, S], BF16, 'qeT') for _ in range(1)]
        keT = [P.sb(st, [128, S], BF16, 'keT') for _ in range(1)]
        ketok = [P.sb(st, [128, NT, 128], BF16, 'ketok') for _ in range(1)]
        vtok = [P.sb(st, [128, NT, 128], BF16, 'vtok') for _ in range(1)]
        sgtok = [P.sb(st, [128, NT, 128], BF16, 'sgtok') for _ in range(1)]
        decay = [P.sb(st, [128, 64], F32, 'decay') for _ in range(1)]
        lbl = P.sb(st, [128, 2, 6], F32, 'lbl')
        oml = P.sb(st, [128, 6], F32, 'oml')
        gn = P.sb(st, [128, 128], F32, 'gn')
        cmask = P.sb(st, [128, 128], BF16, 'cmask')
        smask = P.sb(st, [128, 512], F32, 'smask')
        et = P.sb(st, [128, 512], F32, 'et')
        dt_ = P.sb(st, [128, 512], F32, 'dt')
        kk = P.sb(st, [128, 512], F32, 'kk')
        gt = P.sb(st, [128, 512], F32, 'gt')
        bt = P.sb(st, [128, 512], F32, 'bt')
        eb = P.sb(st, [128, 512], F32, 'eb')
        enb = P.sb(st, [128, 512], F32, 'enb')
        Sst = P.sb(st, [128, 128], F32, 'Sst')
        Sbf = P.sb(st, [128, 128], BF16, 'Sbf')
        at = [P.sb(st, [128, 128], BF16, 'at') for _ in range(2)]
        o1 = [P.sb(st, [128, 128], F32, 'o1') for _ in range(2)]
        ob = [P.sb(st, [128, 128], BF16, 'ob') for _ in range(2)]
        junk = P.sb(st, [128, 128], F32, 'junk')
        sm = {n: [P.sb(st, [128, 1], F32, n) for _ in range(2)] for n in ('ssq', 'ms', 'lnm', 'rstd')}
        pq = P.ps(st, [128, 512], F32, 'pq')
        qraw = P.sb(st, [128, 512], F32, 'qraw')
        ptr = P.ps(st, [128, 4, 128], BF16, 'ptr')
        pvg2 = [P.ps(st, [128, 128], F32, 'pvg') for _ in range(2)]
        pat = P.ps(st, [128, 128], F32, 'pat')
        pkv2 = [P.ps(st, [128, 128], F32, 'pkv') for _ in range(2)]
        po = [P.ps(st, [128, 128], F32, 'po') for _ in range(1)]

        P.dma('pool', cmask[:], inp['c_cmask'], writes=['cmask'], cast=True)
        P.dma('sp', smask[:], inp['c_smask'], writes=['smask'])
        P.dma('sp', gn[:], inp['b_out_norm'][j:j + 1, :].partition_broadcast(128), writes=['gn'])
        lbf = P.sb(st, [128, 1], F32, 'lbf')
        P.dma('sp', lbf[:], inp['c_lbflag'][j], writes=['lbf'])
        lbl2 = P.sb(st, [2, 768], F32, 'lbl2')
        lbT = P.sb(st, [128, 6, 2], F32, 'lbT')
        P.dma('sp', lbl2[:], inp['b_lb_logits'], writes=['lbl2'])
        P.trg([(pvg2[0][:, 2 * h:2 * h + 2], lbl2[0:2, h * 128:(h + 1) * 128], k.identf[0:2, 0:2]) for h in range(6)], reads=['lbl2'], writes=[('pvg', 0)])
        P.copy('dve', lbT[:], pvg2[0][:, 0:12].rearrange("p (h l) -> p h l", l=2), reads=[('pvg', 0)], writes=['lbl'])
        P.tt('dve', oml[:], lbT[:, :, 0], lbT[:, :, 1], ALU.subtract, reads=['lbl'], writes=['oml'])
        P.act(oml[:], oml[:], AF.Exp, reads=['oml'], writes=['oml'])
        P.ts('dve', oml[:], oml[:], 1.0, None, ALU.add, None, reads=['oml'], writes=['oml'])
        P.recip(oml[:], oml[:], reads=['oml'], writes=['oml'])
        P.ts('dve', oml[:], oml[:], lbf[:, 0:1], 1.0, ALU.mult, ALU.add, reads=['oml', 'lbf'], writes=['oml'])

        def project(h):
            b = 0
            for i, c0 in enumerate([h * 128, HG_W + h * 128, 2 * HG_W + h * 128, 3 * HG_W + h * 128]):
                P.dma('pool', W[b][:, :, i * 128:(i + 1) * 128], w_in[:, :, c0:c0 + 128], writes=[('w', b, i)], cast=True)
            for tb in range(8):
                blk = slice(tb * 512, (tb + 1) * 512)
                P.mmg([(pq[:], W[b][:, dc, 0:128], hT[:, dc, blk], dc == 0, dc == 7) for dc in range(8)], reads=[('w', b, 0)], writes=['pq'])
                P.copy('act', qraw[:], pq[:], reads=['pq'], writes=['qraw'])
                P.mmg([(pq[:], W[b][:, dc, 128:256], hT[:, dc, blk], dc == 0, dc == 7) for dc in range(8)], reads=[('w', b, 1)], writes=['pq'])
                P.act(et[:], pq[:], AF.Exp, reads=['pq'], writes=['et'], scale=-1.0)
                P.ts('dve', dt_[:], et[:], 1.0, None, ALU.add, None, reads=['et'], writes=['dt'])
                P.recip(dt_[:], dt_[:], reads=['dt'], writes=['dt'])
                P.stt(kk[:], et[:], oml[:, h:h + 1], dt_[:], ALU.mult, ALU.mult, reads=['et', 'dt', 'oml'], writes=['kk'])
                P.act(gt[:], kk[:], AF.Ln, reads=['kk'], writes=['gt'], scale=-1.0, bias=1.0)
                P.scan(bt[:], smask[:], gt[:], reads=['smask', 'gt'], writes=['bt'])
                P.act(eb[:], bt[:], AF.Exp, reads=['bt'], writes=['eb'])
                P.act(enb[:], bt[:], AF.Exp, reads=['bt'], writes=['enb'], scale=-1.0)
                P.tt('dve', qeT[b][:, blk], qraw[:], eb[:], ALU.mult, reads=['qraw', 'eb'], writes=[('qe', b, tb)])
                P.tt('dve', keT[b][:, blk], kk[:], enb[:], ALU.mult, reads=['kk', 'enb'], writes=[('ke', b, tb)])
                P.copy('act', decay[b][:, tb * 8:(tb + 1) * 8], eb[:].rearrange("p (c t) -> p c t", t=64)[:, :, 63], reads=['eb'], writes=[('dec', b, tb)])
                P.trg([(ptr[:, i, :], keT[b][:, tb * 512 + i * 128: tb * 512 + (i + 1) * 128], k.ident[:]) for i in range(4)],
                      reads=[('ke', b, tb)], writes=['ptr'])
                P.copy('act', ketok[b][:, tb * 4:(tb + 1) * 4, :], ptr[:], reads=['ptr'], writes=[('ketok', b, tb)])
                for tq in range(4):
                    tt = tb * 4 + tq
                    tok = slice(tt * 128, (tt + 1) * 128)
                    P.mmg([(pvg2[0][:], hT[:, dc, tok], W[b][:, dc, 256:384], dc == 0, dc == 7) for dc in range(8)], reads=[('w', b, 2)], writes=[('pvg', 0)])
                    P.copy('dve', vtok[b][:, tt, :], pvg2[0][:], reads=[('pvg', 0)], writes=[('vtok', b, tt)])
                    P.mmg([(pvg2[1][:], hT[:, dc, tok], W[b][:, dc, 384:512], dc == 0, dc == 7) for dc in range(8)], reads=[('w', b, 3)], writes=[('pvg', 1)])
                    P.act(sgtok[b][:, tt, :], pvg2[1][:], AF.Silu, reads=[('pvg', 1)], writes=[('sgtok', b, tt)])

        def recur(h):
            b = 0
            P.memset('pool', Sst[:], 0.0, writes=['Sst'])
            P.memset('pool', Sbf[:], 0.0, writes=['Sbf'])
            for tt in range(NT):
                e = 0
                tb = tt // 4
                tok = slice(tt * 128, (tt + 1) * 128)
                P.mmg([(pat[:], keT[b][:, tok], qeT[b][:, tok], True, True)], reads=[('ke', b, tb), ('qe', b, tb)], writes=['pat'])
                P.tt('dve', at[e][:], pat[:], cmask[:], ALU.mult, reads=['pat', 'cmask'], writes=[('at', e)])
                P.mmg([(pkv2[ci][:], ketok[b][ci * 64:(ci + 1) * 64, tt, :], vtok[b][ci * 64:(ci + 1) * 64, tt, :], True, True) for ci in range(2)],
                      reads=[('ketok', b, tb), ('vtok', b, tt)], writes=['pkv'])
                P.mmg([(po[e][:], at[e][:], vtok[b][:, tt, :], True, False),
                       (po[e][0:64, :], qeT[b][:, tt * 128:tt * 128 + 64], Sbf[:], False, False)],
                      reads=[('at', e), ('vtok', b, tt), ('qe', b, tb), 'Sbf'], writes=[('po', e)])
                for ci in range(2):
                    c = 2 * tt + ci
                    P.tt('dve', Sst[:], pkv2[ci][:], Sst[:], ALU.add, reads=['pkv', 'Sst'], writes=['Sst'])
                    P.ts('dve', Sst[:], Sst[:], decay[b][:, c:c + 1], None, ALU.mult, None, reads=['Sst', ('dec', b, tb)], writes=['Sst'])
                    P.copy('act', Sbf[:], Sst[:], reads=['Sst'], writes=['Sbf'])
                    if ci == 0:
                        P.mmg([(po[e][64:128, :], qeT[b][:, tt * 128 + 64:(tt + 1) * 128], Sbf[:], False, True)],
                              reads=[('qe', b, tb), 'Sbf'], writes=[('po', e)])
                P.act(junk[:], po[e][:], AF.Square, reads=[('po', e)], writes=['junk', ('ssq', e)], accum=sm['ssq'][e][:])
                P.ts('dve', sm['ms'][e][:], sm['ssq'][e][:], 1.0 / 128, EPS, ALU.mult, ALU.add, reads=[('ssq', e)], writes=[('ms', e)])
                P.act(sm['lnm'][e][:], sm['ms'][e][:], AF.Ln, reads=[('ms', e)], writes=[('lnm', e)])
                P.act(sm['rstd'][e][:], sm['lnm'][e][:], AF.Exp, reads=[('lnm', e)], writes=[('rstd', e)], scale=-0.5)
                P.stt(o1[e][:], po[e][:], sm['rstd'][e][:, 0:1], gn[:], ALU.mult, ALU.mult, reads=[('po', e), ('rstd', e), 'gn'], writes=[('o1', e)])
                P.tt('dve', ob[e][:], o1[e][:], sgtok[b][:, tt, :], ALU.mult, reads=[('o1', e), ('sgtok', b, tt)], writes=[('ob', e)])
                P.dma('sp', o_s[tt * 128:(tt + 1) * 128, h * 128:(h + 1) * 128], ob[e][:], reads=[('ob', e)], writes=[('o_s', tt, h)])

        import os
        dbgm = int(os.environ.get('HG_DBG', '0'))
        for h in range(6):
            if dbgm == 1:
                break
            project(h)
            if dbgm == 2:
                continue
            recur(h)
        P.flush()


def phase_ffn(P, k, x_src, xacc_src, x_dst, gain_row, w_gu_list, w_dn_list, dff, router_ap=None, esel_ap=None):
    HT = S // 2
    NTH = NT // 2
    ngrp = (dff + 511) // 512
    moe = router_ap is not None
    same = xacc_src is x_src
    nexp = len(w_gu_list)
    for half in range(2):
        with ExitStack() as st0:
            xacc = P.sb(st0, [128, NTH, D], F32, 'xacc')
            hTh = P.sb(st0, [128, 8, HT], BF16, 'hTh')
            csel = P.sb(st0, [128, NTH], F32, 'csel')
            comb = P.sb(st0, [128, NTH, 8], F32, 'comb')
            with ExitStack() as st:
                gbc = P.sb(st, [128, D], F32, 'gbc')
                hb = [P.sb(st, [128, D], BF16, 'hb') for _ in range(2)]
                pt = [P.ps(st, [128, 8, 128], BF16, 'pt') for _ in range(2)]
                alloc_norm_tmps(P, k, st, ['n0', 'n1'])
                P.dma('sp', gbc[:], gain_row.partition_broadcast(128), writes=['gbc'])
                if moe:
                    xp = [P.sb(st, [128, D], F32, 'xp') for _ in range(2)]
                    hf2 = [P.sb(st, [128, D], F32, 'hf') for _ in range(2)]
                    hTf = P.sb(st, [128, 8, 128], F32, 'hTf')
                    wr = P.sb(st, [128, 8, 8], F32, 'wr')
                    esel = P.sb(st, [128, 8], F32, 'esel')
                    ptf = [P.ps(st, [128, 4, 128], F32, 'ptf') for _ in range(2)]
                    plg = P.ps(st, [128, 8], F32, 'plg')
                    lg = P.sb(st, [128, 8], F32, 'lg')
                    lg2 = P.sb(st, [128, 8], F32, 'lg2')
                    eq1 = P.sb(st, [128, 8], F32, 'eq1')
                    eq2 = P.sb(st, [128, 8], F32, 'eq2')
                    m1 = P.sb(st, [128, 1], F32, 'm1')
                    m2 = P.sb(st, [128, 1], F32, 'm2')
                    w1 = P.sb(st, [128, 1], F32, 'w1')
                    w2 = P.sb(st, [128, 1], F32, 'w2')
                    P.dma('sp', wr[:], wview(router_ap), writes=['wr'])
                    if esel_ap is not None:
                        P.dma('sp', esel[:], esel_ap, writes=['esel'])
                for tl in range(NTH):
                    tt = half * NTH + tl
                    b = tl % 2
                    tag = 'n%d' % b
                    rows = slice(tt * 128, (tt + 1) * 128)
                    if moe:
                        hf = hf2[b]
                        if same:
                            P.dma('sp', xacc[:, tl, :], x_src[rows, :], writes=[tag + 'x', ('xacc', tl)])
                            norm_rows(P, k, st, xacc[:, tl, :], gbc[:], hb[b][:], tag, want_f32=hf[:])
                        else:
                            P.dma('sp', xacc[:, tl, :], xacc_src[rows, :], writes=[('xacc', tl)])
                            P.dma('sp', xp[b][:], x_src[rows, :], writes=[tag + 'x'])
                            norm_rows(P, k, st, xp[b][:], gbc[:], hb[b][:], tag, want_f32=hf[:])
                        for q4 in range(2):
                            P.trg([(ptf[q4][:, c, :], hf[:, (q4 * 4 + c) * 128:(q4 * 4 + c + 1) * 128], k.identf[:]) for c in range(4)],
                                  reads=[tag + 'hf'], writes=[('ptf', q4)])
                            P.copy('dve', hTf[:, q4 * 4:(q4 + 1) * 4, :], ptf[q4][:], reads=[('ptf', q4)], writes=[('hTf', q4)])
                        P.mmg([(plg[:], hTf[:, dc, :], wr[:, dc, :], dc == 0, dc == 7) for dc in range(8)],
                              reads=[('hTf', 0), ('hTf', 1), 'wr'], writes=['plg'])
                        P.copy('dve', lg[:], plg[:], reads=['plg'], writes=['lg'])
                        P.reduce(m1[:], lg[:], ALU.max, reads=['lg'], writes=['m1'])
                        P.ts('dve', eq1[:], lg[:], m1[:, 0:1], None, ALU.is_equal, None, reads=['lg', 'm1'], writes=['eq1'])
                        P.stt(lg2[:], eq1[:], -1e30, lg[:], ALU.mult, ALU.add, reads=['eq1', 'lg'], writes=['lg2'])
                        P.reduce(m2[:], lg2[:], ALU.max, reads=['lg2'], writes=['m2'])
                        P.ts('dve', eq2[:], lg2[:], m2[:, 0:1], None, ALU.is_equal, None, reads=['lg2', 'm2'], writes=['eq2'])
                        P.tt('dve', w2[:], m2[:], m1[:], ALU.subtract, reads=['m1', 'm2'], writes=['w2'])
                        P.act(w2[:], w2[:], AF.Exp, reads=['w2'], writes=['w2'])
                        P.ts('dve', w1[:], w2[:], 1.0, None, ALU.add, None, reads=['w2'], writes=['w1'])
                        P.recip(w1[:], w1[:], reads=['w1'], writes=['w1'])
                        P.tt('dve', w2[:], w2[:], w1[:], ALU.mult, reads=['w1', 'w2'], writes=['w2'])
                        P.ts('dve', eq1[:], eq1[:], w1[:, 0:1], None, ALU.mult, None, reads=['eq1', 'w1'], writes=['eq1'])
                        if esel_ap is None:
                            P.stt(comb[:, tl, :], eq2[:], w2[:, 0:1], eq1[:], ALU.mult, ALU.add, reads=['eq2', 'w2', 'eq1'], writes=[('comb', tl)])
                        else:
                            P.stt(eq2[:], eq2[:], w2[:, 0:1], eq1[:], ALU.mult, ALU.add, reads=['eq2', 'w2', 'eq1'], writes=['eq2'])
                            P.tt('dve', eq2[:], eq2[:], esel[:], ALU.mult, reads=['eq2', 'esel'], writes=['eq2'])
                            P.reduce(csel[:, tl:tl + 1], eq2[:], ALU.add, reads=['eq2'], writes=[('csel', tl)])
                    else:
                        P.dma('sp', xacc[:, tl, :], x_src[rows, :], writes=[tag + 'x', ('xacc', tl)])
                        norm_rows(P, k, st, xacc[:, tl, :], gbc[:], hb[b][:], tag)
                    P.trg([(pt[b][:, c, :], hb[b][:, c * 128:(c + 1) * 128], k.ident[:]) for c in range(8)],
                          reads=[tag + 'hb'], writes=[tag + 'pt'])
                    P.copy('act' if tl % 2 else 'dve', hTh[:, :, tl * 128:(tl + 1) * 128], pt[b][:], reads=[tag + 'pt'], writes=[('hT', tl)])
                P.flush()
            with ExitStack() as st:
                wg = [P.sb(st, [128, 8, 512], BF16, 'wg') for _ in range(2)]
                wu = [P.sb(st, [128, 8, 512], BF16, 'wu') for _ in range(2)]
                wd = [P.sb(st, [128, 4, D], BF16, 'wd') for _ in range(2)]
                sg = [P.sb(st, [128, 512], F32, 'sg') for _ in range(2)]
                aT = [P.sb(st, [128, 4, 512], BF16, 'aT') for _ in range(2)]
                pg = [P.ps(st, [128, 512], F32, 'pg') for _ in range(2)]
                pu = [P.ps(st, [128, 512], F32, 'pu') for _ in range(2)]
                po = [P.ps(st, [128, 512], F32, 'po') for _ in range(4)]
                it = 0
                gi = 0
                oi = 0
                ai = 0
                for e in range(nexp):
                    gu = wview(w_gu_list[e])
                    w_dn_ap = w_dn_list[e]
                    for fg in range(ngrp):
                        F = min(512, dff - fg * 512)
                        nfc = F // 128
                        wb = it % 2
                        it += 1
                        P.dma('pool', wg[wb][:, :, 0:F], gu[:, :, fg * 512:fg * 512 + F], writes=[('wg', wb)], cast=True)
                        P.dma('pool', wu[wb][:, :, 0:F], gu[:, :, dff + fg * 512:dff + fg * 512 + F], writes=[('wu', wb)], cast=True)
                        P.dma('pool', wd[wb][:, 0:nfc, :], wview(w_dn_ap[fg * 512:fg * 512 + F, :]), writes=[('wd', wb)], cast=True)
                        for tb in range(HT // 512):
                            blk = slice(tb * 512, (tb + 1) * 512)
                            ab = ai % 2
                            ai += 1
                            for fc in range(nfc):
                                g = gi % 2
                                gi += 1
                                P.mmg([(pg[g][:], wg[wb][:, dc, fc * 128:(fc + 1) * 128], hTh[:, dc, blk], dc == 0, dc == 7) for dc in range(8)],
                                      reads=[('wg', wb)], writes=[('pg', g)])
                                P.mmg([(pu[g][:], wu[wb][:, dc, fc * 128:(fc + 1) * 128], hTh[:, dc, blk], dc == 0, dc == 7) for dc in range(8)],
                                      reads=[('wu', wb)], writes=[('pu', g)])
                                P.act(sg[g][:], pg[g][:], AF.Silu, reads=[('pg', g)], writes=[('sg', g)])
                                P.tt('dve', aT[ab][:, fc, :], pu[g][:], sg[g][:], ALU.mult, reads=[('pu', g), ('sg', g)], writes=[('aT', ab, fc)])
                            for tq in range(4):
                                tl = tb * 4 + tq
                                for hh in range(2):
                                    o = oi % 4
                                    oi += 1
                                    P.mmg([(po[o][:], aT[ab][:, fc, tq * 128:(tq + 1) * 128], wd[wb][:, fc, hh * 512:(hh + 1) * 512], fc == 0, fc == nfc - 1) for fc in range(nfc)],
                                          reads=[('aT', ab, fc) for fc in range(nfc)] + [('wd', wb)], writes=[('po', o)])
                                    sc_ = (comb[:, tl, e:e + 1] if esel_ap is None else csel[:, tl:tl + 1]) if moe else 1.0
                                    P.stt(xacc[:, tl, hh * 512:(hh + 1) * 512], po[o][:], sc_, xacc[:, tl, hh * 512:(hh + 1) * 512], ALU.mult, ALU.add,
                                          reads=[('po', o), ('xacc', tl, hh)], writes=[('xacc', tl, hh)])
                for tl in range(NTH):
                    tt = half * NTH + tl
                    P.dma('sp', x_dst[tt * 128:(tt + 1) * 128, :], xacc[:, tl, :], reads=[('xacc', tl, 0), ('xacc', tl, 1)], writes=[('xd', tt)])
                P.flush()


def phase_fnorm(P, k, x_src, gain_row, out_ap):
    with ExitStack() as st:
        gbc = P.sb(st, [128, D], F32, 'gbc')
        xt = [P.sb(st, [128, D], F32, 'xt') for _ in range(2)]
        yo = [P.sb(st, [128, D], F32, 'yo') for _ in range(2)]
        alloc_norm_tmps(P, k, st, ['n0', 'n1'])
        P.dma('sp', gbc[:], gain_row.partition_broadcast(128), writes=['gbc'])
        for tt in range(NT):
            b = tt % 2
            tag = 'n%d' % b
            t = k.nt[tag]
            P.dma('sp', xt[b][:], x_src[tt * 128:(tt + 1) * 128, :], writes=[tag + 'x'])
            P.act(t['junk'][:], xt[b][:], AF.Square, reads=[tag + 'x'], writes=[tag + 'junk', tag + 'ssq'], accum=t['ssq'][:])
            P.ts('dve', t['ms'][:], t['ssq'][:], 1.0 / D, EPS, ALU.mult, ALU.add, reads=[tag + 'ssq'], writes=[tag + 'ms'])
            P.tt('pool', t['rstd'][:], t['ms'][:], k.neghalf[:], ALU.pow, reads=[tag + 'ms'], writes=[tag + 'rstd'])
            P.stt(yo[b][:], xt[b][:], t['rstd'][:, 0:1], gbc[:], ALU.mult, ALU.mult, reads=[tag + 'x', tag + 'rstd', 'gbc'], writes=[('yo', b)])
            P.dma('sp', out_ap[tt * 128:(tt + 1) * 128, :], yo[b][:], reads=[('yo', b)], writes=[('out', tt)])
        P.flush()


CONST_SHAPES = {"c_ident": [128, 128], "c_kaug": [6, 6, S], "c_qaug": [6, 6, S], "c_dmask": [6, 128, 128], "c_bias": [128, 6 * 35],
                "c_cmask": [128, 128], "c_smask": [128, 512]}
STEP_INPUTS = {
    'attn': {"x": [S, D], "mem": [MEM_LEN, D], "a_norm_mix": [1, D], "a_w_in": [1, D, 2560], "a_lam_q1": [1, 64], "a_lam_k1": [1, 64],
             "a_lam_q2": [1, 64], "a_lam_k2": [1, 64], "a_subln": [1, 128], "a_mem_norm": [1, D], "a_w_mem_kv": [1, D, 512],
             "a_w_out": [1, D, D], "c_lam": [1, 128, 2], "c_ident": 0, "c_kaug": 0, "c_qaug": 0, "c_dmask": 0, "c_bias": 0},
    'hgrn': {"x": [S, D], "mem": [MEM_LEN, D], "b_norm_mix": [1, D], "b_w_in": [1, D, 3328], "b_lb_logits": [2, 768], "b_out_norm": [1, 128],
             "b_mem_norm": [1, D], "b_w_mem_kv": [1, D, 512], "b_w_out": [1, D, D], "c_lbflag": [1, 128, 1], "c_ident": 0, "c_cmask": 0, "c_smask": 0},
    'dense': {"x": [S, D], "norm": [1, D], "w_gu": [D, 2 * DFF_D], "w_dn": [DFF_D, D], "c_ident": 0},
    'moe1': {"x": [S, D], "xacc": [S, D], "norm": [1, D], "router": [D, 8], "w_gu": [D, 2 * DFF_E], "w_dn": [DFF_E, D], "esel": [128, 8], "c_ident": 0},
    'fnorm': {"x": [S, D], "gain": [1, D], "c_ident": 0},
}


def build_step(kind):
    nc = bass.Bass("TRN2", target_bir_lowering=False)
    inp = {}
    for n, shp in STEP_INPUTS[kind].items():
        if shp == 0:
            shp = CONST_SHAPES[n]
        inp[n] = nc.dram_tensor(n, shp, F32, kind="ExternalInput").ap()
    out = nc.dram_tensor("out", [S, D], F32, kind="ExternalOutput").ap()
    o_s = nc.dram_tensor("o_s", [S, D], BF16, kind="Internal").ap()
    with ExitStack() as st:
        P = Prog(nc, st)
        k = K()
        k.inp = inp
        k.ident = P.sb(st, [128, 128], BF16, 'ident')
        k.identf = P.sb(st, [128, 128], F32, 'identf')
        k.neghalf = P.sb(st, [128, 1], F32, 'neghalf')
        P.dma('pool', k.ident[:], inp['c_ident'], writes=['ident'], cast=True)
        P.dma('sp', k.identf[:], inp['c_ident'], writes=['identf'])
        P.memset('pool', k.neghalf[:], -0.5, writes=['neghalf'])
        P.flush()
        if kind in ('attn', 'hgrn'):
            pre = 'a_' if kind == 'attn' else 'b_'
            with ExitStack() as stm:
                hT = P.sb(stm, [128, 8, S], BF16, 'hT')
                kmT = P.sb(stm, [128, 2, MEM_LEN], BF16, 'kmT')
                vm = P.sb(stm, [128, 2, 4, 65], BF16, 'vm')
                phase_hT(P, k, inp['x'], inp[pre + 'norm_mix'][0:1, :], hT)
                phase_memkv(P, k, inp['mem'], inp[pre + 'mem_norm'][0:1, :], inp[pre + 'w_mem_kv'][0], kmT, vm)
                if kind == 'attn':
                    phase_diffattn(P, k, hT, 0, None, o_s)
                    phase_memattn(P, k, hT, inp['a_w_in'][0], 3 * DA_W, kmT, vm, o_s)
                else:
                    phase_hgrn(P, k, hT, 0, o_s)
                    phase_memattn(P, k, hT, inp['b_w_in'][0], 4 * HG_W, kmT, vm, o_s)
            phase_outproj(P, k, o_s, inp[pre + 'w_out'][0], inp['x'], out)
        elif kind == 'dense':
            phase_ffn(P, k, inp['x'], inp['x'], out, inp['norm'][0:1, :], [inp['w_gu']], [inp['w_dn']], DFF_D)
        elif kind == 'moe1':
            phase_ffn(P, k, inp['x'], inp['xacc'], out, inp['norm'][0:1, :], [inp['w_gu']], [inp['w_dn']], DFF_E,
                      router_ap=inp['router'], esel_ap=inp['esel'])
        elif kind == 'fnorm':
            phase_fnorm(P, k, inp['x'], inp['gain'][0:1, :], out)
        P.add('sp', lambda e: e.nop(), reads=[], writes=[])
        P.flush()
    return nc


def make_consts():
    slopes = 2.0 ** (-8.0 * np.arange(1, 7) / 6.0)
    import ml_dtypes
    bf = ml_dtypes.bfloat16

    def hi_lo(v):
        hi = np.float32(np.float32(v).astype(bf).astype(np.float32))
        lo = np.float32(np.float32(v - hi).astype(bf).astype(np.float32))
        return hi, lo
    c = {}
    c['c_ident'] = np.eye(128, dtype=np.float32)
    pos = np.arange(S)
    krel = (pos % 128).astype(np.float32)
    qrel = pos % 512
    qhi = (qrel & ~3).astype(np.float32)
    qlo = (qrel & 3).astype(np.float32)
    kaug = np.zeros((6, 6, S), np.float32)
    qaug = np.zeros((6, 6, S), np.float32)
    for h in range(6):
        hi, lo = hi_lo(slopes[h])
        kaug[h, 0] = krel
        kaug[h, 1] = krel
        kaug[h, 2] = hi
        kaug[h, 3] = lo
        kaug[h, 4] = hi
        kaug[h, 5] = lo
        qaug[h, 0] = hi
        qaug[h, 1] = lo
        qaug[h, 2] = -qhi
        qaug[h, 3] = -qhi
        qaug[h, 4] = -qlo
        qaug[h, 5] = -qlo
    c['c_kaug'] = kaug
    c['c_qaug'] = qaug
    kk = np.arange(128)[:, None]
    qq = np.arange(128)[None, :]
    dm = np.zeros((6, 128, 128), np.float32)
    for h in range(6):
        allowed = (kk // 64) <= (qq // 64)
        val = np.where(kk <= qq, 1.0, np.exp(-2.0 * slopes[h] * (kk - qq)))
        dm[h] = np.where(allowed, val, 0.0)
    c['c_dmask'] = dm
    cb = np.zeros((128, 6 * 35), np.float32)
    for h in range(6):
        for idx in range(35):
            d = idx - 3
            cb[:, h * 35 + idx] = -slopes[h] * 128.0 * d
    c['c_bias'] = cb
    s_ = np.arange(128)[:, None]
    t_ = np.arange(128)[None, :]
    c['c_cmask'] = ((s_ <= t_) & ((s_ // 64) == (t_ // 64))).astype(np.float32)
    sm = np.ones((128, 512), np.float32)
    sm[:, ::64] = 0.0
    c['c_smask'] = sm
    return c


_CACHE = {}
_CONSTS = {}


def launch(kind, per_core, shared, n_cores):
    if kind not in _CACHE:
        _CACHE[kind] = build_step(kind)
    if not _CONSTS:
        _CONSTS.update(make_consts())
    nc = _CACHE[kind]
    sh = {}
    for n, shp in STEP_INPUTS[kind].items():
        if n in per_core:
            continue
        if shp == 0:
            sh[n] = _CONSTS[n]
        else:
            sh[n] = np.ascontiguousarray(np.asarray(shared[n], dtype=np.float32)).reshape(shp)
    in_maps = []
    for c in range(n_cores):
        m = dict(sh)
        for n, a in per_core.items():
            m[n] = np.ascontiguousarray(a[c])
        in_maps.append(m)
    res = run_bass_kernel_spmd(nc, in_maps, core_ids=list(range(n_cores)))
    return np.stack([np.asarray(r['out'], dtype=np.float32).reshape(S, D) for r in res.results], axis=0)


def run_step(i, which, x, inputs, n_cores):
    f = lambda n: np.asarray(inputs[n], dtype=np.float32)
    j = i // 2
    mem = f('mem')[:n_cores]
    if which == 'mix' and i % 2 == 0:
        lam_init = 0.8 - 0.6 * float(np.exp(-0.3 * i))
        clam = np.zeros((1, 128, 2), np.float32)
        clam[0, :, 0] = 1.0 - lam_init
        clam[0, :, 1] = -lam_init
        sh = {n: f(n)[j:j + 1] for n in ['a_norm_mix', 'a_w_in', 'a_lam_q1', 'a_lam_k1', 'a_lam_q2', 'a_lam_k2', 'a_subln', 'a_mem_norm', 'a_w_mem_kv', 'a_w_out']}
        sh['c_lam'] = clam
        return launch('attn', {'x': x, 'mem': mem}, sh, n_cores)
    if which == 'mix':
        sh = {n: f(n)[j:j + 1] for n in ['b_norm_mix', 'b_w_in', 'b_out_norm', 'b_mem_norm', 'b_w_mem_kv', 'b_w_out']}
        sh['b_lb_logits'] = f('b_lb_logits')
        sh['c_lbflag'] = np.full((1, 128, 1), -float(j), np.float32)
        return launch('hgrn', {'x': x, 'mem': mem}, sh, n_cores)
    if i % 2 == 0:
        sh = {'norm': f('dense_norm')[j:j + 1], 'w_gu': f('dense_w_gate_up')[j], 'w_dn': f('dense_w_down')[j]}
        return launch('dense', {'x': x}, sh, n_cores)
    xacc = x
    for e in range(NEXP):
        esel = np.zeros((128, 8), np.float32)
        esel[:, e] = 1.0
        sh = {'norm': f('moe_norm')[j:j + 1], 'router': f('moe_router')[j], 'w_gu': f('moe_w_gate_up')[j, e], 'w_dn': f('moe_w_down')[j, e], 'esel': esel}
        xacc = launch('moe1', {'x': x, 'xacc': xacc}, sh, n_cores)
    return xacc


FULL_SHAPES = {
    "x": [S, D], "mem": [MEM_LEN, D],
    "a_norm_mix": [2, D], "a_w_in": [2, D, 2560], "a_lam_q1": [2, 64], "a_lam_k1": [2, 64], "a_lam_q2": [2, 64], "a_lam_k2": [2, 64],
    "a_subln": [2, 128], "a_mem_norm": [2, D], "a_w_mem_kv": [2, D, 512], "a_w_out": [2, D, D],
    "b_norm_mix": [2, D], "b_w_in": [2, D, 3328], "b_lb_logits": [2, 768], "b_out_norm": [2, 128], "b_mem_norm": [2, D],
    "b_w_mem_kv": [2, D, 512], "b_w_out": [2, D, D],
    "dense_norm": [2, D], "dense_w_gate_up": [2, D, 2 * DFF_D], "dense_w_down": [2, DFF_D, D],
    "moe_norm": [2, D], "moe_router": [2, D, 8], "moe_w_gate_up": [2, 8, D, 2 * DFF_E], "moe_w_down": [2, 8, DFF_E, D],
    "final_norm": [1, D], "c_lam": [2, 128, 2], "c_lbflag": [2, 128, 1],
}
FULL_SHAPES.update(CONST_SHAPES)


def build_fused():
    nc = bass.Bass("TRN2", target_bir_lowering=False)
    inp = {n: nc.dram_tensor(n, shp, F32, kind="ExternalInput").ap() for n, shp in FULL_SHAPES.items()}
    out = nc.dram_tensor("out", [S, D], F32, kind="ExternalOutput").ap()
    xres = nc.dram_tensor("xres", [S, D], F32, kind="Internal").ap()
    o_s = nc.dram_tensor("o_s", [S, D], BF16, kind="Internal").ap()
    with ExitStack() as st:
        P = Prog(nc, st)
        k = K()
        k.inp = inp
        k.ident = P.sb(st, [128, 128], BF16, 'ident')
        k.identf = P.sb(st, [128, 128], F32, 'identf')
        k.neghalf = P.sb(st, [128, 1], F32, 'neghalf')
        P.dma('pool', k.ident[:], inp['c_ident'], writes=['ident'], cast=True)
        P.dma('sp', k.identf[:], inp['c_ident'], writes=['identf'])
        P.memset('pool', k.neghalf[:], -0.5, writes=['neghalf'])
        P.flush()
        x_cur = inp['x']
        for i in range(DEPTH):
            j = i // 2
            pre = 'a_' if i % 2 == 0 else 'b_'
            with ExitStack() as stm:
                hT = P.sb(stm, [128, 8, S], BF16, 'hT')
                kmT = P.sb(stm, [128, 2, MEM_LEN], BF16, 'kmT')
                vm = P.sb(stm, [128, 2, 4, 65], BF16, 'vm')
                phase_hT(P, k, x_cur, inp[pre + 'norm_mix'][j:j + 1, :], hT)
                phase_memkv(P, k, inp['mem'], inp[pre + 'mem_norm'][j:j + 1, :], inp[pre + 'w_mem_kv'][j], kmT, vm)
                if i % 2 == 0:
                    phase_diffattn(P, k, hT, j, None, o_s)
                    phase_memattn(P, k, hT, inp['a_w_in'][j], 3 * DA_W, kmT, vm, o_s)
                else:
                    phase_hgrn(P, k, hT, j, o_s)
                    phase_memattn(P, k, hT, inp['b_w_in'][j], 4 * HG_W, kmT, vm, o_s)
            phase_outproj(P, k, o_s, inp[pre + 'w_out'][j], x_cur, xres)
            x_cur = xres
            if i % 2 == 0:
                phase_ffn(P, k, xres, xres, xres, inp['dense_norm'][j:j + 1, :], [inp['dense_w_gate_up'][j]], [inp['dense_w_down'][j]], DFF_D)
            else:
                phase_ffn(P, k, xres, xres, xres, inp['moe_norm'][j:j + 1, :],
                          [inp['moe_w_gate_up'][j, e] for e in range(NEXP)], [inp['moe_w_down'][j, e] for e in range(NEXP)], DFF_E,
                          router_ap=inp['moe_router'][j])
        phase_fnorm(P, k, xres, inp['final_norm'][0:1, :], out)
        P.add('sp', lambda e: e.nop(), reads=[], writes=[])
        P.flush()
    return nc


def fused_consts():
    c = dict(make_consts())
    clam = np.zeros((2, 128, 2), np.float32)
    for j in range(2):
        lam_init = 0.8 - 0.6 * float(np.exp(-0.3 * (2 * j)))
        clam[j, :, 0] = 1.0 - lam_init
        clam[j, :, 1] = -lam_init
    c['c_lam'] = clam
    lbf = np.zeros((2, 128, 1), np.float32)
    lbf[1] = -1.0
    c['c_lbflag'] = lbf
    return c


def kernel_unfused(**inputs):
    n_cores = 8
    x = np.asarray(inputs['x'], dtype=np.float32)
    for i in range(DEPTH):
        x = run_step(i, 'mix', x, inputs, n_cores)
        x = run_step(i, 'ffn', x, inputs, n_cores)
    return launch('fnorm', {'x': x}, {'gain': np.asarray(inputs['final_norm'], dtype=np.float32).reshape(1, D)}, n_cores)


def kernel(**inputs):
    n_cores = 8
    if 'fused' not in _CACHE:
        _CACHE['fused'] = build_fused()
    nc = _CACHE['fused']
    consts = fused_consts()
    shared = {}
    for n, shp in FULL_SHAPES.items():
        if n in ('x', 'mem'):
            continue
        if n in consts:
            shared[n] = consts[n]
        else:
            shared[n] = np.ascontiguousarray(np.asarray(inputs[n], dtype=np.float32)).reshape(shp)
    x = np.asarray(inputs['x'], dtype=np.float32)
    mem = np.asarray(inputs['mem'], dtype=np.float32)
    in_maps = []
    for c in range(n_cores):
        m = dict(shared)
        m['x'] = np.ascontiguousarray(x[c])
        m['mem'] = np.ascontiguousarray(mem[c])
        in_maps.append(m)
    res = run_bass_kernel_spmd(nc, in_maps, core_ids=list(range(n_cores)))
    return np.stack([np.asarray(r['out'], dtype=np.float32).reshape(S, D) for r in res.results], axis=0)
```

```python
import numpy as np
import concourse.bass as bass
import concourse.mybir as mybir
from concourse.bass_utils import run_bass_kernel_spmd
from contextlib import ExitStack

F32 = mybir.dt.float32
BF16 = mybir.dt.bfloat16
ALU = mybir.AluOpType
AF = mybir.ActivationFunctionType
AX = mybir.AxisListType

S = 4096
D = 1024
NT = 32
EPS = 1e-6
DEPTH = 4
DA_W = 768
HG_W = 768
MEM_W = 256
DFF_D = 2816
DFF_E = 3584
NEXP = 8
MEM_LEN = 256

ENGS = ['pe', 'act', 'dve', 'pool', 'sp']
DMA_POOL = {'sp': 24, 'pool': 24, 'act': 4}


class Op:
    __slots__ = ('eng', 'fn', 'deps', 'need', 'sig', 'is_dma', 'dsem', 'dval', 'uid')


class Prog:
    def __init__(self, nc, stack, strict=True):
        self.nc = nc
        self.stack = stack
        self.strict = strict
        self.ops = {e: [] for e in ENGS}
        self.lastw = {}
        self.reads = {}
        self.uid = 0
        self.esem = {e: stack.enter_context(nc.semaphore('s_' + e)) for e in ENGS}
        self.sigc = {e: 0 for e in ENGS}
        self.dsems = {}
        self.dcur = {}
        self.dlast = {}
        self.dfence = {}
        for q, n in DMA_POOL.items():
            self.dsems[q] = [stack.enter_context(nc.semaphore('d_%s%d' % (q, i))) for i in range(n)]
            self.dcur[q] = 0
            self.dlast[q] = [None] * n
            self.dfence[q] = [0] * n
        self.efence = {e: 0 for e in ENGS}
        self.ntile = 0
        self.nflush = 0

    def sb(self, st, shape, dtype, name=None):
        self.ntile += 1
        name = (name or 't') + '_%d' % self.ntile
        return st.enter_context(self.nc.sbuf_tensor(name, list(shape), dtype))

    def ps(self, st, shape, dtype=F32, name=None):
        self.ntile += 1
        name = (name or 'p') + '_%d' % self.ntile
        return st.enter_context(self.nc.psum_tensor(name, list(shape), dtype))

    def add(self, eng, fn, reads=(), writes=(), dma=False):
        op = Op()
        op.eng = eng
        op.fn = fn
        op.need = False
        op.sig = None
        op.is_dma = dma
        op.uid = self.uid
        self.uid += 1
        deps = []
        for k in reads:
            w = self.lastw.get(k)
            if w is not None:
                deps.append((w, 'raw'))
        for k in writes:
            w = self.lastw.get(k)
            if w is not None:
                deps.append((w, 'waw'))
            rd = self.reads.get(k)
            if rd:
                for r in rd.values():
                    deps.append((r, 'war'))
        fdeps = []
        seen = set()
        for d, kind in deps:
            if d.uid in seen:
                continue
            if (not d.is_dma) and (not dma) and d.eng == eng:
                if eng == 'pe' or kind == 'war' or not self.strict:
                    continue
            seen.add(d.uid)
            fdeps.append(d)
        if dma:
            q = eng
            i = self.dcur[q]
            self.dcur[q] = (i + 1) % len(self.dsems[q])
            prev = self.dlast[q][i]
            op.dsem = self.dsems[q][i]
            op.dval = (prev.dval if prev is not None else 0) + 16
            if prev is not None and prev.uid not in seen and prev.dval > self.dfence[q][i]:
                fdeps.append(prev)
                seen.add(prev.uid)
            self.dlast[q][i] = op
        for d in fdeps:
            d.need = True
        op.deps = fdeps
        rk = ('dma', op.uid) if dma else eng
        for k in writes:
            self.lastw[k] = op
            self.reads[k] = {}
        for k in reads:
            self.reads.setdefault(k, {})[rk] = op
        self.ops[eng].append(op)
        return op

    def flush(self):
        for e in ENGS:
            comp = [op for op in self.ops[e] if not op.is_dma and op.fn is not None]
            if comp:
                comp[-1].need = True
            c = self.sigc[e]
            for op in self.ops[e]:
                if op.need and not op.is_dma:
                    c += 1
                    op.sig = c
            self.sigc[e] = c
        efence = dict(self.efence)
        dfence = {q: list(v) for q, v in self.dfence.items()}

        def run(e, engobj):
            waited = {}
            for e2 in ENGS:
                if efence[e2] > 0:
                    engobj.wait_ge(self.esem[e2], efence[e2])
                    waited[id(self.esem[e2])] = efence[e2]
            for q in dfence:
                for i, v in enumerate(dfence[q]):
                    if v > 0:
                        engobj.wait_ge(self.dsems[q][i], v)
                        waited[id(self.dsems[q][i])] = v
            for op in self.ops[e]:
                for d in op.deps:
                    if d.is_dma:
                        sem, val = d.dsem, d.dval
                    else:
                        sem, val = self.esem[d.eng], d.sig
                    key = id(sem)
                    if waited.get(key, 0) < val:
                        engobj.wait_ge(sem, val)
                        waited[key] = val
                ins = op.fn(engobj)
                if op.is_dma:
                    ins.then_inc(op.dsem, 16)
                elif op.need:
                    ins.then_inc(self.esem[e], 1)

        with self.nc.Block() as block:
            @block.tensor
            def _(t):
                run('pe', t)

            @block.scalar
            def _(t):
                run('act', t)

            @block.vector
            def _(t):
                run('dve', t)

            @block.gpsimd
            def _(t):
                run('pool', t)

            @block.sync
            def _(t):
                run('sp', t)

        for e in ENGS:
            self.efence[e] = self.sigc[e]
            self.ops[e] = []
        for q in self.dlast:
            for i, op in enumerate(self.dlast[q]):
                if op is not None:
                    self.dfence[q][i] = op.dval
        self.lastw = {}
        self.reads = {}
        self.nflush += 1

    def dma(self, q, out, in_, reads=(), writes=(), cast=False, slow=False):
        if slow:
            fn = lambda e, o=out, i=in_: e.dma_start(out=o, in_=i, allow_slow_non_contiguous=True)
        elif cast:
            fn = lambda e, o=out, i=in_: e.dma_start(out=o, in_=i, max_dma_last_dim=4096)
        else:
            fn = lambda e, o=out, i=in_: e.dma_start(out=o, in_=i)
        return self.add(q, fn, reads, writes, dma=True)

    def mmg(self, mms, reads, writes):
        mms = list(mms)

        def fn(e, mms=mms):
            ins = None
            for (o, l, r, s0, s1) in mms:
                ins = e.matmul(o, l, r, start=s0, stop=s1)
            return ins
        return self.add('pe', fn, reads, writes)

    def trg(self, trs, reads, writes):
        trs = list(trs)

        def fn(e, trs=trs):
            ins = None
            for (o, i, idn) in trs:
                ins = e.transpose(o, i, idn)
            return ins
        return self.add('pe', fn, reads, writes)

    def act(self, out, in_, func, reads, writes, bias=None, scale=None, accum=None):
        kw = {}
        if bias is not None:
            kw['bias'] = bias
        if scale is not None:
            kw['scale'] = scale
        if accum is not None:
            kw['accum_out'] = accum
        return self.add('act', lambda e, o=out, i=in_, f=func, kw=kw: e.activation(out=o, in_=i, func=f, **kw), reads, writes)

    def tt(self, eng, out, in0, in1, op, reads, writes):
        return self.add(eng, lambda e, o=out, a=in0, b=in1, p=op: e.tensor_tensor(out=o, in0=a, in1=b, op=p), reads, writes)

    def ts(self, eng, out, in0, s1, s2, op0, op1, reads, writes):
        if op1 is None:
            return self.add(eng, lambda e, o=out, a=in0, s1=s1, p0=op0: e.tensor_scalar(out=o, in0=a, scalar1=s1, scalar2=None, op0=p0), reads, writes)
        return self.add(eng, lambda e, o=out, a=in0, s1=s1, s2=s2, p0=op0, p1=op1: e.tensor_scalar(out=o, in0=a, scalar1=s1, scalar2=s2, op0=p0, op1=p1), reads, writes)

    def stt(self, out, in0, scalar, in1, op0, op1, reads, writes):
        return self.add('dve', lambda e, o=out, a=in0, s=scalar, b=in1, p0=op0, p1=op1: e.scalar_tensor_tensor(out=o, in0=a, scalar=s, in1=b, op0=p0, op1=p1), reads, writes)

    def copy(self, eng, out, in_, reads, writes):
        if eng == 'act':
            return self.add('act', lambda e, o=out, i=in_: e.copy(out=o, in_=i), reads, writes)
        return self.add(eng, lambda e, o=out, i=in_: e.tensor_copy(out=o, in_=i), reads, writes)

    def memset(self, eng, ap, val, writes):
        return self.add(eng, lambda e, a=ap, v=val: e.memset(a, v), (), writes)

    def recip(self, out, in_, reads, writes):
        return self.add('dve', lambda e, o=out, i=in_: e.reciprocal(out=o, in_=i), reads, writes)

    def reduce(self, out, in_, op, reads, writes):
        return self.add('dve', lambda e, o=out, i=in_, p=op: e.tensor_reduce(out=o, in_=i, axis=AX.X, op=p), reads, writes)

    def scan(self, out, d0, d1, reads, writes):
        return self.add('dve', lambda e, o=out, a=d0, b=d1: e.tensor_tensor_scan(out=o, data0=a, data1=b, initial=0.0, op0=ALU.mult, op1=ALU.add), reads, writes)


class K:
    pass


def wview(ap2d):
    return ap2d.rearrange("(c p) n -> p c n", p=128)


def norm_rows(P, k, st, x_ap, gbc, hb_out, tag, want_f32=None):
    t = k.nt[tag]
    P.act(t['junk'][:], x_ap, AF.Square, reads=[tag + 'x'], writes=[tag + 'junk', tag + 'ssq'], accum=t['ssq'][:])
    P.ts('dve', t['ms'][:], t['ssq'][:], 1.0 / D, EPS, ALU.mult, ALU.add, reads=[tag + 'ssq'], writes=[tag + 'ms'])
    P.tt('pool', t['rstd'][:], t['ms'][:], k.neghalf[:], ALU.pow, reads=[tag + 'ms'], writes=[tag + 'rstd'])
    if want_f32 is not None:
        P.stt(want_f32, x_ap, t['rstd'][:, 0:1], gbc, ALU.mult, ALU.mult, reads=[tag + 'x', tag + 'rstd', 'gbc'], writes=[tag + 'hf'])
        P.copy('act', hb_out, want_f32, reads=[tag + 'hf'], writes=[tag + 'hb'])
    else:
        P.stt(hb_out, x_ap, t['rstd'][:, 0:1], gbc, ALU.mult, ALU.mult, reads=[tag + 'x', tag + 'rstd', 'gbc'], writes=[tag + 'hb'])


def alloc_norm_tmps(P, k, st, tags):
    k.nt = {}
    for tag in tags:
        k.nt[tag] = {
            'junk': P.sb(st, [128, D], F32, 'junk'),
            'ssq': P.sb(st, [128, 1], F32, 'ssq'),
            'ms': P.sb(st, [128, 1], F32, 'ms'),
            'rstd': P.sb(st, [128, 1], F32, 'rstd'),
        }


def phase_hT(P, k, x_src, gain_row, hT):
    with ExitStack() as st:
        gbc = P.sb(st, [128, D], F32, 'gbc')
        xt = [P.sb(st, [128, D], F32, 'xt') for _ in range(2)]
        hb = [P.sb(st, [128, D], BF16, 'hb') for _ in range(2)]
        pt = [P.ps(st, [128, 8, 128], BF16, 'pt') for _ in range(2)]
        alloc_norm_tmps(P, k, st, ['n0', 'n1'])
        P.dma('sp', gbc[:], gain_row.partition_broadcast(128), writes=['gbc'])
        for tt in range(NT):
            b = tt % 2
            tag = 'n%d' % b
            P.dma('sp', xt[b][:], x_src[tt * 128:(tt + 1) * 128, :], writes=[tag + 'x'])
            norm_rows(P, k, st, xt[b][:], gbc[:], hb[b][:], tag)
            P.trg([(pt[b][:, c, :], hb[b][:, c * 128:(c + 1) * 128], k.ident[:]) for c in range(8)],
                  reads=[tag + 'hb'], writes=[tag + 'pt'])
            P.copy('act' if tt % 2 else 'dve', hT[:, :, tt * 128:(tt + 1) * 128], pt[b][:], reads=[tag + 'pt'], writes=[('hT', tt)])
        P.flush()


def phase_memkv(P, k, mem_ap, gain_row, wkv_ap, kmT, vm):
    with ExitStack() as st:
        gbc = P.sb(st, [128, D], F32, 'gbc')
        xt = [P.sb(st, [128, D], F32, 'xt') for _ in range(2)]
        hb = [P.sb(st, [128, D], BF16, 'hb') for _ in range(2)]
        pt = [P.ps(st, [128, 8, 128], BF16, 'pt') for _ in range(2)]
        memT = P.sb(st, [128, 8, MEM_LEN], BF16, 'memT')
        wkv = P.sb(st, [128, 8, 512], BF16, 'wkv')
        pk = P.ps(st, [128, 256], F32, 'pk')
        alloc_norm_tmps(P, k, st, ['n0', 'n1'])
        P.dma('sp', gbc[:], gain_row.partition_broadcast(128), writes=['gbc'])
        P.dma('pool', wkv[:], wview(wkv_ap), writes=['wkv'], cast=True)
        P.memset('pool', vm[:], 1.0, writes=['vm'])
        for mt in range(2):
            tag = 'n%d' % mt
            P.dma('sp', xt[mt][:], mem_ap[mt * 128:(mt + 1) * 128, :], writes=[tag + 'x'])
            norm_rows(P, k, st, xt[mt][:], gbc[:], hb[mt][:], tag)
            P.trg([(pt[mt][:, c, :], hb[mt][:, c * 128:(c + 1) * 128], k.ident[:]) for c in range(8)],
                  reads=[tag + 'hb'], writes=[tag + 'pt'])
            P.copy('dve', memT[:, :, mt * 128:(mt + 1) * 128], pt[mt][:], reads=[tag + 'pt'], writes=[('memT', mt)])
        for p in range(2):
            P.mmg([(pk[:], wkv[:, dc, p * 128:(p + 1) * 128], memT[:, dc, :], dc == 0, dc == 7) for dc in range(8)],
                  reads=['wkv', ('memT', 0), ('memT', 1)], writes=['pk'])
            P.copy('dve', kmT[:, p, :], pk[:], reads=['pk'], writes=[('kmT', p)])
        for mt in range(2):
            P.mmg([(pk[:], memT[:, dc, mt * 128:(mt + 1) * 128], wkv[:, dc, 256:512], dc == 0, dc == 7) for dc in range(8)],
                  reads=['wkv', ('memT', 0), ('memT', 1)], writes=['pk'])
            P.copy('dve', vm[:, mt, :, 0:64], pk[:].rearrange("p (h d) -> p h d", h=4), reads=['pk', 'vm'], writes=[('vm', mt)])
        P.flush()


def phase_memattn(P, k, hT, w_in_ap, col0, kmT, vm, o_s):
    with ExitStack() as st:
        wqm = P.sb(st, [128, 8, 256], BF16, 'wqm')
        qmT = P.sb(st, [128, 2, S], BF16, 'qmT')
        pq = [P.ps(st, [128, 512], F32, 'pq') for _ in range(2)]
        sc = [P.ps(st, [128, 512], F32, 'sc') for _ in range(2)]
        pacc = [P.ps(st, [128, 512], F32, 'pacc') for _ in range(4)]
        pT = [P.sb(st, [128, 512], BF16, 'pT') for _ in range(2)]
        rr = [P.sb(st, [128, 1], F32, 'rr') for _ in range(4)]
        omb = [P.sb(st, [128, 4, 256], BF16, 'omb') for _ in range(2)]
        P.dma('pool', wqm[:], wview(w_in_ap)[:, :, col0:col0 + 256], writes=['wqm'], cast=True)
        for tb in range(8):
            for p in range(2):
                b = (tb * 2 + p) % 2
                P.mmg([(pq[b][:], wqm[:, dc, p * 128:(p + 1) * 128], hT[:, dc, tb * 512:(tb + 1) * 512], dc == 0, dc == 7) for dc in range(8)],
                      reads=['wqm'], writes=[('pq', b)])
                P.act(qmT[:, p, tb * 512:(tb + 1) * 512], pq[b][:], AF.Identity, reads=[('pq', b)], writes=[('qmT', tb, p)], scale=0.125)
        cnt = 0
        for tb in range(8):
            ob = omb[tb % 2]
            for hm in range(4):
                p = hm // 2
                base = (hm % 2) * 64
                for mt in range(2):
                    b = cnt % 2
                    cnt += 1
                    P.mmg([(sc[b][:], kmT[base:base + 64, p, mt * 128:(mt + 1) * 128], qmT[base:base + 64, p, tb * 512:(tb + 1) * 512], True, True)],
                          reads=[('qmT', tb, p)], writes=[('sc', b)])
                    P.act(pT[b][:], sc[b][:], AF.Exp, reads=[('sc', b)], writes=[('pT', b)])
                    P.mmg([(pacc[qs][:, 0:65], pT[b][:, qs * 128:(qs + 1) * 128], vm[:, mt, hm, :], mt == 0, mt == 1) for qs in range(4)],
                          reads=[('pT', b)], writes=[('pacc', qs) for qs in range(4)])
                for qs in range(4):
                    P.recip(rr[qs][:], pacc[qs][:, 64:65], reads=[('pacc', qs)], writes=[('rr', qs)])
                    P.act(ob[:, qs, hm * 64:(hm + 1) * 64], pacc[qs][:, 0:64], AF.Identity, reads=[('pacc', qs), ('rr', qs)],
                          writes=[('omb', tb % 2, qs)], scale=rr[qs][:, 0:1])
            for qs in range(4):
                tt = tb * 4 + qs
                P.dma('sp', o_s[tt * 128:(tt + 1) * 128, 768:1024], ob[:, qs, :], reads=[('omb', tb % 2, qs)], writes=[('o_s', tt, 'm')])
        P.flush()


def phase_outproj(P, k, o_s, w_out_ap, x_src, x_dst):
    with ExitStack() as st:
        wo = P.sb(st, [128, 8, D], BF16, 'wo')
        ot = [P.sb(st, [128, D], BF16, 'ot') for _ in range(2)]
        oT = [P.sb(st, [128, 8, 128], BF16, 'oT') for _ in range(2)]
        xt = [P.sb(st, [128, D], F32, 'xt') for _ in range(2)]
        xn = [P.sb(st, [128, D], F32, 'xn') for _ in range(2)]
        pt = [P.ps(st, [128, 8, 128], BF16, 'pt') for _ in range(2)]
        po = [P.ps(st, [128, 512], F32, 'po') for _ in range(4)]
        P.dma('pool', wo[:], wview(w_out_ap), writes=['wo'], cast=True)
        for tt in range(NT):
            b = tt % 2
            P.dma('sp', ot[b][:], o_s[tt * 128:(tt + 1) * 128, :], writes=[('ot', b)])
            P.dma('sp', xt[b][:], x_src[tt * 128:(tt + 1) * 128, :], writes=[('xt', b)])
            P.trg([(pt[b][:, c, :], ot[b][:, c * 128:(c + 1) * 128], k.ident[:]) for c in range(8)],
                  reads=[('ot', b)], writes=[('pt', b)])
            P.copy('act', oT[b][:], pt[b][:], reads=[('pt', b)], writes=[('oT', b)])
            for hh in range(2):
                pb = b * 2 + hh
                P.mmg([(po[pb][:], oT[b][:, fc, :], wo[:, fc, hh * 512:(hh + 1) * 512], fc == 0, fc == 7) for fc in range(8)],
                      reads=[('oT', b), 'wo'], writes=[('po', pb)])
                P.tt('dve', xn[b][:, hh * 512:(hh + 1) * 512], po[pb][:], xt[b][:, hh * 512:(hh + 1) * 512], ALU.add,
                     reads=[('po', pb), ('xt', b)], writes=[('xn', b, hh)])
            P.dma('sp', x_dst[tt * 128:(tt + 1) * 128, :], xn[b][:], reads=[('xn', b, 0), ('xn', b, 1)], writes=[('xd', tt)])
        P.flush()


def phase_diffattn(P, k, hT, j, lam_init, o_s):
    inp = k.inp
    w_in = wview(inp['a_w_in'][j])
    with ExitStack() as st:
        wqkv = [P.sb(st, [128, 8, 384], BF16, 'wqkv') for _ in range(2)]
        qT = [P.sb(st, [128, 2, S], BF16, 'qT') for _ in range(2)]
        kT = [P.sb(st, [128, 2, S], BF16, 'kT') for _ in range(2)]
        va = [P.sb(st, [128, NT, 129], BF16, 'va') for _ in range(2)]
        dmask = P.sb(st, [128, 6, 128], BF16, 'dmask')
        cbias = P.sb(st, [128, 6 * 35], F32, 'cbias')
        gsub = P.sb(st, [128, 128], F32, 'gsub')
        lv = [P.sb(st, [128, 64], F32, 'lv') for _ in range(4)]
        lt = P.sb(st, [128, 64], F32, 'lt')
        ls = [P.sb(st, [128, 1], F32, 'ls') for _ in range(2)]
        neglam = P.sb(st, [128, 1], F32, 'neglam')
        clam = P.sb(st, [128, 2], F32, 'clam')
        pT = [P.sb(st, [128, 512], BF16, 'pT') for _ in range(3)]
        o0 = P.sb(st, [128, 4, 128], F32, 'o0')
        oo = [P.sb(st, [128, 128], F32, 'oo') for _ in range(2)]
        ob = [P.sb(st, [128, 128], BF16, 'ob') for _ in range(2)]
        junk = P.sb(st, [128, 128], F32, 'junk')
        sm = {n: [P.sb(st, [128, 1], F32, n) for _ in range(2)] for n in ('r0', 'r1', 'ssq', 'ms', 'lnm', 'rstd')}
        pp = [P.ps(st, [128, 512], F32, 'pp') for _ in range(2)]
        sc = [P.ps(st, [128, 512], F32, 'sc') for _ in range(2)]
        pacc = [P.ps(st, [128, 512], F32, 'pacc') for _ in range(4)]

        P.dma('pool', dmask[:], inp['c_dmask'].rearrange("h k q -> k h q"), writes=['dmask'], cast=True)
        P.dma('sp', cbias[:], inp['c_bias'], writes=['cbias'])
        P.dma('sp', gsub[:], inp['a_subln'][j:j + 1, :].partition_broadcast(128), writes=['gsub'])
        P.dma('sp', clam[:], inp['c_lam'][j], writes=['clam'])
        P.ts('dve', gsub[:], gsub[:], clam[:, 0:1], None, ALU.mult, None, reads=['gsub', 'clam'], writes=['gsub'])
        for i, nm in enumerate(['a_lam_q1', 'a_lam_k1', 'a_lam_q2', 'a_lam_k2']):
            P.dma('sp', lv[i][:], inp[nm][j:j + 1, :].partition_broadcast(128), writes=[('lv', i)])
        for i in range(2):
            P.tt('dve', lt[:], lv[2 * i][:], lv[2 * i + 1][:], ALU.mult, reads=[('lv', 2 * i), ('lv', 2 * i + 1)], writes=['lt'])
            P.reduce(ls[i][:], lt[:], ALU.add, reads=['lt'], writes=[('ls', i)])
            P.act(ls[i][:], ls[i][:], AF.Exp, reads=[('ls', i)], writes=[('ls', i)])
        P.tt('dve', neglam[:], ls[1][:], ls[0][:], ALU.subtract, reads=[('ls', 0), ('ls', 1)], writes=['neglam'])
        P.ts('dve', neglam[:], neglam[:], clam[:, 1:2], None, ALU.add, None, reads=['neglam', 'clam'], writes=['neglam'])
        for b in range(2):
            P.memset('pool', va[b][:, :, 128:129], 1.0, writes=[('va1', b)])

        scb = [sc[0], sc[1], pp[1]]
        pT4 = pT + [P.sb(st, [128, 512], BF16, 'pT')]

        def project_units(h):
            b = h % 2
            W = wqkv[b]
            units = []

            def u0():
                for i, c0 in enumerate([h * 128, DA_W + h * 128, 2 * DA_W + h * 128]):
                    P.dma('pool', W[:, :, i * 128:(i + 1) * 128], w_in[:, :, c0:c0 + 128], writes=[('w', b, i)], cast=True)
                for m in range(2):
                    P.dma('pool', qT[b][64:70, m, :], inp['c_qaug'][h], writes=[('qa', b, m)], cast=True)
                    P.dma('pool', kT[b][64:70, m, :], inp['c_kaug'][h], writes=[('ka', b, m)], cast=True)
            units.append(u0)
            for tb in range(8):
                for m in range(2):
                    for isk in range(2):
                        def u(tb=tb, m=m, isk=isk):
                            c0 = isk * 128 + m * 64
                            P.mmg([(pp[0][0:64, :], W[:, dc, c0:c0 + 64], hT[:, dc, tb * 512:(tb + 1) * 512], dc == 0, dc == 7) for dc in range(8)],
                                  reads=[('w', b, isk)], writes=[('pp', 0)])
                            if isk == 0:
                                P.ts('dve', qT[b][0:64, m, tb * 512:(tb + 1) * 512], pp[0][0:64, :], 0.125, None, ALU.mult, None,
                                     reads=[('pp', 0)], writes=[('q', b, m, tb)])
                            else:
                                P.copy('dve', kT[b][0:64, m, tb * 512:(tb + 1) * 512], pp[0][0:64, :], reads=[('pp', 0)], writes=[('k', b, m, tb)])
                        units.append(u)
                for tq in range(4):
                    def uv(tt=tb * 4 + tq):
                        P.mmg([(pp[0][:, 0:128], hT[:, dc, tt * 128:(tt + 1) * 128], W[:, dc, 256:384], dc == 0, dc == 7) for dc in range(8)],
                              reads=[('w', b, 2)], writes=[('pp', 0)])
                        P.copy('dve', va[b][:, tt, 0:128], pp[0][:, 0:128], reads=[('pp', 0), ('va1', b)], writes=[('v', b, tt)])
                    units.append(uv)
            return units

        def attend(h, nxt):
            b = h % 2
            blocks = [(jq, m, kt) for jq in range(8) for m in range(2) for kt in range(4 * jq + 4)]
            nb = len(blocks)
            evc = [0]

            def geom(i):
                jq, m, kt = blocks[i]
                r = kt - 4 * jq
                off = max(r, 0) * 128
                return jq, m, kt, r, off, 512 - off

            def emit_qk(i):
                jq, m, kt, r, off, N = geom(i)
                sb_ = i % 3
                P.mmg([(scb[sb_][:, 0:N], kT[b][0:70, m, kt * 128:(kt + 1) * 128], qT[b][0:70, m, jq * 512 + off:(jq + 1) * 512], True, True)],
                      reads=[('q', b, m, jq), ('qa', b, m), ('ka', b, m), ('k', b, m, kt // 4)], writes=[('sc', sb_)])

            def emit_exp(i):
                jq, m, kt, r, off, N = geom(i)
                sb_ = i % 3
                pb = i % 4
                bi = h * 35 + (4 * jq - kt + 3)
                P.act(pT4[pb][:, off:512], scb[sb_][:, 0:N], AF.Exp, reads=[('sc', sb_), 'cbias'], writes=[('pT', pb)], bias=cbias[:, bi:bi + 1])
                if r >= 0:
                    P.tt('dve', pT4[pb][:, off:off + 128], pT4[pb][:, off:off + 128], dmask[:, h, :], ALU.mult,
                         reads=[('pT', pb), 'dmask'], writes=[('pT', pb)])

            def emit_pv(i):
                jq, m, kt, r, off, N = geom(i)
                pb = i % 4
                qs0 = max(r, 0)
                P.mmg([(pacc[qs][:, 0:129], pT4[pb][:, qs * 128:(qs + 1) * 128], va[b][:, kt, :], kt == 0, kt == 4 * jq + qs) for qs in range(qs0, 4)],
                      reads=[('pT', pb), ('v', b, kt), ('va1', b)], writes=[('pacc', qs) for qs in range(qs0, 4)])
                if kt == 4 * jq + 3:
                    for qs in range(4):
                        e = evc[0] % 2
                        evc[0] += 1
                        if m == 0:
                            P.recip(sm['r0'][e][:], pacc[qs][:, 128:129], reads=[('pacc', qs)], writes=[('r0', e)])
                            P.ts('dve', o0[:, qs, :], pacc[qs][:, 0:128], sm['r0'][e][:, 0:1], None, ALU.mult, None,
                                 reads=[('pacc', qs), ('r0', e)], writes=[('o0', qs)])
                        else:
                            P.recip(sm['r1'][e][:], pacc[qs][:, 128:129], reads=[('pacc', qs)], writes=[('r1', e)])
                            P.tt('dve', sm['r1'][e][:], sm['r1'][e][:], neglam[:], ALU.mult, reads=[('r1', e), 'neglam'], writes=[('r1', e)])
                            P.stt(oo[e][:], pacc[qs][:, 0:128], sm['r1'][e][:, 0:1], o0[:, qs, :], ALU.mult, ALU.add,
                                  reads=[('pacc', qs), ('r1', e), ('o0', qs)], writes=[('oo', e)])
                            P.add('dve', lambda eng, o=junk[:], a=oo[e][:], acc=sm['ssq'][e][:]: eng.scalar_tensor_tensor(
                                out=o, in0=a, scalar=1.0, in1=a, op0=ALU.mult, op1=ALU.mult, accum_out=acc),
                                reads=[('oo', e)], writes=['junk', ('ssq', e)])
                            P.ts('dve', sm['ms'][e][:], sm['ssq'][e][:], 1.0 / 128, EPS, ALU.mult, ALU.add, reads=[('ssq', e)], writes=[('ms', e)])
                            P.tt('pool', sm['rstd'][e][:], sm['ms'][e][:], k.neghalf[:], ALU.pow, reads=[('ms', e)], writes=[('rstd', e)])
                            P.stt(ob[e][:], oo[e][:], sm['rstd'][e][:, 0:1], gsub[:], ALU.mult, ALU.mult,
                                  reads=[('oo', e), ('rstd', e), 'gsub'], writes=[('ob', e)])
                            tt = jq * 4 + qs
                            P.dma('sp', o_s[tt * 128:(tt + 1) * 128, h * 128:(h + 1) * 128], ob[e][:], reads=[('ob', e)], writes=[('o_s', tt, h)])

            emit_qk(0)
            emit_qk(1)
            for i in range(nb):
                emit_exp(i)
                if i + 2 < nb:
                    emit_qk(i + 2)
                emit_pv(i)
                if i % 4 == 3 and nxt:
                    nxt.pop(0)()
            while nxt:
                nxt.pop(0)()

        for u in project_units(0):
            u()
        for h in range(6):
            attend(h, project_units(h + 1) if h + 1 < 6 else [])
        P.flush()


def phase_hgrn(P, k, hT, j, o_s):
    inp = k.inp
    w_in = wview(inp['b_w_in'][j])
    with ExitStack() as st:
        W = [P.sb(st, [128, 8, 512], BF16, 'W') for _ in range(2)]
        qeT = [P.sb(st, [128, S], BF16, 'qeT') for _ in range(2)]
        keT = [P.sb(st, [128, S], BF16, 'keT') for _ in range(2)]
        ketok = [P.sb(st, [128, NT, 128], BF16, 'ketok') for _ in range(2)]
        vtok = [P.sb(st, [128, NT, 128], BF16, 'vtok') for _ in range(2)]
        sgtok = [P.sb(st, [128, NT, 128], BF16, 'sgtok') for _ in range(2)]
        decay = [P.sb(st, [128, 64], F32, 'decay') for _ in range(2)]
        oml = P.sb(st, [128, 6], F32, 'oml')
        gn = P.sb(st, [128, 128], F32, 'gn')
        cmask = P.sb(st, [128, 128], BF16, 'cmask')
        smask = P.sb(st, [128, 512], F32, 'smask')
        tmp = {n: [P.sb(st, [128, 512], F32, n) for _ in range(2)] for n in ('et', 'dt', 'kk', 'gt', 'bt', 'eb', 'enb', 'qraw')}
        Sst = P.sb(st, [128, 128], F32, 'Sst')
        Sbf = P.sb(st, [128, 128], BF16, 'Sbf')
        at = [P.sb(st, [128, 128], BF16, 'at') for _ in range(2)]
        o1 = [P.sb(st, [128, 128], F32, 'o1') for _ in range(2)]
        ob = [P.sb(st, [128, 128], BF16, 'ob') for _ in range(2)]
        junk = P.sb(st, [128, 128], F32, 'junk')
        sm = {n: [P.sb(st, [128, 1], F32, n) for _ in range(2)] for n in ('ssq', 'ms', 'rstd')}
        pq = P.ps(st, [128, 512], F32, 'pq')
        ptr = P.ps(st, [128, 4, 128], BF16, 'ptr')
        pvg = P.ps(st, [128, 128], F32, 'pvg')
        pat = P.ps(st, [128, 128], F32, 'pat')
        pkv2 = [P.ps(st, [128, 128], F32, 'pkv') for _ in range(2)]
        po = [P.ps(st, [128, 128], F32, 'po') for _ in range(2)]

        P.dma('pool', cmask[:], inp['c_cmask'], writes=['cmask'], cast=True)
        P.dma('sp', smask[:], inp['c_smask'], writes=['smask'])
        P.dma('sp', gn[:], inp['b_out_norm'][j:j + 1, :].partition_broadcast(128), writes=['gn'])
        lbf = P.sb(st, [128, 1], F32, 'lbf')
        lbl2 = P.sb(st, [2, 768], F32, 'lbl2')
        lbT = P.sb(st, [128, 6, 2], F32, 'lbT')
        P.dma('sp', lbf[:], inp['c_lbflag'][j], writes=['lbf'])
        P.dma('sp', lbl2[:], inp['b_lb_logits'], writes=['lbl2'])
        P.trg([(pvg[:, 2 * h:2 * h + 2], lbl2[0:2, h * 128:(h + 1) * 128], k.identf[0:2, 0:2]) for h in range(6)], reads=['lbl2'], writes=['pvg'])
        P.copy('dve', lbT[:], pvg[:, 0:12].rearrange("p (h l) -> p h l", l=2), reads=['pvg'], writes=['lbl'])
        P.tt('dve', oml[:], lbT[:, :, 0], lbT[:, :, 1], ALU.subtract, reads=['lbl'], writes=['oml'])
        P.act(oml[:], oml[:], AF.Exp, reads=['oml'], writes=['oml'])
        P.ts('dve', oml[:], oml[:], 1.0, None, ALU.add, None, reads=['oml'], writes=['oml'])
        P.recip(oml[:], oml[:], reads=['oml'], writes=['oml'])
        P.ts('dve', oml[:], oml[:], lbf[:, 0:1], 1.0, ALU.mult, ALU.add, reads=['oml', 'lbf'], writes=['oml'])

        def project_units(h):
            b = h % 2
            units = []

            def u0():
                for i, c0 in enumerate([h * 128, HG_W + h * 128, 2 * HG_W + h * 128, 3 * HG_W + h * 128]):
                    P.dma('pool', W[b][:, :, i * 128:(i + 1) * 128], w_in[:, :, c0:c0 + 128], writes=[('w', b, i)], cast=True)
            units.append(u0)
            for tb in range(8):
                def ua(tb=tb):
                    blk = slice(tb * 512, (tb + 1) * 512)
                    x = tb % 2
                    et, dt_, kk, gt, bt, eb, enb, qraw = (tmp[n][x] for n in ('et', 'dt', 'kk', 'gt', 'bt', 'eb', 'enb', 'qraw'))
                    T = lambda n: (n, x)
                    P.mmg([(pq[:], W[b][:, dc, 0:128], hT[:, dc, blk], dc == 0, dc == 7) for dc in range(8)], reads=[('w', b, 0)], writes=['pq'])
                    P.copy('act', qraw[:], pq[:], reads=['pq'], writes=[T('qraw')])
                    P.mmg([(pq[:], W[b][:, dc, 128:256], hT[:, dc, blk], dc == 0, dc == 7) for dc in range(8)], reads=[('w', b, 1)], writes=['pq'])
                    P.act(et[:], pq[:], AF.Exp, reads=['pq'], writes=[T('et')], scale=-1.0)
                    P.ts('dve', dt_[:], et[:], 1.0, None, ALU.add, None, reads=[T('et')], writes=[T('dt')])
                    P.recip(dt_[:], dt_[:], reads=[T('dt')], writes=[T('dt')])
                    P.stt(kk[:], et[:], oml[:, h:h + 1], dt_[:], ALU.mult, ALU.mult, reads=[T('et'), T('dt'), 'oml'], writes=[T('kk')])
                    P.act(gt[:], kk[:], AF.Ln, reads=[T('kk')], writes=[T('gt')], scale=-1.0, bias=1.0)
                    P.scan(bt[:], smask[:], gt[:], reads=['smask', T('gt')], writes=[T('bt')])
                    P.act(eb[:], bt[:], AF.Exp, reads=[T('bt')], writes=[T('eb')])
                    P.act(enb[:], bt[:], AF.Exp, reads=[T('bt')], writes=[T('enb')], scale=-1.0)
                    P.tt('dve', qeT[b][:, blk], qraw[:], eb[:], ALU.mult, reads=[T('qraw'), T('eb')], writes=[('qe', b, tb)])
                    P.tt('dve', keT[b][:, blk], kk[:], enb[:], ALU.mult, reads=[T('kk'), T('enb')], writes=[('ke', b, tb)])
                    P.copy('act', decay[b][:, tb * 8:(tb + 1) * 8], eb[:].rearrange("p (c t) -> p c t", t=64)[:, :, 63], reads=[T('eb')], writes=[('dec', b, tb)])
                    P.trg([(ptr[:, i, :], keT[b][:, tb * 512 + i * 128: tb * 512 + (i + 1) * 128], k.ident[:]) for i in range(4)],
                          reads=[('ke', b, tb)], writes=['ptr'])
                    P.copy('act', ketok[b][:, tb * 4:(tb + 1) * 4, :], ptr[:], reads=['ptr'], writes=[('ketok', b, tb)])
                units.append(ua)
                for tq in range(4):
                    def uv(tt=tb * 4 + tq):
                        tok = slice(tt * 128, (tt + 1) * 128)
                        P.mmg([(pvg[:], hT[:, dc, tok], W[b][:, dc, 256:384], dc == 0, dc == 7) for dc in range(8)], reads=[('w', b, 2)], writes=['pvg'])
                        P.copy('dve', vtok[b][:, tt, :], pvg[:], reads=['pvg'], writes=[('vtok', b, tt)])
                        P.mmg([(pvg[:], hT[:, dc, tok], W[b][:, dc, 384:512], dc == 0, dc == 7) for dc in range(8)], reads=[('w', b, 3)], writes=['pvg'])
                        P.act(sgtok[b][:, tt, :], pvg[:], AF.Silu, reads=['pvg'], writes=[('sgtok', b, tt)])
                    units.append(uv)
            return units

        def recur(h, nxt):
            b = h % 2
            P.memset('pool', Sst[:], 0.0, writes=['Sst'])
            P.memset('pool', Sbf[:], 0.0, writes=['Sbf'])
            for tt in range(NT):
                e = tt % 2
                tb = tt // 4
                tok = slice(tt * 128, (tt + 1) * 128)
                P.mmg([(pat[:], keT[b][:, tok], qeT[b][:, tok], True, True)], reads=[('ke', b, tb), ('qe', b, tb)], writes=['pat'])
                P.tt('dve', at[e][:], pat[:], cmask[:], ALU.mult, reads=['pat', 'cmask'], writes=[('at', e)])
                P.mmg([(pkv2[ci][:], ketok[b][ci * 64:(ci + 1) * 64, tt, :], vtok[b][ci * 64:(ci + 1) * 64, tt, :], True, True) for ci in range(2)],
                      reads=[('ketok', b, tb), ('vtok', b, tt)], writes=['pkv'])
                P.mmg([(po[e][:], at[e][:], vtok[b][:, tt, :], True, False),
                       (po[e][0:64, :], qeT[b][:, tt * 128:tt * 128 + 64], Sbf[:], False, False)],
                      reads=[('at', e), ('vtok', b, tt), ('qe', b, tb), 'Sbf'], writes=[('po', e)])
                for ci in range(2):
                    c = 2 * tt + ci
                    P.tt('dve', Sst[:], pkv2[ci][:], Sst[:], ALU.add, reads=['pkv', 'Sst'], writes=['Sst'])
                    P.ts('dve', Sst[:], Sst[:], decay[b][:, c:c + 1], None, ALU.mult, None, reads=['Sst', ('dec', b, tb)], writes=['Sst'])
                    P.copy('act', Sbf[:], Sst[:], reads=['Sst'], writes=['Sbf'])
                    if ci == 0:
                        P.mmg([(po[e][64:128, :], qeT[b][:, tt * 128 + 64:(tt + 1) * 128], Sbf[:], False, True)],
                              reads=[('qe', b, tb), 'Sbf'], writes=[('po', e)])
                P.act(junk[:], po[e][:], AF.Square, reads=[('po', e)], writes=['junk', ('ssq', e)], accum=sm['ssq'][e][:])
                P.ts('dve', sm['ms'][e][:], sm['ssq'][e][:], 1.0 / 128, EPS, ALU.mult, ALU.add, reads=[('ssq', e)], writes=[('ms', e)])
                P.tt('pool', sm['rstd'][e][:], sm['ms'][e][:], k.neghalf[:], ALU.pow, reads=[('ms', e)], writes=[('rstd', e)])
                P.stt(o1[e][:], po[e][:], sm['rstd'][e][:, 0:1], gn[:], ALU.mult, ALU.mult, reads=[('po', e), ('rstd', e), 'gn'], writes=[('o1', e)])
                P.tt('dve', ob[e][:], o1[e][:], sgtok[b][:, tt, :], ALU.mult, reads=[('o1', e), ('sgtok', b, tt)], writes=[('ob', e)])
                P.dma('sp', o_s[tt * 128:(tt + 1) * 128, h * 128:(h + 1) * 128], ob[e][:], reads=[('ob', e)], writes=[('o_s', tt, h)])
                for _ in range(2):
                    if nxt:
                        nxt.pop(0)()
            while nxt:
                nxt.pop(0)()

        for u in project_units(0):
            u()
        for h in range(6):
            recur(h, project_units(h + 1) if h + 1 < 6 else [])
        P.flush()


def phase_ffn(P, k, x_src, xacc_src, x_dst, gain_row, w_gu_list, w_dn_list, dff, router_ap=None, esel_ap=None):
    HT = S // 2
    NTH = NT // 2
    ngrp = (dff + 511) // 512
    moe = router_ap is not None
    same = xacc_src is x_src
    nexp = len(w_gu_list)
    for half in range(2):
        with ExitStack() as st0:
            xacc = P.sb(st0, [128, NTH, D], F32, 'xacc')
            hTh = P.sb(st0, [128, 8, HT], BF16, 'hTh')
            csel = P.sb(st0, [128, NTH], F32, 'csel')
            comb = P.sb(st0, [128, NTH, 8], F32, 'comb')
            with ExitStack() as st:
                gbc = P.sb(st, [128, D], F32, 'gbc')
                hb = [P.sb(st, [128, D], BF16, 'hb') for _ in range(2)]
                pt = [P.ps(st, [128, 8, 128], BF16, 'pt') for _ in range(2)]
                alloc_norm_tmps(P, k, st, ['n0', 'n1'])
                P.dma('sp', gbc[:], gain_row.partition_broadcast(128), writes=['gbc'])
                if moe:
                    xp = [P.sb(st, [128, D], F32, 'xp') for _ in range(2)]
                    hf2 = [P.sb(st, [128, D], F32, 'hf') for _ in range(2)]
                    hTf = P.sb(st, [128, 8, 128], F32, 'hTf')
                    wr = P.sb(st, [128, 8, 8], F32, 'wr')
                    esel = P.sb(st, [128, 8], F32, 'esel')
                    ptf = [P.ps(st, [128, 4, 128], F32, 'ptf') for _ in range(2)]
                    plg = P.ps(st, [128, 8], F32, 'plg')
                    lg = P.sb(st, [128, 8], F32, 'lg')
                    lg2 = P.sb(st, [128, 8], F32, 'lg2')
                    eq1 = P.sb(st, [128, 8], F32, 'eq1')
                    eq2 = P.sb(st, [128, 8], F32, 'eq2')
                    m1 = P.sb(st, [128, 1], F32, 'm1')
                    m2 = P.sb(st, [128, 1], F32, 'm2')
                    w1 = P.sb(st, [128, 1], F32, 'w1')
                    w2 = P.sb(st, [128, 1], F32, 'w2')
                    P.dma('sp', wr[:], wview(router_ap), writes=['wr'])
                    if esel_ap is not None:
                        P.dma('sp', esel[:], esel_ap, writes=['esel'])
                for tl in range(NTH):
                    tt = half * NTH + tl
                    b = tl % 2
                    tag = 'n%d' % b
                    rows = slice(tt * 128, (tt + 1) * 128)
                    if moe:
                        hf = hf2[b]
                        if same:
                            P.dma('sp', xacc[:, tl, :], x_src[rows, :], writes=[tag + 'x', ('xacc', tl)])
                            norm_rows(P, k, st, xacc[:, tl, :], gbc[:], hb[b][:], tag, want_f32=hf[:])
                        else:
                            P.dma('sp', xacc[:, tl, :], xacc_src[rows, :], writes=[('xacc', tl)])
                            P.dma('sp', xp[b][:], x_src[rows, :], writes=[tag + 'x'])
                            norm_rows(P, k, st, xp[b][:], gbc[:], hb[b][:], tag, want_f32=hf[:])
                        for q4 in range(2):
                            P.trg([(ptf[q4][:, c, :], hf[:, (q4 * 4 + c) * 128:(q4 * 4 + c + 1) * 128], k.identf[:]) for c in range(4)],
                                  reads=[tag + 'hf'], writes=[('ptf', q4)])
                            P.copy('dve', hTf[:, q4 * 4:(q4 + 1) * 4, :], ptf[q4][:], reads=[('ptf', q4)], writes=[('hTf', q4)])
                        P.mmg([(plg[:], hTf[:, dc, :], wr[:, dc, :], dc == 0, dc == 7) for dc in range(8)],
                              reads=[('hTf', 0), ('hTf', 1), 'wr'], writes=['plg'])
                        P.copy('dve', lg[:], plg[:], reads=['plg'], writes=['lg'])
                        P.reduce(m1[:], lg[:], ALU.max, reads=['lg'], writes=['m1'])
                        P.ts('dve', eq1[:], lg[:], m1[:, 0:1], None, ALU.is_equal, None, reads=['lg', 'm1'], writes=['eq1'])
                        P.stt(lg2[:], eq1[:], -1e30, lg[:], ALU.mult, ALU.add, reads=['eq1', 'lg'], writes=['lg2'])
                        P.reduce(m2[:], lg2[:], ALU.max, reads=['lg2'], writes=['m2'])
                        P.ts('dve', eq2[:], lg2[:], m2[:, 0:1], None, ALU.is_equal, None, reads=['lg2', 'm2'], writes=['eq2'])
                        P.tt('dve', w2[:], m2[:], m1[:], ALU.subtract, reads=['m1', 'm2'], writes=['w2'])
                        P.act(w2[:], w2[:], AF.Exp, reads=['w2'], writes=['w2'])
                        P.ts('dve', w1[:], w2[:], 1.0, None, ALU.add, None, reads=['w2'], writes=['w1'])
                        P.recip(w1[:], w1[:], reads=['w1'], writes=['w1'])
                        P.tt('dve', w2[:], w2[:], w1[:], ALU.mult, reads=['w1', 'w2'], writes=['w2'])
                        P.ts('dve', eq1[:], eq1[:], w1[:, 0:1], None, ALU.mult, None, reads=['eq1', 'w1'], writes=['eq1'])
                        if esel_ap is None:
                            P.stt(comb[:, tl, :], eq2[:], w2[:, 0:1], eq1[:], ALU.mult, ALU.add, reads=['eq2', 'w2', 'eq1'], writes=[('comb', tl)])
                        else:
                            P.stt(eq2[:], eq2[:], w2[:, 0:1], eq1[:], ALU.mult, ALU.add, reads=['eq2', 'w2', 'eq1'], writes=['eq2'])
                            P.tt('dve', eq2[:], eq2[:], esel[:], ALU.mult, reads=['eq2', 'esel'], writes=['eq2'])
                            P.reduce(csel[:, tl:tl + 1], eq2[:], ALU.add, reads=['eq2'], writes=[('csel', tl)])
                    else:
                        P.dma('sp', xacc[:, tl, :], x_src[rows, :], writes=[tag + 'x', ('xacc', tl)])
                        norm_rows(P, k, st, xacc[:, tl, :], gbc[:], hb[b][:], tag)
                    P.trg([(pt[b][:, c, :], hb[b][:, c * 128:(c + 1) * 128], k.ident[:]) for c in range(8)],
                          reads=[tag + 'hb'], writes=[tag + 'pt'])
                    P.copy('act' if tl % 2 else 'dve', hTh[:, :, tl * 128:(tl + 1) * 128], pt[b][:], reads=[tag + 'pt'], writes=[('hT', tl)])
                P.flush()
            with ExitStack() as st:
                wg = [P.sb(st, [128, 8, 512], BF16, 'wg') for _ in range(2)]
                wu = [P.sb(st, [128, 8, 512], BF16, 'wu') for _ in range(2)]
                wd = [P.sb(st, [128, 4, D], BF16, 'wd') for _ in range(2)]
                sg = [P.sb(st, [128, 512], F32, 'sg') for _ in range(2)]
                aT = [P.sb(st, [128, 4, 512], BF16, 'aT') for _ in range(2)]
                pg = [P.ps(st, [128, 512], F32, 'pg') for _ in range(2)]
                pu = [P.ps(st, [128, 512], F32, 'pu') for _ in range(2)]
                po = [P.ps(st, [128, 512], F32, 'po') for _ in range(4)]
                it = 0
                gi = 0
                oi = 0
                ai = 0
                for e in range(nexp):
                    gu = wview(w_gu_list[e])
                    w_dn_ap = w_dn_list[e]
                    for fg in range(ngrp):
                        F = min(512, dff - fg * 512)
                        nfc = F // 128
                        wb = it % 2
                        it += 1
                        P.dma('pool', wg[wb][:, :, 0:F], gu[:, :, fg * 512:fg * 512 + F], writes=[('wg', wb)], cast=True)
                        P.dma('pool', wu[wb][:, :, 0:F], gu[:, :, dff + fg * 512:dff + fg * 512 + F], writes=[('wu', wb)], cast=True)
                        P.dma('pool', wd[wb][:, 0:nfc, :], wview(w_dn_ap[fg * 512:fg * 512 + F, :]), writes=[('wd', wb)], cast=True)
                        for tb in range(HT // 512):
                            blk = slice(tb * 512, (tb + 1) * 512)
                            ab = ai % 2
                            ai += 1
                            for fc in range(nfc):
                                g = gi % 2
                                gi += 1
                                P.mmg([(pg[g][:], wg[wb][:, dc, fc * 128:(fc + 1) * 128], hTh[:, dc, blk], dc == 0, dc == 7) for dc in range(8)],
                                      reads=[('wg', wb)], writes=[('pg', g)])
                                P.mmg([(pu[g][:], wu[wb][:, dc, fc * 128:(fc + 1) * 128], hTh[:, dc, blk], dc == 0, dc == 7) for dc in range(8)],
                                      reads=[('wu', wb)], writes=[('pu', g)])
                                P.act(sg[g][:], pg[g][:], AF.Silu, reads=[('pg', g)], writes=[('sg', g)])
                                P.tt('dve', aT[ab][:, fc, :], pu[g][:], sg[g][:], ALU.mult, reads=[('pu', g), ('sg', g)], writes=[('aT', ab, fc)])
                            for tq in range(4):
                                tl = tb * 4 + tq
                                for hh in range(2):
                                    o = oi % 4
                                    oi += 1
                                    P.mmg([(po[o][:], aT[ab][:, fc, tq * 128:(tq + 1) * 128], wd[wb][:, fc, hh * 512:(hh + 1) * 512], fc == 0, fc == nfc - 1) for fc in range(nfc)],
                                          reads=[('aT', ab, fc) for fc in range(nfc)] + [('wd', wb)], writes=[('po', o)])
                                    sc_ = (comb[:, tl, e:e + 1] if esel_ap is None else csel[:, tl:tl + 1]) if moe else 1.0
                                    P.stt(xacc[:, tl, hh * 512:(hh + 1) * 512], po[o][:], sc_, xacc[:, tl, hh * 512:(hh + 1) * 512], ALU.mult, ALU.add,
                                          reads=[('po', o), ('xacc', tl, hh)], writes=[('xacc', tl, hh)])
                for tl in range(NTH):
                    tt = half * NTH + tl
                    P.dma('sp', x_dst[tt * 128:(tt + 1) * 128, :], xacc[:, tl, :], reads=[('xacc', tl, 0), ('xacc', tl, 1)], writes=[('xd', tt)])
                P.flush()


def phase_fnorm(P, k, x_src, gain_row, out_ap):
    with ExitStack() as st:
        gbc = P.sb(st, [128, D], F32, 'gbc')
        xt = [P.sb(st, [128, D], F32, 'xt') for _ in range(2)]
        yo = [P.sb(st, [128, D], F32, 'yo') for _ in range(2)]
        alloc_norm_tmps(P, k, st, ['n0', 'n1'])
        P.dma('sp', gbc[:], gain_row.partition_broadcast(128), writes=['gbc'])
        for tt in range(NT):
            b = tt % 2
            tag = 'n%d' % b
            t = k.nt[tag]
            P.dma('sp', xt[b][:], x_src[tt * 128:(tt + 1) * 128, :], writes=[tag + 'x'])
            P.act(t['junk'][:], xt[b][:], AF.Square, reads=[tag + 'x'], writes=[tag + 'junk', tag + 'ssq'], accum=t['ssq'][:])
            P.ts('dve', t['ms'][:], t['ssq'][:], 1.0 / D, EPS, ALU.mult, ALU.add, reads=[tag + 'ssq'], writes=[tag + 'ms'])
            P.tt('pool', t['rstd'][:], t['ms'][:], k.neghalf[:], ALU.pow, reads=[tag + 'ms'], writes=[tag + 'rstd'])
            P.stt(yo[b][:], xt[b][:], t['rstd'][:, 0:1], gbc[:], ALU.mult, ALU.mult, reads=[tag + 'x', tag + 'rstd', 'gbc'], writes=[('yo', b)])
            P.dma('sp', out_ap[tt * 128:(tt + 1) * 128, :], yo[b][:], reads=[('yo', b)], writes=[('out', tt)])
        P.flush()


CONST_SHAPES = {"c_ident": [128, 128], "c_kaug": [6, 6, S], "c_qaug": [6, 6, S], "c_dmask": [6, 128, 128], "c_bias": [128, 6 * 35],
                "c_cmask": [128, 128], "c_smask": [128, 512]}
STEP_INPUTS = {
    'attn': {"x": [S, D], "mem": [MEM_LEN, D], "a_norm_mix": [1, D], "a_w_in": [1, D, 2560], "a_lam_q1": [1, 64], "a_lam_k1": [1, 64],
             "a_lam_q2": [1, 64], "a_lam_k2": [1, 64], "a_subln": [1, 128], "a_mem_norm": [1, D], "a_w_mem_kv": [1, D, 512],
             "a_w_out": [1, D, D], "c_lam": [1, 128, 2], "c_ident": 0, "c_kaug": 0, "c_qaug": 0, "c_dmask": 0, "c_bias": 0},
    'hgrn': {"x": [S, D], "mem": [MEM_LEN, D], "b_norm_mix": [1, D], "b_w_in": [1, D, 3328], "b_lb_logits": [2, 768], "b_out_norm": [1, 128],
             "b_mem_norm": [1, D], "b_w_mem_kv": [1, D, 512], "b_w_out": [1, D, D], "c_lbflag": [1, 128, 1], "c_ident": 0, "c_cmask": 0, "c_smask": 0},
    'dense': {"x": [S, D], "norm": [1, D], "w_gu": [D, 2 * DFF_D], "w_dn": [DFF_D, D], "c_ident": 0},
    'moe1': {"x": [S, D], "xacc": [S, D], "norm": [1, D], "router": [D, 8], "w_gu": [D, 2 * DFF_E], "w_dn": [DFF_E, D], "esel": [128, 8], "c_ident": 0},
    'fnorm': {"x": [S, D], "gain": [1, D], "c_ident": 0},
}


def build_step(kind):
    nc = bass.Bass("TRN2", target_bir_lowering=False)
    inp = {}
    for n, shp in STEP_INPUTS[kind].items():
        if shp == 0:
            shp = CONST_SHAPES[n]
        inp[n] = nc.dram_tensor(n, shp, F32, kind="ExternalInput").ap()
    out = nc.dram_tensor("out", [S, D], F32, kind="ExternalOutput").ap()
    o_s = nc.dram_tensor("o_s", [S, D], BF16, kind="Internal").ap()
    with ExitStack() as st:
        P = Prog(nc, st)
        k = K()
        k.inp = inp
        k.ident = P.sb(st, [128, 128], BF16, 'ident')
        k.identf = P.sb(st, [128, 128], F32, 'identf')
        k.neghalf = P.sb(st, [128, 1], F32, 'neghalf')
        P.dma('pool', k.ident[:], inp['c_ident'], writes=['ident'], cast=True)
        P.dma('sp', k.identf[:], inp['c_ident'], writes=['identf'])
        P.memset('pool', k.neghalf[:], -0.5, writes=['neghalf'])
        P.flush()
        if kind in ('attn', 'hgrn'):
            pre = 'a_' if kind == 'attn' else 'b_'
            with ExitStack() as stm:
                hT = P.sb(stm, [128, 8, S], BF16, 'hT')
                kmT = P.sb(stm, [128, 2, MEM_LEN], BF16, 'kmT')
                vm = P.sb(stm, [128, 2, 4, 65], BF16, 'vm')
                phase_hT(P, k, inp['x'], inp[pre + 'norm_mix'][0:1, :], hT)
                phase_memkv(P, k, inp['mem'], inp[pre + 'mem_norm'][0:1, :], inp[pre + 'w_mem_kv'][0], kmT, vm)
                if kind == 'attn':
                    phase_diffattn(P, k, hT, 0, None, o_s)
                    phase_memattn(P, k, hT, inp['a_w_in'][0], 3 * DA_W, kmT, vm, o_s)
                else:
                    phase_hgrn(P, k, hT, 0, o_s)
                    phase_memattn(P, k, hT, inp['b_w_in'][0], 4 * HG_W, kmT, vm, o_s)
            phase_outproj(P, k, o_s, inp[pre + 'w_out'][0], inp['x'], out)
        elif kind == 'dense':
            phase_ffn(P, k, inp['x'], inp['x'], out, inp['norm'][0:1, :], [inp['w_gu']], [inp['w_dn']], DFF_D)
        elif kind == 'moe1':
            phase_ffn(P, k, inp['x'], inp['xacc'], out, inp['norm'][0:1, :], [inp['w_gu']], [inp['w_dn']], DFF_E,
                      router_ap=inp['router'], esel_ap=inp['esel'])
        elif kind == 'fnorm':
            phase_fnorm(P, k, inp['x'], inp['gain'][0:1, :], out)
        P.add('sp', lambda e: e.nop(), reads=[], writes=[])
        P.flush()
    return nc


def make_consts():
    slopes = 2.0 ** (-8.0 * np.arange(1, 7) / 6.0)
    import ml_dtypes
    bf = ml_dtypes.bfloat16

    def hi_lo(v):
        hi = np.float32(np.float32(v).astype(bf).astype(np.float32))
        lo = np.float32(np.float32(v - hi).astype(bf).astype(np.float32))
        return hi, lo
    c = {}
    c['c_ident'] = np.eye(128, dtype=np.float32)
    pos = np.arange(S)
    krel = (pos % 128).astype(np.float32)
    qrel = pos % 512
    qhi = (qrel & ~3).astype(np.float32)
    qlo = (qrel & 3).astype(np.float32)
    kaug = np.zeros((6, 6, S), np.float32)
    qaug = np.zeros((6, 6, S), np.float32)
    for h in range(6):
        hi, lo = hi_lo(slopes[h])
        kaug[h, 0] = krel
        kaug[h, 1] = krel
        kaug[h, 2] = hi
        kaug[h, 3] = lo
        kaug[h, 4] = hi
        kaug[h, 5] = lo
        qaug[h, 0] = hi
        qaug[h, 1] = lo
        qaug[h, 2] = -qhi
        qaug[h, 3] = -qhi
        qaug[h, 4] = -qlo
        qaug[h, 5] = -qlo
    c['c_kaug'] = kaug
    c['c_qaug'] = qaug
    kk = np.arange(128)[:, None]
    qq = np.arange(128)[None, :]
    dm = np.zeros((6, 128, 128), np.float32)
    for h in range(6):
        allowed = (kk // 64) <= (qq // 64)
        val = np.where(kk <= qq, 1.0, np.exp(-2.0 * slopes[h] * (kk - qq)))
        dm[h] = np.where(allowed, val, 0.0)
    c['c_dmask'] = dm
    cb = np.zeros((128, 6 * 35), np.float32)
    for h in range(6):
        for idx in range(35):
            d = idx - 3
            cb[:, h * 35 + idx] = -slopes[h] * 128.0 * d
    c['c_bias'] = cb
    s_ = np.arange(128)[:, None]
    t_ = np.arange(128)[None, :]
    c['c_cmask'] = ((s_ <= t_) & ((s_ // 64) == (t_ // 64))).astype(np.float32)
    sm = np.ones((128, 512), np.float32)
    sm[:, ::64] = 0.0
    c['c_smask'] = sm
    return c


_CACHE = {}
_CONSTS = {}


def launch(kind, per_core, shared, n_cores):
    if kind not in _CACHE:
        _CACHE[kind] = build_step(kind)
    if not _CONSTS:
        _CONSTS.update(make_consts())
    nc = _CACHE[kind]
    sh = {}
    for n, shp in STEP_INPUTS[kind].items():
        if n in per_core:
            continue
        if shp == 0:
            sh[n] = _CONSTS[n]
        else:
            sh[n] = np.ascontiguousarray(np.asarray(shared[n], dtype=np.float32)).reshape(shp)
    in_maps = []
    for c in range(n_cores):
        m = dict(sh)
        for n, a in per_core.items():
            m[n] = np.ascontiguousarray(a[c])
        in_maps.append(m)
    import os
    tr = bool(os.environ.get('K_TRACE'))
    res = run_bass_kernel_spmd(nc, in_maps, core_ids=list(range(n_cores)), trace=tr)
    if tr:
        print('EXEC_NS', kind, res.exec_time_ns)
    return np.stack([np.asarray(r['out'], dtype=np.float32).reshape(S, D) for r in res.results], axis=0)


def run_step(i, which, x, inputs, n_cores):
    f = lambda n: np.asarray(inputs[n], dtype=np.float32)
    j = i // 2
    mem = f('mem')[:n_cores]
    if which == 'mix' and i % 2 == 0:
        lam_init = 0.8 - 0.6 * float(np.exp(-0.3 * i))
        clam = np.zeros((1, 128, 2), np.float32)
        clam[0, :, 0] = 1.0 - lam_init
        clam[0, :, 1] = -lam_init
        sh = {n: f(n)[j:j + 1] for n in ['a_norm_mix', 'a_w_in', 'a_lam_q1', 'a_lam_k1', 'a_lam_q2', 'a_lam_k2', 'a_subln', 'a_mem_norm', 'a_w_mem_kv', 'a_w_out']}
        sh['c_lam'] = clam
        return launch('attn', {'x': x, 'mem': mem}, sh, n_cores)
    if which == 'mix':
        sh = {n: f(n)[j:j + 1] for n in ['b_norm_mix', 'b_w_in', 'b_out_norm', 'b_mem_norm', 'b_w_mem_kv', 'b_w_out']}
        sh['b_lb_logits'] = f('b_lb_logits')
        sh['c_lbflag'] = np.full((1, 128, 1), -float(j), np.float32)
        return launch('hgrn', {'x': x, 'mem': mem}, sh, n_cores)
    if i % 2 == 0:
        sh = {'norm': f('dense_norm')[j:j + 1], 'w_gu': f('dense_w_gate_up')[j], 'w_dn': f('dense_w_down')[j]}
        return launch('dense', {'x': x}, sh, n_cores)
    xacc = x
    for e in range(NEXP):
        esel = np.zeros((128, 8), np.float32)
        esel[:, e] = 1.0
        sh = {'norm': f('moe_norm')[j:j + 1], 'router': f('moe_router')[j], 'w_gu': f('moe_w_gate_up')[j, e], 'w_dn': f('moe_w_down')[j, e], 'esel': esel}
        xacc = launch('moe1', {'x': x, 'xacc': xacc}, sh, n_cores)
    return xacc


FULL_SHAPES = {
    "x": [S, D], "mem": [MEM_LEN, D],
    "a_norm_mix": [2, D], "a_w_in": [2, D, 2560], "a_lam_q1": [2, 64], "a_lam_k1": [2, 64], "a_lam_q2": [2, 64], "a_lam_k2": [2, 64],
    "a_subln": [2, 128], "a_mem_norm": [2, D], "a_w_mem_kv": [2, D, 512], "a_w_out": [2, D, D],
    "b_norm_mix": [2, D], "b_w_in": [2, D, 3328], "b_lb_logits": [2, 768], "b_out_norm": [2, 128], "b_mem_norm": [2, D],
    "b_w_mem_kv": [2, D, 512], "b_w_out": [2, D, D],
    "dense_norm": [2, D], "dense_w_gate_up": [2, D, 2 * DFF_D], "dense_w_down": [2, DFF_D, D],
    "moe_norm": [2, D], "moe_router": [2, D, 8], "moe_w_gate_up": [2, 8, D, 2 * DFF_E], "moe_w_down": [2, 8, DFF_E, D],
    "final_norm": [1, D], "c_lam": [2, 128, 2], "c_lbflag": [2, 128, 1],
}
FULL_SHAPES.update(CONST_SHAPES)


def build_fused():
    nc = bass.Bass("TRN2", target_bir_lowering=False)
    inp = {n: nc.dram_tensor(n, shp, F32, kind="ExternalInput").ap() for n, shp in FULL_SHAPES.items()}
    out = nc.dram_tensor("out", [S, D], F32, kind="ExternalOutput").ap()
    xres = nc.dram_tensor("xres", [S, D], F32, kind="Internal").ap()
    o_s = nc.dram_tensor("o_s", [S, D], BF16, kind="Internal").ap()
    with ExitStack() as st:
        P = Prog(nc, st)
        k = K()
        k.inp = inp
        k.ident = P.sb(st, [128, 128], BF16, 'ident')
        k.identf = P.sb(st, [128, 128], F32, 'identf')
        k.neghalf = P.sb(st, [128, 1], F32, 'neghalf')
        P.dma('pool', k.ident[:], inp['c_ident'], writes=['ident'], cast=True)
        P.dma('sp', k.identf[:], inp['c_ident'], writes=['identf'])
        P.memset('pool', k.neghalf[:], -0.5, writes=['neghalf'])
        P.flush()
        x_cur = inp['x']
        for i in range(DEPTH):
            j = i // 2
            pre = 'a_' if i % 2 == 0 else 'b_'
            with ExitStack() as stm:
                hT = P.sb(stm, [128, 8, S], BF16, 'hT')
                kmT = P.sb(stm, [128, 2, MEM_LEN], BF16, 'kmT')
                vm = P.sb(stm, [128, 2, 4, 65], BF16, 'vm')
                phase_hT(P, k, x_cur, inp[pre + 'norm_mix'][j:j + 1, :], hT)
                phase_memkv(P, k, inp['mem'], inp[pre + 'mem_norm'][j:j + 1, :], inp[pre + 'w_mem_kv'][j], kmT, vm)
                if i % 2 == 0:
                    phase_diffattn(P, k, hT, j, None, o_s)
                    phase_memattn(P, k, hT, inp['a_w_in'][j], 3 * DA_W, kmT, vm, o_s)
                else:
                    phase_hgrn(P, k, hT, j, o_s)
                    phase_memattn(P, k, hT, inp['b_w_in'][j], 4 * HG_W, kmT, vm, o_s)
            phase_outproj(P, k, o_s, inp[pre + 'w_out'][j], x_cur, xres)
            x_cur = xres
            if i % 2 == 0:
                phase_ffn(P, k, xres, xres, xres, inp['dense_norm'][j:j + 1, :], [inp['dense_w_gate_up'][j]], [inp['dense_w_down'][j]], DFF_D)
            else:
                phase_ffn(P, k, xres, xres, xres, inp['moe_norm'][j:j + 1, :],
                          [inp['moe_w_gate_up'][j, e] for e in range(NEXP)], [inp['moe_w_down'][j, e] for e in range(NEXP)], DFF_E,
                          router_ap=inp['moe_router'][j])
        phase_fnorm(P, k, xres, inp['final_norm'][0:1, :], out)
        P.add('sp', lambda e: e.nop(), reads=[], writes=[])
        P.flush()
    return nc


def fused_consts():
    c = dict(make_consts())
    clam = np.zeros((2, 128, 2), np.float32)
    for j in range(2):
        lam_init = 0.8 - 0.6 * float(np.exp(-0.3 * (2 * j)))
        clam[j, :, 0] = 1.0 - lam_init
        clam[j, :, 1] = -lam_init
    c['c_lam'] = clam
    lbf = np.zeros((2, 128, 1), np.float32)
    lbf[1] = -1.0
    c['c_lbflag'] = lbf
    return c


def kernel_unfused(**inputs):
    n_cores = 8
    x = np.asarray(inputs['x'], dtype=np.float32)
    for i in range(DEPTH):
        x = run_step(i, 'mix', x, inputs, n_cores)
        x = run_step(i, 'ffn', x, inputs, n_cores)
    return launch('fnorm', {'x': x}, {'gain': np.asarray(inputs['final_norm'], dtype=np.float32).reshape(1, D)}, n_cores)


def kernel(**inputs):
    n_cores = 8
    if 'fused' not in _CACHE:
        _CACHE['fused'] = build_fused()
    nc = _CACHE['fused']
    consts = fused_consts()
    shared = {}
    for n, shp in FULL_SHAPES.items():
        if n in ('x', 'mem'):
            continue
        if n in consts:
            shared[n] = consts[n]
        else:
            shared[n] = np.ascontiguousarray(np.asarray(inputs[n], dtype=np.float32)).reshape(shp)
    x = np.asarray(inputs['x'], dtype=np.float32)
    mem = np.asarray(inputs['mem'], dtype=np.float32)
    in_maps = []
    for c in range(n_cores):
        m = dict(shared)
        m['x'] = np.ascontiguousarray(x[c])
        m['mem'] = np.ascontiguousarray(mem[c])
        in_maps.append(m)
    res = run_bass_kernel_spmd(nc, in_maps, core_ids=list(range(n_cores)))
    return np.stack([np.asarray(r['out'], dtype=np.float32).reshape(S, D) for r in res.results], axis=0)
```

```python
import numpy as np
import concourse.bass as bass
import concourse.mybir as mybir
from concourse.bass_utils import run_bass_kernel_spmd
from contextlib import ExitStack

F32 = mybir.dt.float32
BF16 = mybir.dt.bfloat16
ALU = mybir.AluOpType
AF = mybir.ActivationFunctionType
AX = mybir.AxisListType

S = 4096
D = 1024
NT = 32
EPS = 1e-6
DEPTH = 4
DA_W = 768
HG_W = 768
MEM_W = 256
DFF_D = 2816
DFF_E = 3584
NEXP = 8
MEM_LEN = 256

ENGS = ['pe', 'act', 'dve', 'pool', 'sp']
DMA_POOL = {'sp': 24, 'pool': 24, 'act': 4}


class Op:
    __slots__ = ('eng', 'fn', 'deps', 'need', 'sig', 'is_dma', 'dsem', 'dval', 'uid')


class Prog:
    def __init__(self, nc, stack, strict=True):
        self.nc = nc
        self.stack = stack
        self.strict = strict
        self.ops = {e: [] for e in ENGS}
        self.lastw = {}
        self.reads = {}
        self.uid = 0
        self.esem = {e: stack.enter_context(nc.semaphore('s_' + e)) for e in ENGS}
        self.sigc = {e: 0 for e in ENGS}
        self.dsems = {}
        self.dcur = {}
        self.dlast = {}
        self.dfence = {}
        for q, n in DMA_POOL.items():
            self.dsems[q] = [stack.enter_context(nc.semaphore('d_%s%d' % (q, i))) for i in range(n)]
            self.dcur[q] = 0
            self.dlast[q] = [None] * n
            self.dfence[q] = [0] * n
        self.efence = {e: 0 for e in ENGS}
        self.ntile = 0
        self.nflush = 0

    def sb(self, st, shape, dtype, name=None):
        self.ntile += 1
        name = (name or 't') + '_%d' % self.ntile
        return st.enter_context(self.nc.sbuf_tensor(name, list(shape), dtype))

    def ps(self, st, shape, dtype=F32, name=None):
        self.ntile += 1
        name = (name or 'p') + '_%d' % self.ntile
        return st.enter_context(self.nc.psum_tensor(name, list(shape), dtype))

    def add(self, eng, fn, reads=(), writes=(), dma=False):
        op = Op()
        op.eng = eng
        op.fn = fn
        op.need = False
        op.sig = None
        op.is_dma = dma
        op.uid = self.uid
        self.uid += 1
        deps = []
        for k in reads:
            w = self.lastw.get(k)
            if w is not None:
                deps.append((w, 'raw'))
        for k in writes:
            w = self.lastw.get(k)
            if w is not None:
                deps.append((w, 'waw'))
            rd = self.reads.get(k)
            if rd:
                for r in rd.values():
                    deps.append((r, 'war'))
        fdeps = []
        seen = set()
        for d, kind in deps:
            if d.uid in seen:
                continue
            if (not d.is_dma) and (not dma) and d.eng == eng:
                if eng == 'pe' or not self.strict:
                    continue
            seen.add(d.uid)
            fdeps.append(d)
        if dma:
            q = eng
            i = self.dcur[q]
            self.dcur[q] = (i + 1) % len(self.dsems[q])
            prev = self.dlast[q][i]
            op.dsem = self.dsems[q][i]
            op.dval = (prev.dval if prev is not None else 0) + 16
            if prev is not None and prev.uid not in seen and prev.dval > self.dfence[q][i]:
                fdeps.append(prev)
                seen.add(prev.uid)
            self.dlast[q][i] = op
        for d in fdeps:
            d.need = True
        op.deps = fdeps
        rk = ('dma', op.uid) if dma else eng
        for k in writes:
            self.lastw[k] = op
            self.reads[k] = {}
        for k in reads:
            self.reads.setdefault(k, {})[rk] = op
        self.ops[eng].append(op)
        return op

    def flush(self):
        for e in ENGS:
            comp = [op for op in self.ops[e] if not op.is_dma and op.fn is not None]
            if comp:
                comp[-1].need = True
            c = self.sigc[e]
            for op in self.ops[e]:
                if op.need and not op.is_dma:
                    c += 1
                    op.sig = c
            self.sigc[e] = c
        efence = dict(self.efence)
        dfence = {q: list(v) for q, v in self.dfence.items()}

        def run(e, engobj):
            waited = {}
            for e2 in ENGS:
                if efence[e2] > 0:
                    engobj.wait_ge(self.esem[e2], efence[e2])
                    waited[id(self.esem[e2])] = efence[e2]
            for q in dfence:
                for i, v in enumerate(dfence[q]):
                    if v > 0:
                        engobj.wait_ge(self.dsems[q][i], v)
                        waited[id(self.dsems[q][i])] = v
            for op in self.ops[e]:
                for d in op.deps:
                    if d.is_dma:
                        sem, val = d.dsem, d.dval
                    else:
                        sem, val = self.esem[d.eng], d.sig
                    key = id(sem)
                    if waited.get(key, 0) < val:
                        engobj.wait_ge(sem, val)
                        waited[key] = val
                ins = op.fn(engobj)
                if op.is_dma:
                    ins.then_inc(op.dsem, 16)
                elif op.need:
                    ins.then_inc(self.esem[e], 1)

        with self.nc.Block() as block:
            @block.tensor
            def _(t):
                run('pe', t)

            @block.scalar
            def _(t):
                run('act', t)

            @block.vector
            def _(t):
                run('dve', t)

            @block.gpsimd
            def _(t):
                run('pool', t)

            @block.sync
            def _(t):
                run('sp', t)

        for e in ENGS:
            self.efence[e] = self.sigc[e]
            self.ops[e] = []
        for q in self.dlast:
            for i, op in enumerate(self.dlast[q]):
                if op is not None:
                    self.dfence[q][i] = op.dval
        self.lastw = {}
        self.reads = {}
        self.nflush += 1

    def flush_branch(self, flag_ap, recA, recB):
        assert all(len(self.ops[e]) == 0 for e in ENGS)
        st0 = (dict(self.sigc), {q: list(v) for q, v in self.dlast.items()}, dict(self.dcur))

        def record(rec):
            self.sigc = dict(st0[0])
            self.dlast = {q: list(v) for q, v in st0[1].items()}
            self.dcur = dict(st0[2])
            self.lastw = {}
            self.reads = {}
            rec()
            for e in ENGS:
                comp = [op for op in self.ops[e] if not op.is_dma]
                if comp:
                    comp[-1].need = True
                c = self.sigc[e]
                for op in self.ops[e]:
                    if op.need and not op.is_dma:
                        c += 1
                        op.sig = c
                self.sigc[e] = c
            ops = self.ops
            self.ops = {e: [] for e in ENGS}
            dv = {q: [(o.dval if o is not None else 0) for o in self.dlast[q]] for q in self.dlast}
            return ops, dict(self.sigc), dv
        opsA, sigA, dvA = record(recA)
        opsB, sigB, dvB = record(recB)
        fin = {e: max(sigA[e], sigB[e]) for e in ENGS}
        dfin = {q: [max(a, b) for a, b in zip(dvA[q], dvB[q])] for q in dvA}
        efence = dict(self.efence)
        dfence = {q: list(v) for q, v in self.dfence.items()}

        def run_ops(e, engobj, ops, waited):
            for op in ops[e]:
                for d in op.deps:
                    if d.is_dma:
                        sem, val = d.dsem, d.dval
                    else:
                        sem, val = self.esem[d.eng], d.sig
                    key = id(sem)
                    if waited.get(key, 0) < val:
                        engobj.wait_ge(sem, val)
                        waited[key] = val
                ins = op.fn(engobj)
                if op.is_dma:
                    ins.then_inc(op.dsem, 16)
                elif op.need:
                    ins.then_inc(self.esem[e], 1)

        def catchup(e, engobj, sig, dv):
            if sig[e] > 0:
                engobj.wait_ge(self.esem[e], sig[e])
            if fin[e] > sig[e]:
                engobj.sem_inc(self.esem[e], fin[e] - sig[e])
            if e in dv:
                for i, v in enumerate(dv[e]):
                    if dfin[e][i] > v:
                        if v > 0:
                            engobj.wait_ge(self.dsems[e][i], v)
                        engobj.sem_inc(self.dsems[e][i], dfin[e][i] - v)

        def run(e, engobj):
            waited = {}
            for e2 in ENGS:
                if efence[e2] > 0:
                    engobj.wait_ge(self.esem[e2], efence[e2])
                    waited[id(self.esem[e2])] = efence[e2]
            for q in dfence:
                for i, v in enumerate(dfence[q]):
                    if v > 0:
                        engobj.wait_ge(self.dsems[q][i], v)
                        waited[id(self.dsems[q][i])] = v
            reg = engobj.alloc_register('flag_' + e + '_%d' % self.nflush)
            engobj.reg_load(reg, flag_ap)
            with engobj.If_eq(reg, 0):
                run_ops(e, engobj, opsA, dict(waited))
                catchup(e, engobj, sigA, dvA)
            with engobj.Else():
                run_ops(e, engobj, opsB, dict(waited))
                catchup(e, engobj, sigB, dvB)

        with self.nc.Block() as block:
            @block.tensor
            def _(t):
                run('pe', t)

            @block.scalar
            def _(t):
                run('act', t)

            @block.vector
            def _(t):
                run('dve', t)

            @block.gpsimd
            def _(t):
                run('pool', t)

            @block.sync
            def _(t):
                run('sp', t)

        self.sigc = dict(fin)
        for e in ENGS:
            self.efence[e] = fin[e]
        for q in dfin:
            for i, v in enumerate(dfin[q]):
                if v > 0:
                    o = Op()
                    o.dval = v
                    o.dsem = self.dsems[q][i]
                    o.is_dma = True
                    o.uid = -1
                    self.dlast[q][i] = o
                else:
                    self.dlast[q][i] = None
                self.dfence[q][i] = v
            self.dcur[q] = 0
        self.lastw = {}
        self.reads = {}
        self.nflush += 1

    def dma(self, q, out, in_, reads=(), writes=(), cast=False, slow=False):
        if slow:
            fn = lambda e, o=out, i=in_: e.dma_start(out=o, in_=i, allow_slow_non_contiguous=True)
        elif cast:
            fn = lambda e, o=out, i=in_: e.dma_start(out=o, in_=i, max_dma_last_dim=4096)
        else:
            fn = lambda e, o=out, i=in_: e.dma_start(out=o, in_=i)
        return self.add(q, fn, reads, writes, dma=True)

    def mmg(self, mms, reads, writes):
        mms = list(mms)

        def fn(e, mms=mms):
            ins = None
            for (o, l, r, s0, s1) in mms:
                ins = e.matmul(o, l, r, start=s0, stop=s1)
            return ins
        return self.add('pe', fn, reads, writes)

    def trg(self, trs, reads, writes):
        trs = list(trs)

        def fn(e, trs=trs):
            ins = None
            for (o, i, idn) in trs:
                ins = e.transpose(o, i, idn)
            return ins
        return self.add('pe', fn, reads, writes)

    def act(self, out, in_, func, reads, writes, bias=None, scale=None, accum=None):
        kw = {}
        if bias is not None:
            kw['bias'] = bias
        if scale is not None:
            kw['scale'] = scale
        if accum is not None:
            kw['accum_out'] = accum
        return self.add('act', lambda e, o=out, i=in_, f=func, kw=kw: e.activation(out=o, in_=i, func=f, **kw), reads, writes)

    def tt(self, eng, out, in0, in1, op, reads, writes):
        return self.add(eng, lambda e, o=out, a=in0, b=in1, p=op: e.tensor_tensor(out=o, in0=a, in1=b, op=p), reads, writes)

    def ts(self, eng, out, in0, s1, s2, op0, op1, reads, writes):
        if op1 is None:
            return self.add(eng, lambda e, o=out, a=in0, s1=s1, p0=op0: e.tensor_scalar(out=o, in0=a, scalar1=s1, scalar2=None, op0=p0), reads, writes)
        return self.add(eng, lambda e, o=out, a=in0, s1=s1, s2=s2, p0=op0, p1=op1: e.tensor_scalar(out=o, in0=a, scalar1=s1, scalar2=s2, op0=p0, op1=p1), reads, writes)

    def stt(self, out, in0, scalar, in1, op0, op1, reads, writes):
        return self.add('dve', lambda e, o=out, a=in0, s=scalar, b=in1, p0=op0, p1=op1: e.scalar_tensor_tensor(out=o, in0=a, scalar=s, in1=b, op0=p0, op1=p1), reads, writes)

    def copy(self, eng, out, in_, reads, writes):
        if eng == 'act':
            return self.add('act', lambda e, o=out, i=in_: e.copy(out=o, in_=i), reads, writes)
        return self.add(eng, lambda e, o=out, i=in_: e.tensor_copy(out=o, in_=i), reads, writes)

    def memset(self, eng, ap, val, writes):
        return self.add(eng, lambda e, a=ap, v=val: e.memset(a, v), (), writes)

    def recip(self, out, in_, reads, writes):
        return self.add('dve', lambda e, o=out, i=in_: e.reciprocal(out=o, in_=i), reads, writes)

    def reduce(self, out, in_, op, reads, writes):
        return self.add('dve', lambda e, o=out, i=in_, p=op: e.tensor_reduce(out=o, in_=i, axis=AX.X, op=p), reads, writes)

    def scan(self, out, d0, d1, reads, writes):
        return self.add('dve', lambda e, o=out, a=d0, b=d1: e.tensor_tensor_scan(out=o, data0=a, data1=b, initial=0.0, op0=ALU.mult, op1=ALU.add), reads, writes)


class K:
    pass


def wview(ap2d):
    return ap2d.rearrange("(c p) n -> p c n", p=128)


def norm_rows(P, k, st, x_ap, gbc, hb_out, tag, want_f32=None):
    t = k.nt[tag]
    P.act(t['junk'][:], x_ap, AF.Square, reads=[tag + 'x'], writes=[tag + 'junk', tag + 'ssq'], accum=t['ssq'][:])
    P.ts('dve', t['ms'][:], t['ssq'][:], 1.0 / D, EPS, ALU.mult, ALU.add, reads=[tag + 'ssq'], writes=[tag + 'ms'])
    P.tt('pool', t['rstd'][:], t['ms'][:], k.neghalf[:], ALU.pow, reads=[tag + 'ms'], writes=[tag + 'rstd'])
    if want_f32 is not None:
        P.stt(want_f32, x_ap, t['rstd'][:, 0:1], gbc, ALU.mult, ALU.mult, reads=[tag + 'x', tag + 'rstd', 'gbc'], writes=[tag + 'hf'])
        P.copy('act', hb_out, want_f32, reads=[tag + 'hf'], writes=[tag + 'hb'])
    else:
        P.stt(hb_out, x_ap, t['rstd'][:, 0:1], gbc, ALU.mult, ALU.mult, reads=[tag + 'x', tag + 'rstd', 'gbc'], writes=[tag + 'hb'])


def alloc_norm_tmps(P, k, st, tags):
    k.nt = {}
    for tag in tags:
        k.nt[tag] = {
            'junk': P.sb(st, [128, D], F32, 'junk'),
            'ssq': P.sb(st, [128, 1], F32, 'ssq'),
            'ms': P.sb(st, [128, 1], F32, 'ms'),
            'rstd': P.sb(st, [128, 1], F32, 'rstd'),
        }


def phase_hT(P, k, x_src, gain_row, hT):
    with ExitStack() as st:
        gbc = P.sb(st, [128, D], F32, 'gbc')
        xt = [P.sb(st, [128, D], F32, 'xt') for _ in range(2)]
        hb = [P.sb(st, [128, D], BF16, 'hb') for _ in range(2)]
        pt = [P.ps(st, [128, 8, 128], BF16, 'pt') for _ in range(2)]
        alloc_norm_tmps(P, k, st, ['n0', 'n1'])
        P.dma('sp', gbc[:], gain_row.partition_broadcast(128), writes=['gbc'])
        for tt in range(NT):
            b = tt % 2
            tag = 'n%d' % b
            P.dma('sp', xt[b][:], x_src[tt * 128:(tt + 1) * 128, :], writes=[tag + 'x'])
            norm_rows(P, k, st, xt[b][:], gbc[:], hb[b][:], tag)
            P.trg([(pt[b][:, c, :], hb[b][:, c * 128:(c + 1) * 128], k.ident[:]) for c in range(8)],
                  reads=[tag + 'hb'], writes=[tag + 'pt'])
            P.copy('act' if tt % 2 else 'dve', hT[:, :, tt * 128:(tt + 1) * 128], pt[b][:], reads=[tag + 'pt'], writes=[('hT', tt)])
        P.flush()


def phase_memkv(P, k, mem_ap, gain_row, wkv_ap, kmT, vm):
    with ExitStack() as st:
        gbc = P.sb(st, [128, D], F32, 'gbc')
        xt = [P.sb(st, [128, D], F32, 'xt') for _ in range(2)]
        hb = [P.sb(st, [128, D], BF16, 'hb') for _ in range(2)]
        pt = [P.ps(st, [128, 8, 128], BF16, 'pt') for _ in range(2)]
        memT = P.sb(st, [128, 8, MEM_LEN], BF16, 'memT')
        wkv = P.sb(st, [128, 8, 512], BF16, 'wkv')
        pk = P.ps(st, [128, 256], F32, 'pk')
        alloc_norm_tmps(P, k, st, ['n0', 'n1'])
        P.dma('sp', gbc[:], gain_row.partition_broadcast(128), writes=['gbc'])
        P.dma('pool', wkv[:], wview(wkv_ap), writes=['wkv'], cast=True)
        P.memset('pool', vm[:], 1.0, writes=['vm'])
        for mt in range(2):
            tag = 'n%d' % mt
            P.dma('sp', xt[mt][:], mem_ap[mt * 128:(mt + 1) * 128, :], writes=[tag + 'x'])
            norm_rows(P, k, st, xt[mt][:], gbc[:], hb[mt][:], tag)
            P.trg([(pt[mt][:, c, :], hb[mt][:, c * 128:(c + 1) * 128], k.ident[:]) for c in range(8)],
                  reads=[tag + 'hb'], writes=[tag + 'pt'])
            P.copy('dve', memT[:, :, mt * 128:(mt + 1) * 128], pt[mt][:], reads=[tag + 'pt'], writes=[('memT', mt)])
        for p in range(2):
            P.mmg([(pk[:], wkv[:, dc, p * 128:(p + 1) * 128], memT[:, dc, :], dc == 0, dc == 7) for dc in range(8)],
                  reads=['wkv', ('memT', 0), ('memT', 1)], writes=['pk'])
            P.copy('dve', kmT[:, p, :], pk[:], reads=['pk'], writes=[('kmT', p)])
        for mt in range(2):
            P.mmg([(pk[:], memT[:, dc, mt * 128:(mt + 1) * 128], wkv[:, dc, 256:512], dc == 0, dc == 7) for dc in range(8)],
                  reads=['wkv', ('memT', 0), ('memT', 1)], writes=['pk'])
            P.copy('dve', vm[:, mt, :, 0:64], pk[:].rearrange("p (h d) -> p h d", h=4), reads=['pk', 'vm'], writes=[('vm', mt)])
        P.flush()


def phase_memattn(P, k, hT, w_in_ap, col0, kmT, vm, o_s):
    with ExitStack() as st:
        wqm = P.sb(st, [128, 8, 256], BF16, 'wqm')
        qmT = P.sb(st, [128, 2, S], BF16, 'qmT')
        pq = [P.ps(st, [128, 512], F32, 'pq') for _ in range(2)]
        sc = [P.ps(st, [128, 512], F32, 'sc') for _ in range(2)]
        pacc = [P.ps(st, [128, 512], F32, 'pacc') for _ in range(4)]
        pT = [P.sb(st, [128, 512], BF16, 'pT') for _ in range(2)]
        rr = [P.sb(st, [128, 1], F32, 'rr') for _ in range(4)]
        omb = [P.sb(st, [128, 4, 256], BF16, 'omb') for _ in range(2)]
        P.dma('pool', wqm[:], wview(w_in_ap)[:, :, col0:col0 + 256], writes=['wqm'], cast=True)
        for tb in range(8):
            for p in range(2):
                b = (tb * 2 + p) % 2
                P.mmg([(pq[b][:], wqm[:, dc, p * 128:(p + 1) * 128], hT[:, dc, tb * 512:(tb + 1) * 512], dc == 0, dc == 7) for dc in range(8)],
                      reads=['wqm'], writes=[('pq', b)])
                P.act(qmT[:, p, tb * 512:(tb + 1) * 512], pq[b][:], AF.Identity, reads=[('pq', b)], writes=[('qmT', tb, p)], scale=0.125)
        cnt = 0
        for tb in range(8):
            ob = omb[tb % 2]
            for hm in range(4):
                p = hm // 2
                base = (hm % 2) * 64
                for mt in range(2):
                    b = cnt % 2
                    cnt += 1
                    P.mmg([(sc[b][:], kmT[base:base + 64, p, mt * 128:(mt + 1) * 128], qmT[base:base + 64, p, tb * 512:(tb + 1) * 512], True, True)],
                          reads=[('qmT', tb, p)], writes=[('sc', b)])
                    P.act(pT[b][:], sc[b][:], AF.Exp, reads=[('sc', b)], writes=[('pT', b)])
                    P.mmg([(pacc[qs][:, 0:65], pT[b][:, qs * 128:(qs + 1) * 128], vm[:, mt, hm, :], mt == 0, mt == 1) for qs in range(4)],
                          reads=[('pT', b)], writes=[('pacc', qs) for qs in range(4)])
                for qs in range(4):
                    P.recip(rr[qs][:], pacc[qs][:, 64:65], reads=[('pacc', qs)], writes=[('rr', qs)])
                    P.act(ob[:, qs, hm * 64:(hm + 1) * 64], pacc[qs][:, 0:64], AF.Identity, reads=[('pacc', qs), ('rr', qs)],
                          writes=[('omb', tb % 2, qs)], scale=rr[qs][:, 0:1])
            for qs in range(4):
                tt = tb * 4 + qs
                P.dma('sp', o_s[tt * 128:(tt + 1) * 128, 768:1024], ob[:, qs, :], reads=[('omb', tb % 2, qs)], writes=[('o_s', tt, 'm')])
        P.flush()


def phase_outproj(P, k, o_s, w_out_ap, x_src, x_dst):
    with ExitStack() as st:
        wo = P.sb(st, [128, 8, D], BF16, 'wo')
        ot = [P.sb(st, [128, D], BF16, 'ot') for _ in range(2)]
        oT = [P.sb(st, [128, 8, 128], BF16, 'oT') for _ in range(2)]
        xt = [P.sb(st, [128, D], F32, 'xt') for _ in range(2)]
        xn = [P.sb(st, [128, D], F32, 'xn') for _ in range(2)]
        pt = [P.ps(st, [128, 8, 128], BF16, 'pt') for _ in range(2)]
        po = [P.ps(st, [128, 512], F32, 'po') for _ in range(4)]
        P.dma('pool', wo[:], wview(w_out_ap), writes=['wo'], cast=True)
        for tt in range(NT):
            b = tt % 2
            P.dma('sp', ot[b][:], o_s[tt * 128:(tt + 1) * 128, :], writes=[('ot', b)])
            P.dma('sp', xt[b][:], x_src[tt * 128:(tt + 1) * 128, :], writes=[('xt', b)])
            P.trg([(pt[b][:, c, :], ot[b][:, c * 128:(c + 1) * 128], k.ident[:]) for c in range(8)],
                  reads=[('ot', b)], writes=[('pt', b)])
            P.copy('act', oT[b][:], pt[b][:], reads=[('pt', b)], writes=[('oT', b)])
            for hh in range(2):
                pb = b * 2 + hh
                P.mmg([(po[pb][:], oT[b][:, fc, :], wo[:, fc, hh * 512:(hh + 1) * 512], fc == 0, fc == 7) for fc in range(8)],
                      reads=[('oT', b), 'wo'], writes=[('po', pb)])
                P.tt('dve', xn[b][:, hh * 512:(hh + 1) * 512], po[pb][:], xt[b][:, hh * 512:(hh + 1) * 512], ALU.add,
                     reads=[('po', pb), ('xt', b)], writes=[('xn', b, hh)])
            P.dma('sp', x_dst[tt * 128:(tt + 1) * 128, :], xn[b][:], reads=[('xn', b, 0), ('xn', b, 1)], writes=[('xd', tt)])
        P.flush()


def phase_diffattn(P, k, hT, j, lam_init, o_s):
    inp = k.inp
    w_in = wview(inp['a_w_in'][j])
    with ExitStack() as st:
        wqkv = [P.sb(st, [128, 8, 384], BF16, 'wqkv') for _ in range(2)]
        qT = [P.sb(st, [128, 2, S], BF16, 'qT') for _ in range(2)]
        kT = [P.sb(st, [128, 2, S], BF16, 'kT') for _ in range(2)]
        va = [P.sb(st, [128, NT, 129], BF16, 'va') for _ in range(2)]
        dmask = P.sb(st, [128, 6, 128], BF16, 'dmask')
        cbias = P.sb(st, [128, 6 * 35], F32, 'cbias')
        gsub = P.sb(st, [128, 128], F32, 'gsub')
        lv = [P.sb(st, [128, 64], F32, 'lv') for _ in range(4)]
        lt = P.sb(st, [128, 64], F32, 'lt')
        ls = [P.sb(st, [128, 1], F32, 'ls') for _ in range(2)]
        neglam = P.sb(st, [128, 1], F32, 'neglam')
        clam = P.sb(st, [128, 2], F32, 'clam')
        pT = [P.sb(st, [128, 512], BF16, 'pT') for _ in range(3)]
        o0 = P.sb(st, [128, 4, 128], F32, 'o0')
        oo = [P.sb(st, [128, 128], F32, 'oo') for _ in range(2)]
        ob = [P.sb(st, [128, 128], BF16, 'ob') for _ in range(2)]
        junk = P.sb(st, [128, 128], F32, 'junk')
        sm = {n: [P.sb(st, [128, 1], F32, n) for _ in range(2)] for n in ('r0', 'r1', 'ssq', 'ms', 'lnm', 'rstd')}
        pp = [P.ps(st, [128, 512], F32, 'pp') for _ in range(2)]
        sc = [P.ps(st, [128, 512], F32, 'sc') for _ in range(2)]
        pacc = [P.ps(st, [128, 512], F32, 'pacc') for _ in range(4)]

        P.dma('pool', dmask[:], inp['c_dmask'].rearrange("h k q -> k h q"), writes=['dmask'], cast=True)
        P.dma('sp', cbias[:], inp['c_bias'], writes=['cbias'])
        P.dma('sp', gsub[:], inp['a_subln'][j:j + 1, :].partition_broadcast(128), writes=['gsub'])
        P.dma('sp', clam[:], inp['c_lam'][j], writes=['clam'])
        P.ts('dve', gsub[:], gsub[:], clam[:, 0:1], None, ALU.mult, None, reads=['gsub', 'clam'], writes=['gsub'])
        for i, nm in enumerate(['a_lam_q1', 'a_lam_k1', 'a_lam_q2', 'a_lam_k2']):
            P.dma('sp', lv[i][:], inp[nm][j:j + 1, :].partition_broadcast(128), writes=[('lv', i)])
        for i in range(2):
            P.tt('dve', lt[:], lv[2 * i][:], lv[2 * i + 1][:], ALU.mult, reads=[('lv', 2 * i), ('lv', 2 * i + 1)], writes=['lt'])
            P.reduce(ls[i][:], lt[:], ALU.add, reads=['lt'], writes=[('ls', i)])
            P.act(ls[i][:], ls[i][:], AF.Exp, reads=[('ls', i)], writes=[('ls', i)])
        P.tt('dve', neglam[:], ls[1][:], ls[0][:], ALU.subtract, reads=[('ls', 0), ('ls', 1)], writes=['neglam'])
        P.ts('dve', neglam[:], neglam[:], clam[:, 1:2], None, ALU.add, None, reads=['neglam', 'clam'], writes=['neglam'])
        for b in range(2):
            P.memset('pool', va[b][:, :, 128:129], 1.0, writes=[('va1', b)])

        scb = [sc[0], sc[1], pp[1]]
        pT4 = pT + [P.sb(st, [128, 512], BF16, 'pT')]

        def project_units(h):
            b = h % 2
            W = wqkv[b]
            units = []

            def u0():
                for i, c0 in enumerate([h * 128, DA_W + h * 128, 2 * DA_W + h * 128]):
                    P.dma('pool', W[:, :, i * 128:(i + 1) * 128], w_in[:, :, c0:c0 + 128], writes=[('w', b, i)], cast=True)
                for m in range(2):
                    P.dma('pool', qT[b][64:70, m, :], inp['c_qaug'][h], writes=[('qa', b, m)], cast=True)
                    P.dma('pool', kT[b][64:70, m, :], inp['c_kaug'][h], writes=[('ka', b, m)], cast=True)
            units.append(u0)
            for tb in range(8):
                for m in range(2):
                    for isk in range(2):
                        def u(tb=tb, m=m, isk=isk):
                            c0 = isk * 128 + m * 64
                            P.mmg([(pp[0][0:64, :], W[:, dc, c0:c0 + 64], hT[:, dc, tb * 512:(tb + 1) * 512], dc == 0, dc == 7) for dc in range(8)],
                                  reads=[('w', b, isk)], writes=[('pp', 0)])
                            if isk == 0:
                                P.ts('dve', qT[b][0:64, m, tb * 512:(tb + 1) * 512], pp[0][0:64, :], 0.125, None, ALU.mult, None,
                                     reads=[('pp', 0)], writes=[('q', b, m, tb)])
                            else:
                                P.copy('dve', kT[b][0:64, m, tb * 512:(tb + 1) * 512], pp[0][0:64, :], reads=[('pp', 0)], writes=[('k', b, m, tb)])
                        units.append(u)
                for tq in range(4):
                    def uv(tt=tb * 4 + tq):
                        P.mmg([(pp[0][:, 0:128], hT[:, dc, tt * 128:(tt + 1) * 128], W[:, dc, 256:384], dc == 0, dc == 7) for dc in range(8)],
                              reads=[('w', b, 2)], writes=[('pp', 0)])
                        P.copy('dve', va[b][:, tt, 0:128], pp[0][:, 0:128], reads=[('pp', 0), ('va1', b)], writes=[('v', b, tt)])
                    units.append(uv)
            return units

        def attend(h, nxt):
            b = h % 2
            blocks = [(jq, m, kt) for jq in range(8) for m in range(2) for kt in range(4 * jq + 4)]
            nb = len(blocks)
            evc = [0]

            def geom(i):
                jq, m, kt = blocks[i]
                r = kt - 4 * jq
                off = max(r, 0) * 128
                return jq, m, kt, r, off, 512 - off

            def emit_qk(i):
                jq, m, kt, r, off, N = geom(i)
                sb_ = i % 3
                P.mmg([(scb[sb_][:, 0:N], kT[b][0:70, m, kt * 128:(kt + 1) * 128], qT[b][0:70, m, jq * 512 + off:(jq + 1) * 512], True, True)],
                      reads=[('q', b, m, jq), ('qa', b, m), ('ka', b, m), ('k', b, m, kt // 4)], writes=[('sc', sb_)])

            def emit_exp(i):
                jq, m, kt, r, off, N = geom(i)
                sb_ = i % 3
                pb = i % 4
                bi = h * 35 + (4 * jq - kt + 3)
                P.act(pT4[pb][:, off:512], scb[sb_][:, 0:N], AF.Exp, reads=[('sc', sb_), 'cbias'], writes=[('pT', pb)], bias=cbias[:, bi:bi + 1])
                if r >= 0:
                    P.tt('dve', pT4[pb][:, off:off + 128], pT4[pb][:, off:off + 128], dmask[:, h, :], ALU.mult,
                         reads=[('pT', pb), 'dmask'], writes=[('pT', pb)])

            def emit_pv(i):
                jq, m, kt, r, off, N = geom(i)
                pb = i % 4
                qs0 = max(r, 0)
                P.mmg([(pacc[qs][:, 0:129], pT4[pb][:, qs * 128:(qs + 1) * 128], va[b][:, kt, :], kt == 0, kt == 4 * jq + qs) for qs in range(qs0, 4)],
                      reads=[('pT', pb), ('v', b, kt), ('va1', b)], writes=[('pacc', qs) for qs in range(qs0, 4)])
                if kt == 4 * jq + 3:
                    for qs in range(4):
                        e = evc[0] % 2
                        evc[0] += 1
                        if m == 0:
                            P.recip(sm['r0'][e][:], pacc[qs][:, 128:129], reads=[('pacc', qs)], writes=[('r0', e)])
                            P.ts('dve', o0[:, qs, :], pacc[qs][:, 0:128], sm['r0'][e][:, 0:1], None, ALU.mult, None,
                                 reads=[('pacc', qs), ('r0', e)], writes=[('o0', qs)])
                        else:
                            P.recip(sm['r1'][e][:], pacc[qs][:, 128:129], reads=[('pacc', qs)], writes=[('r1', e)])
                            P.tt('dve', sm['r1'][e][:], sm['r1'][e][:], neglam[:], ALU.mult, reads=[('r1', e), 'neglam'], writes=[('r1', e)])
                            P.stt(oo[e][:], pacc[qs][:, 0:128], sm['r1'][e][:, 0:1], o0[:, qs, :], ALU.mult, ALU.add,
                                  reads=[('pacc', qs), ('r1', e), ('o0', qs)], writes=[('oo', e)])
                            P.add('dve', lambda eng, o=junk[:], a=oo[e][:], acc=sm['ssq'][e][:]: eng.scalar_tensor_tensor(
                                out=o, in0=a, scalar=1.0, in1=a, op0=ALU.mult, op1=ALU.mult, accum_out=acc),
                                reads=[('oo', e)], writes=['junk', ('ssq', e)])
                            P.ts('dve', sm['ms'][e][:], sm['ssq'][e][:], 1.0 / 128, EPS, ALU.mult, ALU.add, reads=[('ssq', e)], writes=[('ms', e)])
                            P.tt('pool', sm['rstd'][e][:], sm['ms'][e][:], k.neghalf[:], ALU.pow, reads=[('ms', e)], writes=[('rstd', e)])
                            P.stt(ob[e][:], oo[e][:], sm['rstd'][e][:, 0:1], gsub[:], ALU.mult, ALU.mult,
                                  reads=[('oo', e), ('rstd', e), 'gsub'], writes=[('ob', e)])
                            tt = jq * 4 + qs
                            P.dma('sp', o_s[tt * 128:(tt + 1) * 128, h * 128:(h + 1) * 128], ob[e][:], reads=[('ob', e)], writes=[('o_s', tt, h)])

            emit_qk(0)
            emit_qk(1)
            for i in range(nb):
                emit_exp(i)
                if i + 2 < nb:
                    emit_qk(i + 2)
                emit_pv(i)
                if i % 4 == 3 and nxt:
                    nxt.pop(0)()
            while nxt:
                nxt.pop(0)()

        for u in project_units(0):
            u()
        for h in range(6):
            attend(h, project_units(h + 1) if h + 1 < 6 else [])
        P.flush()


def phase_hgrn(P, k, hT, j, o_s):
    inp = k.inp
    w_in = wview(inp['b_w_in'][j])
    with ExitStack() as st:
        W = [P.sb(st, [128, 8, 512], BF16, 'W') for _ in range(2)]
        qeT = [P.sb(st, [128, S], BF16, 'qeT') for _ in range(2)]
        keT = [P.sb(st, [128, S], BF16, 'keT') for _ in range(2)]
        ketok = [P.sb(st, [128, NT, 128], BF16, 'ketok') for _ in range(2)]
        vtok = [P.sb(st, [128, NT, 128], BF16, 'vtok') for _ in range(2)]
        sgtok = [P.sb(st, [128, NT, 128], BF16, 'sgtok') for _ in range(2)]
        decay = [P.sb(st, [128, 64], F32, 'decay') for _ in range(2)]
        oml = P.sb(st, [128, 6], F32, 'oml')
        gn = P.sb(st, [128, 128], F32, 'gn')
        cmask = P.sb(st, [128, 128], BF16, 'cmask')
        smask = P.sb(st, [128, 512], F32, 'smask')
        tmp = {n: [P.sb(st, [128, 512], F32, n) for _ in range(2)] for n in ('et', 'dt', 'kk', 'gt', 'bt', 'eb', 'enb', 'qraw')}
        Sst = P.sb(st, [128, 128], F32, 'Sst')
        Sbf = P.sb(st, [128, 128], BF16, 'Sbf')
        at = [P.sb(st, [128, 128], BF16, 'at') for _ in range(2)]
        o1 = [P.sb(st, [128, 128], F32, 'o1') for _ in range(2)]
        ob = [P.sb(st, [128, 128], BF16, 'ob') for _ in range(2)]
        junk = P.sb(st, [128, 128], F32, 'junk')
        sm = {n: [P.sb(st, [128, 1], F32, n) for _ in range(2)] for n in ('ssq', 'ms', 'rstd')}
        pq = P.ps(st, [128, 512], F32, 'pq')
        ptr = P.ps(st, [128, 4, 128], BF16, 'ptr')
        pvg = P.ps(st, [128, 128], F32, 'pvg')
        pat = P.ps(st, [128, 128], F32, 'pat')
        pkv2 = [P.ps(st, [128, 128], F32, 'pkv') for _ in range(2)]
        po = [P.ps(st, [128, 128], F32, 'po') for _ in range(2)]

        P.dma('pool', cmask[:], inp['c_cmask'], writes=['cmask'], cast=True)
        P.dma('sp', smask[:], inp['c_smask'], writes=['smask'])
        P.dma('sp', gn[:], inp['b_out_norm'][j:j + 1, :].partition_broadcast(128), writes=['gn'])
        lbf = P.sb(st, [128, 1], F32, 'lbf')
        lbl2 = P.sb(st, [2, 768], F32, 'lbl2')
        lbT = P.sb(st, [128, 6, 2], F32, 'lbT')
        P.dma('sp', lbf[:], inp['c_lbflag'][j], writes=['lbf'])
        P.dma('sp', lbl2[:], inp['b_lb_logits'], writes=['lbl2'])
        P.trg([(pvg[:, 2 * h:2 * h + 2], lbl2[0:2, h * 128:(h + 1) * 128], k.identf[0:2, 0:2]) for h in range(6)], reads=['lbl2'], writes=['pvg'])
        P.copy('dve', lbT[:], pvg[:, 0:12].rearrange("p (h l) -> p h l", l=2), reads=['pvg'], writes=['lbl'])
        P.tt('dve', oml[:], lbT[:, :, 0], lbT[:, :, 1], ALU.subtract, reads=['lbl'], writes=['oml'])
        P.act(oml[:], oml[:], AF.Exp, reads=['oml'], writes=['oml'])
        P.ts('dve', oml[:], oml[:], 1.0, None, ALU.add, None, reads=['oml'], writes=['oml'])
        P.recip(oml[:], oml[:], reads=['oml'], writes=['oml'])
        P.ts('dve', oml[:], oml[:], lbf[:, 0:1], 1.0, ALU.mult, ALU.add, reads=['oml', 'lbf'], writes=['oml'])

        def project_units(h):
            b = h % 2
            units = []

            def u0():
                for i, c0 in enumerate([h * 128, HG_W + h * 128, 2 * HG_W + h * 128, 3 * HG_W + h * 128]):
                    P.dma('pool', W[b][:, :, i * 128:(i + 1) * 128], w_in[:, :, c0:c0 + 128], writes=[('w', b, i)], cast=True)
            units.append(u0)
            for tb in range(8):
                def ua(tb=tb):
                    blk = slice(tb * 512, (tb + 1) * 512)
                    x = tb % 2
                    et, dt_, kk, gt, bt, eb, enb, qraw = (tmp[n][x] for n in ('et', 'dt', 'kk', 'gt', 'bt', 'eb', 'enb', 'qraw'))
                    T = lambda n: (n, x)
                    P.mmg([(pq[:], W[b][:, dc, 0:128], hT[:, dc, blk], dc == 0, dc == 7) for dc in range(8)], reads=[('w', b, 0)], writes=['pq'])
                    P.copy('act', qraw[:], pq[:], reads=['pq'], writes=[T('qraw')])
                    P.mmg([(pq[:], W[b][:, dc, 128:256], hT[:, dc, blk], dc == 0, dc == 7) for dc in range(8)], reads=[('w', b, 1)], writes=['pq'])
                    P.act(et[:], pq[:], AF.Exp, reads=['pq'], writes=[T('et')], scale=-1.0)
                    P.ts('dve', dt_[:], et[:], 1.0, None, ALU.add, None, reads=[T('et')], writes=[T('dt')])
                    P.recip(dt_[:], dt_[:], reads=[T('dt')], writes=[T('dt')])
                    P.stt(kk[:], et[:], oml[:, h:h + 1], dt_[:], ALU.mult, ALU.mult, reads=[T('et'), T('dt'), 'oml'], writes=[T('kk')])
                    P.act(gt[:], kk[:], AF.Ln, reads=[T('kk')], writes=[T('gt')], scale=-1.0, bias=1.0)
                    P.scan(bt[:], smask[:], gt[:], reads=['smask', T('gt')], writes=[T('bt')])
                    P.act(eb[:], bt[:], AF.Exp, reads=[T('bt')], writes=[T('eb')])
                    P.act(enb[:], bt[:], AF.Exp, reads=[T('bt')], writes=[T('enb')], scale=-1.0)
                    P.tt('dve', qeT[b][:, blk], qraw[:], eb[:], ALU.mult, reads=[T('qraw'), T('eb')], writes=[('qe', b, tb)])
                    P.tt('dve', keT[b][:, blk], kk[:], enb[:], ALU.mult, reads=[T('kk'), T('enb')], writes=[('ke', b, tb)])
                    P.copy('act', decay[b][:, tb * 8:(tb + 1) * 8], eb[:].rearrange("p (c t) -> p c t", t=64)[:, :, 63], reads=[T('eb')], writes=[('dec', b, tb)])
                    P.trg([(ptr[:, i, :], keT[b][:, tb * 512 + i * 128: tb * 512 + (i + 1) * 128], k.ident[:]) for i in range(4)],
                          reads=[('ke', b, tb)], writes=['ptr'])
                    P.copy('act', ketok[b][:, tb * 4:(tb + 1) * 4, :], ptr[:], reads=['ptr'], writes=[('ketok', b, tb)])
                units.append(ua)
                for tq in range(4):
                    def uv(tt=tb * 4 + tq):
                        tok = slice(tt * 128, (tt + 1) * 128)
                        P.mmg([(pvg[:], hT[:, dc, tok], W[b][:, dc, 256:384], dc == 0, dc == 7) for dc in range(8)], reads=[('w', b, 2)], writes=['pvg'])
                        P.copy('dve', vtok[b][:, tt, :], pvg[:], reads=['pvg'], writes=[('vtok', b, tt)])
                        P.mmg([(pvg[:], hT[:, dc, tok], W[b][:, dc, 384:512], dc == 0, dc == 7) for dc in range(8)], reads=[('w', b, 3)], writes=['pvg'])
                        P.act(sgtok[b][:, tt, :], pvg[:], AF.Silu, reads=['pvg'], writes=[('sgtok', b, tt)])
                    units.append(uv)
            return units

        def recur(h, nxt):
            b = h % 2
            P.memset('pool', Sst[:], 0.0, writes=['Sst'])
            P.memset('pool', Sbf[:], 0.0, writes=['Sbf'])
            for tt in range(NT):
                e = tt % 2
                tb = tt // 4
                tok = slice(tt * 128, (tt + 1) * 128)
                P.mmg([(pat[:], keT[b][:, tok], qeT[b][:, tok], True, True)], reads=[('ke', b, tb), ('qe', b, tb)], writes=['pat'])
                P.tt('dve', at[e][:], pat[:], cmask[:], ALU.mult, reads=['pat', 'cmask'], writes=[('at', e)])
                P.mmg([(pkv2[ci][:], ketok[b][ci * 64:(ci + 1) * 64, tt, :], vtok[b][ci * 64:(ci + 1) * 64, tt, :], True, True) for ci in range(2)],
                      reads=[('ketok', b, tb), ('vtok', b, tt)], writes=['pkv'])
                P.mmg([(po[e][:], at[e][:], vtok[b][:, tt, :], True, False),
                       (po[e][0:64, :], qeT[b][:, tt * 128:tt * 128 + 64], Sbf[:], False, False)],
                      reads=[('at', e), ('vtok', b, tt), ('qe', b, tb), 'Sbf'], writes=[('po', e)])
                for ci in range(2):
                    c = 2 * tt + ci
                    P.tt('dve', Sst[:], pkv2[ci][:], Sst[:], ALU.add, reads=['pkv', 'Sst'], writes=['Sst'])
                    P.ts('dve', Sst[:], Sst[:], decay[b][:, c:c + 1], None, ALU.mult, None, reads=['Sst', ('dec', b, tb)], writes=['Sst'])
                    P.copy('act', Sbf[:], Sst[:], reads=['Sst'], writes=['Sbf'])
                    if ci == 0:
                        P.mmg([(po[e][64:128, :], qeT[b][:, tt * 128 + 64:(tt + 1) * 128], Sbf[:], False, True)],
                              reads=[('qe', b, tb), 'Sbf'], writes=[('po', e)])
                P.act(junk[:], po[e][:], AF.Square, reads=[('po', e)], writes=['junk', ('ssq', e)], accum=sm['ssq'][e][:])
                P.ts('dve', sm['ms'][e][:], sm['ssq'][e][:], 1.0 / 128, EPS, ALU.mult, ALU.add, reads=[('ssq', e)], writes=[('ms', e)])
                P.tt('pool', sm['rstd'][e][:], sm['ms'][e][:], k.neghalf[:], ALU.pow, reads=[('ms', e)], writes=[('rstd', e)])
                P.stt(o1[e][:], po[e][:], sm['rstd'][e][:, 0:1], gn[:], ALU.mult, ALU.mult, reads=[('po', e), ('rstd', e), 'gn'], writes=[('o1', e)])
                P.tt('dve', ob[e][:], o1[e][:], sgtok[b][:, tt, :], ALU.mult, reads=[('o1', e), ('sgtok', b, tt)], writes=[('ob', e)])
                P.dma('sp', o_s[tt * 128:(tt + 1) * 128, h * 128:(h + 1) * 128], ob[e][:], reads=[('ob', e)], writes=[('o_s', tt, h)])
                for _ in range(2):
                    if nxt:
                        nxt.pop(0)()
            while nxt:
                nxt.pop(0)()

        for u in project_units(0):
            u()
        for h in range(6):
            recur(h, project_units(h + 1) if h + 1 < 6 else [])
        P.flush()


def phase_ffn(P, k, x_src, xacc_src, x_dst, gain_row, w_gu_list, w_dn_list, dff, router_ap=None, esel_ap=None):
    HT = S // 2
    NTH = NT // 2
    ngrp = (dff + 511) // 512
    moe = router_ap is not None
    same = xacc_src is x_src
    nexp = len(w_gu_list)
    for half in range(2):
        with ExitStack() as st0:
            xacc = P.sb(st0, [128, NTH, D], F32, 'xacc')
            hTh = P.sb(st0, [128, 8, HT], BF16, 'hTh')
            csel = P.sb(st0, [128, NTH], F32, 'csel')
            comb = P.sb(st0, [128, NTH, 8], F32, 'comb')
            with ExitStack() as st:
                gbc = P.sb(st, [128, D], F32, 'gbc')
                hb = [P.sb(st, [128, D], BF16, 'hb') for _ in range(2)]
                pt = [P.ps(st, [128, 8, 128], BF16, 'pt') for _ in range(2)]
                alloc_norm_tmps(P, k, st, ['n0', 'n1'])
                P.dma('sp', gbc[:], gain_row.partition_broadcast(128), writes=['gbc'])
                if moe:
                    xp = [P.sb(st, [128, D], F32, 'xp') for _ in range(2)]
                    hf2 = [P.sb(st, [128, D], F32, 'hf') for _ in range(2)]
                    hTf = P.sb(st, [128, 8, 128], F32, 'hTf')
                    wr = P.sb(st, [128, 8, 8], F32, 'wr')
                    esel = P.sb(st, [128, 8], F32, 'esel')
                    ptf = [P.ps(st, [128, 4, 128], F32, 'ptf') for _ in range(2)]
                    plg = P.ps(st, [128, 8], F32, 'plg')
                    lg = P.sb(st, [128, 8], F32, 'lg')
                    lg2 = P.sb(st, [128, 8], F32, 'lg2')
                    eq1 = P.sb(st, [128, 8], F32, 'eq1')
                    eq2 = P.sb(st, [128, 8], F32, 'eq2')
                    m1 = P.sb(st, [128, 1], F32, 'm1')
                    m2 = P.sb(st, [128, 1], F32, 'm2')
                    w1 = P.sb(st, [128, 1], F32, 'w1')
                    w2 = P.sb(st, [128, 1], F32, 'w2')
                    P.dma('sp', wr[:], wview(router_ap), writes=['wr'])
                    if esel_ap is not None:
                        P.dma('sp', esel[:], esel_ap, writes=['esel'])
                for tl in range(NTH):
                    tt = half * NTH + tl
                    b = tl % 2
                    tag = 'n%d' % b
                    rows = slice(tt * 128, (tt + 1) * 128)
                    if moe:
                        hf = hf2[b]
                        if same:
                            P.dma('sp', xacc[:, tl, :], x_src[rows, :], writes=[tag + 'x', ('xacc', tl)])
                            norm_rows(P, k, st, xacc[:, tl, :], gbc[:], hb[b][:], tag, want_f32=hf[:])
                        else:
                            P.dma('sp', xacc[:, tl, :], xacc_src[rows, :], writes=[('xacc', tl)])
                            P.dma('sp', xp[b][:], x_src[rows, :], writes=[tag + 'x'])
                            norm_rows(P, k, st, xp[b][:], gbc[:], hb[b][:], tag, want_f32=hf[:])
                        for q4 in range(2):
                            P.trg([(ptf[q4][:, c, :], hf[:, (q4 * 4 + c) * 128:(q4 * 4 + c + 1) * 128], k.identf[:]) for c in range(4)],
                                  reads=[tag + 'hf'], writes=[('ptf', q4)])
                            P.copy('dve', hTf[:, q4 * 4:(q4 + 1) * 4, :], ptf[q4][:], reads=[('ptf', q4)], writes=[('hTf', q4)])
                        P.mmg([(plg[:], hTf[:, dc, :], wr[:, dc, :], dc == 0, dc == 7) for dc in range(8)],
                              reads=[('hTf', 0), ('hTf', 1), 'wr'], writes=['plg'])
                        P.copy('dve', lg[:], plg[:], reads=['plg'], writes=['lg'])
                        P.reduce(m1[:], lg[:], ALU.max, reads=['lg'], writes=['m1'])
                        P.ts('dve', eq1[:], lg[:], m1[:, 0:1], None, ALU.is_equal, None, reads=['lg', 'm1'], writes=['eq1'])
                        P.stt(lg2[:], eq1[:], -1e30, lg[:], ALU.mult, ALU.add, reads=['eq1', 'lg'], writes=['lg2'])
                        P.reduce(m2[:], lg2[:], ALU.max, reads=['lg2'], writes=['m2'])
                        P.ts('dve', eq2[:], lg2[:], m2[:, 0:1], None, ALU.is_equal, None, reads=['lg2', 'm2'], writes=['eq2'])
                        P.tt('dve', w2[:], m2[:], m1[:], ALU.subtract, reads=['m1', 'm2'], writes=['w2'])
                        P.act(w2[:], w2[:], AF.Exp, reads=['w2'], writes=['w2'])
                        P.ts('dve', w1[:], w2[:], 1.0, None, ALU.add, None, reads=['w2'], writes=['w1'])
                        P.recip(w1[:], w1[:], reads=['w1'], writes=['w1'])
                        P.tt('dve', w2[:], w2[:], w1[:], ALU.mult, reads=['w1', 'w2'], writes=['w2'])
                        P.ts('dve', eq1[:], eq1[:], w1[:, 0:1], None, ALU.mult, None, reads=['eq1', 'w1'], writes=['eq1'])
                        if esel_ap is None:
                            P.stt(comb[:, tl, :], eq2[:], w2[:, 0:1], eq1[:], ALU.mult, ALU.add, reads=['eq2', 'w2', 'eq1'], writes=[('comb', tl)])
                        else:
                            P.stt(eq2[:], eq2[:], w2[:, 0:1], eq1[:], ALU.mult, ALU.add, reads=['eq2', 'w2', 'eq1'], writes=['eq2'])
                            P.tt('dve', eq2[:], eq2[:], esel[:], ALU.mult, reads=['eq2', 'esel'], writes=['eq2'])
                            P.reduce(csel[:, tl:tl + 1], eq2[:], ALU.add, reads=['eq2'], writes=[('csel', tl)])
                    else:
                        P.dma('sp', xacc[:, tl, :], x_src[rows, :], writes=[tag + 'x', ('xacc', tl)])
                        norm_rows(P, k, st, xacc[:, tl, :], gbc[:], hb[b][:], tag)
                    P.trg([(pt[b][:, c, :], hb[b][:, c * 128:(c + 1) * 128], k.ident[:]) for c in range(8)],
                          reads=[tag + 'hb'], writes=[tag + 'pt'])
                    P.copy('act' if tl % 2 else 'dve', hTh[:, :, tl * 128:(tl + 1) * 128], pt[b][:], reads=[tag + 'pt'], writes=[('hT', tl)])
                P.flush()
            with ExitStack() as st:
                wg = [P.sb(st, [128, 8, 512], BF16, 'wg') for _ in range(2)]
                wu = [P.sb(st, [128, 8, 512], BF16, 'wu') for _ in range(2)]
                wd = [P.sb(st, [128, 4, D], BF16, 'wd') for _ in range(2)]
                sg = [P.sb(st, [128, 512], F32, 'sg') for _ in range(2)]
                aT = [P.sb(st, [128, 4, 512], BF16, 'aT') for _ in range(2)]
                pg = [P.ps(st, [128, 512], F32, 'pg') for _ in range(2)]
                pu = [P.ps(st, [128, 512], F32, 'pu') for _ in range(2)]
                po = [P.ps(st, [128, 512], F32, 'po') for _ in range(4)]
                it = 0
                gi = 0
                oi = 0
                ai = 0
                for e in range(nexp):
                    gu = wview(w_gu_list[e])
                    w_dn_ap = w_dn_list[e]
                    for fg in range(ngrp):
                        F = min(512, dff - fg * 512)
                        nfc = F // 128
                        wb = it % 2
                        it += 1
                        P.dma('pool', wg[wb][:, :, 0:F], gu[:, :, fg * 512:fg * 512 + F], writes=[('wg', wb)], cast=True)
                        P.dma('pool', wu[wb][:, :, 0:F], gu[:, :, dff + fg * 512:dff + fg * 512 + F], writes=[('wu', wb)], cast=True)
                        P.dma('pool', wd[wb][:, 0:nfc, :], wview(w_dn_ap[fg * 512:fg * 512 + F, :]), writes=[('wd', wb)], cast=True)
                        for tb in range(HT // 512):
                            blk = slice(tb * 512, (tb + 1) * 512)
                            ab = ai % 2
                            ai += 1
                            for fc in range(nfc):
                                g = gi % 2
                                gi += 1
                                P.mmg([(pg[g][:], wg[wb][:, dc, fc * 128:(fc + 1) * 128], hTh[:, dc, blk], dc == 0, dc == 7) for dc in range(8)],
                                      reads=[('wg', wb)], writes=[('pg', g)])
                                P.mmg([(pu[g][:], wu[wb][:, dc, fc * 128:(fc + 1) * 128], hTh[:, dc, blk], dc == 0, dc == 7) for dc in range(8)],
                                      reads=[('wu', wb)], writes=[('pu', g)])
                                P.act(sg[g][:], pg[g][:], AF.Silu, reads=[('pg', g)], writes=[('sg', g)])
                                P.tt('dve', aT[ab][:, fc, :], pu[g][:], sg[g][:], ALU.mult, reads=[('pu', g), ('sg', g)], writes=[('aT', ab, fc)])
                            for tq in range(4):
                                tl = tb * 4 + tq
                                for hh in range(2):
                                    o = oi % 4
                                    oi += 1
                                    P.mmg([(po[o][:], aT[ab][:, fc, tq * 128:(tq + 1) * 128], wd[wb][:, fc, hh * 512:(hh + 1) * 512], fc == 0, fc == nfc - 1) for fc in range(nfc)],
                                          reads=[('aT', ab, fc) for fc in range(nfc)] + [('wd', wb)], writes=[('po', o)])
                                    sc_ = (comb[:, tl, e:e + 1] if esel_ap is None else csel[:, tl:tl + 1]) if moe else 1.0
                                    P.stt(xacc[:, tl, hh * 512:(hh + 1) * 512], po[o][:], sc_, xacc[:, tl, hh * 512:(hh + 1) * 512], ALU.mult, ALU.add,
                                          reads=[('po', o), ('xacc', tl, hh)], writes=[('xacc', tl, hh)])
                for tl in range(NTH):
                    tt = half * NTH + tl
                    P.dma('sp', x_dst[tt * 128:(tt + 1) * 128, :], xacc[:, tl, :], reads=[('xacc', tl, 0), ('xacc', tl, 1)], writes=[('xd', tt)])
                P.flush()


def phase_moe_sparse(P, k, x_src, x_dst, gain_row, w_gu_list, w_dn_list, router_ap, hbk):
    I32 = mybir.dt.int32
    NQ = 4
    NTQ = NT // NQ
    dff = DFF_E
    ngrp = dff // 512
    import os
    thr = float(os.environ.get('K_MOE_THR', '64'))
    for qt in range(NQ):
        with ExitStack() as st0:
            xacc = P.sb(st0, [128, NTQ, D], F32, 'xacc')
            comb = P.sb(st0, [128, NTQ * 8], F32, 'comb')
            maskf = P.sb(st0, [128, NTQ * 8], F32, 'maskf')
            pos = P.sb(st0, [128, NTQ * 8], F32, 'pos')
            flag = P.sb(st0, [128, 1], I32, 'flag')
            iota = P.sb(st0, [128, 128], F32, 'iota')
            with ExitStack() as st:
                gbc = P.sb(st, [128, D], F32, 'gbc')
                hb = [P.sb(st, [128, D], BF16, 'hb') for _ in range(2)]
                hf2 = [P.sb(st, [128, D], F32, 'hf') for _ in range(2)]
                alloc_norm_tmps(P, k, st, ['n0', 'n1'])
                hTf = P.sb(st, [128, 8, 128], F32, 'hTf')
                wr = P.sb(st, [128, 8, 8], F32, 'wr')
                ltri = P.sb(st, [128, 128], F32, 'ltri')
                ones = P.sb(st, [128, 128], F32, 'ones')
                cnt = P.sb(st, [128, NTQ * 8], F32, 'cnt')
                mx = P.sb(st, [128, 1], F32, 'mx')
                ptf = [P.ps(st, [128, 4, 128], F32, 'ptf') for _ in range(2)]
                plg = P.ps(st, [128, 8], F32, 'plg')
                ppos = P.ps(st, [128, NTQ * 8], F32, 'ppos')
                pcnt = P.ps(st, [128, NTQ * 8], F32, 'pcnt')
                lg = P.sb(st, [128, 8], F32, 'lg')
                lg2 = P.sb(st, [128, 8], F32, 'lg2')
                eq1 = P.sb(st, [128, 8], F32, 'eq1')
                eq2 = P.sb(st, [128, 8], F32, 'eq2')
                m1 = P.sb(st, [128, 1], F32, 'm1')
                m2 = P.sb(st, [128, 1], F32, 'm2')
                w1 = P.sb(st, [128, 1], F32, 'w1')
                w2 = P.sb(st, [128, 1], F32, 'w2')
                P.dma('sp', gbc[:], gain_row.partition_broadcast(128), writes=['gbc'])
                P.dma('sp', wr[:], wview(router_ap), writes=['wr'])
                P.dma('sp', ltri[:], k.inp['c_ltri'], writes=['ltri'])
                P.dma('sp', iota[:], k.inp['c_iota'], writes=['iota'])
                P.memset('pool', ones[:], 1.0, writes=['ones'])
                for tl in range(NTQ):
                    tt = qt * NTQ + tl
                    b = tl % 2
                    tag = 'n%d' % b
                    rows = slice(tt * 128, (tt + 1) * 128)
                    hf = hf2[b]
                    P.dma('sp', xacc[:, tl, :], x_src[rows, :], writes=[tag + 'x', ('xacc', tl)])
                    norm_rows(P, k, st, xacc[:, tl, :], gbc[:], hb[b][:], tag, want_f32=hf[:])
                    P.dma('sp', hbk[rows, :], hb[b][:], reads=[tag + 'hb'], writes=[('hbk', tt)])
                    for q4 in range(2):
                        P.trg([(ptf[q4][:, c, :], hf[:, (q4 * 4 + c) * 128:(q4 * 4 + c + 1) * 128], k.identf[:]) for c in range(4)],
                              reads=[tag + 'hf'], writes=[('ptf', q4)])
                        P.copy('dve', hTf[:, q4 * 4:(q4 + 1) * 4, :], ptf[q4][:], reads=[('ptf', q4)], writes=[('hTf', q4)])
                    P.mmg([(plg[:], hTf[:, dc, :], wr[:, dc, :], dc == 0, dc == 7) for dc in range(8)],
                          reads=[('hTf', 0), ('hTf', 1), 'wr'], writes=['plg'])
                    P.copy('dve', lg[:], plg[:], reads=['plg'], writes=['lg'])
                    P.reduce(m1[:], lg[:], ALU.max, reads=['lg'], writes=['m1'])
                    P.ts('dve', eq1[:], lg[:], m1[:, 0:1], None, ALU.is_equal, None, reads=['lg', 'm1'], writes=['eq1'])
                    P.stt(lg2[:], eq1[:], -1e30, lg[:], ALU.mult, ALU.add, reads=['eq1', 'lg'], writes=['lg2'])
                    P.reduce(m2[:], lg2[:], ALU.max, reads=['lg2'], writes=['m2'])
                    P.ts('dve', eq2[:], lg2[:], m2[:, 0:1], None, ALU.is_equal, None, reads=['lg2', 'm2'], writes=['eq2'])
                    P.tt('dve', w2[:], m2[:], m1[:], ALU.subtract, reads=['m1', 'm2'], writes=['w2'])
                    P.act(w2[:], w2[:], AF.Exp, reads=['w2'], writes=['w2'])
                    P.ts('dve', w1[:], w2[:], 1.0, None, ALU.add, None, reads=['w2'], writes=['w1'])
                    P.recip(w1[:], w1[:], reads=['w1'], writes=['w1'])
                    P.tt('dve', w2[:], w2[:], w1[:], ALU.mult, reads=['w1', 'w2'], writes=['w2'])
                    P.tt('dve', maskf[:, tl * 8:(tl + 1) * 8], eq1[:], eq2[:], ALU.add, reads=['eq1', 'eq2'], writes=[('mask', tl)])
                    P.ts('dve', eq1[:], eq1[:], w1[:, 0:1], None, ALU.mult, None, reads=['eq1', 'w1'], writes=['eq1'])
                    P.stt(comb[:, tl * 8:(tl + 1) * 8], eq2[:], w2[:, 0:1], eq1[:], ALU.mult, ALU.add, reads=['eq2', 'w2', 'eq1'], writes=[('comb', tl)])
                allm = [('mask', tl) for tl in range(NTQ)]
                P.mmg([(ppos[:], ltri[:], maskf[:], True, True)], reads=allm + ['ltri'], writes=['ppos'])
                P.mmg([(pcnt[:], ones[:], maskf[:], True, True)], reads=allm + ['ones'], writes=['pcnt'])
                P.copy('dve', pos[:], ppos[:], reads=['ppos'], writes=['pos'])
                P.copy('dve', cnt[:], pcnt[:], reads=['pcnt'], writes=['cnt'])
                P.reduce(mx[:], cnt[:], ALU.max, reads=['cnt'], writes=['mx'])
                P.ts('dve', flag[:], mx[:], thr, None, ALU.is_gt, None, reads=['mx'], writes=['flag'])
                P.flush()
            with ExitStack() as st:
                wg = [P.sb(st, [128, 8, 512], BF16, 'wg') for _ in range(2)]
                wu = [P.sb(st, [128, 8, 512], BF16, 'wu') for _ in range(2)]
                wd = [P.sb(st, [128, 4, D], BF16, 'wd') for _ in range(2)]
                sg = [P.sb(st, [128, 512], F32, 'sg') for _ in range(2)]
                aT = [P.sb(st, [128, 4, 512], BF16, 'aT') for _ in range(2)]
                hbt = [P.sb(st, [128, D], BF16, 'hbt') for _ in range(2)]
                sel = P.sb(st, [128, NTQ, 128], BF16, 'sel')
                selT = P.sb(st, [128, NTQ, 128], BF16, 'selT')
                hTg = P.sb(st, [128, 8, NTQ * 128], BF16, 'hTg')
                yacc = P.sb(st, [128, NTQ, D], F32, 'yacc')
                ybf = [P.sb(st, [128, D], BF16, 'ybf') for _ in range(2)]
                pg = [P.ps(st, [128, 512], F32, 'pg') for _ in range(2)]
                pu = [P.ps(st, [128, 512], F32, 'pu') for _ in range(2)]
                po = [P.ps(st, [128, 512], F32, 'po') for _ in range(3)]
                pts = P.ps(st, [128, NTQ, 128], BF16, 'pts')

                def body(cap):
                    NS = NTQ * cap
                    nsb = NS // 512
                    nst = NS // 128
                    ctr = {'it': 0, 'gi': 0, 'oi': 0, 'ai': 0, 'hb': 0, 'yb': 0}
                    for e in range(NEXP):
                        gu = wview(w_gu_list[e])
                        w_dn_ap = w_dn_list[e]
                        for tl in range(NTQ):
                            col = tl * 8 + e
                            P.ts('dve', sel[:, tl, 0:cap], iota[:, 0:cap], pos[:, col:col + 1], maskf[:, col:col + 1], ALU.is_equal, ALU.mult,
                                 reads=[], writes=[('sel', tl)])
                        P.trg([(pts[(tl * cap) % 128:(tl * cap) % 128 + cap, tl, :], sel[:, tl, 0:cap], k.ident[:]) for tl in range(NTQ)],
                              reads=[('sel', tl) for tl in range(NTQ)], writes=['pts'])
                        P.copy('act', selT[:, :, :], pts[:, :, :], reads=['pts'], writes=['selT'])
                        for tl in range(NTQ):
                            tt = qt * NTQ + tl
                            hbi = ctr['hb'] % 2
                            ctr['hb'] += 1
                            P.dma('sp', hbt[hbi][:], hbk[tt * 128:(tt + 1) * 128, :], writes=[('hbt', hbi)])
                            ndc = 512 // cap
                            for g0 in range(0, 8, ndc):
                                o = ctr['oi'] % 3
                                ctr['oi'] += 1
                                pv = po[o][:].rearrange("p (c s) -> p c s", s=cap)
                                P.mmg([(pv[:, dc - g0, :], hbt[hbi][:, dc * 128:(dc + 1) * 128], sel[:, tl, 0:cap], True, True) for dc in range(g0, g0 + ndc)],
                                      reads=[('hbt', hbi), ('sel', tl)], writes=[('po', o)])
                                P.copy('act' if (tl % 2) else 'dve', hTg[:, g0:g0 + ndc, tl * cap:(tl + 1) * cap], pv, reads=[('po', o)], writes=[('hTg', tl, g0)])
                        allg = [('hTg', tl, g0) for tl in range(NTQ) for g0 in range(0, 8, 512 // cap)]
                        for fg in range(ngrp):
                            wb = ctr['it'] % 2
                            ctr['it'] += 1
                            P.dma('pool', wg[wb][:], gu[:, :, fg * 512:(fg + 1) * 512], writes=[('wg', wb)], cast=True)
                            P.dma('pool', wu[wb][:], gu[:, :, dff + fg * 512:dff + (fg + 1) * 512], writes=[('wu', wb)], cast=True)
                            P.dma('pool', wd[wb][:], wview(w_dn_ap[fg * 512:(fg + 1) * 512, :]), writes=[('wd', wb)], cast=True)
                            for sb_ in range(nsb):
                                blk = slice(sb_ * 512, (sb_ + 1) * 512)
                                ab = ctr['ai'] % 2
                                ctr['ai'] += 1
                                for fc in range(4):
                                    g = ctr['gi'] % 2
                                    ctr['gi'] += 1
                                    P.mmg([(pg[g][:], wg[wb][:, dc, fc * 128:(fc + 1) * 128], hTg[:, dc, blk], dc == 0, dc == 7) for dc in range(8)],
                                          reads=[('wg', wb)] + allg, writes=[('pg', g)])
                                    P.mmg([(pu[g][:], wu[wb][:, dc, fc * 128:(fc + 1) * 128], hTg[:, dc, blk], dc == 0, dc == 7) for dc in range(8)],
                                          reads=[('wu', wb)] + allg, writes=[('pu', g)])
                                    P.act(sg[g][:], pg[g][:], AF.Silu, reads=[('pg', g)], writes=[('sg', g)])
                                    P.tt('dve', aT[ab][:, fc, :], pu[g][:], sg[g][:], ALU.mult, reads=[('pu', g), ('sg', g)], writes=[('aT', ab, fc)])
                                for tq in range(4):
                                    s_t = sb_ * 4 + tq
                                    for hh in range(2):
                                        o = ctr['oi'] % 3
                                        ctr['oi'] += 1
                                        P.mmg([(po[o][:], aT[ab][:, fc, tq * 128:(tq + 1) * 128], wd[wb][:, fc, hh * 512:(hh + 1) * 512], fc == 0, fc == 3) for fc in range(4)],
                                              reads=[('aT', ab, fc) for fc in range(4)] + [('wd', wb)], writes=[('po', o)])
                                        ydst = yacc[:, s_t, hh * 512:(hh + 1) * 512]
                                        if fg == 0:
                                            P.copy('dve', ydst, po[o][:], reads=[('po', o)], writes=[('yacc', s_t, hh)])
                                        else:
                                            P.tt('dve', ydst, po[o][:], ydst, ALU.add, reads=[('po', o), ('yacc', s_t, hh)], writes=[('yacc', s_t, hh)])
                        for s_t in range(nst):
                            yb = ctr['yb'] % 2
                            ctr['yb'] += 1
                            P.copy('act', ybf[yb][:], yacc[:, s_t, :], reads=[('yacc', s_t, 0), ('yacc', s_t, 1)], writes=[('ybf', yb)])
                            for tl in range(s_t * 128 // cap, (s_t + 1) * 128 // cap):
                                p0 = (tl * cap) % 128
                                col = tl * 8 + e
                                for hh in range(2):
                                    o = ctr['oi'] % 3
                                    ctr['oi'] += 1
                                    P.mmg([(po[o][:], selT[p0:p0 + cap, tl, :], ybf[yb][p0:p0 + cap, hh * 512:(hh + 1) * 512], True, True)],
                                          reads=['selT', ('ybf', yb)], writes=[('po', o)])
                                    P.stt(xacc[:, tl, hh * 512:(hh + 1) * 512], po[o][:], comb[:, col:col + 1], xacc[:, tl, hh * 512:(hh + 1) * 512], ALU.mult, ALU.add,
                                          reads=[('po', o), ('xacc', tl, hh)], writes=[('xacc', tl, hh)])
                    for tl in range(NTQ):
                        tt = qt * NTQ + tl
                        P.dma('sp', x_dst[tt * 128:(tt + 1) * 128, :], xacc[:, tl, :], reads=[('xacc', tl, 0), ('xacc', tl, 1)], writes=[('xd', tt)])

                P.flush_branch(flag[0:1, 0:1], lambda: body(64), lambda: body(128))


def phase_fnorm(P, k, x_src, gain_row, out_ap):
    with ExitStack() as st:
        gbc = P.sb(st, [128, D], F32, 'gbc')
        xt = [P.sb(st, [128, D], F32, 'xt') for _ in range(2)]
        yo = [P.sb(st, [128, D], F32, 'yo') for _ in range(2)]
        alloc_norm_tmps(P, k, st, ['n0', 'n1'])
        P.dma('sp', gbc[:], gain_row.partition_broadcast(128), writes=['gbc'])
        for tt in range(NT):
            b = tt % 2
            tag = 'n%d' % b
            t = k.nt[tag]
            P.dma('sp', xt[b][:], x_src[tt * 128:(tt + 1) * 128, :], writes=[tag + 'x'])
            P.act(t['junk'][:], xt[b][:], AF.Square, reads=[tag + 'x'], writes=[tag + 'junk', tag + 'ssq'], accum=t['ssq'][:])
            P.ts('dve', t['ms'][:], t['ssq'][:], 1.0 / D, EPS, ALU.mult, ALU.add, reads=[tag + 'ssq'], writes=[tag + 'ms'])
            P.tt('pool', t['rstd'][:], t['ms'][:], k.neghalf[:], ALU.pow, reads=[tag + 'ms'], writes=[tag + 'rstd'])
            P.stt(yo[b][:], xt[b][:], t['rstd'][:, 0:1], gbc[:], ALU.mult, ALU.mult, reads=[tag + 'x', tag + 'rstd', 'gbc'], writes=[('yo', b)])
            P.dma('sp', out_ap[tt * 128:(tt + 1) * 128, :], yo[b][:], reads=[('yo', b)], writes=[('out', tt)])
        P.flush()


CONST_SHAPES = {"c_ident": [128, 128], "c_kaug": [6, 6, S], "c_qaug": [6, 6, S], "c_dmask": [6, 128, 128], "c_bias": [128, 6 * 35],
                "c_cmask": [128, 128], "c_smask": [128, 512], "c_ltri": [128, 128], "c_iota": [128, 128]}
STEP_INPUTS = {
    'attn': {"x": [S, D], "mem": [MEM_LEN, D], "a_norm_mix": [1, D], "a_w_in": [1, D, 2560], "a_lam_q1": [1, 64], "a_lam_k1": [1, 64],
             "a_lam_q2": [1, 64], "a_lam_k2": [1, 64], "a_subln": [1, 128], "a_mem_norm": [1, D], "a_w_mem_kv": [1, D, 512],
             "a_w_out": [1, D, D], "c_lam": [1, 128, 2], "c_ident": 0, "c_kaug": 0, "c_qaug": 0, "c_dmask": 0, "c_bias": 0},
    'hgrn': {"x": [S, D], "mem": [MEM_LEN, D], "b_norm_mix": [1, D], "b_w_in": [1, D, 3328], "b_lb_logits": [2, 768], "b_out_norm": [1, 128],
             "b_mem_norm": [1, D], "b_w_mem_kv": [1, D, 512], "b_w_out": [1, D, D], "c_lbflag": [1, 128, 1], "c_ident": 0, "c_cmask": 0, "c_smask": 0},
    'dense': {"x": [S, D], "norm": [1, D], "w_gu": [D, 2 * DFF_D], "w_dn": [DFF_D, D], "c_ident": 0},
    'moe1': {"x": [S, D], "xacc": [S, D], "norm": [1, D], "router": [D, 8], "w_gu": [D, 2 * DFF_E], "w_dn": [DFF_E, D], "esel": [128, 8], "c_ident": 0},
    'fnorm': {"x": [S, D], "gain": [1, D], "c_ident": 0},
    'moes': {"x": [S, D], "norm": [1, D], "router": [D, 8], "w_gu": [8, D, 2 * DFF_E], "w_dn": [8, DFF_E, D], "c_ident": 0, "c_ltri": 0, "c_iota": 0},
}


def build_step(kind):
    nc = bass.Bass("TRN2", target_bir_lowering=False)
    inp = {}
    for n, shp in STEP_INPUTS[kind].items():
        if shp == 0:
            shp = CONST_SHAPES[n]
        inp[n] = nc.dram_tensor(n, shp, F32, kind="ExternalInput").ap()
    out = nc.dram_tensor("out", [S, D], F32, kind="ExternalOutput").ap()
    o_s = nc.dram_tensor("o_s", [S, D], BF16, kind="Internal").ap()
    with ExitStack() as st:
        P = Prog(nc, st)
        k = K()
        k.inp = inp
        k.ident = P.sb(st, [128, 128], BF16, 'ident')
        k.identf = P.sb(st, [128, 128], F32, 'identf')
        k.neghalf = P.sb(st, [128, 1], F32, 'neghalf')
        P.dma('pool', k.ident[:], inp['c_ident'], writes=['ident'], cast=True)
        P.dma('sp', k.identf[:], inp['c_ident'], writes=['identf'])
        P.memset('pool', k.neghalf[:], -0.5, writes=['neghalf'])
        P.flush()
        if kind in ('attn', 'hgrn'):
            pre = 'a_' if kind == 'attn' else 'b_'
            with ExitStack() as stm:
                hT = P.sb(stm, [128, 8, S], BF16, 'hT')
                kmT = P.sb(stm, [128, 2, MEM_LEN], BF16, 'kmT')
                vm = P.sb(stm, [128, 2, 4, 65], BF16, 'vm')
                phase_hT(P, k, inp['x'], inp[pre + 'norm_mix'][0:1, :], hT)
                phase_memkv(P, k, inp['mem'], inp[pre + 'mem_norm'][0:1, :], inp[pre + 'w_mem_kv'][0], kmT, vm)
                if kind == 'attn':
                    phase_diffattn(P, k, hT, 0, None, o_s)
                    phase_memattn(P, k, hT, inp['a_w_in'][0], 3 * DA_W, kmT, vm, o_s)
                else:
                    phase_hgrn(P, k, hT, 0, o_s)
                    phase_memattn(P, k, hT, inp['b_w_in'][0], 4 * HG_W, kmT, vm, o_s)
            phase_outproj(P, k, o_s, inp[pre + 'w_out'][0], inp['x'], out)
        elif kind == 'dense':
            phase_ffn(P, k, inp['x'], inp['x'], out, inp['norm'][0:1, :], [inp['w_gu']], [inp['w_dn']], DFF_D)
        elif kind == 'moe1':
            phase_ffn(P, k, inp['x'], inp['xacc'], out, inp['norm'][0:1, :], [inp['w_gu']], [inp['w_dn']], DFF_E,
                      router_ap=inp['router'], esel_ap=inp['esel'])
        elif kind == 'fnorm':
            phase_fnorm(P, k, inp['x'], inp['gain'][0:1, :], out)
        elif kind == 'moes':
            hbk = nc.dram_tensor("hbk", [S, D], BF16, kind="Internal").ap()
            phase_moe_sparse(P, k, inp['x'], out, inp['norm'][0:1, :], [inp['w_gu'][e] for e in range(NEXP)], [inp['w_dn'][e] for e in range(NEXP)], inp['router'], hbk)
        P.add('sp', lambda e: e.nop(), reads=[], writes=[])
        P.flush()
    return nc


def make_consts():
    slopes = 2.0 ** (-8.0 * np.arange(1, 7) / 6.0)
    import ml_dtypes
    bf = ml_dtypes.bfloat16

    def hi_lo(v):
        hi = np.float32(np.float32(v).astype(bf).astype(np.float32))
        lo = np.float32(np.float32(v - hi).astype(bf).astype(np.float32))
        return hi, lo
    c = {}
    c['c_ident'] = np.eye(128, dtype=np.float32)
    pos = np.arange(S)
    krel = (pos % 128).astype(np.float32)
    qrel = pos % 512
    qhi = (qrel & ~3).astype(np.float32)
    qlo = (qrel & 3).astype(np.float32)
    kaug = np.zeros((6, 6, S), np.float32)
    qaug = np.zeros((6, 6, S), np.float32)
    for h in range(6):
        hi, lo = hi_lo(slopes[h])
        kaug[h, 0] = krel
        kaug[h, 1] = krel
        kaug[h, 2] = hi
        kaug[h, 3] = lo
        kaug[h, 4] = hi
        kaug[h, 5] = lo
        qaug[h, 0] = hi
        qaug[h, 1] = lo
        qaug[h, 2] = -qhi
        qaug[h, 3] = -qhi
        qaug[h, 4] = -qlo
        qaug[h, 5] = -qlo
    c['c_kaug'] = kaug
    c['c_qaug'] = qaug
    kk = np.arange(128)[:, None]
    qq = np.arange(128)[None, :]
    dm = np.zeros((6, 128, 128), np.float32)
    for h in range(6):
        allowed = (kk // 64) <= (qq // 64)
        val = np.where(kk <= qq, 1.0, np.exp(-2.0 * slopes[h] * (kk - qq)))
        dm[h] = np.where(allowed, val, 0.0)
    c['c_dmask'] = dm
    cb = np.zeros((128, 6 * 35), np.float32)
    for h in range(6):
        for idx in range(35):
            d = idx - 3
            cb[:, h * 35 + idx] = -slopes[h] * 128.0 * d
    c['c_bias'] = cb
    s_ = np.arange(128)[:, None]
    t_ = np.arange(128)[None, :]
    c['c_cmask'] = ((s_ <= t_) & ((s_ // 64) == (t_ // 64))).astype(np.float32)
    sm = np.ones((128, 512), np.float32)
    sm[:, ::64] = 0.0
    c['c_smask'] = sm
    c['c_ltri'] = (s_ < t_).astype(np.float32)
    c['c_iota'] = np.broadcast_to(np.arange(128, dtype=np.float32)[None, :], (128, 128)).copy()
    return c


_CACHE = {}
_CONSTS = {}


def launch(kind, per_core, shared, n_cores):
    if kind not in _CACHE:
        _CACHE[kind] = build_step(kind)
    if not _CONSTS:
        _CONSTS.update(make_consts())
    nc = _CACHE[kind]
    sh = {}
    for n, shp in STEP_INPUTS[kind].items():
        if n in per_core:
            continue
        if shp == 0:
            sh[n] = _CONSTS[n]
        else:
            sh[n] = np.ascontiguousarray(np.asarray(shared[n], dtype=np.float32)).reshape(shp)
    in_maps = []
    for c in range(n_cores):
        m = dict(sh)
        for n, a in per_core.items():
            m[n] = np.ascontiguousarray(a[c])
        in_maps.append(m)
    import os
    tr = bool(os.environ.get('K_TRACE'))
    res = run_bass_kernel_spmd(nc, in_maps, core_ids=list(range(n_cores)), trace=tr)
    if tr:
        print('EXEC_NS', kind, res.exec_time_ns)
    return np.stack([np.asarray(r['out'], dtype=np.float32).reshape(S, D) for r in res.results], axis=0)


def run_step(i, which, x, inputs, n_cores):
    f = lambda n: np.asarray(inputs[n], dtype=np.float32)
    j = i // 2
    mem = f('mem')[:n_cores]
    if which == 'mix' and i % 2 == 0:
        lam_init = 0.8 - 0.6 * float(np.exp(-0.3 * i))
        clam = np.zeros((1, 128, 2), np.float32)
        clam[0, :, 0] = 1.0 - lam_init
        clam[0, :, 1] = -lam_init
        sh = {n: f(n)[j:j + 1] for n in ['a_norm_mix', 'a_w_in', 'a_lam_q1', 'a_lam_k1', 'a_lam_q2', 'a_lam_k2', 'a_subln', 'a_mem_norm', 'a_w_mem_kv', 'a_w_out']}
        sh['c_lam'] = clam
        return launch('attn', {'x': x, 'mem': mem}, sh, n_cores)
    if which == 'mix':
        sh = {n: f(n)[j:j + 1] for n in ['b_norm_mix', 'b_w_in', 'b_out_norm', 'b_mem_norm', 'b_w_mem_kv', 'b_w_out']}
        sh['b_lb_logits'] = f('b_lb_logits')
        sh['c_lbflag'] = np.full((1, 128, 1), -float(j), np.float32)
        return launch('hgrn', {'x': x, 'mem': mem}, sh, n_cores)
    if i % 2 == 0:
        sh = {'norm': f('dense_norm')[j:j + 1], 'w_gu': f('dense_w_gate_up')[j], 'w_dn': f('dense_w_down')[j]}
        return launch('dense', {'x': x}, sh, n_cores)
    xacc = x
    for e in range(NEXP):
        esel = np.zeros((128, 8), np.float32)
        esel[:, e] = 1.0
        sh = {'norm': f('moe_norm')[j:j + 1], 'router': f('moe_router')[j], 'w_gu': f('moe_w_gate_up')[j, e], 'w_dn': f('moe_w_down')[j, e], 'esel': esel}
        xacc = launch('moe1', {'x': x, 'xacc': xacc}, sh, n_cores)
    return xacc


FULL_SHAPES = {
    "x": [S, D], "mem": [MEM_LEN, D],
    "a_norm_mix": [2, D], "a_w_in": [2, D, 2560], "a_lam_q1": [2, 64], "a_lam_k1": [2, 64], "a_lam_q2": [2, 64], "a_lam_k2": [2, 64],
    "a_subln": [2, 128], "a_mem_norm": [2, D], "a_w_mem_kv": [2, D, 512], "a_w_out": [2, D, D],
    "b_norm_mix": [2, D], "b_w_in": [2, D, 3328], "b_lb_logits": [2, 768], "b_out_norm": [2, 128], "b_mem_norm": [2, D],
    "b_w_mem_kv": [2, D, 512], "b_w_out": [2, D, D],
    "dense_norm": [2, D], "dense_w_gate_up": [2, D, 2 * DFF_D], "dense_w_down": [2, DFF_D, D],
    "moe_norm": [2, D], "moe_router": [2, D, 8], "moe_w_gate_up": [2, 8, D, 2 * DFF_E], "moe_w_down": [2, 8, DFF_E, D],
    "final_norm": [1, D], "c_lam": [2, 128, 2], "c_lbflag": [2, 128, 1],
}
FULL_SHAPES.update(CONST_SHAPES)


def build_fused():
    nc = bass.Bass("TRN2", target_bir_lowering=False)
    inp = {n: nc.dram_tensor(n, shp, F32, kind="ExternalInput").ap() for n, shp in FULL_SHAPES.items()}
    out = nc.dram_tensor("out", [S, D], F32, kind="ExternalOutput").ap()
    xres = nc.dram_tensor("xres", [S, D], F32, kind="Internal").ap()
    o_s = nc.dram_tensor("o_s", [S, D], BF16, kind="Internal").ap()
    hbk = nc.dram_tensor("hbk", [S, D], BF16, kind="Internal").ap()
    with ExitStack() as st:
        P = Prog(nc, st)
        k = K()
        k.inp = inp
        k.ident = P.sb(st, [128, 128], BF16, 'ident')
        k.identf = P.sb(st, [128, 128], F32, 'identf')
        k.neghalf = P.sb(st, [128, 1], F32, 'neghalf')
        P.dma('pool', k.ident[:], inp['c_ident'], writes=['ident'], cast=True)
        P.dma('sp', k.identf[:], inp['c_ident'], writes=['identf'])
        P.memset('pool', k.neghalf[:], -0.5, writes=['neghalf'])
        P.flush()
        x_cur = inp['x']
        for i in range(DEPTH):
            j = i // 2
            pre = 'a_' if i % 2 == 0 else 'b_'
            with ExitStack() as stm:
                hT = P.sb(stm, [128, 8, S], BF16, 'hT')
                kmT = P.sb(stm, [128, 2, MEM_LEN], BF16, 'kmT')
                vm = P.sb(stm, [128, 2, 4, 65], BF16, 'vm')
                phase_hT(P, k, x_cur, inp[pre + 'norm_mix'][j:j + 1, :], hT)
                phase_memkv(P, k, inp['mem'], inp[pre + 'mem_norm'][j:j + 1, :], inp[pre + 'w_mem_kv'][j], kmT, vm)
                if i % 2 == 0:
                    phase_diffattn(P, k, hT, j, None, o_s)
                    phase_memattn(P, k, hT, inp['a_w_in'][j], 3 * DA_W, kmT, vm, o_s)
                else:
                    phase_hgrn(P, k, hT, j, o_s)
                    phase_memattn(P, k, hT, inp['b_w_in'][j], 4 * HG_W, kmT, vm, o_s)
            phase_outproj(P, k, o_s, inp[pre + 'w_out'][j], x_cur, xres)
            x_cur = xres
            if i % 2 == 0:
                phase_ffn(P, k, xres, xres, xres, inp['dense_norm'][j:j + 1, :], [inp['dense_w_gate_up'][j]], [inp['dense_w_down'][j]], DFF_D)
            else:
                phase_moe_sparse(P, k, xres, xres, inp['moe_norm'][j:j + 1, :],
                                 [inp['moe_w_gate_up'][j, e] for e in range(NEXP)], [inp['moe_w_down'][j, e] for e in range(NEXP)],
                                 inp['moe_router'][j], hbk)
        phase_fnorm(P, k, xres, inp['final_norm'][0:1, :], out)
        P.add('sp', lambda e: e.nop(), reads=[], writes=[])
        P.flush()
    return nc


def fused_consts():
    c = dict(make_consts())
    clam = np.zeros((2, 128, 2), np.float32)
    for j in range(2):
        lam_init = 0.8 - 0.6 * float(np.exp(-0.3 * (2 * j)))
        clam[j, :, 0] = 1.0 - lam_init
        clam[j, :, 1] = -lam_init
    c['c_lam'] = clam
    lbf = np.zeros((2, 128, 1), np.float32)
    lbf[1] = -1.0
    c['c_lbflag'] = lbf
    return c


def kernel_unfused(**inputs):
    n_cores = 8
    x = np.asarray(inputs['x'], dtype=np.float32)
    for i in range(DEPTH):
        x = run_step(i, 'mix', x, inputs, n_cores)
        x = run_step(i, 'ffn', x, inputs, n_cores)
    return launch('fnorm', {'x': x}, {'gain': np.asarray(inputs['final_norm'], dtype=np.float32).reshape(1, D)}, n_cores)


def kernel(**inputs):
    n_cores = 8
    if 'fused' not in _CACHE:
        _CACHE['fused'] = build_fused()
    nc = _CACHE['fused']
    consts = fused_consts()
    shared = {}
    for n, shp in FULL_SHAPES.items():
        if n in ('x', 'mem'):
            continue
        if n in consts:
            shared[n] = consts[n]
        else:
            shared[n] = np.ascontiguousarray(np.asarray(inputs[n], dtype=np.float32)).reshape(shp)
    x = np.asarray(inputs['x'], dtype=np.float32)
    mem = np.asarray(inputs['mem'], dtype=np.float32)
    in_maps = []
    for c in range(n_cores):
        m = dict(shared)
        m['x'] = np.ascontiguousarray(x[c])
        m['mem'] = np.ascontiguousarray(mem[c])
        in_maps.append(m)
    res = run_bass_kernel_spmd(nc, in_maps, core_ids=list(range(n_cores)))
    return np.stack([np.asarray(r['out'], dtype=np.float32).reshape(S, D) for r in res.results], axis=0)
```

```python
import numpy as np
import concourse.bass as bass
import concourse.mybir as mybir
from concourse.bass_utils import run_bass_kernel_spmd
from contextlib import ExitStack

F32 = mybir.dt.float32
BF16 = mybir.dt.bfloat16
ALU = mybir.AluOpType
AF = mybir.ActivationFunctionType
AX = mybir.AxisListType

S = 4096
D = 1024
NT = 32
EPS = 1e-6
DEPTH = 4
DA_W = 768
HG_W = 768
MEM_W = 256
DFF_D = 2816
DFF_E = 3584
NEXP = 8
MEM_LEN = 256

ENGS = ['pe', 'act', 'dve', 'pool', 'sp']
DMA_POOL = {'sp': 24, 'pool': 24, 'act': 4}


class Op:
    __slots__ = ('eng', 'fn', 'deps', 'need', 'sig', 'is_dma', 'dsem', 'dval', 'uid')


class Prog:
    def __init__(self, nc, stack, strict=True):
        self.nc = nc
        self.stack = stack
        self.strict = strict
        self.ops = {e: [] for e in ENGS}
        self.lastw = {}
        self.reads = {}
        self.uid = 0
        self.esem = {e: stack.enter_context(nc.semaphore('s_' + e)) for e in ENGS}
        self.sigc = {e: 0 for e in ENGS}
        self.dsems = {}
        self.dcur = {}
        self.dlast = {}
        self.dfence = {}
        for q, n in DMA_POOL.items():
            self.dsems[q] = [stack.enter_context(nc.semaphore('d_%s%d' % (q, i))) for i in range(n)]
            self.dcur[q] = 0
            self.dlast[q] = [None] * n
            self.dfence[q] = [0] * n
        self.efence = {e: 0 for e in ENGS}
        self.ntile = 0
        self.nflush = 0

    def sb(self, st, shape, dtype, name=None):
        self.ntile += 1
        name = (name or 't') + '_%d' % self.ntile
        return st.enter_context(self.nc.sbuf_tensor(name, list(shape), dtype))

    def ps(self, st, shape, dtype=F32, name=None):
        self.ntile += 1
        name = (name or 'p') + '_%d' % self.ntile
        return st.enter_context(self.nc.psum_tensor(name, list(shape), dtype))

    def add(self, eng, fn, reads=(), writes=(), dma=False):
        op = Op()
        op.eng = eng
        op.fn = fn
        op.need = False
        op.sig = None
        op.is_dma = dma
        op.uid = self.uid
        self.uid += 1
        deps = []
        for k in reads:
            w = self.lastw.get(k)
            if w is not None:
                deps.append((w, 'raw'))
        for k in writes:
            w = self.lastw.get(k)
            if w is not None:
                deps.append((w, 'waw'))
            rd = self.reads.get(k)
            if rd:
                for r in rd.values():
                    deps.append((r, 'war'))
        fdeps = []
        seen = set()
        for d, kind in deps:
            if d.uid in seen:
                continue
            if (not d.is_dma) and (not dma) and d.eng == eng:
                if eng == 'pe' or not self.strict:
                    continue
            seen.add(d.uid)
            fdeps.append(d)
        if dma:
            q = eng
            i = self.dcur[q]
            self.dcur[q] = (i + 1) % len(self.dsems[q])
            prev = self.dlast[q][i]
            op.dsem = self.dsems[q][i]
            op.dval = (prev.dval if prev is not None else 0) + 16
            if prev is not None and prev.uid not in seen and prev.dval > self.dfence[q][i]:
                fdeps.append(prev)
                seen.add(prev.uid)
            self.dlast[q][i] = op
        for d in fdeps:
            d.need = True
        op.deps = fdeps
        rk = ('dma', op.uid) if dma else eng
        for k in writes:
            self.lastw[k] = op
            self.reads[k] = {}
        for k in reads:
            self.reads.setdefault(k, {})[rk] = op
        self.ops[eng].append(op)
        return op

    def flush(self):
        for e in ENGS:
            comp = [op for op in self.ops[e] if not op.is_dma and op.fn is not None]
            if comp:
                comp[-1].need = True
            c = self.sigc[e]
            for op in self.ops[e]:
                if op.need and not op.is_dma:
                    c += 1
                    op.sig = c
            self.sigc[e] = c
        efence = dict(self.efence)
        dfence = {q: list(v) for q, v in self.dfence.items()}

        def run(e, engobj):
            waited = {}
            for e2 in ENGS:
                if efence[e2] > 0:
                    engobj.wait_ge(self.esem[e2], efence[e2])
                    waited[id(self.esem[e2])] = efence[e2]
            for q in dfence:
                for i, v in enumerate(dfence[q]):
                    if v > 0:
                        engobj.wait_ge(self.dsems[q][i], v)
                        waited[id(self.dsems[q][i])] = v
            for op in self.ops[e]:
                for d in op.deps:
                    if d.is_dma:
                        sem, val = d.dsem, d.dval
                    else:
                        sem, val = self.esem[d.eng], d.sig
                    key = id(sem)
                    if waited.get(key, 0) < val:
                        engobj.wait_ge(sem, val)
                        waited[key] = val
                ins = op.fn(engobj)
                if op.is_dma:
                    ins.then_inc(op.dsem, 16)
                elif op.need:
                    ins.then_inc(self.esem[e], 1)

        with self.nc.Block() as block:
            @block.tensor
            def _(t):
                run('pe', t)

            @block.scalar
            def _(t):
                run('act', t)

            @block.vector
            def _(t):
                run('dve', t)

            @block.gpsimd
            def _(t):
                run('pool', t)

            @block.sync
            def _(t):
                run('sp', t)

        for e in ENGS:
            self.efence[e] = self.sigc[e]
            self.ops[e] = []
        for q in self.dlast:
            for i, op in enumerate(self.dlast[q]):
                if op is not None:
                    self.dfence[q][i] = op.dval
        self.lastw = {}
        self.reads = {}
        self.nflush += 1

    def flush_branch(self, flag_ap, recA, recB):
        assert all(len(self.ops[e]) == 0 for e in ENGS)
        st0 = (dict(self.sigc), {q: list(v) for q, v in self.dlast.items()}, dict(self.dcur))

        def record(rec):
            self.sigc = dict(st0[0])
            self.dlast = {q: list(v) for q, v in st0[1].items()}
            self.dcur = dict(st0[2])
            self.lastw = {}
            self.reads = {}
            rec()
            for e in ENGS:
                comp = [op for op in self.ops[e] if not op.is_dma]
                if comp:
                    comp[-1].need = True
                c = self.sigc[e]
                for op in self.ops[e]:
                    if op.need and not op.is_dma:
                        c += 1
                        op.sig = c
                self.sigc[e] = c
            ops = self.ops
            self.ops = {e: [] for e in ENGS}
            dv = {q: [(o.dval if o is not None else 0) for o in self.dlast[q]] for q in self.dlast}
            return ops, dict(self.sigc), dv
        opsA, sigA, dvA = record(recA)
        opsB, sigB, dvB = record(recB)
        fin = {e: max(sigA[e], sigB[e]) for e in ENGS}
        dfin = {q: [max(a, b) for a, b in zip(dvA[q], dvB[q])] for q in dvA}
        efence = dict(self.efence)
        dfence = {q: list(v) for q, v in self.dfence.items()}

        def run_ops(e, engobj, ops, waited):
            for op in ops[e]:
                for d in op.deps:
                    if d.is_dma:
                        sem, val = d.dsem, d.dval
                    else:
                        sem, val = self.esem[d.eng], d.sig
                    key = id(sem)
                    if waited.get(key, 0) < val:
                        engobj.wait_ge(sem, val)
                        waited[key] = val
                ins = op.fn(engobj)
                if op.is_dma:
                    ins.then_inc(op.dsem, 16)
                elif op.need:
                    ins.then_inc(self.esem[e], 1)

        def catchup(e, engobj, sig, dv):
            if sig[e] > 0:
                engobj.wait_ge(self.esem[e], sig[e])
            if fin[e] > sig[e]:
                engobj.sem_inc(self.esem[e], fin[e] - sig[e])
            if e in dv:
                for i, v in enumerate(dv[e]):
                    if dfin[e][i] > v:
                        if v > 0:
                            engobj.wait_ge(self.dsems[e][i], v)
                        engobj.sem_inc(self.dsems[e][i], dfin[e][i] - v)

        def run(e, engobj):
            waited = {}
            for e2 in ENGS:
                if efence[e2] > 0:
                    engobj.wait_ge(self.esem[e2], efence[e2])
                    waited[id(self.esem[e2])] = efence[e2]
            for q in dfence:
                for i, v in enumerate(dfence[q]):
                    if v > 0:
                        engobj.wait_ge(self.dsems[q][i], v)
                        waited[id(self.dsems[q][i])] = v
            reg = engobj.alloc_register('flag_' + e + '_%d' % self.nflush)
            engobj.reg_load(reg, flag_ap)
            with engobj.If_eq(reg, 0):
                run_ops(e, engobj, opsA, dict(waited))
                catchup(e, engobj, sigA, dvA)
            with engobj.Else():
                run_ops(e, engobj, opsB, dict(waited))
                catchup(e, engobj, sigB, dvB)

        with self.nc.Block() as block:
            @block.tensor
            def _(t):
                run('pe', t)

            @block.scalar
            def _(t):
                run('act', t)

            @block.vector
            def _(t):
                run('dve', t)

            @block.gpsimd
            def _(t):
                run('pool', t)

            @block.sync
            def _(t):
                run('sp', t)

        self.sigc = dict(fin)
        for e in ENGS:
            self.efence[e] = fin[e]
        for q in dfin:
            for i, v in enumerate(dfin[q]):
                if v > 0:
                    o = Op()
                    o.dval = v
                    o.dsem = self.dsems[q][i]
                    o.is_dma = True
                    o.uid = -1
                    self.dlast[q][i] = o
                else:
                    self.dlast[q][i] = None
                self.dfence[q][i] = v
            self.dcur[q] = 0
        self.lastw = {}
        self.reads = {}
        self.nflush += 1

    def dma(self, q, out, in_, reads=(), writes=(), cast=False, slow=False):
        if slow:
            fn = lambda e, o=out, i=in_: e.dma_start(out=o, in_=i, allow_slow_non_contiguous=True)
        elif cast:
            fn = lambda e, o=out, i=in_: e.dma_start(out=o, in_=i, max_dma_last_dim=4096)
        else:
            fn = lambda e, o=out, i=in_: e.dma_start(out=o, in_=i)
        return self.add(q, fn, reads, writes, dma=True)

    def mmg(self, mms, reads, writes):
        mms = list(mms)

        def fn(e, mms=mms):
            ins = None
            for (o, l, r, s0, s1) in mms:
                ins = e.matmul(o, l, r, start=s0, stop=s1)
            return ins
        return self.add('pe', fn, reads, writes)

    def trg(self, trs, reads, writes):
        trs = list(trs)

        def fn(e, trs=trs):
            ins = None
            for (o, i, idn) in trs:
                ins = e.transpose(o, i, idn)
            return ins
        return self.add('pe', fn, reads, writes)

    def act(self, out, in_, func, reads, writes, bias=None, scale=None, accum=None):
        kw = {}
        if bias is not None:
            kw['bias'] = bias
        if scale is not None:
            kw['scale'] = scale
        if accum is not None:
            kw['accum_out'] = accum
        return self.add('act', lambda e, o=out, i=in_, f=func, kw=kw: e.activation(out=o, in_=i, func=f, **kw), reads, writes)

    def tt(self, eng, out, in0, in1, op, reads, writes):
        return self.add(eng, lambda e, o=out, a=in0, b=in1, p=op: e.tensor_tensor(out=o, in0=a, in1=b, op=p), reads, writes)

    def ts(self, eng, out, in0, s1, s2, op0, op1, reads, writes):
        if op1 is None:
            return self.add(eng, lambda e, o=out, a=in0, s1=s1, p0=op0: e.tensor_scalar(out=o, in0=a, scalar1=s1, scalar2=None, op0=p0), reads, writes)
        return self.add(eng, lambda e, o=out, a=in0, s1=s1, s2=s2, p0=op0, p1=op1: e.tensor_scalar(out=o, in0=a, scalar1=s1, scalar2=s2, op0=p0, op1=p1), reads, writes)

    def stt(self, out, in0, scalar, in1, op0, op1, reads, writes):
        return self.add('dve', lambda e, o=out, a=in0, s=scalar, b=in1, p0=op0, p1=op1: e.scalar_tensor_tensor(out=o, in0=a, scalar=s, in1=b, op0=p0, op1=p1), reads, writes)

    def copy(self, eng, out, in_, reads, writes):
        if eng == 'act':
            return self.add('act', lambda e, o=out, i=in_: e.copy(out=o, in_=i), reads, writes)
        return self.add(eng, lambda e, o=out, i=in_: e.tensor_copy(out=o, in_=i), reads, writes)

    def memset(self, eng, ap, val, writes):
        return self.add(eng, lambda e, a=ap, v=val: e.memset(a, v), (), writes)

    def recip(self, out, in_, reads, writes):
        return self.add('dve', lambda e, o=out, i=in_: e.reciprocal(out=o, in_=i), reads, writes)

    def reduce(self, out, in_, op, reads, writes):
        return self.add('dve', lambda e, o=out, i=in_, p=op: e.tensor_reduce(out=o, in_=i, axis=AX.X, op=p), reads, writes)

    def scan(self, out, d0, d1, reads, writes):
        return self.add('dve', lambda e, o=out, a=d0, b=d1: e.tensor_tensor_scan(out=o, data0=a, data1=b, initial=0.0, op0=ALU.mult, op1=ALU.add), reads, writes)


class K:
    pass


def wview(ap2d):
    return ap2d.rearrange("(c p) n -> p c n", p=128)


def norm_rows(P, k, st, x_ap, gbc, hb_out, tag, want_f32=None):
    t = k.nt[tag]
    P.act(t['junk'][:], x_ap, AF.Square, reads=[tag + 'x'], writes=[tag + 'junk', tag + 'ssq'], accum=t['ssq'][:])
    P.ts('dve', t['ms'][:], t['ssq'][:], 1.0 / D, EPS, ALU.mult, ALU.add, reads=[tag + 'ssq'], writes=[tag + 'ms'])
    P.tt('pool', t['rstd'][:], t['ms'][:], k.neghalf[:], ALU.pow, reads=[tag + 'ms'], writes=[tag + 'rstd'])
    if want_f32 is not None:
        P.stt(want_f32, x_ap, t['rstd'][:, 0:1], gbc, ALU.mult, ALU.mult, reads=[tag + 'x', tag + 'rstd', 'gbc'], writes=[tag + 'hf'])
        P.copy('act', hb_out, want_f32, reads=[tag + 'hf'], writes=[tag + 'hb'])
    else:
        P.stt(hb_out, x_ap, t['rstd'][:, 0:1], gbc, ALU.mult, ALU.mult, reads=[tag + 'x', tag + 'rstd', 'gbc'], writes=[tag + 'hb'])


def alloc_norm_tmps(P, k, st, tags):
    k.nt = {}
    for tag in tags:
        k.nt[tag] = {
            'junk': P.sb(st, [128, D], F32, 'junk'),
            'ssq': P.sb(st, [128, 1], F32, 'ssq'),
            'ms': P.sb(st, [128, 1], F32, 'ms'),
            'rstd': P.sb(st, [128, 1], F32, 'rstd'),
        }


def phase_hT(P, k, x_src, gain_row, hT):
    with ExitStack() as st:
        gbc = P.sb(st, [128, D], F32, 'gbc')
        xt = [P.sb(st, [128, D], F32, 'xt') for _ in range(3)]
        hb = [P.sb(st, [128, D], BF16, 'hb') for _ in range(3)]
        pt = [P.ps(st, [128, 8, 128], BF16, 'pt') for _ in range(3)]
        alloc_norm_tmps(P, k, st, ['n0', 'n1', 'n2'])
        P.dma('sp', gbc[:], gain_row.partition_broadcast(128), writes=['gbc'])
        for tt in range(NT):
            b = tt % 3
            tag = 'n%d' % b
            P.dma('sp' if tt % 2 else 'pool', xt[b][:], x_src[tt * 128:(tt + 1) * 128, :], writes=[tag + 'x'])
            norm_rows(P, k, st, xt[b][:], gbc[:], hb[b][:], tag)
            P.trg([(pt[b][:, c, :], hb[b][:, c * 128:(c + 1) * 128], k.ident[:]) for c in range(8)],
                  reads=[tag + 'hb'], writes=[tag + 'pt'])
            P.copy('act' if tt % 2 else 'dve', hT[:, :, tt * 128:(tt + 1) * 128], pt[b][:], reads=[tag + 'pt'], writes=[('hT', tt)])
        P.flush()


def phase_memkv(P, k, mem_ap, gain_row, wkv_ap, kmT, vm):
    with ExitStack() as st:
        gbc = P.sb(st, [128, D], F32, 'gbc')
        xt = [P.sb(st, [128, D], F32, 'xt') for _ in range(2)]
        hb = [P.sb(st, [128, D], BF16, 'hb') for _ in range(2)]
        pt = [P.ps(st, [128, 8, 128], BF16, 'pt') for _ in range(2)]
        memT = P.sb(st, [128, 8, MEM_LEN], BF16, 'memT')
        wkv = P.sb(st, [128, 8, 512], BF16, 'wkv')
        pk = P.ps(st, [128, 256], F32, 'pk')
        alloc_norm_tmps(P, k, st, ['n0', 'n1'])
        P.dma('sp', gbc[:], gain_row.partition_broadcast(128), writes=['gbc'])
        P.dma('pool', wkv[:], wview(wkv_ap), writes=['wkv'], cast=True)
        P.memset('pool', vm[:], 1.0, writes=['vm'])
        for mt in range(2):
            tag = 'n%d' % mt
            P.dma('sp', xt[mt][:], mem_ap[mt * 128:(mt + 1) * 128, :], writes=[tag + 'x'])
            norm_rows(P, k, st, xt[mt][:], gbc[:], hb[mt][:], tag)
            P.trg([(pt[mt][:, c, :], hb[mt][:, c * 128:(c + 1) * 128], k.ident[:]) for c in range(8)],
                  reads=[tag + 'hb'], writes=[tag + 'pt'])
            P.copy('dve', memT[:, :, mt * 128:(mt + 1) * 128], pt[mt][:], reads=[tag + 'pt'], writes=[('memT', mt)])
        for p in range(2):
            P.mmg([(pk[:], wkv[:, dc, p * 128:(p + 1) * 128], memT[:, dc, :], dc == 0, dc == 7) for dc in range(8)],
                  reads=['wkv', ('memT', 0), ('memT', 1)], writes=['pk'])
            P.copy('dve', kmT[:, p, :], pk[:], reads=['pk'], writes=[('kmT', p)])
        for mt in range(2):
            P.mmg([(pk[:], memT[:, dc, mt * 128:(mt + 1) * 128], wkv[:, dc, 256:512], dc == 0, dc == 7) for dc in range(8)],
                  reads=['wkv', ('memT', 0), ('memT', 1)], writes=['pk'])
            P.copy('dve', vm[:, mt, :, 0:64], pk[:].rearrange("p (h d) -> p h d", h=4), reads=['pk', 'vm'], writes=[('vm', mt)])
        P.flush()


def phase_memattn(P, k, hT, w_in_ap, col0, kmT, vm, o_s):
    with ExitStack() as st:
        wqm = P.sb(st, [128, 8, 256], BF16, 'wqm')
        qmT = P.sb(st, [128, 2, S], BF16, 'qmT')
        pq = [P.ps(st, [128, 512], F32, 'pq') for _ in range(2)]
        sc = [P.ps(st, [128, 512], F32, 'sc') for _ in range(2)]
        pacc = [P.ps(st, [128, 512], F32, 'pacc') for _ in range(4)]
        pT = [P.sb(st, [128, 512], BF16, 'pT') for _ in range(2)]
        rr = [P.sb(st, [128, 1], F32, 'rr') for _ in range(4)]
        omb = [P.sb(st, [128, 4, 256], BF16, 'omb') for _ in range(2)]
        P.dma('pool', wqm[:], wview(w_in_ap)[:, :, col0:col0 + 256], writes=['wqm'], cast=True)
        for tb in range(8):
            for p in range(2):
                b = (tb * 2 + p) % 2
                P.mmg([(pq[b][:], wqm[:, dc, p * 128:(p + 1) * 128], hT[:, dc, tb * 512:(tb + 1) * 512], dc == 0, dc == 7) for dc in range(8)],
                      reads=['wqm'], writes=[('pq', b)])
                P.act(qmT[:, p, tb * 512:(tb + 1) * 512], pq[b][:], AF.Identity, reads=[('pq', b)], writes=[('qmT', tb, p)], scale=0.125)
        cnt = 0
        for tb in range(8):
            ob = omb[tb % 2]
            for hm in range(4):
                p = hm // 2
                base = (hm % 2) * 64
                for mt in range(2):
                    b = cnt % 2
                    cnt += 1
                    P.mmg([(sc[b][:], kmT[base:base + 64, p, mt * 128:(mt + 1) * 128], qmT[base:base + 64, p, tb * 512:(tb + 1) * 512], True, True)],
                          reads=[('qmT', tb, p)], writes=[('sc', b)])
                    P.act(pT[b][:], sc[b][:], AF.Exp, reads=[('sc', b)], writes=[('pT', b)])
                    P.mmg([(pacc[qs][:, 0:65], pT[b][:, qs * 128:(qs + 1) * 128], vm[:, mt, hm, :], mt == 0, mt == 1) for qs in range(4)],
                          reads=[('pT', b)], writes=[('pacc', qs) for qs in range(4)])
                for qs in range(4):
                    P.recip(rr[qs][:], pacc[qs][:, 64:65], reads=[('pacc', qs)], writes=[('rr', qs)])
                    P.act(ob[:, qs, hm * 64:(hm + 1) * 64], pacc[qs][:, 0:64], AF.Identity, reads=[('pacc', qs), ('rr', qs)],
                          writes=[('omb', tb % 2, qs)], scale=rr[qs][:, 0:1])
            for qs in range(4):
                tt = tb * 4 + qs
                P.dma('sp', o_s[tt * 128:(tt + 1) * 128, 768:1024], ob[:, qs, :], reads=[('omb', tb % 2, qs)], writes=[('o_s', tt, 'm')])
        P.flush()


def phase_outproj(P, k, o_s, w_out_ap, x_src, x_dst):
    NB = 3
    with ExitStack() as st:
        wo = P.sb(st, [128, 8, D], BF16, 'wo')
        ot = [P.sb(st, [128, D], BF16, 'ot') for _ in range(NB)]
        oT = [P.sb(st, [128, 8, 128], BF16, 'oT') for _ in range(NB)]
        xt = [P.sb(st, [128, D], F32, 'xt') for _ in range(NB)]
        xn = [P.sb(st, [128, D], F32, 'xn') for _ in range(NB)]
        pt = [P.ps(st, [128, 8, 128], BF16, 'pt') for _ in range(2)]
        po = [P.ps(st, [128, 512], F32, 'po') for _ in range(4)]
        P.dma('pool', wo[:], wview(w_out_ap), writes=['wo'], cast=True)

        def stage_a(tt):
            b = tt % NB
            pb2 = tt % 2
            P.dma('sp', ot[b][:], o_s[tt * 128:(tt + 1) * 128, :], writes=[('ot', b)])
            P.dma('pool', xt[b][:], x_src[tt * 128:(tt + 1) * 128, :], writes=[('xt', b)])
            P.trg([(pt[pb2][:, c, :], ot[b][:, c * 128:(c + 1) * 128], k.ident[:]) for c in range(8)],
                  reads=[('ot', b)], writes=[('pt', pb2)])
            P.copy('act', oT[b][:], pt[pb2][:], reads=[('pt', pb2)], writes=[('oT', b)])

        def stage_b(tt):
            b = tt % NB
            pb2 = tt % 2
            for hh in range(2):
                pb = pb2 * 2 + hh
                P.mmg([(po[pb][:], oT[b][:, fc, :], wo[:, fc, hh * 512:(hh + 1) * 512], fc == 0, fc == 7) for fc in range(8)],
                      reads=[('oT', b), 'wo'], writes=[('po', pb)])
                P.tt('dve', xn[b][:, hh * 512:(hh + 1) * 512], po[pb][:], xt[b][:, hh * 512:(hh + 1) * 512], ALU.add,
                     reads=[('po', pb), ('xt', b)], writes=[('xn', b, hh)])
            P.dma('sp', x_dst[tt * 128:(tt + 1) * 128, :], xn[b][:], reads=[('xn', b, 0), ('xn', b, 1)], writes=[('xd', tt)])

        stage_a(0)
        for tt in range(NT):
            if tt + 1 < NT:
                stage_a(tt + 1)
            stage_b(tt)
        P.flush()


def phase_diffattn(P, k, hT, j, lam_init, o_s):
    inp = k.inp
    w_in = wview(inp['a_w_in'][j])
    with ExitStack() as st:
        wqkv = [P.sb(st, [128, 8, 384], BF16, 'wqkv') for _ in range(2)]
        qT = [P.sb(st, [128, 2, S], BF16, 'qT') for _ in range(2)]
        kT = [P.sb(st, [128, 2, S], BF16, 'kT') for _ in range(2)]
        va = [P.sb(st, [128, NT, 129], BF16, 'va') for _ in range(2)]
        dmask = P.sb(st, [128, 6, 128], BF16, 'dmask')
        cbias = P.sb(st, [128, 6 * 35], F32, 'cbias')
        gsub = P.sb(st, [128, 128], F32, 'gsub')
        lv = [P.sb(st, [128, 64], F32, 'lv') for _ in range(4)]
        lt = P.sb(st, [128, 64], F32, 'lt')
        ls = [P.sb(st, [128, 1], F32, 'ls') for _ in range(2)]
        neglam = P.sb(st, [128, 1], F32, 'neglam')
        clam = P.sb(st, [128, 2], F32, 'clam')
        pT = [P.sb(st, [128, 512], BF16, 'pT') for _ in range(3)]
        o0 = P.sb(st, [128, 4, 128], F32, 'o0')
        oo = [P.sb(st, [128, 128], F32, 'oo') for _ in range(2)]
        ob = [P.sb(st, [128, 128], BF16, 'ob') for _ in range(2)]
        junk = P.sb(st, [128, 128], F32, 'junk')
        sm = {n: [P.sb(st, [128, 1], F32, n) for _ in range(2)] for n in ('r0', 'r1', 'ssq', 'ms', 'lnm', 'rstd')}
        pp = [P.ps(st, [128, 512], F32, 'pp') for _ in range(2)]
        sc = [P.ps(st, [128, 512], F32, 'sc') for _ in range(2)]
        pacc = [P.ps(st, [128, 512], F32, 'pacc') for _ in range(4)]

        P.dma('pool', dmask[:], inp['c_dmask'].rearrange("h k q -> k h q"), writes=['dmask'], cast=True)
        P.dma('sp', cbias[:], inp['c_bias'], writes=['cbias'])
        P.dma('sp', gsub[:], inp['a_subln'][j:j + 1, :].partition_broadcast(128), writes=['gsub'])
        P.dma('sp', clam[:], inp['c_lam'][j], writes=['clam'])
        P.ts('dve', gsub[:], gsub[:], clam[:, 0:1], None, ALU.mult, None, reads=['gsub', 'clam'], writes=['gsub'])
        for i, nm in enumerate(['a_lam_q1', 'a_lam_k1', 'a_lam_q2', 'a_lam_k2']):
            P.dma('sp', lv[i][:], inp[nm][j:j + 1, :].partition_broadcast(128), writes=[('lv', i)])
        for i in range(2):
            P.tt('dve', lt[:], lv[2 * i][:], lv[2 * i + 1][:], ALU.mult, reads=[('lv', 2 * i), ('lv', 2 * i + 1)], writes=['lt'])
            P.reduce(ls[i][:], lt[:], ALU.add, reads=['lt'], writes=[('ls', i)])
            P.act(ls[i][:], ls[i][:], AF.Exp, reads=[('ls', i)], writes=[('ls', i)])
        P.tt('dve', neglam[:], ls[1][:], ls[0][:], ALU.subtract, reads=[('ls', 0), ('ls', 1)], writes=['neglam'])
        P.ts('dve', neglam[:], neglam[:], clam[:, 1:2], None, ALU.add, None, reads=['neglam', 'clam'], writes=['neglam'])
        for b in range(2):
            P.memset('pool', va[b][:, :, 128:129], 1.0, writes=[('va1', b)])

        scb = [sc[0], sc[1], pp[1]]
        pT4 = pT + [P.sb(st, [128, 512], BF16, 'pT')]

        def project_units(h):
            b = h % 2
            W = wqkv[b]
            units = []

            def u0():
                for i, c0 in enumerate([h * 128, DA_W + h * 128, 2 * DA_W + h * 128]):
                    P.dma('pool', W[:, :, i * 128:(i + 1) * 128], w_in[:, :, c0:c0 + 128], writes=[('w', b, i)], cast=True)
                for m in range(2):
                    P.dma('pool', qT[b][64:70, m, :], inp['c_qaug'][h], writes=[('qa', b, m)], cast=True)
                    P.dma('pool', kT[b][64:70, m, :], inp['c_kaug'][h], writes=[('ka', b, m)], cast=True)
            units.append(u0)
            for tb in range(8):
                for m in range(2):
                    for isk in range(2):
                        def u(tb=tb, m=m, isk=isk):
                            c0 = isk * 128 + m * 64
                            P.mmg([(pp[0][0:64, :], W[:, dc, c0:c0 + 64], hT[:, dc, tb * 512:(tb + 1) * 512], dc == 0, dc == 7) for dc in range(8)],
                                  reads=[('w', b, isk)], writes=[('pp', 0)])
                            if isk == 0:
                                P.ts('dve', qT[b][0:64, m, tb * 512:(tb + 1) * 512], pp[0][0:64, :], 0.125, None, ALU.mult, None,
                                     reads=[('pp', 0)], writes=[('q', b, m, tb)])
                            else:
                                P.copy('dve', kT[b][0:64, m, tb * 512:(tb + 1) * 512], pp[0][0:64, :], reads=[('pp', 0)], writes=[('k', b, m, tb)])
                        units.append(u)
                for tq in range(4):
                    def uv(tt=tb * 4 + tq):
                        P.mmg([(pp[0][:, 0:128], hT[:, dc, tt * 128:(tt + 1) * 128], W[:, dc, 256:384], dc == 0, dc == 7) for dc in range(8)],
                              reads=[('w', b, 2)], writes=[('pp', 0)])
                        P.copy('dve', va[b][:, tt, 0:128], pp[0][:, 0:128], reads=[('pp', 0), ('va1', b)], writes=[('v', b, tt)])
                    units.append(uv)
            return units

        def attend(h, nxt):
            b = h % 2
            blocks = [(jq, m, kt) for jq in range(8) for m in range(2) for kt in range(4 * jq + 4)]
            nb = len(blocks)
            evc = [0]

            def geom(i):
                jq, m, kt = blocks[i]
                r = kt - 4 * jq
                off = max(r, 0) * 128
                return jq, m, kt, r, off, 512 - off

            def emit_qk(i):
                jq, m, kt, r, off, N = geom(i)
                sb_ = i % 3
                P.mmg([(scb[sb_][:, 0:N], kT[b][0:70, m, kt * 128:(kt + 1) * 128], qT[b][0:70, m, jq * 512 + off:(jq + 1) * 512], True, True)],
                      reads=[('q', b, m, jq), ('qa', b, m), ('ka', b, m), ('k', b, m, kt // 4)], writes=[('sc', sb_)])

            def emit_exp(i):
                jq, m, kt, r, off, N = geom(i)
                sb_ = i % 3
                pb = i % 4
                bi = h * 35 + (4 * jq - kt + 3)
                P.act(pT4[pb][:, off:512], scb[sb_][:, 0:N], AF.Exp, reads=[('sc', sb_), 'cbias'], writes=[('pT', pb)], bias=cbias[:, bi:bi + 1])
                if r >= 0:
                    P.tt('dve', pT4[pb][:, off:off + 128], pT4[pb][:, off:off + 128], dmask[:, h, :], ALU.mult,
                         reads=[('pT', pb), 'dmask'], writes=[('pT', pb)])

            def emit_pv(i):
                jq, m, kt, r, off, N = geom(i)
                pb = i % 4
                qs0 = max(r, 0)
                P.mmg([(pacc[qs][:, 0:129], pT4[pb][:, qs * 128:(qs + 1) * 128], va[b][:, kt, :], kt == 0, kt == 4 * jq + qs) for qs in range(qs0, 4)],
                      reads=[('pT', pb), ('v', b, kt), ('va1', b)], writes=[('pacc', qs) for qs in range(qs0, 4)])
                if kt == 4 * jq + 3:
                    for qs in range(4):
                        e = evc[0] % 2
                        evc[0] += 1
                        if m == 0:
                            P.recip(sm['r0'][e][:], pacc[qs][:, 128:129], reads=[('pacc', qs)], writes=[('r0', e)])
                            P.ts('dve', o0[:, qs, :], pacc[qs][:, 0:128], sm['r0'][e][:, 0:1], None, ALU.mult, None,
                                 reads=[('pacc', qs), ('r0', e)], writes=[('o0', qs)])
                        else:
                            P.recip(sm['r1'][e][:], pacc[qs][:, 128:129], reads=[('pacc', qs)], writes=[('r1', e)])
                            P.tt('dve', sm['r1'][e][:], sm['r1'][e][:], neglam[:], ALU.mult, reads=[('r1', e), 'neglam'], writes=[('r1', e)])
                            P.stt(oo[e][:], pacc[qs][:, 0:128], sm['r1'][e][:, 0:1], o0[:, qs, :], ALU.mult, ALU.add,
                                  reads=[('pacc', qs), ('r1', e), ('o0', qs)], writes=[('oo', e)])
                            P.add('dve', lambda eng, o=junk[:], a=oo[e][:], acc=sm['ssq'][e][:]: eng.scalar_tensor_tensor(
                                out=o, in0=a, scalar=1.0, in1=a, op0=ALU.mult, op1=ALU.mult, accum_out=acc),
                                reads=[('oo', e)], writes=['junk', ('ssq', e)])
                            P.ts('dve', sm['ms'][e][:], sm['ssq'][e][:], 1.0 / 128, EPS, ALU.mult, ALU.add, reads=[('ssq', e)], writes=[('ms', e)])
                            P.tt('pool', sm['rstd'][e][:], sm['ms'][e][:], k.neghalf[:], ALU.pow, reads=[('ms', e)], writes=[('rstd', e)])
                            P.stt(ob[e][:], oo[e][:], sm['rstd'][e][:, 0:1], gsub[:], ALU.mult, ALU.mult,
                                  reads=[('oo', e), ('rstd', e), 'gsub'], writes=[('ob', e)])
                            tt = jq * 4 + qs
                            P.dma('sp', o_s[tt * 128:(tt + 1) * 128, h * 128:(h + 1) * 128], ob[e][:], reads=[('ob', e)], writes=[('o_s', tt, h)])

            emit_qk(0)
            emit_qk(1)
            for i in range(nb):
                emit_exp(i)
                if i + 2 < nb:
                    emit_qk(i + 2)
                emit_pv(i)
                if i % 4 == 3 and nxt:
                    nxt.pop(0)()
            while nxt:
                nxt.pop(0)()

        for u in project_units(0):
            u()
        for h in range(6):
            attend(h, project_units(h + 1) if h + 1 < 6 else [])
        P.flush()


def phase_hgrn(P, k, hT, j, o_s):
    inp = k.inp
    w_in = wview(inp['b_w_in'][j])
    with ExitStack() as st:
        W = [P.sb(st, [128, 8, 512], BF16, 'W') for _ in range(2)]
        qeT = [P.sb(st, [128, S], BF16, 'qeT') for _ in range(2)]
        keT = [P.sb(st, [128, S], BF16, 'keT') for _ in range(2)]
        ketok = [P.sb(st, [128, NT, 128], BF16, 'ketok') for _ in range(2)]
        vtok = [P.sb(st, [128, NT, 128], BF16, 'vtok') for _ in range(2)]
        sgtok = [P.sb(st, [128, NT, 128], BF16, 'sgtok') for _ in range(2)]
        decay = [P.sb(st, [128, 64], F32, 'decay') for _ in range(2)]
        oml = P.sb(st, [128, 6], F32, 'oml')
        gn = P.sb(st, [128, 128], F32, 'gn')
        cmask = P.sb(st, [128, 128], BF16, 'cmask')
        smask = P.sb(st, [128, 512], F32, 'smask')
        tmp = {n: [P.sb(st, [128, 512], F32, n) for _ in range(2)] for n in ('et', 'dt', 'kk', 'gt', 'bt', 'eb', 'enb', 'qraw')}
        Sst = P.sb(st, [128, 128], F32, 'Sst')
        Sbf = P.sb(st, [128, 128], BF16, 'Sbf')
        at = [P.sb(st, [128, 128], BF16, 'at') for _ in range(2)]
        o1 = [P.sb(st, [128, 128], F32, 'o1') for _ in range(2)]
        ob = [P.sb(st, [128, 128], BF16, 'ob') for _ in range(2)]
        junk = P.sb(st, [128, 128], F32, 'junk')
        sm = {n: [P.sb(st, [128, 1], F32, n) for _ in range(2)] for n in ('ssq', 'ms', 'rstd')}
        pq = P.ps(st, [128, 512], F32, 'pq')
        ptr = P.ps(st, [128, 4, 128], BF16, 'ptr')
        pvg = P.ps(st, [128, 128], F32, 'pvg')
        pat = P.ps(st, [128, 128], F32, 'pat')
        pkv2 = [P.ps(st, [128, 128], F32, 'pkv') for _ in range(2)]
        po = [P.ps(st, [128, 128], F32, 'po') for _ in range(2)]

        P.dma('pool', cmask[:], inp['c_cmask'], writes=['cmask'], cast=True)
        P.dma('sp', smask[:], inp['c_smask'], writes=['smask'])
        P.dma('sp', gn[:], inp['b_out_norm'][j:j + 1, :].partition_broadcast(128), writes=['gn'])
        lbf = P.sb(st, [128, 1], F32, 'lbf')
        lbl2 = P.sb(st, [2, 768], F32, 'lbl2')
        lbT = P.sb(st, [128, 6, 2], F32, 'lbT')
        P.dma('sp', lbf[:], inp['c_lbflag'][j], writes=['lbf'])
        P.dma('sp', lbl2[:], inp['b_lb_logits'], writes=['lbl2'])
        P.trg([(pvg[:, 2 * h:2 * h + 2], lbl2[0:2, h * 128:(h + 1) * 128], k.identf[0:2, 0:2]) for h in range(6)], reads=['lbl2'], writes=['pvg'])
        P.copy('dve', lbT[:], pvg[:, 0:12].rearrange("p (h l) -> p h l", l=2), reads=['pvg'], writes=['lbl'])
        P.tt('dve', oml[:], lbT[:, :, 0], lbT[:, :, 1], ALU.subtract, reads=['lbl'], writes=['oml'])
        P.act(oml[:], oml[:], AF.Exp, reads=['oml'], writes=['oml'])
        P.ts('dve', oml[:], oml[:], 1.0, None, ALU.add, None, reads=['oml'], writes=['oml'])
        P.recip(oml[:], oml[:], reads=['oml'], writes=['oml'])
        P.ts('dve', oml[:], oml[:], lbf[:, 0:1], 1.0, ALU.mult, ALU.add, reads=['oml', 'lbf'], writes=['oml'])

        def project_units(h):
            b = h % 2
            units = []

            def u0():
                for i, c0 in enumerate([h * 128, HG_W + h * 128, 2 * HG_W + h * 128, 3 * HG_W + h * 128]):
                    P.dma('pool', W[b][:, :, i * 128:(i + 1) * 128], w_in[:, :, c0:c0 + 128], writes=[('w', b, i)], cast=True)
            units.append(u0)
            for tb in range(8):
                def ua(tb=tb):
                    blk = slice(tb * 512, (tb + 1) * 512)
                    x = tb % 2
                    et, dt_, kk, gt, bt, eb, enb, qraw = (tmp[n][x] for n in ('et', 'dt', 'kk', 'gt', 'bt', 'eb', 'enb', 'qraw'))
                    T = lambda n: (n, x)
                    P.mmg([(pq[:], W[b][:, dc, 0:128], hT[:, dc, blk], dc == 0, dc == 7) for dc in range(8)], reads=[('w', b, 0)], writes=['pq'])
                    P.copy('act', qraw[:], pq[:], reads=['pq'], writes=[T('qraw')])
                    P.mmg([(pq[:], W[b][:, dc, 128:256], hT[:, dc, blk], dc == 0, dc == 7) for dc in range(8)], reads=[('w', b, 1)], writes=['pq'])
                    P.act(et[:], pq[:], AF.Exp, reads=['pq'], writes=[T('et')], scale=-1.0)
                    P.ts('dve', dt_[:], et[:], 1.0, None, ALU.add, None, reads=[T('et')], writes=[T('dt')])
                    P.recip(dt_[:], dt_[:], reads=[T('dt')], writes=[T('dt')])
                    P.stt(kk[:], et[:], oml[:, h:h + 1], dt_[:], ALU.mult, ALU.mult, reads=[T('et'), T('dt'), 'oml'], writes=[T('kk')])
                    P.act(gt[:], kk[:], AF.Ln, reads=[T('kk')], writes=[T('gt')], scale=-1.0, bias=1.0)
                    P.scan(bt[:], smask[:], gt[:], reads=['smask', T('gt')], writes=[T('bt')])
                    P.act(eb[:], bt[:], AF.Exp, reads=[T('bt')], writes=[T('eb')])
                    P.act(enb[:], bt[:], AF.Exp, reads=[T('bt')], writes=[T('enb')], scale=-1.0)
                    P.tt('dve', qeT[b][:, blk], qraw[:], eb[:], ALU.mult, reads=[T('qraw'), T('eb')], writes=[('qe', b, tb)])
                    P.tt('dve', keT[b][:, blk], kk[:], enb[:], ALU.mult, reads=[T('kk'), T('enb')], writes=[('ke', b, tb)])
                    P.copy('act', decay[b][:, tb * 8:(tb + 1) * 8], eb[:].rearrange("p (c t) -> p c t", t=64)[:, :, 63], reads=[T('eb')], writes=[('dec', b, tb)])
                    P.trg([(ptr[:, i, :], keT[b][:, tb * 512 + i * 128: tb * 512 + (i + 1) * 128], k.ident[:]) for i in range(4)],
                          reads=[('ke', b, tb)], writes=['ptr'])
                    P.copy('act', ketok[b][:, tb * 4:(tb + 1) * 4, :], ptr[:], reads=['ptr'], writes=[('ketok', b, tb)])
                units.append(ua)
                for tq in range(4):
                    def uv(tt=tb * 4 + tq):
                        tok = slice(tt * 128, (tt + 1) * 128)
                        P.mmg([(pvg[:], hT[:, dc, tok], W[b][:, dc, 256:384], dc == 0, dc == 7) for dc in range(8)], reads=[('w', b, 2)], writes=['pvg'])
                        P.copy('dve', vtok[b][:, tt, :], pvg[:], reads=['pvg'], writes=[('vtok', b, tt)])
                        P.mmg([(pvg[:], hT[:, dc, tok], W[b][:, dc, 384:512], dc == 0, dc == 7) for dc in range(8)], reads=[('w', b, 3)], writes=['pvg'])
                        P.act(sgtok[b][:, tt, :], pvg[:], AF.Silu, reads=['pvg'], writes=[('sgtok', b, tt)])
                    units.append(uv)
            return units

        def recur(h, nxt):
            b = h % 2
            P.memset('pool', Sst[:], 0.0, writes=['Sst'])
            P.memset('pool', Sbf[:], 0.0, writes=['Sbf'])
            for tt in range(NT):
                e = tt % 2
                tb = tt // 4
                tok = slice(tt * 128, (tt + 1) * 128)
                P.mmg([(pat[:], keT[b][:, tok], qeT[b][:, tok], True, True)], reads=[('ke', b, tb), ('qe', b, tb)], writes=['pat'])
                P.tt('dve', at[e][:], pat[:], cmask[:], ALU.mult, reads=['pat', 'cmask'], writes=[('at', e)])
                P.mmg([(pkv2[ci][:], ketok[b][ci * 64:(ci + 1) * 64, tt, :], vtok[b][ci * 64:(ci + 1) * 64, tt, :], True, True) for ci in range(2)],
                      reads=[('ketok', b, tb), ('vtok', b, tt)], writes=['pkv'])
                P.mmg([(po[e][:], at[e][:], vtok[b][:, tt, :], True, False),
                       (po[e][0:64, :], qeT[b][:, tt * 128:tt * 128 + 64], Sbf[:], False, False)],
                      reads=[('at', e), ('vtok', b, tt), ('qe', b, tb), 'Sbf'], writes=[('po', e)])
                for ci in range(2):
                    c = 2 * tt + ci
                    P.tt('dve', Sst[:], pkv2[ci][:], Sst[:], ALU.add, reads=['pkv', 'Sst'], writes=['Sst'])
                    P.ts('dve', Sst[:], Sst[:], decay[b][:, c:c + 1], None, ALU.mult, None, reads=['Sst', ('dec', b, tb)], writes=['Sst'])
                    P.copy('act', Sbf[:], Sst[:], reads=['Sst'], writes=['Sbf'])
                    if ci == 0:
                        P.mmg([(po[e][64:128, :], qeT[b][:, tt * 128 + 64:(tt + 1) * 128], Sbf[:], False, True)],
                              reads=[('qe', b, tb), 'Sbf'], writes=[('po', e)])
                P.act(junk[:], po[e][:], AF.Square, reads=[('po', e)], writes=['junk', ('ssq', e)], accum=sm['ssq'][e][:])
                P.ts('dve', sm['ms'][e][:], sm['ssq'][e][:], 1.0 / 128, EPS, ALU.mult, ALU.add, reads=[('ssq', e)], writes=[('ms', e)])
                P.tt('pool', sm['rstd'][e][:], sm['ms'][e][:], k.neghalf[:], ALU.pow, reads=[('ms', e)], writes=[('rstd', e)])
                P.stt(o1[e][:], po[e][:], sm['rstd'][e][:, 0:1], gn[:], ALU.mult, ALU.mult, reads=[('po', e), ('rstd', e), 'gn'], writes=[('o1', e)])
                P.tt('dve', ob[e][:], o1[e][:], sgtok[b][:, tt, :], ALU.mult, reads=[('o1', e), ('sgtok', b, tt)], writes=[('ob', e)])
                P.dma('sp', o_s[tt * 128:(tt + 1) * 128, h * 128:(h + 1) * 128], ob[e][:], reads=[('ob', e)], writes=[('o_s', tt, h)])
                for _ in range(2):
                    if nxt:
                        nxt.pop(0)()
            while nxt:
                nxt.pop(0)()

        for u in project_units(0):
            u()
        for h in range(6):
            recur(h, project_units(h + 1) if h + 1 < 6 else [])
        P.flush()


def phase_ffn(P, k, x_src, xacc_src, x_dst, gain_row, w_gu_list, w_dn_list, dff, router_ap=None, esel_ap=None):
    HT = S // 2
    NTH = NT // 2
    ngrp = (dff + 511) // 512
    moe = router_ap is not None
    same = xacc_src is x_src
    nexp = len(w_gu_list)
    for half in range(2):
        with ExitStack() as st0:
            xacc = P.sb(st0, [128, NTH, D], F32, 'xacc')
            hTh = P.sb(st0, [128, 8, HT], BF16, 'hTh')
            csel = P.sb(st0, [128, NTH], F32, 'csel')
            comb = P.sb(st0, [128, NTH, 8], F32, 'comb')
            with ExitStack() as st:
                gbc = P.sb(st, [128, D], F32, 'gbc')
                hb = [P.sb(st, [128, D], BF16, 'hb') for _ in range(2)]
                pt = [P.ps(st, [128, 8, 128], BF16, 'pt') for _ in range(2)]
                alloc_norm_tmps(P, k, st, ['n0', 'n1'])
                P.dma('sp', gbc[:], gain_row.partition_broadcast(128), writes=['gbc'])
                if moe:
                    xp = [P.sb(st, [128, D], F32, 'xp') for _ in range(2)]
                    hf2 = [P.sb(st, [128, D], F32, 'hf') for _ in range(2)]
                    hTf = P.sb(st, [128, 8, 128], F32, 'hTf')
                    wr = P.sb(st, [128, 8, 8], F32, 'wr')
                    esel = P.sb(st, [128, 8], F32, 'esel')
                    ptf = [P.ps(st, [128, 4, 128], F32, 'ptf') for _ in range(2)]
                    plg = P.ps(st, [128, 8], F32, 'plg')
                    lg = P.sb(st, [128, 8], F32, 'lg')
                    lg2 = P.sb(st, [128, 8], F32, 'lg2')
                    eq1 = P.sb(st, [128, 8], F32, 'eq1')
                    eq2 = P.sb(st, [128, 8], F32, 'eq2')
                    m1 = P.sb(st, [128, 1], F32, 'm1')
                    m2 = P.sb(st, [128, 1], F32, 'm2')
                    w1 = P.sb(st, [128, 1], F32, 'w1')
                    w2 = P.sb(st, [128, 1], F32, 'w2')
                    P.dma('sp', wr[:], wview(router_ap), writes=['wr'])
                    if esel_ap is not None:
                        P.dma('sp', esel[:], esel_ap, writes=['esel'])
                for tl in range(NTH):
                    tt = half * NTH + tl
                    b = tl % 2
                    tag = 'n%d' % b
                    rows = slice(tt * 128, (tt + 1) * 128)
                    if moe:
                        hf = hf2[b]
                        if same:
                            P.dma('sp', xacc[:, tl, :], x_src[rows, :], writes=[tag + 'x', ('xacc', tl)])
                            norm_rows(P, k, st, xacc[:, tl, :], gbc[:], hb[b][:], tag, want_f32=hf[:])
                        else:
                            P.dma('sp', xacc[:, tl, :], xacc_src[rows, :], writes=[('xacc', tl)])
                            P.dma('sp', xp[b][:], x_src[rows, :], writes=[tag + 'x'])
                            norm_rows(P, k, st, xp[b][:], gbc[:], hb[b][:], tag, want_f32=hf[:])
                        for q4 in range(2):
                            P.trg([(ptf[q4][:, c, :], hf[:, (q4 * 4 + c) * 128:(q4 * 4 + c + 1) * 128], k.identf[:]) for c in range(4)],
                                  reads=[tag + 'hf'], writes=[('ptf', q4)])
                            P.copy('dve', hTf[:, q4 * 4:(q4 + 1) * 4, :], ptf[q4][:], reads=[('ptf', q4)], writes=[('hTf', q4)])
                        P.mmg([(plg[:], hTf[:, dc, :], wr[:, dc, :], dc == 0, dc == 7) for dc in range(8)],
                              reads=[('hTf', 0), ('hTf', 1), 'wr'], writes=['plg'])
                        P.copy('dve', lg[:], plg[:], reads=['plg'], writes=['lg'])
                        P.reduce(m1[:], lg[:], ALU.max, reads=['lg'], writes=['m1'])
                        P.ts('dve', eq1[:], lg[:], m1[:, 0:1], None, ALU.is_equal, None, reads=['lg', 'm1'], writes=['eq1'])
                        P.stt(lg2[:], eq1[:], -1e30, lg[:], ALU.mult, ALU.add, reads=['eq1', 'lg'], writes=['lg2'])
                        P.reduce(m2[:], lg2[:], ALU.max, reads=['lg2'], writes=['m2'])
                        P.ts('dve', eq2[:], lg2[:], m2[:, 0:1], None, ALU.is_equal, None, reads=['lg2', 'm2'], writes=['eq2'])
                        P.tt('dve', w2[:], m2[:], m1[:], ALU.subtract, reads=['m1', 'm2'], writes=['w2'])
                        P.act(w2[:], w2[:], AF.Exp, reads=['w2'], writes=['w2'])
                        P.ts('dve', w1[:], w2[:], 1.0, None, ALU.add, None, reads=['w2'], writes=['w1'])
                        P.recip(w1[:], w1[:], reads=['w1'], writes=['w1'])
                        P.tt('dve', w2[:], w2[:], w1[:], ALU.mult, reads=['w1', 'w2'], writes=['w2'])
                        P.ts('dve', eq1[:], eq1[:], w1[:, 0:1], None, ALU.mult, None, reads=['eq1', 'w1'], writes=['eq1'])
                        if esel_ap is None:
                            P.stt(comb[:, tl, :], eq2[:], w2[:, 0:1], eq1[:], ALU.mult, ALU.add, reads=['eq2', 'w2', 'eq1'], writes=[('comb', tl)])
                        else:
                            P.stt(eq2[:], eq2[:], w2[:, 0:1], eq1[:], ALU.mult, ALU.add, reads=['eq2', 'w2', 'eq1'], writes=['eq2'])
                            P.tt('dve', eq2[:], eq2[:], esel[:], ALU.mult, reads=['eq2', 'esel'], writes=['eq2'])
                            P.reduce(csel[:, tl:tl + 1], eq2[:], ALU.add, reads=['eq2'], writes=[('csel', tl)])
                    else:
                        P.dma('sp', xacc[:, tl, :], x_src[rows, :], writes=[tag + 'x', ('xacc', tl)])
                        norm_rows(P, k, st, xacc[:, tl, :], gbc[:], hb[b][:], tag)
                    P.trg([(pt[b][:, c, :], hb[b][:, c * 128:(c + 1) * 128], k.ident[:]) for c in range(8)],
                          reads=[tag + 'hb'], writes=[tag + 'pt'])
                    P.copy('act' if tl % 2 else 'dve', hTh[:, :, tl * 128:(tl + 1) * 128], pt[b][:], reads=[tag + 'pt'], writes=[('hT', tl)])
                P.flush()
            with ExitStack() as st:
                wg = [P.sb(st, [128, 8, 512], BF16, 'wg') for _ in range(2)]
                wu = [P.sb(st, [128, 8, 512], BF16, 'wu') for _ in range(2)]
                wd = [P.sb(st, [128, 4, D], BF16, 'wd') for _ in range(2)]
                sg = [P.sb(st, [128, 512], F32, 'sg') for _ in range(2)]
                aT = [P.sb(st, [128, 4, 512], BF16, 'aT') for _ in range(2)]
                pg = [P.ps(st, [128, 512], F32, 'pg') for _ in range(2)]
                pu = [P.ps(st, [128, 512], F32, 'pu') for _ in range(2)]
                po = [P.ps(st, [128, 512], F32, 'po') for _ in range(4)]
                it = 0
                gi = 0
                oi = 0
                ai = 0
                for e in range(nexp):
                    gu = wview(w_gu_list[e])
                    w_dn_ap = w_dn_list[e]
                    for fg in range(ngrp):
                        F = min(512, dff - fg * 512)
                        nfc = F // 128
                        wb = it % 2
                        it += 1
                        P.dma('pool', wg[wb][:, :, 0:F], gu[:, :, fg * 512:fg * 512 + F], writes=[('wg', wb)], cast=True)
                        P.dma('pool', wu[wb][:, :, 0:F], gu[:, :, dff + fg * 512:dff + fg * 512 + F], writes=[('wu', wb)], cast=True)
                        P.dma('pool', wd[wb][:, 0:nfc, :], wview(w_dn_ap[fg * 512:fg * 512 + F, :]), writes=[('wd', wb)], cast=True)
                        for tb in range(HT // 512):
                            blk = slice(tb * 512, (tb + 1) * 512)
                            ab = ai % 2
                            ai += 1
                            for fc in range(nfc):
                                g = gi % 2
                                gi += 1
                                P.mmg([(pg[g][:], wg[wb][:, dc, fc * 128:(fc + 1) * 128], hTh[:, dc, blk], dc == 0, dc == 7) for dc in range(8)],
                                      reads=[('wg', wb)], writes=[('pg', g)])
                                P.mmg([(pu[g][:], wu[wb][:, dc, fc * 128:(fc + 1) * 128], hTh[:, dc, blk], dc == 0, dc == 7) for dc in range(8)],
                                      reads=[('wu', wb)], writes=[('pu', g)])
                                P.act(sg[g][:], pg[g][:], AF.Silu, reads=[('pg', g)], writes=[('sg', g)])
                                P.tt('dve', aT[ab][:, fc, :], pu[g][:], sg[g][:], ALU.mult, reads=[('pu', g), ('sg', g)], writes=[('aT', ab, fc)])
                            for tq in range(4):
                                tl = tb * 4 + tq
                                for hh in range(2):
                                    o = oi % 4
                                    oi += 1
                                    P.mmg([(po[o][:], aT[ab][:, fc, tq * 128:(tq + 1) * 128], wd[wb][:, fc, hh * 512:(hh + 1) * 512], fc == 0, fc == nfc - 1) for fc in range(nfc)],
                                          reads=[('aT', ab, fc) for fc in range(nfc)] + [('wd', wb)], writes=[('po', o)])
                                    sc_ = (comb[:, tl, e:e + 1] if esel_ap is None else csel[:, tl:tl + 1]) if moe else 1.0
                                    P.stt(xacc[:, tl, hh * 512:(hh + 1) * 512], po[o][:], sc_, xacc[:, tl, hh * 512:(hh + 1) * 512], ALU.mult, ALU.add,
                                          reads=[('po', o), ('xacc', tl, hh)], writes=[('xacc', tl, hh)])
                for tl in range(NTH):
                    tt = half * NTH + tl
                    P.dma('sp', x_dst[tt * 128:(tt + 1) * 128, :], xacc[:, tl, :], reads=[('xacc', tl, 0), ('xacc', tl, 1)], writes=[('xd', tt)])
                P.flush()


def phase_moe_sparse(P, k, x_src, x_dst, gain_row, w_gu_list, w_dn_list, router_ap, hbk):
    I32 = mybir.dt.int32
    NQ = 4
    NTQ = NT // NQ
    dff = DFF_E
    ngrp = dff // 512
    import os
    thr = float(os.environ.get('K_MOE_THR', '64'))
    for qt in range(NQ):
        with ExitStack() as st0:
            xacc = P.sb(st0, [128, NTQ, D], F32, 'xacc')
            comb = P.sb(st0, [128, NTQ * 8], F32, 'comb')
            maskf = P.sb(st0, [128, NTQ * 8], F32, 'maskf')
            pos = P.sb(st0, [128, NTQ * 8], F32, 'pos')
            flag = P.sb(st0, [128, 1], I32, 'flag')
            iota = P.sb(st0, [128, 128], F32, 'iota')
            with ExitStack() as st:
                gbc = P.sb(st, [128, D], F32, 'gbc')
                hb = [P.sb(st, [128, D], BF16, 'hb') for _ in range(2)]
                hf2 = [P.sb(st, [128, D], F32, 'hf') for _ in range(2)]
                alloc_norm_tmps(P, k, st, ['n0', 'n1'])
                hTf = P.sb(st, [128, 8, 128], F32, 'hTf')
                wr = P.sb(st, [128, 8, 8], F32, 'wr')
                ltri = P.sb(st, [128, 128], F32, 'ltri')
                ones = P.sb(st, [128, 128], F32, 'ones')
                cnt = P.sb(st, [128, NTQ * 8], F32, 'cnt')
                mx = P.sb(st, [128, 1], F32, 'mx')
                ptf = [P.ps(st, [128, 4, 128], F32, 'ptf') for _ in range(2)]
                plg = P.ps(st, [128, 8], F32, 'plg')
                ppos = P.ps(st, [128, NTQ * 8], F32, 'ppos')
                pcnt = P.ps(st, [128, NTQ * 8], F32, 'pcnt')
                lg = P.sb(st, [128, 8], F32, 'lg')
                lg2 = P.sb(st, [128, 8], F32, 'lg2')
                eq1 = P.sb(st, [128, 8], F32, 'eq1')
                eq2 = P.sb(st, [128, 8], F32, 'eq2')
                m1 = P.sb(st, [128, 1], F32, 'm1')
                m2 = P.sb(st, [128, 1], F32, 'm2')
                w1 = P.sb(st, [128, 1], F32, 'w1')
                w2 = P.sb(st, [128, 1], F32, 'w2')
                P.dma('sp', gbc[:], gain_row.partition_broadcast(128), writes=['gbc'])
                P.dma('sp', wr[:], wview(router_ap), writes=['wr'])
                P.dma('sp', ltri[:], k.inp['c_ltri'], writes=['ltri'])
                P.dma('sp', iota[:], k.inp['c_iota'], writes=['iota'])
                P.memset('pool', ones[:], 1.0, writes=['ones'])
                for tl in range(NTQ):
                    tt = qt * NTQ + tl
                    b = tl % 2
                    tag = 'n%d' % b
                    rows = slice(tt * 128, (tt + 1) * 128)
                    hf = hf2[b]
                    P.dma('sp', xacc[:, tl, :], x_src[rows, :], writes=[tag + 'x', ('xacc', tl)])
                    norm_rows(P, k, st, xacc[:, tl, :], gbc[:], hb[b][:], tag, want_f32=hf[:])
                    P.dma('sp', hbk[rows, :], hb[b][:], reads=[tag + 'hb'], writes=[('hbk', tt)])
                    for q4 in range(2):
                        P.trg([(ptf[q4][:, c, :], hf[:, (q4 * 4 + c) * 128:(q4 * 4 + c + 1) * 128], k.identf[:]) for c in range(4)],
                              reads=[tag + 'hf'], writes=[('ptf', q4)])
                        P.copy('dve', hTf[:, q4 * 4:(q4 + 1) * 4, :], ptf[q4][:], reads=[('ptf', q4)], writes=[('hTf', q4)])
                    P.mmg([(plg[:], hTf[:, dc, :], wr[:, dc, :], dc == 0, dc == 7) for dc in range(8)],
                          reads=[('hTf', 0), ('hTf', 1), 'wr'], writes=['plg'])
                    P.copy('dve', lg[:], plg[:], reads=['plg'], writes=['lg'])
                    P.reduce(m1[:], lg[:], ALU.max, reads=['lg'], writes=['m1'])
                    P.ts('dve', eq1[:], lg[:], m1[:, 0:1], None, ALU.is_equal, None, reads=['lg', 'm1'], writes=['eq1'])
                    P.stt(lg2[:], eq1[:], -1e30, lg[:], ALU.mult, ALU.add, reads=['eq1', 'lg'], writes=['lg2'])
                    P.reduce(m2[:], lg2[:], ALU.max, reads=['lg2'], writes=['m2'])
                    P.ts('dve', eq2[:], lg2[:], m2[:, 0:1], None, ALU.is_equal, None, reads=['lg2', 'm2'], writes=['eq2'])
                    P.tt('dve', w2[:], m2[:], m1[:], ALU.subtract, reads=['m1', 'm2'], writes=['w2'])
                    P.act(w2[:], w2[:], AF.Exp, reads=['w2'], writes=['w2'])
                    P.ts('dve', w1[:], w2[:], 1.0, None, ALU.add, None, reads=['w2'], writes=['w1'])
                    P.recip(w1[:], w1[:], reads=['w1'], writes=['w1'])
                    P.tt('dve', w2[:], w2[:], w1[:], ALU.mult, reads=['w1', 'w2'], writes=['w2'])
                    P.tt('dve', maskf[:, tl * 8:(tl + 1) * 8], eq1[:], eq2[:], ALU.add, reads=['eq1', 'eq2'], writes=[('mask', tl)])
                    P.ts('dve', eq1[:], eq1[:], w1[:, 0:1], None, ALU.mult, None, reads=['eq1', 'w1'], writes=['eq1'])
                    P.stt(comb[:, tl * 8:(tl + 1) * 8], eq2[:], w2[:, 0:1], eq1[:], ALU.mult, ALU.add, reads=['eq2', 'w2', 'eq1'], writes=[('comb', tl)])
                allm = [('mask', tl) for tl in range(NTQ)]
                P.mmg([(ppos[:], ltri[:], maskf[:], True, True)], reads=allm + ['ltri'], writes=['ppos'])
                P.mmg([(pcnt[:], ones[:], maskf[:], True, True)], reads=allm + ['ones'], writes=['pcnt'])
                P.copy('dve', pos[:], ppos[:], reads=['ppos'], writes=['pos'])
                P.copy('dve', cnt[:], pcnt[:], reads=['pcnt'], writes=['cnt'])
                P.reduce(mx[:], cnt[:], ALU.max, reads=['cnt'], writes=['mx'])
                P.ts('dve', flag[:], mx[:], thr, None, ALU.is_gt, None, reads=['mx'], writes=['flag'])
                P.flush()
            with ExitStack() as st:
                wg = [P.sb(st, [128, 8, 512], BF16, 'wg') for _ in range(2)]
                wu = [P.sb(st, [128, 8, 512], BF16, 'wu') for _ in range(2)]
                wd = [P.sb(st, [128, 4, D], BF16, 'wd') for _ in range(2)]
                sg = [P.sb(st, [128, 512], F32, 'sg') for _ in range(2)]
                aT = [P.sb(st, [128, 4, 512], BF16, 'aT') for _ in range(2)]
                hbt = [P.sb(st, [128, D], BF16, 'hbt') for _ in range(2)]
                sel2 = [P.sb(st, [128, NTQ, 128], BF16, 'sel') for _ in range(2)]
                selT2 = [P.sb(st, [128, NTQ, 128], BF16, 'selT') for _ in range(2)]
                hTg2 = [P.sb(st, [128, 8, NTQ * 128], BF16, 'hTg') for _ in range(2)]
                yacc = P.sb(st, [128, NTQ, D], F32, 'yacc')
                ybf = [P.sb(st, [128, D], BF16, 'ybf') for _ in range(2)]
                pg = [P.ps(st, [128, 512], F32, 'pg') for _ in range(2)]
                pu = [P.ps(st, [128, 512], F32, 'pu') for _ in range(2)]
                po = [P.ps(st, [128, 512], F32, 'po') for _ in range(3)]
                pts = P.ps(st, [128, NTQ, 128], BF16, 'pts')

                def body(cap):
                    NS = NTQ * cap
                    nsb = NS // 512
                    nst = NS // 128
                    ctr = {'it': 0, 'gi': 0, 'oi': 0, 'ai': 0, 'hb': 0, 'yb': 0}
                    def pre_units(e):
                        x = e % 2
                        sel, selT, hTg = sel2[x], selT2[x], hTg2[x]
                        units = []

                        def usel():
                            for tl in range(NTQ):
                                col = tl * 8 + e
                                P.ts('dve', sel[:, tl, 0:cap], iota[:, 0:cap], pos[:, col:col + 1], maskf[:, col:col + 1], ALU.is_equal, ALU.mult,
                                     reads=[], writes=[('sel', x, tl)])
                            P.trg([(pts[(tl * cap) % 128:(tl * cap) % 128 + cap, tl, :], sel[:, tl, 0:cap], k.ident[:]) for tl in range(NTQ)],
                                  reads=[('sel', x, tl) for tl in range(NTQ)], writes=['pts'])
                            P.copy('act', selT[:, :, :], pts[:, :, :], reads=['pts'], writes=[('selT', x)])
                        units.append(usel)
                        for tl in range(NTQ):
                            def ug(tl=tl):
                                tt = qt * NTQ + tl
                                hbi = ctr['hb'] % 2
                                ctr['hb'] += 1
                                P.dma('sp', hbt[hbi][:], hbk[tt * 128:(tt + 1) * 128, :], writes=[('hbt', hbi)])
                                ndc = 512 // cap
                                for g0 in range(0, 8, ndc):
                                    o = ctr['oi'] % 3
                                    ctr['oi'] += 1
                                    pv = po[o][:].rearrange("p (c s) -> p c s", s=cap)
                                    P.mmg([(pv[:, dc - g0, :], hbt[hbi][:, dc * 128:(dc + 1) * 128], sel[:, tl, 0:cap], True, True) for dc in range(g0, g0 + ndc)],
                                          reads=[('hbt', hbi), ('sel', x, tl)], writes=[('po', o)])
                                    P.copy('act' if (tl % 2) else 'dve', hTg[:, g0:g0 + ndc, tl * cap:(tl + 1) * cap], pv, reads=[('po', o)], writes=[('hTg', x, tl, g0)])
                            units.append(ug)
                        return units

                    for u in pre_units(0):
                        u()
                    for e in range(NEXP):
                        gu = wview(w_gu_list[e])
                        w_dn_ap = w_dn_list[e]
                        x = e % 2
                        selT, hTg = selT2[x], hTg2[x]
                        nxt = pre_units(e + 1) if e + 1 < NEXP else []
                        allg = [('hTg', x, tl, g0) for tl in range(NTQ) for g0 in range(0, 8, 512 // cap)]
                        for fg in range(ngrp):
                            wb = ctr['it'] % 2
                            ctr['it'] += 1
                            P.dma('pool', wg[wb][:], gu[:, :, fg * 512:(fg + 1) * 512], writes=[('wg', wb)], cast=True)
                            P.dma('pool', wu[wb][:], gu[:, :, dff + fg * 512:dff + (fg + 1) * 512], writes=[('wu', wb)], cast=True)
                            P.dma('pool', wd[wb][:], wview(w_dn_ap[fg * 512:(fg + 1) * 512, :]), writes=[('wd', wb)], cast=True)
                            for sb_ in range(nsb):
                                blk = slice(sb_ * 512, (sb_ + 1) * 512)
                                ab = ctr['ai'] % 2
                                ctr['ai'] += 1
                                for fc in range(4):
                                    g = ctr['gi'] % 2
                                    ctr['gi'] += 1
                                    P.mmg([(pg[g][:], wg[wb][:, dc, fc * 128:(fc + 1) * 128], hTg[:, dc, blk], dc == 0, dc == 7) for dc in range(8)],
                                          reads=[('wg', wb)] + allg, writes=[('pg', g)])
                                    P.mmg([(pu[g][:], wu[wb][:, dc, fc * 128:(fc + 1) * 128], hTg[:, dc, blk], dc == 0, dc == 7) for dc in range(8)],
                                          reads=[('wu', wb)] + allg, writes=[('pu', g)])
                                    P.act(sg[g][:], pg[g][:], AF.Silu, reads=[('pg', g)], writes=[('sg', g)])
                                    P.tt('dve', aT[ab][:, fc, :], pu[g][:], sg[g][:], ALU.mult, reads=[('pu', g), ('sg', g)], writes=[('aT', ab, fc)])
                                for tq in range(4):
                                    s_t = sb_ * 4 + tq
                                    for hh in range(2):
                                        o = ctr['oi'] % 3
                                        ctr['oi'] += 1
                                        P.mmg([(po[o][:], aT[ab][:, fc, tq * 128:(tq + 1) * 128], wd[wb][:, fc, hh * 512:(hh + 1) * 512], fc == 0, fc == 3) for fc in range(4)],
                                              reads=[('aT', ab, fc) for fc in range(4)] + [('wd', wb)], writes=[('po', o)])
                                        ydst = yacc[:, s_t, hh * 512:(hh + 1) * 512]
                                        if fg == 0:
                                            P.copy('dve', ydst, po[o][:], reads=[('po', o)], writes=[('yacc', s_t, hh)])
                                        else:
                                            P.tt('dve', ydst, po[o][:], ydst, ALU.add, reads=[('po', o), ('yacc', s_t, hh)], writes=[('yacc', s_t, hh)])
                            for _ in range(2):
                                if nxt:
                                    nxt.pop(0)()
                        while nxt:
                            nxt.pop(0)()
                        for s_t in range(nst):
                            yb = ctr['yb'] % 2
                            ctr['yb'] += 1
                            P.copy('act', ybf[yb][:], yacc[:, s_t, :], reads=[('yacc', s_t, 0), ('yacc', s_t, 1)], writes=[('ybf', yb)])
                            for tl in range(s_t * 128 // cap, (s_t + 1) * 128 // cap):
                                p0 = (tl * cap) % 128
                                col = tl * 8 + e
                                for hh in range(2):
                                    o = ctr['oi'] % 3
                                    ctr['oi'] += 1
                                    P.mmg([(po[o][:], selT[p0:p0 + cap, tl, :], ybf[yb][p0:p0 + cap, hh * 512:(hh + 1) * 512], True, True)],
                                          reads=[('selT', x), ('ybf', yb)], writes=[('po', o)])
                                    P.stt(xacc[:, tl, hh * 512:(hh + 1) * 512], po[o][:], comb[:, col:col + 1], xacc[:, tl, hh * 512:(hh + 1) * 512], ALU.mult, ALU.add,
                                          reads=[('po', o), ('xacc', tl, hh)], writes=[('xacc', tl, hh)])
                    for tl in range(NTQ):
                        tt = qt * NTQ + tl
                        P.dma('sp', x_dst[tt * 128:(tt + 1) * 128, :], xacc[:, tl, :], reads=[('xacc', tl, 0), ('xacc', tl, 1)], writes=[('xd', tt)])

                P.flush_branch(flag[0:1, 0:1], lambda: body(64), lambda: body(128))


def phase_fnorm(P, k, x_src, gain_row, out_ap):
    with ExitStack() as st:
        gbc = P.sb(st, [128, D], F32, 'gbc')
        xt = [P.sb(st, [128, D], F32, 'xt') for _ in range(2)]
        yo = [P.sb(st, [128, D], F32, 'yo') for _ in range(2)]
        alloc_norm_tmps(P, k, st, ['n0', 'n1'])
        P.dma('sp', gbc[:], gain_row.partition_broadcast(128), writes=['gbc'])
        for tt in range(NT):
            b = tt % 2
            tag = 'n%d' % b
            t = k.nt[tag]
            P.dma('sp', xt[b][:], x_src[tt * 128:(tt + 1) * 128, :], writes=[tag + 'x'])
            P.act(t['junk'][:], xt[b][:], AF.Square, reads=[tag + 'x'], writes=[tag + 'junk', tag + 'ssq'], accum=t['ssq'][:])
            P.ts('dve', t['ms'][:], t['ssq'][:], 1.0 / D, EPS, ALU.mult, ALU.add, reads=[tag + 'ssq'], writes=[tag + 'ms'])
            P.tt('pool', t['rstd'][:], t['ms'][:], k.neghalf[:], ALU.pow, reads=[tag + 'ms'], writes=[tag + 'rstd'])
            P.stt(yo[b][:], xt[b][:], t['rstd'][:, 0:1], gbc[:], ALU.mult, ALU.mult, reads=[tag + 'x', tag + 'rstd', 'gbc'], writes=[('yo', b)])
            P.dma('sp', out_ap[tt * 128:(tt + 1) * 128, :], yo[b][:], reads=[('yo', b)], writes=[('out', tt)])
        P.flush()


CONST_SHAPES = {"c_ident": [128, 128], "c_kaug": [6, 6, S], "c_qaug": [6, 6, S], "c_dmask": [6, 128, 128], "c_bias": [128, 6 * 35],
                "c_cmask": [128, 128], "c_smask": [128, 512], "c_ltri": [128, 128], "c_iota": [128, 128]}
STEP_INPUTS = {
    'attn': {"x": [S, D], "mem": [MEM_LEN, D], "a_norm_mix": [1, D], "a_w_in": [1, D, 2560], "a_lam_q1": [1, 64], "a_lam_k1": [1, 64],
             "a_lam_q2": [1, 64], "a_lam_k2": [1, 64], "a_subln": [1, 128], "a_mem_norm": [1, D], "a_w_mem_kv": [1, D, 512],
             "a_w_out": [1, D, D], "c_lam": [1, 128, 2], "c_ident": 0, "c_kaug": 0, "c_qaug": 0, "c_dmask": 0, "c_bias": 0},
    'hgrn': {"x": [S, D], "mem": [MEM_LEN, D], "b_norm_mix": [1, D], "b_w_in": [1, D, 3328], "b_lb_logits": [2, 768], "b_out_norm": [1, 128],
             "b_mem_norm": [1, D], "b_w_mem_kv": [1, D, 512], "b_w_out": [1, D, D], "c_lbflag": [1, 128, 1], "c_ident": 0, "c_cmask": 0, "c_smask": 0},
    'dense': {"x": [S, D], "norm": [1, D], "w_gu": [D, 2 * DFF_D], "w_dn": [DFF_D, D], "c_ident": 0},
    'moe1': {"x": [S, D], "xacc": [S, D], "norm": [1, D], "router": [D, 8], "w_gu": [D, 2 * DFF_E], "w_dn": [DFF_E, D], "esel": [128, 8], "c_ident": 0},
    'fnorm': {"x": [S, D], "gain": [1, D], "c_ident": 0},
    'moes': {"x": [S, D], "norm": [1, D], "router": [D, 8], "w_gu": [8, D, 2 * DFF_E], "w_dn": [8, DFF_E, D], "c_ident": 0, "c_ltri": 0, "c_iota": 0},
}


def build_step(kind):
    nc = bass.Bass("TRN2", target_bir_lowering=False)
    inp = {}
    for n, shp in STEP_INPUTS[kind].items():
        if shp == 0:
            shp = CONST_SHAPES[n]
        inp[n] = nc.dram_tensor(n, shp, F32, kind="ExternalInput").ap()
    out = nc.dram_tensor("out", [S, D], F32, kind="ExternalOutput").ap()
    o_s = nc.dram_tensor("o_s", [S, D], BF16, kind="Internal").ap()
    with ExitStack() as st:
        P = Prog(nc, st)
        k = K()
        k.inp = inp
        k.ident = P.sb(st, [128, 128], BF16, 'ident')
        k.identf = P.sb(st, [128, 128], F32, 'identf')
        k.neghalf = P.sb(st, [128, 1], F32, 'neghalf')
        P.dma('pool', k.ident[:], inp['c_ident'], writes=['ident'], cast=True)
        P.dma('sp', k.identf[:], inp['c_ident'], writes=['identf'])
        P.memset('pool', k.neghalf[:], -0.5, writes=['neghalf'])
        P.flush()
        if kind in ('attn', 'hgrn'):
            pre = 'a_' if kind == 'attn' else 'b_'
            with ExitStack() as stm:
                hT = P.sb(stm, [128, 8, S], BF16, 'hT')
                kmT = P.sb(stm, [128, 2, MEM_LEN], BF16, 'kmT')
                vm = P.sb(stm, [128, 2, 4, 65], BF16, 'vm')
                phase_hT(P, k, inp['x'], inp[pre + 'norm_mix'][0:1, :], hT)
                phase_memkv(P, k, inp['mem'], inp[pre + 'mem_norm'][0:1, :], inp[pre + 'w_mem_kv'][0], kmT, vm)
                if kind == 'attn':
                    phase_diffattn(P, k, hT, 0, None, o_s)
                    phase_memattn(P, k, hT, inp['a_w_in'][0], 3 * DA_W, kmT, vm, o_s)
                else:
                    phase_hgrn(P, k, hT, 0, o_s)
                    phase_memattn(P, k, hT, inp['b_w_in'][0], 4 * HG_W, kmT, vm, o_s)
            phase_outproj(P, k, o_s, inp[pre + 'w_out'][0], inp['x'], out)
        elif kind == 'dense':
            phase_ffn(P, k, inp['x'], inp['x'], out, inp['norm'][0:1, :], [inp['w_gu']], [inp['w_dn']], DFF_D)
        elif kind == 'moe1':
            phase_ffn(P, k, inp['x'], inp['xacc'], out, inp['norm'][0:1, :], [inp['w_gu']], [inp['w_dn']], DFF_E,
                      router_ap=inp['router'], esel_ap=inp['esel'])
        elif kind == 'fnorm':
            phase_fnorm(P, k, inp['x'], inp['gain'][0:1, :], out)
        elif kind == 'moes':
            hbk = nc.dram_tensor("hbk", [S, D], BF16, kind="Internal").ap()
            phase_moe_sparse(P, k, inp['x'], out, inp['norm'][0:1, :], [inp['w_gu'][e] for e in range(NEXP)], [inp['w_dn'][e] for e in range(NEXP)], inp['router'], hbk)
        P.add('sp', lambda e: e.nop(), reads=[], writes=[])
        P.flush()
    return nc


def make_consts():
    slopes = 2.0 ** (-8.0 * np.arange(1, 7) / 6.0)
    import ml_dtypes
    bf = ml_dtypes.bfloat16

    def hi_lo(v):
        hi = np.float32(np.float32(v).astype(bf).astype(np.float32))
        lo = np.float32(np.float32(v - hi).astype(bf).astype(np.float32))
        return hi, lo
    c = {}
    c['c_ident'] = np.eye(128, dtype=np.float32)
    pos = np.arange(S)
    krel = (pos % 128).astype(np.float32)
    qrel = pos % 512
    qhi = (qrel & ~3).astype(np.float32)
    qlo = (qrel & 3).astype(np.float32)
    kaug = np.zeros((6, 6, S), np.float32)
    qaug = np.zeros((6, 6, S), np.float32)
    for h in range(6):
        hi, lo = hi_lo(slopes[h])
        kaug[h, 0] = krel
        kaug[h, 1] = krel
        kaug[h, 2] = hi
        kaug[h, 3] = lo
        kaug[h, 4] = hi
        kaug[h, 5] = lo
        qaug[h, 0] = hi
        qaug[h, 1] = lo
        qaug[h, 2] = -qhi
        qaug[h, 3] = -qhi
        qaug[h, 4] = -qlo
        qaug[h, 5] = -qlo
    c['c_kaug'] = kaug
    c['c_qaug'] = qaug
    kk = np.arange(128)[:, None]
    qq = np.arange(128)[None, :]
    dm = np.zeros((6, 128, 128), np.float32)
    for h in range(6):
        allowed = (kk // 64) <= (qq // 64)
        val = np.where(kk <= qq, 1.0, np.exp(-2.0 * slopes[h] * (kk - qq)))
        dm[h] = np.where(allowed, val, 0.0)
    c['c_dmask'] = dm
    cb = np.zeros((128, 6 * 35), np.float32)
    for h in range(6):
        for idx in range(35):
            d = idx - 3
            cb[:, h * 35 + idx] = -slopes[h] * 128.0 * d
    c['c_bias'] = cb
    s_ = np.arange(128)[:, None]
    t_ = np.arange(128)[None, :]
    c['c_cmask'] = ((s_ <= t_) & ((s_ // 64) == (t_ // 64))).astype(np.float32)
    sm = np.ones((128, 512), np.float32)
    sm[:, ::64] = 0.0
    c['c_smask'] = sm
    c['c_ltri'] = (s_ < t_).astype(np.float32)
    c['c_iota'] = np.broadcast_to(np.arange(128, dtype=np.float32)[None, :], (128, 128)).copy()
    return c


_CACHE = {}
_CONSTS = {}


def launch(kind, per_core, shared, n_cores):
    if kind not in _CACHE:
        _CACHE[kind] = build_step(kind)
    if not _CONSTS:
        _CONSTS.update(make_consts())
    nc = _CACHE[kind]
    sh = {}
    for n, shp in STEP_INPUTS[kind].items():
        if n in per_core:
            continue
        if shp == 0:
            sh[n] = _CONSTS[n]
        else:
            sh[n] = np.ascontiguousarray(np.asarray(shared[n], dtype=np.float32)).reshape(shp)
    in_maps = []
    for c in range(n_cores):
        m = dict(sh)
        for n, a in per_core.items():
            m[n] = np.ascontiguousarray(a[c])
        in_maps.append(m)
    import os
    tr = bool(os.environ.get('K_TRACE'))
    res = run_bass_kernel_spmd(nc, in_maps, core_ids=list(range(n_cores)), trace=tr)
    if tr:
        print('EXEC_NS', kind, res.exec_time_ns)
    return np.stack([np.asarray(r['out'], dtype=np.float32).reshape(S, D) for r in res.results], axis=0)


def run_step(i, which, x, inputs, n_cores):
    f = lambda n: np.asarray(inputs[n], dtype=np.float32)
    j = i // 2
    mem = f('mem')[:n_cores]
    if which == 'mix' and i % 2 == 0:
        lam_init = 0.8 - 0.6 * float(np.exp(-0.3 * i))
        clam = np.zeros((1, 128, 2), np.float32)
        clam[0, :, 0] = 1.0 - lam_init
        clam[0, :, 1] = -lam_init
        sh = {n: f(n)[j:j + 1] for n in ['a_norm_mix', 'a_w_in', 'a_lam_q1', 'a_lam_k1', 'a_lam_q2', 'a_lam_k2', 'a_subln', 'a_mem_norm', 'a_w_mem_kv', 'a_w_out']}
        sh['c_lam'] = clam
        return launch('attn', {'x': x, 'mem': mem}, sh, n_cores)
    if which == 'mix':
        sh = {n: f(n)[j:j + 1] for n in ['b_norm_mix', 'b_w_in', 'b_out_norm', 'b_mem_norm', 'b_w_mem_kv', 'b_w_out']}
        sh['b_lb_logits'] = f('b_lb_logits')
        sh['c_lbflag'] = np.full((1, 128, 1), -float(j), np.float32)
        return launch('hgrn', {'x': x, 'mem': mem}, sh, n_cores)
    if i % 2 == 0:
        sh = {'norm': f('dense_norm')[j:j + 1], 'w_gu': f('dense_w_gate_up')[j], 'w_dn': f('dense_w_down')[j]}
        return launch('dense', {'x': x}, sh, n_cores)
    xacc = x
    for e in range(NEXP):
        esel = np.zeros((128, 8), np.float32)
        esel[:, e] = 1.0
        sh = {'norm': f('moe_norm')[j:j + 1], 'router': f('moe_router')[j], 'w_gu': f('moe_w_gate_up')[j, e], 'w_dn': f('moe_w_down')[j, e], 'esel': esel}
        xacc = launch('moe1', {'x': x, 'xacc': xacc}, sh, n_cores)
    return xacc


FULL_SHAPES = {
    "x": [S, D], "mem": [MEM_LEN, D],
    "a_norm_mix": [2, D], "a_w_in": [2, D, 2560], "a_lam_q1": [2, 64], "a_lam_k1": [2, 64], "a_lam_q2": [2, 64], "a_lam_k2": [2, 64],
    "a_subln": [2, 128], "a_mem_norm": [2, D], "a_w_mem_kv": [2, D, 512], "a_w_out": [2, D, D],
    "b_norm_mix": [2, D], "b_w_in": [2, D, 3328], "b_lb_logits": [2, 768], "b_out_norm": [2, 128], "b_mem_norm": [2, D],
    "b_w_mem_kv": [2, D, 512], "b_w_out": [2, D, D],
    "dense_norm": [2, D], "dense_w_gate_up": [2, D, 2 * DFF_D], "dense_w_down": [2, DFF_D, D],
    "moe_norm": [2, D], "moe_router": [2, D, 8], "moe_w_gate_up": [2, 8, D, 2 * DFF_E], "moe_w_down": [2, 8, DFF_E, D],
    "final_norm": [1, D], "c_lam": [2, 128, 2], "c_lbflag": [2, 128, 1],
}
FULL_SHAPES.update(CONST_SHAPES)


def build_fused():
    nc = bass.Bass("TRN2", target_bir_lowering=False)
    inp = {n: nc.dram_tensor(n, shp, F32, kind="ExternalInput").ap() for n, shp in FULL_SHAPES.items()}
    out = nc.dram_tensor("out", [S, D], F32, kind="ExternalOutput").ap()
    xres = nc.dram_tensor("xres", [S, D], F32, kind="Internal").ap()
    o_s = nc.dram_tensor("o_s", [S, D], BF16, kind="Internal").ap()
    hbk = nc.dram_tensor("hbk", [S, D], BF16, kind="Internal").ap()
    with ExitStack() as st:
        P = Prog(nc, st)
        k = K()
        k.inp = inp
        k.ident = P.sb(st, [128, 128], BF16, 'ident')
        k.identf = P.sb(st, [128, 128], F32, 'identf')
        k.neghalf = P.sb(st, [128, 1], F32, 'neghalf')
        P.dma('pool', k.ident[:], inp['c_ident'], writes=['ident'], cast=True)
        P.dma('sp', k.identf[:], inp['c_ident'], writes=['identf'])
        P.memset('pool', k.neghalf[:], -0.5, writes=['neghalf'])
        P.flush()
        x_cur = inp['x']
        for i in range(DEPTH):
            j = i // 2
            pre = 'a_' if i % 2 == 0 else 'b_'
            with ExitStack() as stm:
                hT = P.sb(stm, [128, 8, S], BF16, 'hT')
                kmT = P.sb(stm, [128, 2, MEM_LEN], BF16, 'kmT')
                vm = P.sb(stm, [128, 2, 4, 65], BF16, 'vm')
                phase_hT(P, k, x_cur, inp[pre + 'norm_mix'][j:j + 1, :], hT)
                phase_memkv(P, k, inp['mem'], inp[pre + 'mem_norm'][j:j + 1, :], inp[pre + 'w_mem_kv'][j], kmT, vm)
                if i % 2 == 0:
                    phase_diffattn(P, k, hT, j, None, o_s)
                    phase_memattn(P, k, hT, inp['a_w_in'][j], 3 * DA_W, kmT, vm, o_s)
                else:
                    phase_hgrn(P, k, hT, j, o_s)
                    phase_memattn(P, k, hT, inp['b_w_in'][j], 4 * HG_W, kmT, vm, o_s)
            phase_outproj(P, k, o_s, inp[pre + 'w_out'][j], x_cur, xres)
            x_cur = xres
            if i % 2 == 0:
                phase_ffn(P, k, xres, xres, xres, inp['dense_norm'][j:j + 1, :], [inp['dense_w_gate_up'][j]], [inp['dense_w_down'][j]], DFF_D)
            else:
                phase_moe_sparse(P, k, xres, xres, inp['moe_norm'][j:j + 1, :],
                                 [inp['moe_w_gate_up'][j, e] for e in range(NEXP)], [inp['moe_w_down'][j, e] for e in range(NEXP)],
                                 inp['moe_router'][j], hbk)
        phase_fnorm(P, k, xres, inp['final_norm'][0:1, :], out)
        P.add('sp', lambda e: e.nop(), reads=[], writes=[])
        P.flush()
    return nc


def fused_consts():
    c = dict(make_consts())
    clam = np.zeros((2, 128, 2), np.float32)
    for j in range(2):
        lam_init = 0.8 - 0.6 * float(np.exp(-0.3 * (2 * j)))
        clam[j, :, 0] = 1.0 - lam_init
        clam[j, :, 1] = -lam_init
    c['c_lam'] = clam
    lbf = np.zeros((2, 128, 1), np.float32)
    lbf[1] = -1.0
    c['c_lbflag'] = lbf
    return c


def kernel_unfused(**inputs):
    n_cores = 8
    x = np.asarray(inputs['x'], dtype=np.float32)
    for i in range(DEPTH):
        x = run_step(i, 'mix', x, inputs, n_cores)
        x = run_step(i, 'ffn', x, inputs, n_cores)
    return launch('fnorm', {'x': x}, {'gain': np.asarray(inputs['final_norm'], dtype=np.float32).reshape(1, D)}, n_cores)


def kernel(**inputs):
    n_cores = 8
    if 'fused' not in _CACHE:
        _CACHE['fused'] = build_fused()
    nc = _CACHE['fused']
    consts = fused_consts()
    shared = {}
    for n, shp in FULL_SHAPES.items():
        if n in ('x', 'mem'):
            continue
        if n in consts:
            shared[n] = consts[n]
        else:
            shared[n] = np.ascontiguousarray(np.asarray(inputs[n], dtype=np.float32)).reshape(shp)
    x = np.asarray(inputs['x'], dtype=np.float32)
    mem = np.asarray(inputs['mem'], dtype=np.float32)
    in_maps = []
    for c in range(n_cores):
        m = dict(shared)
        m['x'] = np.ascontiguousarray(x[c])
        m['mem'] = np.ascontiguousarray(mem[c])
        in_maps.append(m)
    res = run_bass_kernel_spmd(nc, in_maps, core_ids=list(range(n_cores)))
    return np.stack([np.asarray(r['out'], dtype=np.float32).reshape(S, D) for r in res.results], axis=0)
```

```python
import numpy as np
import concourse.bass as bass
import concourse.mybir as mybir
from concourse.bass_utils import run_bass_kernel_spmd
from contextlib import ExitStack

F32 = mybir.dt.float32
BF16 = mybir.dt.bfloat16
ALU = mybir.AluOpType
AF = mybir.ActivationFunctionType
AX = mybir.AxisListType

S = 4096
D = 1024
NT = 32
EPS = 1e-6
DEPTH = 4
DA_W = 768
HG_W = 768
MEM_W = 256
DFF_D = 2816
DFF_E = 3584
NEXP = 8
MEM_LEN = 256

ENGS = ['pe', 'act', 'dve', 'pool', 'sp']
DMA_POOL = {'sp': 24, 'pool': 24, 'act': 4}


class Op:
    __slots__ = ('eng', 'fn', 'deps', 'need', 'sig', 'is_dma', 'dsem', 'dval', 'uid')


class Prog:
    def __init__(self, nc, stack, strict=True):
        self.nc = nc
        self.stack = stack
        self.strict = strict
        self.ops = {e: [] for e in ENGS}
        self.lastw = {}
        self.reads = {}
        self.uid = 0
        self.esem = {e: stack.enter_context(nc.semaphore('s_' + e)) for e in ENGS}
        self.sigc = {e: 0 for e in ENGS}
        self.dsems = {}
        self.dcur = {}
        self.dlast = {}
        self.dfence = {}
        for q, n in DMA_POOL.items():
            self.dsems[q] = [stack.enter_context(nc.semaphore('d_%s%d' % (q, i))) for i in range(n)]
            self.dcur[q] = 0
            self.dlast[q] = [None] * n
            self.dfence[q] = [0] * n
        self.efence = {e: 0 for e in ENGS}
        self.ntile = 0
        self.nflush = 0

    def sb(self, st, shape, dtype, name=None):
        self.ntile += 1
        name = (name or 't') + '_%d' % self.ntile
        return st.enter_context(self.nc.sbuf_tensor(name, list(shape), dtype))

    def ps(self, st, shape, dtype=F32, name=None):
        self.ntile += 1
        name = (name or 'p') + '_%d' % self.ntile
        return st.enter_context(self.nc.psum_tensor(name, list(shape), dtype))

    def add(self, eng, fn, reads=(), writes=(), dma=False):
        op = Op()
        op.eng = eng
        op.fn = fn
        op.need = False
        op.sig = None
        op.is_dma = dma
        op.uid = self.uid
        self.uid += 1
        deps = []
        for k in reads:
            w = self.lastw.get(k)
            if w is not None:
                deps.append((w, 'raw'))
        for k in writes:
            w = self.lastw.get(k)
            if w is not None:
                deps.append((w, 'waw'))
            rd = self.reads.get(k)
            if rd:
                for r in rd.values():
                    deps.append((r, 'war'))
        fdeps = []
        seen = set()
        for d, kind in deps:
            if d.uid in seen:
                continue
            if (not d.is_dma) and (not dma) and d.eng == eng:
                if eng == 'pe' or not self.strict:
                    continue
            seen.add(d.uid)
            fdeps.append(d)
        if dma:
            q = eng
            i = self.dcur[q]
            self.dcur[q] = (i + 1) % len(self.dsems[q])
            prev = self.dlast[q][i]
            op.dsem = self.dsems[q][i]
            op.dval = (prev.dval if prev is not None else 0) + 16
            if prev is not None and prev.uid not in seen and prev.dval > self.dfence[q][i]:
                fdeps.append(prev)
                seen.add(prev.uid)
            self.dlast[q][i] = op
        for d in fdeps:
            d.need = True
        op.deps = fdeps
        rk = ('dma', op.uid) if dma else eng
        for k in writes:
            self.lastw[k] = op
            self.reads[k] = {}
        for k in reads:
            self.reads.setdefault(k, {})[rk] = op
        self.ops[eng].append(op)
        return op

    def flush(self):
        for e in ENGS:
            comp = [op for op in self.ops[e] if not op.is_dma and op.fn is not None]
            if comp:
                comp[-1].need = True
            c = self.sigc[e]
            for op in self.ops[e]:
                if op.need and not op.is_dma:
                    c += 1
                    op.sig = c
            self.sigc[e] = c
        efence = dict(self.efence)
        dfence = {q: list(v) for q, v in self.dfence.items()}

        def run(e, engobj):
            waited = {}
            for e2 in ENGS:
                if efence[e2] > 0:
                    engobj.wait_ge(self.esem[e2], efence[e2])
                    waited[id(self.esem[e2])] = efence[e2]
            for q in dfence:
                for i, v in enumerate(dfence[q]):
                    if v > 0:
                        engobj.wait_ge(self.dsems[q][i], v)
                        waited[id(self.dsems[q][i])] = v
            for op in self.ops[e]:
                for d in op.deps:
                    if d.is_dma:
                        sem, val = d.dsem, d.dval
                    else:
                        sem, val = self.esem[d.eng], d.sig
                    key = id(sem)
                    if waited.get(key, 0) < val:
                        engobj.wait_ge(sem, val)
                        waited[key] = val
                ins = op.fn(engobj)
                if op.is_dma:
                    ins.then_inc(op.dsem, 16)
                elif op.need:
                    ins.then_inc(self.esem[e], 1)

        with self.nc.Block() as block:
            @block.tensor
            def _(t):
                run('pe', t)

            @block.scalar
            def _(t):
                run('act', t)

            @block.vector
            def _(t):
                run('dve', t)

            @block.gpsimd
            def _(t):
                run('pool', t)

            @block.sync
            def _(t):
                run('sp', t)

        for e in ENGS:
            self.efence[e] = self.sigc[e]
            self.ops[e] = []
        for q in self.dlast:
            for i, op in enumerate(self.dlast[q]):
                if op is not None:
                    self.dfence[q][i] = op.dval
        self.lastw = {}
        self.reads = {}
        self.nflush += 1

    def flush_branch(self, flag_ap, recA, recB):
        assert all(len(self.ops[e]) == 0 for e in ENGS)
        st0 = (dict(self.sigc), {q: list(v) for q, v in self.dlast.items()}, dict(self.dcur))

        def record(rec):
            self.sigc = dict(st0[0])
            self.dlast = {q: list(v) for q, v in st0[1].items()}
            self.dcur = dict(st0[2])
            self.lastw = {}
            self.reads = {}
            rec()
            for e in ENGS:
                comp = [op for op in self.ops[e] if not op.is_dma]
                if comp:
                    comp[-1].need = True
                c = self.sigc[e]
                for op in self.ops[e]:
                    if op.need and not op.is_dma:
                        c += 1
                        op.sig = c
                self.sigc[e] = c
            ops = self.ops
            self.ops = {e: [] for e in ENGS}
            dv = {q: [(o.dval if o is not None else 0) for o in self.dlast[q]] for q in self.dlast}
            return ops, dict(self.sigc), dv
        opsA, sigA, dvA = record(recA)
        opsB, sigB, dvB = record(recB)
        fin = {e: max(sigA[e], sigB[e]) for e in ENGS}
        dfin = {q: [max(a, b) for a, b in zip(dvA[q], dvB[q])] for q in dvA}
        efence = dict(self.efence)
        dfence = {q: list(v) for q, v in self.dfence.items()}

        def run_ops(e, engobj, ops, waited):
            for op in ops[e]:
                for d in op.deps:
                    if d.is_dma:
                        sem, val = d.dsem, d.dval
                    else:
                        sem, val = self.esem[d.eng], d.sig
                    key = id(sem)
                    if waited.get(key, 0) < val:
                        engobj.wait_ge(sem, val)
                        waited[key] = val
                ins = op.fn(engobj)
                if op.is_dma:
                    ins.then_inc(op.dsem, 16)
                elif op.need:
                    ins.then_inc(self.esem[e], 1)

        def catchup(e, engobj, sig, dv):
            if sig[e] > 0:
                engobj.wait_ge(self.esem[e], sig[e])
            if fin[e] > sig[e]:
                engobj.sem_inc(self.esem[e], fin[e] - sig[e])
            if e in dv:
                for i, v in enumerate(dv[e]):
                    if dfin[e][i] > v:
                        if v > 0:
                            engobj.wait_ge(self.dsems[e][i], v)
                        engobj.sem_inc(self.dsems[e][i], dfin[e][i] - v)

        def run(e, engobj):
            waited = {}
            for e2 in ENGS:
                if efence[e2] > 0:
                    engobj.wait_ge(self.esem[e2], efence[e2])
                    waited[id(self.esem[e2])] = efence[e2]
            for q in dfence:
                for i, v in enumerate(dfence[q]):
                    if v > 0:
                        engobj.wait_ge(self.dsems[q][i], v)
                        waited[id(self.dsems[q][i])] = v
            reg = engobj.alloc_register('flag_' + e + '_%d' % self.nflush)
            engobj.reg_load(reg, flag_ap)
            with engobj.If_eq(reg, 0):
                run_ops(e, engobj, opsA, dict(waited))
                catchup(e, engobj, sigA, dvA)
            with engobj.Else():
                run_ops(e, engobj, opsB, dict(waited))
                catchup(e, engobj, sigB, dvB)

        with self.nc.Block() as block:
            @block.tensor
            def _(t):
                run('pe', t)

            @block.scalar
            def _(t):
                run('act', t)

            @block.vector
            def _(t):
                run('dve', t)

            @block.gpsimd
            def _(t):
                run('pool', t)

            @block.sync
            def _(t):
                run('sp', t)

        self.sigc = dict(fin)
        for e in ENGS:
            self.efence[e] = fin[e]
        for q in dfin:
            for i, v in enumerate(dfin[q]):
                if v > 0:
                    o = Op()
                    o.dval = v
                    o.dsem = self.dsems[q][i]
                    o.is_dma = True
                    o.uid = -1
                    self.dlast[q][i] = o
                else:
                    self.dlast[q][i] = None
                self.dfence[q][i] = v
            self.dcur[q] = 0
        self.lastw = {}
        self.reads = {}
        self.nflush += 1

    def dma(self, q, out, in_, reads=(), writes=(), cast=False, slow=False):
        if slow:
            fn = lambda e, o=out, i=in_: e.dma_start(out=o, in_=i, allow_slow_non_contiguous=True)
        elif cast:
            fn = lambda e, o=out, i=in_: e.dma_start(out=o, in_=i, max_dma_last_dim=4096)
        else:
            fn = lambda e, o=out, i=in_: e.dma_start(out=o, in_=i)
        return self.add(q, fn, reads, writes, dma=True)

    def mmg(self, mms, reads, writes):
        mms = list(mms)

        def fn(e, mms=mms):
            ins = None
            for (o, l, r, s0, s1) in mms:
                ins = e.matmul(o, l, r, start=s0, stop=s1)
            return ins
        return self.add('pe', fn, reads, writes)

    def trg(self, trs, reads, writes):
        trs = list(trs)

        def fn(e, trs=trs):
            ins = None
            for (o, i, idn) in trs:
                ins = e.transpose(o, i, idn)
            return ins
        return self.add('pe', fn, reads, writes)

    def act(self, out, in_, func, reads, writes, bias=None, scale=None, accum=None):
        kw = {}
        if bias is not None:
            kw['bias'] = bias
        if scale is not None:
            kw['scale'] = scale
        if accum is not None:
            kw['accum_out'] = accum
        return self.add('act', lambda e, o=out, i=in_, f=func, kw=kw: e.activation(out=o, in_=i, func=f, **kw), reads, writes)

    def tt(self, eng, out, in0, in1, op, reads, writes):
        return self.add(eng, lambda e, o=out, a=in0, b=in1, p=op: e.tensor_tensor(out=o, in0=a, in1=b, op=p), reads, writes)

    def ts(self, eng, out, in0, s1, s2, op0, op1, reads, writes):
        if op1 is None:
            return self.add(eng, lambda e, o=out, a=in0, s1=s1, p0=op0: e.tensor_scalar(out=o, in0=a, scalar1=s1, scalar2=None, op0=p0), reads, writes)
        return self.add(eng, lambda e, o=out, a=in0, s1=s1, s2=s2, p0=op0, p1=op1: e.tensor_scalar(out=o, in0=a, scalar1=s1, scalar2=s2, op0=p0, op1=p1), reads, writes)

    def stt(self, out, in0, scalar, in1, op0, op1, reads, writes):
        return self.add('dve', lambda e, o=out, a=in0, s=scalar, b=in1, p0=op0, p1=op1: e.scalar_tensor_tensor(out=o, in0=a, scalar=s, in1=b, op0=p0, op1=p1), reads, writes)

    def copy(self, eng, out, in_, reads, writes):
        if eng == 'act':
            return self.add('act', lambda e, o=out, i=in_: e.copy(out=o, in_=i), reads, writes)
        return self.add(eng, lambda e, o=out, i=in_: e.tensor_copy(out=o, in_=i), reads, writes)

    def memset(self, eng, ap, val, writes):
        return self.add(eng, lambda e, a=ap, v=val: e.memset(a, v), (), writes)

    def recip(self, out, in_, reads, writes):
        return self.add('dve', lambda e, o=out, i=in_: e.reciprocal(out=o, in_=i), reads, writes)

    def reduce(self, out, in_, op, reads, writes):
        return self.add('dve', lambda e, o=out, i=in_, p=op: e.tensor_reduce(out=o, in_=i, axis=AX.X, op=p), reads, writes)

    def scan(self, out, d0, d1, reads, writes):
        return self.add('dve', lambda e, o=out, a=d0, b=d1: e.tensor_tensor_scan(out=o, data0=a, data1=b, initial=0.0, op0=ALU.mult, op1=ALU.add), reads, writes)


class K:
    pass


def wview(ap2d):
    return ap2d.rearrange("(c p) n -> p c n", p=128)


def norm_rows(P, k, st, x_ap, gbc, hb_out, tag, want_f32=None):
    t = k.nt[tag]
    P.act(t['junk'][:], x_ap, AF.Square, reads=[tag + 'x'], writes=[tag + 'junk', tag + 'ssq'], accum=t['ssq'][:])
    P.ts('dve', t['ms'][:], t['ssq'][:], 1.0 / D, EPS, ALU.mult, ALU.add, reads=[tag + 'ssq'], writes=[tag + 'ms'])
    P.tt('pool', t['rstd'][:], t['ms'][:], k.neghalf[:], ALU.pow, reads=[tag + 'ms'], writes=[tag + 'rstd'])
    if want_f32 is not None:
        P.stt(want_f32, x_ap, t['rstd'][:, 0:1], gbc, ALU.mult, ALU.mult, reads=[tag + 'x', tag + 'rstd', 'gbc'], writes=[tag + 'hf'])
        P.copy('act', hb_out, want_f32, reads=[tag + 'hf'], writes=[tag + 'hb'])
    else:
        P.stt(hb_out, x_ap, t['rstd'][:, 0:1], gbc, ALU.mult, ALU.mult, reads=[tag + 'x', tag + 'rstd', 'gbc'], writes=[tag + 'hb'])


def alloc_norm_tmps(P, k, st, tags):
    k.nt = {}
    for tag in tags:
        k.nt[tag] = {
            'junk': P.sb(st, [128, D], F32, 'junk'),
            'ssq': P.sb(st, [128, 1], F32, 'ssq'),
            'ms': P.sb(st, [128, 1], F32, 'ms'),
            'rstd': P.sb(st, [128, 1], F32, 'rstd'),
        }


def phase_hT(P, k, x_src, gain_row, hT):
    with ExitStack() as st:
        gbc = P.sb(st, [128, D], F32, 'gbc')
        xt = [P.sb(st, [128, D], F32, 'xt') for _ in range(3)]
        hb = [P.sb(st, [128, D], BF16, 'hb') for _ in range(3)]
        pt = [P.ps(st, [128, 8, 128], BF16, 'pt') for _ in range(3)]
        alloc_norm_tmps(P, k, st, ['n0', 'n1', 'n2'])
        P.dma('sp', gbc[:], gain_row.partition_broadcast(128), writes=['gbc'])
        for tt in range(NT):
            b = tt % 3
            tag = 'n%d' % b
            P.dma('sp' if tt % 2 else 'pool', xt[b][:], x_src[tt * 128:(tt + 1) * 128, :], writes=[tag + 'x'])
            norm_rows(P, k, st, xt[b][:], gbc[:], hb[b][:], tag)
            P.trg([(pt[b][:, c, :], hb[b][:, c * 128:(c + 1) * 128], k.ident[:]) for c in range(8)],
                  reads=[tag + 'hb'], writes=[tag + 'pt'])
            P.copy('act' if tt % 2 else 'dve', hT[:, :, tt * 128:(tt + 1) * 128], pt[b][:], reads=[tag + 'pt'], writes=[('hT', tt)])
        P.flush()


def phase_memkv(P, k, mem_ap, gain_row, wkv_ap, kmT, vm):
    with ExitStack() as st:
        gbc = P.sb(st, [128, D], F32, 'gbc')
        xt = [P.sb(st, [128, D], F32, 'xt') for _ in range(2)]
        hb = [P.sb(st, [128, D], BF16, 'hb') for _ in range(2)]
        pt = [P.ps(st, [128, 8, 128], BF16, 'pt') for _ in range(2)]
        memT = P.sb(st, [128, 8, MEM_LEN], BF16, 'memT')
        wkv = P.sb(st, [128, 8, 512], BF16, 'wkv')
        pk = P.ps(st, [128, 256], F32, 'pk')
        alloc_norm_tmps(P, k, st, ['n0', 'n1'])
        P.dma('sp', gbc[:], gain_row.partition_broadcast(128), writes=['gbc'])
        P.dma('pool', wkv[:], wview(wkv_ap), writes=['wkv'], cast=True)
        P.memset('pool', vm[:], 1.0, writes=['vm'])
        for mt in range(2):
            tag = 'n%d' % mt
            P.dma('sp', xt[mt][:], mem_ap[mt * 128:(mt + 1) * 128, :], writes=[tag + 'x'])
            norm_rows(P, k, st, xt[mt][:], gbc[:], hb[mt][:], tag)
            P.trg([(pt[mt][:, c, :], hb[mt][:, c * 128:(c + 1) * 128], k.ident[:]) for c in range(8)],
                  reads=[tag + 'hb'], writes=[tag + 'pt'])
            P.copy('dve', memT[:, :, mt * 128:(mt + 1) * 128], pt[mt][:], reads=[tag + 'pt'], writes=[('memT', mt)])
        for p in range(2):
            P.mmg([(pk[:], wkv[:, dc, p * 128:(p + 1) * 128], memT[:, dc, :], dc == 0, dc == 7) for dc in range(8)],
                  reads=['wkv', ('memT', 0), ('memT', 1)], writes=['pk'])
            P.copy('dve', kmT[:, p, :], pk[:], reads=['pk'], writes=[('kmT', p)])
        for mt in range(2):
            P.mmg([(pk[:], memT[:, dc, mt * 128:(mt + 1) * 128], wkv[:, dc, 256:512], dc == 0, dc == 7) for dc in range(8)],
                  reads=['wkv', ('memT', 0), ('memT', 1)], writes=['pk'])
            P.copy('dve', vm[:, mt, :, 0:64], pk[:].rearrange("p (h d) -> p h d", h=4), reads=['pk', 'vm'], writes=[('vm', mt)])
        P.flush()


def phase_memattn(P, k, hT, w_in_ap, col0, kmT, vm, o_s):
    with ExitStack() as st:
        wqm = P.sb(st, [128, 8, 256], BF16, 'wqm')
        qmT = P.sb(st, [128, 2, S], BF16, 'qmT')
        pq = [P.ps(st, [128, 512], F32, 'pq') for _ in range(2)]
        sc = [P.ps(st, [128, 512], F32, 'sc') for _ in range(2)]
        pacc = [P.ps(st, [128, 512], F32, 'pacc') for _ in range(4)]
        pT = [P.sb(st, [128, 512], BF16, 'pT') for _ in range(2)]
        rr = [P.sb(st, [128, 1], F32, 'rr') for _ in range(4)]
        omb = [P.sb(st, [128, 4, 256], BF16, 'omb') for _ in range(2)]
        P.dma('pool', wqm[:], wview(w_in_ap)[:, :, col0:col0 + 256], writes=['wqm'], cast=True)
        for tb in range(8):
            for p in range(2):
                b = (tb * 2 + p) % 2
                P.mmg([(pq[b][:], wqm[:, dc, p * 128:(p + 1) * 128], hT[:, dc, tb * 512:(tb + 1) * 512], dc == 0, dc == 7) for dc in range(8)],
                      reads=['wqm'], writes=[('pq', b)])
                P.act(qmT[:, p, tb * 512:(tb + 1) * 512], pq[b][:], AF.Identity, reads=[('pq', b)], writes=[('qmT', tb, p)], scale=0.125)
        cnt = 0
        for tb in range(8):
            ob = omb[tb % 2]
            for hm in range(4):
                p = hm // 2
                base = (hm % 2) * 64
                for mt in range(2):
                    b = cnt % 2
                    cnt += 1
                    P.mmg([(sc[b][:], kmT[base:base + 64, p, mt * 128:(mt + 1) * 128], qmT[base:base + 64, p, tb * 512:(tb + 1) * 512], True, True)],
                          reads=[('qmT', tb, p)], writes=[('sc', b)])
                    P.act(pT[b][:], sc[b][:], AF.Exp, reads=[('sc', b)], writes=[('pT', b)])
                    P.mmg([(pacc[qs][:, 0:65], pT[b][:, qs * 128:(qs + 1) * 128], vm[:, mt, hm, :], mt == 0, mt == 1) for qs in range(4)],
                          reads=[('pT', b)], writes=[('pacc', qs) for qs in range(4)])
                for qs in range(4):
                    P.recip(rr[qs][:], pacc[qs][:, 64:65], reads=[('pacc', qs)], writes=[('rr', qs)])
                    P.act(ob[:, qs, hm * 64:(hm + 1) * 64], pacc[qs][:, 0:64], AF.Identity, reads=[('pacc', qs), ('rr', qs)],
                          writes=[('omb', tb % 2, qs)], scale=rr[qs][:, 0:1])
            for qs in range(4):
                tt = tb * 4 + qs
                P.dma('sp', o_s[tt * 128:(tt + 1) * 128, 768:1024], ob[:, qs, :], reads=[('omb', tb % 2, qs)], writes=[('o_s', tt, 'm')])
        P.flush()


def phase_outproj(P, k, o_s, w_out_ap, x_src, x_dst):
    NB = 3
    with ExitStack() as st:
        wo = P.sb(st, [128, 8, D], BF16, 'wo')
        ot = [P.sb(st, [128, D], BF16, 'ot') for _ in range(NB)]
        oT = [P.sb(st, [128, 8, 128], BF16, 'oT') for _ in range(NB)]
        xt = [P.sb(st, [128, D], F32, 'xt') for _ in range(NB)]
        xn = [P.sb(st, [128, D], F32, 'xn') for _ in range(NB)]
        pt = [P.ps(st, [128, 8, 128], BF16, 'pt') for _ in range(2)]
        po = [P.ps(st, [128, 512], F32, 'po') for _ in range(4)]
        P.dma('pool', wo[:], wview(w_out_ap), writes=['wo'], cast=True)

        def stage_a(tt):
            b = tt % NB
            pb2 = tt % 2
            P.dma('sp', ot[b][:], o_s[tt * 128:(tt + 1) * 128, :], writes=[('ot', b)])
            P.dma('pool', xt[b][:], x_src[tt * 128:(tt + 1) * 128, :], writes=[('xt', b)])
            P.trg([(pt[pb2][:, c, :], ot[b][:, c * 128:(c + 1) * 128], k.ident[:]) for c in range(8)],
                  reads=[('ot', b)], writes=[('pt', pb2)])
            P.copy('act', oT[b][:], pt[pb2][:], reads=[('pt', pb2)], writes=[('oT', b)])

        def stage_b(tt):
            b = tt % NB
            pb2 = tt % 2
            for hh in range(2):
                pb = pb2 * 2 + hh
                P.mmg([(po[pb][:], oT[b][:, fc, :], wo[:, fc, hh * 512:(hh + 1) * 512], fc == 0, fc == 7) for fc in range(8)],
                      reads=[('oT', b), 'wo'], writes=[('po', pb)])
                P.tt('dve', xn[b][:, hh * 512:(hh + 1) * 512], po[pb][:], xt[b][:, hh * 512:(hh + 1) * 512], ALU.add,
                     reads=[('po', pb), ('xt', b)], writes=[('xn', b, hh)])
            P.dma('sp', x_dst[tt * 128:(tt + 1) * 128, :], xn[b][:], reads=[('xn', b, 0), ('xn', b, 1)], writes=[('xd', tt)])

        stage_a(0)
        for tt in range(NT):
            if tt + 1 < NT:
                stage_a(tt + 1)
            stage_b(tt)
        P.flush()


def phase_diffattn(P, k, hT, j, lam_init, o_s):
    inp = k.inp
    w_in = wview(inp['a_w_in'][j])
    with ExitStack() as st:
        wqkv = [P.sb(st, [128, 8, 384], BF16, 'wqkv') for _ in range(2)]
        qT = [P.sb(st, [128, 2, S], BF16, 'qT') for _ in range(2)]
        kT = [P.sb(st, [128, 2, S], BF16, 'kT') for _ in range(2)]
        va = [P.sb(st, [128, NT, 129], BF16, 'va') for _ in range(2)]
        dmask = P.sb(st, [128, 6, 128], BF16, 'dmask')
        cbias = P.sb(st, [128, 6 * 35], F32, 'cbias')
        gsub = P.sb(st, [128, 128], F32, 'gsub')
        lv = [P.sb(st, [128, 64], F32, 'lv') for _ in range(4)]
        lt = P.sb(st, [128, 64], F32, 'lt')
        ls = [P.sb(st, [128, 1], F32, 'ls') for _ in range(2)]
        neglam = P.sb(st, [128, 1], F32, 'neglam')
        clam = P.sb(st, [128, 2], F32, 'clam')
        pT = [P.sb(st, [128, 512], BF16, 'pT') for _ in range(3)]
        o0 = P.sb(st, [128, 4, 128], F32, 'o0')
        oo = [P.sb(st, [128, 128], F32, 'oo') for _ in range(2)]
        ob = [P.sb(st, [128, 128], BF16, 'ob') for _ in range(2)]
        junk = P.sb(st, [128, 128], F32, 'junk')
        sm = {n: [P.sb(st, [128, 1], F32, n) for _ in range(2)] for n in ('r0', 'r1', 'ssq', 'ms', 'lnm', 'rstd')}
        pp = [P.ps(st, [128, 512], F32, 'pp') for _ in range(2)]
        sc = [P.ps(st, [128, 512], F32, 'sc') for _ in range(2)]
        pacc = [P.ps(st, [128, 512], F32, 'pacc') for _ in range(4)]

        P.dma('pool', dmask[:], inp['c_dmask'].rearrange("h k q -> k h q"), writes=['dmask'], cast=True)
        P.dma('sp', cbias[:], inp['c_bias'], writes=['cbias'])
        P.dma('sp', gsub[:], inp['a_subln'][j:j + 1, :].partition_broadcast(128), writes=['gsub'])
        P.dma('sp', clam[:], inp['c_lam'][j], writes=['clam'])
        P.ts('dve', gsub[:], gsub[:], clam[:, 0:1], None, ALU.mult, None, reads=['gsub', 'clam'], writes=['gsub'])
        for i, nm in enumerate(['a_lam_q1', 'a_lam_k1', 'a_lam_q2', 'a_lam_k2']):
            P.dma('sp', lv[i][:], inp[nm][j:j + 1, :].partition_broadcast(128), writes=[('lv', i)])
        for i in range(2):
            P.tt('dve', lt[:], lv[2 * i][:], lv[2 * i + 1][:], ALU.mult, reads=[('lv', 2 * i), ('lv', 2 * i + 1)], writes=['lt'])
            P.reduce(ls[i][:], lt[:], ALU.add, reads=['lt'], writes=[('ls', i)])
            P.act(ls[i][:], ls[i][:], AF.Exp, reads=[('ls', i)], writes=[('ls', i)])
        P.tt('dve', neglam[:], ls[1][:], ls[0][:], ALU.subtract, reads=[('ls', 0), ('ls', 1)], writes=['neglam'])
        P.ts('dve', neglam[:], neglam[:], clam[:, 1:2], None, ALU.add, None, reads=['neglam', 'clam'], writes=['neglam'])
        for b in range(2):
            P.memset('pool', va[b][:, :, 128:129], 1.0, writes=[('va1', b)])

        scb = [sc[0], sc[1], pp[1]]
        pT4 = pT + [P.sb(st, [128, 512], BF16, 'pT')]

        def project_units(h):
            b = h % 2
            W = wqkv[b]
            units = []

            def u0():
                for i, c0 in enumerate([h * 128, DA_W + h * 128, 2 * DA_W + h * 128]):
                    P.dma('pool', W[:, :, i * 128:(i + 1) * 128], w_in[:, :, c0:c0 + 128], writes=[('w', b, i)], cast=True)
                for m in range(2):
                    P.dma('pool', qT[b][64:70, m, :], inp['c_qaug'][h], writes=[('qa', b, m)], cast=True)
                    P.dma('pool', kT[b][64:70, m, :], inp['c_kaug'][h], writes=[('ka', b, m)], cast=True)
            units.append(u0)
            for tb in range(8):
                for m in range(2):
                    for isk in range(2):
                        def u(tb=tb, m=m, isk=isk):
                            c0 = isk * 128 + m * 64
                            P.mmg([(pp[0][0:64, :], W[:, dc, c0:c0 + 64], hT[:, dc, tb * 512:(tb + 1) * 512], dc == 0, dc == 7) for dc in range(8)],
                                  reads=[('w', b, isk)], writes=[('pp', 0)])
                            if isk == 0:
                                P.ts('dve', qT[b][0:64, m, tb * 512:(tb + 1) * 512], pp[0][0:64, :], 0.125, None, ALU.mult, None,
                                     reads=[('pp', 0)], writes=[('q', b, m, tb)])
                            else:
                                P.copy('dve', kT[b][0:64, m, tb * 512:(tb + 1) * 512], pp[0][0:64, :], reads=[('pp', 0)], writes=[('k', b, m, tb)])
                        units.append(u)
                for tq in range(4):
                    def uv(tt=tb * 4 + tq):
                        P.mmg([(pp[0][:, 0:128], hT[:, dc, tt * 128:(tt + 1) * 128], W[:, dc, 256:384], dc == 0, dc == 7) for dc in range(8)],
                              reads=[('w', b, 2)], writes=[('pp', 0)])
                        P.copy('dve', va[b][:, tt, 0:128], pp[0][:, 0:128], reads=[('pp', 0), ('va1', b)], writes=[('v', b, tt)])
                    units.append(uv)
            return units

        def attend(h, nxt):
            b = h % 2
            blocks = [(jq, m, kt) for jq in range(8) for m in range(2) for kt in range(4 * jq + 4)]
            nb = len(blocks)
            evc = [0]

            def geom(i):
                jq, m, kt = blocks[i]
                r = kt - 4 * jq
                off = max(r, 0) * 128
                return jq, m, kt, r, off, 512 - off

            def emit_qk(i):
                jq, m, kt, r, off, N = geom(i)
                sb_ = i % 3
                P.mmg([(scb[sb_][:, 0:N], kT[b][0:70, m, kt * 128:(kt + 1) * 128], qT[b][0:70, m, jq * 512 + off:(jq + 1) * 512], True, True)],
                      reads=[('q', b, m, jq), ('qa', b, m), ('ka', b, m), ('k', b, m, kt // 4)], writes=[('sc', sb_)])

            def emit_exp(i):
                jq, m, kt, r, off, N = geom(i)
                sb_ = i % 3
                pb = i % 4
                bi = h * 35 + (4 * jq - kt + 3)
                P.act(pT4[pb][:, off:512], scb[sb_][:, 0:N], AF.Exp, reads=[('sc', sb_), 'cbias'], writes=[('pT', pb)], bias=cbias[:, bi:bi + 1])
                if r >= 0:
                    P.tt('dve', pT4[pb][:, off:off + 128], pT4[pb][:, off:off + 128], dmask[:, h, :], ALU.mult,
                         reads=[('pT', pb), 'dmask'], writes=[('pT', pb)])

            def emit_pv(i):
                jq, m, kt, r, off, N = geom(i)
                pb = i % 4
                qs0 = max(r, 0)
                P.mmg([(pacc[qs][:, 0:129], pT4[pb][:, qs * 128:(qs + 1) * 128], va[b][:, kt, :], kt == 0, kt == 4 * jq + qs) for qs in range(qs0, 4)],
                      reads=[('pT', pb), ('v', b, kt), ('va1', b)], writes=[('pacc', qs) for qs in range(qs0, 4)])
                if kt == 4 * jq + 3:
                    for qs in range(4):
                        e = evc[0] % 2
                        evc[0] += 1
                        if m == 0:
                            P.recip(sm['r0'][e][:], pacc[qs][:, 128:129], reads=[('pacc', qs)], writes=[('r0', e)])
                            P.ts('dve', o0[:, qs, :], pacc[qs][:, 0:128], sm['r0'][e][:, 0:1], None, ALU.mult, None,
                                 reads=[('pacc', qs), ('r0', e)], writes=[('o0', qs)])
                        else:
                            P.recip(sm['r1'][e][:], pacc[qs][:, 128:129], reads=[('pacc', qs)], writes=[('r1', e)])
                            P.tt('dve', sm['r1'][e][:], sm['r1'][e][:], neglam[:], ALU.mult, reads=[('r1', e), 'neglam'], writes=[('r1', e)])
                            P.stt(oo[e][:], pacc[qs][:, 0:128], sm['r1'][e][:, 0:1], o0[:, qs, :], ALU.mult, ALU.add,
                                  reads=[('pacc', qs), ('r1', e), ('o0', qs)], writes=[('oo', e)])
                            P.add('dve', lambda eng, o=junk[:], a=oo[e][:], acc=sm['ssq'][e][:]: eng.scalar_tensor_tensor(
                                out=o, in0=a, scalar=1.0, in1=a, op0=ALU.mult, op1=ALU.mult, accum_out=acc),
                                reads=[('oo', e)], writes=['junk', ('ssq', e)])
                            P.ts('dve', sm['ms'][e][:], sm['ssq'][e][:], 1.0 / 128, EPS, ALU.mult, ALU.add, reads=[('ssq', e)], writes=[('ms', e)])
                            P.tt('pool', sm['rstd'][e][:], sm['ms'][e][:], k.neghalf[:], ALU.pow, reads=[('ms', e)], writes=[('rstd', e)])
                            P.stt(ob[e][:], oo[e][:], sm['rstd'][e][:, 0:1], gsub[:], ALU.mult, ALU.mult,
                                  reads=[('oo', e), ('rstd', e), 'gsub'], writes=[('ob', e)])
                            tt = jq * 4 + qs
                            P.dma('sp', o_s[tt * 128:(tt + 1) * 128, h * 128:(h + 1) * 128], ob[e][:], reads=[('ob', e)], writes=[('o_s', tt, h)])

            emit_qk(0)
            emit_qk(1)
            for i in range(nb):
                emit_exp(i)
                if i + 2 < nb:
                    emit_qk(i + 2)
                emit_pv(i)
                if i % 4 == 3 and nxt:
                    nxt.pop(0)()
            while nxt:
                nxt.pop(0)()

        for u in project_units(0):
            u()
        for h in range(6):
            attend(h, project_units(h + 1) if h + 1 < 6 else [])
        P.flush()


def phase_hgrn(P, k, hT, j, o_s):
    inp = k.inp
    w_in = wview(inp['b_w_in'][j])
    with ExitStack() as st:
        W = [P.sb(st, [128, 8, 512], BF16, 'W') for _ in range(2)]
        qeT = [P.sb(st, [128, S], BF16, 'qeT') for _ in range(2)]
        keT = [P.sb(st, [128, S], BF16, 'keT') for _ in range(2)]
        ketok = [P.sb(st, [128, NT, 128], BF16, 'ketok') for _ in range(2)]
        vtok = [P.sb(st, [128, NT, 128], BF16, 'vtok') for _ in range(2)]
        sgtok = [P.sb(st, [128, NT, 128], BF16, 'sgtok') for _ in range(2)]
        decay = [P.sb(st, [128, 64], F32, 'decay') for _ in range(2)]
        oml = P.sb(st, [128, 6], F32, 'oml')
        gn = P.sb(st, [128, 128], F32, 'gn')
        cmask = P.sb(st, [128, 128], BF16, 'cmask')
        smask = P.sb(st, [128, 512], F32, 'smask')
        tmp = {n: [P.sb(st, [128, 512], F32, n) for _ in range(2)] for n in ('et', 'dt', 'kk', 'gt', 'bt', 'eb', 'enb', 'qraw')}
        Sst = P.sb(st, [128, 128], F32, 'Sst')
        Sbf = P.sb(st, [128, 128], BF16, 'Sbf')
        at = [P.sb(st, [128, 128], BF16, 'at') for _ in range(2)]
        o1 = [P.sb(st, [128, 128], F32, 'o1') for _ in range(2)]
        ob = [P.sb(st, [128, 128], BF16, 'ob') for _ in range(2)]
        junk = P.sb(st, [128, 128], F32, 'junk')
        sm = {n: [P.sb(st, [128, 1], F32, n) for _ in range(2)] for n in ('ssq', 'ms', 'rstd')}
        pq = P.ps(st, [128, 512], F32, 'pq')
        ptr = P.ps(st, [128, 4, 128], BF16, 'ptr')
        pvg = P.ps(st, [128, 128], F32, 'pvg')
        pat = P.ps(st, [128, 128], F32, 'pat')
        pkv2 = [P.ps(st, [128, 128], F32, 'pkv') for _ in range(2)]
        po = [P.ps(st, [128, 128], F32, 'po') for _ in range(2)]

        P.dma('pool', cmask[:], inp['c_cmask'], writes=['cmask'], cast=True)
        P.dma('sp', smask[:], inp['c_smask'], writes=['smask'])
        P.dma('sp', gn[:], inp['b_out_norm'][j:j + 1, :].partition_broadcast(128), writes=['gn'])
        lbf = P.sb(st, [128, 1], F32, 'lbf')
        lbl2 = P.sb(st, [2, 768], F32, 'lbl2')
        lbT = P.sb(st, [128, 6, 2], F32, 'lbT')
        P.dma('sp', lbf[:], inp['c_lbflag'][j], writes=['lbf'])
        P.dma('sp', lbl2[:], inp['b_lb_logits'], writes=['lbl2'])
        P.trg([(pvg[:, 2 * h:2 * h + 2], lbl2[0:2, h * 128:(h + 1) * 128], k.identf[0:2, 0:2]) for h in range(6)], reads=['lbl2'], writes=['pvg'])
        P.copy('dve', lbT[:], pvg[:, 0:12].rearrange("p (h l) -> p h l", l=2), reads=['pvg'], writes=['lbl'])
        P.tt('dve', oml[:], lbT[:, :, 0], lbT[:, :, 1], ALU.subtract, reads=['lbl'], writes=['oml'])
        P.act(oml[:], oml[:], AF.Exp, reads=['oml'], writes=['oml'])
        P.ts('dve', oml[:], oml[:], 1.0, None, ALU.add, None, reads=['oml'], writes=['oml'])
        P.recip(oml[:], oml[:], reads=['oml'], writes=['oml'])
        P.ts('dve', oml[:], oml[:], lbf[:, 0:1], 1.0, ALU.mult, ALU.add, reads=['oml', 'lbf'], writes=['oml'])

        def project_units(h):
            b = h % 2
            units = []

            def u0():
                for i, c0 in enumerate([h * 128, HG_W + h * 128, 2 * HG_W + h * 128, 3 * HG_W + h * 128]):
                    P.dma('pool', W[b][:, :, i * 128:(i + 1) * 128], w_in[:, :, c0:c0 + 128], writes=[('w', b, i)], cast=True)
            units.append(u0)
            for tb in range(8):
                def ua(tb=tb):
                    blk = slice(tb * 512, (tb + 1) * 512)
                    x = tb % 2
                    et, dt_, kk, gt, bt, eb, enb, qraw = (tmp[n][x] for n in ('et', 'dt', 'kk', 'gt', 'bt', 'eb', 'enb', 'qraw'))
                    T = lambda n: (n, x)
                    P.mmg([(pq[:], W[b][:, dc, 0:128], hT[:, dc, blk], dc == 0, dc == 7) for dc in range(8)], reads=[('w', b, 0)], writes=['pq'])
                    P.copy('act', qraw[:], pq[:], reads=['pq'], writes=[T('qraw')])
                    P.mmg([(pq[:], W[b][:, dc, 128:256], hT[:, dc, blk], dc == 0, dc == 7) for dc in range(8)], reads=[('w', b, 1)], writes=['pq'])
                    P.act(et[:], pq[:], AF.Exp, reads=['pq'], writes=[T('et')], scale=-1.0)
                    P.ts('dve', dt_[:], et[:], 1.0, None, ALU.add, None, reads=[T('et')], writes=[T('dt')])
                    P.recip(dt_[:], dt_[:], reads=[T('dt')], writes=[T('dt')])
                    P.stt(kk[:], et[:], oml[:, h:h + 1], dt_[:], ALU.mult, ALU.mult, reads=[T('et'), T('dt'), 'oml'], writes=[T('kk')])
                    P.act(gt[:], kk[:], AF.Ln, reads=[T('kk')], writes=[T('gt')], scale=-1.0, bias=1.0)
                    P.scan(bt[:], smask[:], gt[:], reads=['smask', T('gt')], writes=[T('bt')])
                    P.act(eb[:], bt[:], AF.Exp, reads=[T('bt')], writes=[T('eb')])
                    P.act(enb[:], bt[:], AF.Exp, reads=[T('bt')], writes=[T('enb')], scale=-1.0)
                    P.tt('dve', qeT[b][:, blk], qraw[:], eb[:], ALU.mult, reads=[T('qraw'), T('eb')], writes=[('qe', b, tb)])
                    P.tt('dve', keT[b][:, blk], kk[:], enb[:], ALU.mult, reads=[T('kk'), T('enb')], writes=[('ke', b, tb)])
                    P.copy('act', decay[b][:, tb * 8:(tb + 1) * 8], eb[:].rearrange("p (c t) -> p c t", t=64)[:, :, 63], reads=[T('eb')], writes=[('dec', b, tb)])
                    P.trg([(ptr[:, i, :], keT[b][:, tb * 512 + i * 128: tb * 512 + (i + 1) * 128], k.ident[:]) for i in range(4)],
                          reads=[('ke', b, tb)], writes=['ptr'])
                    P.copy('act', ketok[b][:, tb * 4:(tb + 1) * 4, :], ptr[:], reads=['ptr'], writes=[('ketok', b, tb)])
                units.append(ua)
                for tq in range(4):
                    def uv(tt=tb * 4 + tq):
                        tok = slice(tt * 128, (tt + 1) * 128)
                        P.mmg([(pvg[:], hT[:, dc, tok], W[b][:, dc, 256:384], dc == 0, dc == 7) for dc in range(8)], reads=[('w', b, 2)], writes=['pvg'])
                        P.copy('dve', vtok[b][:, tt, :], pvg[:], reads=['pvg'], writes=[('vtok', b, tt)])
                        P.mmg([(pvg[:], hT[:, dc, tok], W[b][:, dc, 384:512], dc == 0, dc == 7) for dc in range(8)], reads=[('w', b, 3)], writes=['pvg'])
                        P.act(sgtok[b][:, tt, :], pvg[:], AF.Silu, reads=['pvg'], writes=[('sgtok', b, tt)])
                    units.append(uv)
            return units

        def recur(h, nxt):
            b = h % 2
            P.memset('pool', Sst[:], 0.0, writes=['Sst'])
            P.memset('pool', Sbf[:], 0.0, writes=['Sbf'])
            for tt in range(NT):
                e = tt % 2
                tb = tt // 4
                tok = slice(tt * 128, (tt + 1) * 128)
                P.mmg([(pat[:], keT[b][:, tok], qeT[b][:, tok], True, True)], reads=[('ke', b, tb), ('qe', b, tb)], writes=['pat'])
                P.tt('dve', at[e][:], pat[:], cmask[:], ALU.mult, reads=['pat', 'cmask'], writes=[('at', e)])
                P.mmg([(pkv2[ci][:], ketok[b][ci * 64:(ci + 1) * 64, tt, :], vtok[b][ci * 64:(ci + 1) * 64, tt, :], True, True) for ci in range(2)],
                      reads=[('ketok', b, tb), ('vtok', b, tt)], writes=['pkv'])
                P.mmg([(po[e][:], at[e][:], vtok[b][:, tt, :], True, False),
                       (po[e][0:64, :], qeT[b][:, tt * 128:tt * 128 + 64], Sbf[:], False, False)],
                      reads=[('at', e), ('vtok', b, tt), ('qe', b, tb), 'Sbf'], writes=[('po', e)])
                for ci in range(2):
                    c = 2 * tt + ci
                    P.tt('dve', Sst[:], pkv2[ci][:], Sst[:], ALU.add, reads=['pkv', 'Sst'], writes=['Sst'])
                    P.ts('dve', Sst[:], Sst[:], decay[b][:, c:c + 1], None, ALU.mult, None, reads=['Sst', ('dec', b, tb)], writes=['Sst'])
                    P.copy('act', Sbf[:], Sst[:], reads=['Sst'], writes=['Sbf'])
                    if ci == 0:
                        P.mmg([(po[e][64:128, :], qeT[b][:, tt * 128 + 64:(tt + 1) * 128], Sbf[:], False, True)],
                              reads=[('qe', b, tb), 'Sbf'], writes=[('po', e)])
                P.act(junk[:], po[e][:], AF.Square, reads=[('po', e)], writes=['junk', ('ssq', e)], accum=sm['ssq'][e][:])
                P.ts('dve', sm['ms'][e][:], sm['ssq'][e][:], 1.0 / 128, EPS, ALU.mult, ALU.add, reads=[('ssq', e)], writes=[('ms', e)])
                P.tt('pool', sm['rstd'][e][:], sm['ms'][e][:], k.neghalf[:], ALU.pow, reads=[('ms', e)], writes=[('rstd', e)])
                P.stt(o1[e][:], po[e][:], sm['rstd'][e][:, 0:1], gn[:], ALU.mult, ALU.mult, reads=[('po', e), ('rstd', e), 'gn'], writes=[('o1', e)])
                P.tt('dve', ob[e][:], o1[e][:], sgtok[b][:, tt, :], ALU.mult, reads=[('o1', e), ('sgtok', b, tt)], writes=[('ob', e)])
                P.dma('sp', o_s[tt * 128:(tt + 1) * 128, h * 128:(h + 1) * 128], ob[e][:], reads=[('ob', e)], writes=[('o_s', tt, h)])
                for _ in range(2):
                    if nxt:
                        nxt.pop(0)()
            while nxt:
                nxt.pop(0)()

        for u in project_units(0):
            u()
        for h in range(6):
            recur(h, project_units(h + 1) if h + 1 < 6 else [])
        P.flush()


def phase_ffn(P, k, x_src, xacc_src, x_dst, gain_row, w_gu_list, w_dn_list, dff, router_ap=None, esel_ap=None):
    HT = S // 2
    NTH = NT // 2
    ngrp = (dff + 511) // 512
    moe = router_ap is not None
    same = xacc_src is x_src
    nexp = len(w_gu_list)
    for half in range(2):
        with ExitStack() as st0:
            xacc = P.sb(st0, [128, NTH, D], F32, 'xacc')
            hTh = P.sb(st0, [128, 8, HT], BF16, 'hTh')
            csel = P.sb(st0, [128, NTH], F32, 'csel')
            comb = P.sb(st0, [128, NTH, 8], F32, 'comb')
            with ExitStack() as st:
                gbc = P.sb(st, [128, D], F32, 'gbc')
                hb = [P.sb(st, [128, D], BF16, 'hb') for _ in range(2)]
                pt = [P.ps(st, [128, 8, 128], BF16, 'pt') for _ in range(2)]
                alloc_norm_tmps(P, k, st, ['n0', 'n1'])
                P.dma('sp', gbc[:], gain_row.partition_broadcast(128), writes=['gbc'])
                if moe:
                    xp = [P.sb(st, [128, D], F32, 'xp') for _ in range(2)]
                    hf2 = [P.sb(st, [128, D], F32, 'hf') for _ in range(2)]
                    hTf = P.sb(st, [128, 8, 128], F32, 'hTf')
                    wr = P.sb(st, [128, 8, 8], F32, 'wr')
                    esel = P.sb(st, [128, 8], F32, 'esel')
                    ptf = [P.ps(st, [128, 4, 128], F32, 'ptf') for _ in range(2)]
                    plg = P.ps(st, [128, 8], F32, 'plg')
                    lg = P.sb(st, [128, 8], F32, 'lg')
                    lg2 = P.sb(st, [128, 8], F32, 'lg2')
                    eq1 = P.sb(st, [128, 8], F32, 'eq1')
                    eq2 = P.sb(st, [128, 8], F32, 'eq2')
                    m1 = P.sb(st, [128, 1], F32, 'm1')
                    m2 = P.sb(st, [128, 1], F32, 'm2')
                    w1 = P.sb(st, [128, 1], F32, 'w1')
                    w2 = P.sb(st, [128, 1], F32, 'w2')
                    P.dma('sp', wr[:], wview(router_ap), writes=['wr'])
                    if esel_ap is not None:
                        P.dma('sp', esel[:], esel_ap, writes=['esel'])
                for tl in range(NTH):
                    tt = half * NTH + tl
                    b = tl % 2
                    tag = 'n%d' % b
                    rows = slice(tt * 128, (tt + 1) * 128)
                    if moe:
                        hf = hf2[b]
                        if same:
                            P.dma('sp', xacc[:, tl, :], x_src[rows, :], writes=[tag + 'x', ('xacc', tl)])
                            norm_rows(P, k, st, xacc[:, tl, :], gbc[:], hb[b][:], tag, want_f32=hf[:])
                        else:
                            P.dma('sp', xacc[:, tl, :], xacc_src[rows, :], writes=[('xacc', tl)])
                            P.dma('sp', xp[b][:], x_src[rows, :], writes=[tag + 'x'])
                            norm_rows(P, k, st, xp[b][:], gbc[:], hb[b][:], tag, want_f32=hf[:])
                        for q4 in range(2):
                            P.trg([(ptf[q4][:, c, :], hf[:, (q4 * 4 + c) * 128:(q4 * 4 + c + 1) * 128], k.identf[:]) for c in range(4)],
                                  reads=[tag + 'hf'], writes=[('ptf', q4)])
                            P.copy('dve', hTf[:, q4 * 4:(q4 + 1) * 4, :], ptf[q4][:], reads=[('ptf', q4)], writes=[('hTf', q4)])
                        P.mmg([(plg[:], hTf[:, dc, :], wr[:, dc, :], dc == 0, dc == 7) for dc in range(8)],
                              reads=[('hTf', 0), ('hTf', 1), 'wr'], writes=['plg'])
                        P.copy('dve', lg[:], plg[:], reads=['plg'], writes=['lg'])
                        P.reduce(m1[:], lg[:], ALU.max, reads=['lg'], writes=['m1'])
                        P.ts('dve', eq1[:], lg[:], m1[:, 0:1], None, ALU.is_equal, None, reads=['lg', 'm1'], writes=['eq1'])
                        P.stt(lg2[:], eq1[:], -1e30, lg[:], ALU.mult, ALU.add, reads=['eq1', 'lg'], writes=['lg2'])
                        P.reduce(m2[:], lg2[:], ALU.max, reads=['lg2'], writes=['m2'])
                        P.ts('dve', eq2[:], lg2[:], m2[:, 0:1], None, ALU.is_equal, None, reads=['lg2', 'm2'], writes=['eq2'])
                        P.tt('dve', w2[:], m2[:], m1[:], ALU.subtract, reads=['m1', 'm2'], writes=['w2'])
                        P.act(w2[:], w2[:], AF.Exp, reads=['w2'], writes=['w2'])
                        P.ts('dve', w1[:], w2[:], 1.0, None, ALU.add, None, reads=['w2'], writes=['w1'])
                        P.recip(w1[:], w1[:], reads=['w1'], writes=['w1'])
                        P.tt('dve', w2[:], w2[:], w1[:], ALU.mult, reads=['w1', 'w2'], writes=['w2'])
                        P.ts('dve', eq1[:], eq1[:], w1[:, 0:1], None, ALU.mult, None, reads=['eq1', 'w1'], writes=['eq1'])
                        if esel_ap is None:
                            P.stt(comb[:, tl, :], eq2[:], w2[:, 0:1], eq1[:], ALU.mult, ALU.add, reads=['eq2', 'w2', 'eq1'], writes=[('comb', tl)])
                        else:
                            P.stt(eq2[:], eq2[:], w2[:, 0:1], eq1[:], ALU.mult, ALU.add, reads=['eq2', 'w2', 'eq1'], writes=['eq2'])
                            P.tt('dve', eq2[:], eq2[:], esel[:], ALU.mult, reads=['eq2', 'esel'], writes=['eq2'])
                            P.reduce(csel[:, tl:tl + 1], eq2[:], ALU.add, reads=['eq2'], writes=[('csel', tl)])
                    else:
                        P.dma('sp', xacc[:, tl, :], x_src[rows, :], writes=[tag + 'x', ('xacc', tl)])
                        norm_rows(P, k, st, xacc[:, tl, :], gbc[:], hb[b][:], tag)
                    P.trg([(pt[b][:, c, :], hb[b][:, c * 128:(c + 1) * 128], k.ident[:]) for c in range(8)],
                          reads=[tag + 'hb'], writes=[tag + 'pt'])
                    P.copy('act' if tl % 2 else 'dve', hTh[:, :, tl * 128:(tl + 1) * 128], pt[b][:], reads=[tag + 'pt'], writes=[('hT', tl)])
                P.flush()
            with ExitStack() as st:
                wg = [P.sb(st, [128, 8, 512], BF16, 'wg') for _ in range(2)]
                wu = [P.sb(st, [128, 8, 512], BF16, 'wu') for _ in range(2)]
                wd = [P.sb(st, [128, 4, D], BF16, 'wd') for _ in range(2)]
                sg = [P.sb(st, [128, 512], F32, 'sg') for _ in range(2)]
                aT = [P.sb(st, [128, 4, 512], BF16, 'aT') for _ in range(2)]
                pg = [P.ps(st, [128, 512], F32, 'pg') for _ in range(2)]
                pu = [P.ps(st, [128, 512], F32, 'pu') for _ in range(2)]
                po = [P.ps(st, [128, 512], F32, 'po') for _ in range(4)]
                it = 0
                gi = 0
                oi = 0
                ai = 0
                for e in range(nexp):
                    gu = wview(w_gu_list[e])
                    w_dn_ap = w_dn_list[e]
                    for fg in range(ngrp):
                        F = min(512, dff - fg * 512)
                        nfc = F // 128
                        wb = it % 2
                        it += 1
                        P.dma('pool', wg[wb][:, :, 0:F], gu[:, :, fg * 512:fg * 512 + F], writes=[('wg', wb)], cast=True)
                        P.dma('pool', wu[wb][:, :, 0:F], gu[:, :, dff + fg * 512:dff + fg * 512 + F], writes=[('wu', wb)], cast=True)
                        P.dma('pool', wd[wb][:, 0:nfc, :], wview(w_dn_ap[fg * 512:fg * 512 + F, :]), writes=[('wd', wb)], cast=True)
                        for tb in range(HT // 512):
                            blk = slice(tb * 512, (tb + 1) * 512)
                            ab = ai % 2
                            ai += 1
                            for fc in range(nfc):
                                g = gi % 2
                                gi += 1
                                P.mmg([(pg[g][:], wg[wb][:, dc, fc * 128:(fc + 1) * 128], hTh[:, dc, blk], dc == 0, dc == 7) for dc in range(8)],
                                      reads=[('wg', wb)], writes=[('pg', g)])
                                P.mmg([(pu[g][:], wu[wb][:, dc, fc * 128:(fc + 1) * 128], hTh[:, dc, blk], dc == 0, dc == 7) for dc in range(8)],
                                      reads=[('wu', wb)], writes=[('pu', g)])
                                P.act(sg[g][:], pg[g][:], AF.Silu, reads=[('pg', g)], writes=[('sg', g)])
                                P.tt('dve', aT[ab][:, fc, :], pu[g][:], sg[g][:], ALU.mult, reads=[('pu', g), ('sg', g)], writes=[('aT', ab, fc)])
                            for tq in range(4):
                                tl = tb * 4 + tq
                                for hh in range(2):
                                    o = oi % 4
                                    oi += 1
                                    P.mmg([(po[o][:], aT[ab][:, fc, tq * 128:(tq + 1) * 128], wd[wb][:, fc, hh * 512:(hh + 1) * 512], fc == 0, fc == nfc - 1) for fc in range(nfc)],
                                          reads=[('aT', ab, fc) for fc in range(nfc)] + [('wd', wb)], writes=[('po', o)])
                                    sc_ = (comb[:, tl, e:e + 1] if esel_ap is None else csel[:, tl:tl + 1]) if moe else 1.0
                                    P.stt(xacc[:, tl, hh * 512:(hh + 1) * 512], po[o][:], sc_, xacc[:, tl, hh * 512:(hh + 1) * 512], ALU.mult, ALU.add,
                                          reads=[('po', o), ('xacc', tl, hh)], writes=[('xacc', tl, hh)])
                for tl in range(NTH):
                    tt = half * NTH + tl
                    P.dma('sp', x_dst[tt * 128:(tt + 1) * 128, :], xacc[:, tl, :], reads=[('xacc', tl, 0), ('xacc', tl, 1)], writes=[('xd', tt)])
                P.flush()


def phase_moe_sparse(P, k, x_src, x_dst, gain_row, w_gu_list, w_dn_list, router_ap, hbk):
    I32 = mybir.dt.int32
    NQ = 4
    NTQ = NT // NQ
    dff = DFF_E
    ngrp = dff // 512
    import os
    thr = float(os.environ.get('K_MOE_THR', '96'))
    for qt in range(NQ):
        with ExitStack() as st0:
            xacc = P.sb(st0, [128, NTQ, D], F32, 'xacc')
            comb = P.sb(st0, [128, NTQ * 8], F32, 'comb')
            maskf = P.sb(st0, [128, NTQ * 8], F32, 'maskf')
            pos = P.sb(st0, [128, NTQ * 8], F32, 'pos')
            flag = P.sb(st0, [128, 1], I32, 'flag')
            posA = P.sb(st0, [128, NTQ * 8], F32, 'posA')
            iota = P.sb(st0, [128, 128], F32, 'iota')
            with ExitStack() as st:
                gbc = P.sb(st, [128, D], F32, 'gbc')
                hb = [P.sb(st, [128, D], BF16, 'hb') for _ in range(2)]
                hf2 = [P.sb(st, [128, D], F32, 'hf') for _ in range(2)]
                alloc_norm_tmps(P, k, st, ['n0', 'n1'])
                hTf = P.sb(st, [128, 8, 128], F32, 'hTf')
                wr = P.sb(st, [128, 8, 8], F32, 'wr')
                ltri = P.sb(st, [128, 128], F32, 'ltri')
                ones = P.sb(st, [128, 128], F32, 'ones')
                cnt = P.sb(st, [128, NTQ * 8], F32, 'cnt')
                mx = P.sb(st, [128, 1], F32, 'mx')
                ptf = [P.ps(st, [128, 4, 128], F32, 'ptf') for _ in range(2)]
                plg = P.ps(st, [128, 8], F32, 'plg')
                ppos = P.ps(st, [128, NTQ * 8], F32, 'ppos')
                pcnt = P.ps(st, [128, NTQ * 8], F32, 'pcnt')
                lg = P.sb(st, [128, 8], F32, 'lg')
                lg2 = P.sb(st, [128, 8], F32, 'lg2')
                eq1 = P.sb(st, [128, 8], F32, 'eq1')
                eq2 = P.sb(st, [128, 8], F32, 'eq2')
                m1 = P.sb(st, [128, 1], F32, 'm1')
                m2 = P.sb(st, [128, 1], F32, 'm2')
                w1 = P.sb(st, [128, 1], F32, 'w1')
                w2 = P.sb(st, [128, 1], F32, 'w2')
                P.dma('sp', gbc[:], gain_row.partition_broadcast(128), writes=['gbc'])
                P.dma('sp', wr[:], wview(router_ap), writes=['wr'])
                P.dma('sp', ltri[:], k.inp['c_ltri'], writes=['ltri'])
                P.dma('sp', iota[:], k.inp['c_iota'], writes=['iota'])
                P.memset('pool', ones[:], 1.0, writes=['ones'])
                for tl in range(NTQ):
                    tt = qt * NTQ + tl
                    b = tl % 2
                    tag = 'n%d' % b
                    rows = slice(tt * 128, (tt + 1) * 128)
                    hf = hf2[b]
                    P.dma('sp', xacc[:, tl, :], x_src[rows, :], writes=[tag + 'x', ('xacc', tl)])
                    norm_rows(P, k, st, xacc[:, tl, :], gbc[:], hb[b][:], tag, want_f32=hf[:])
                    P.dma('sp', hbk[rows, :], hb[b][:], reads=[tag + 'hb'], writes=[('hbk', tt)])
                    for q4 in range(2):
                        P.trg([(ptf[q4][:, c, :], hf[:, (q4 * 4 + c) * 128:(q4 * 4 + c + 1) * 128], k.identf[:]) for c in range(4)],
                              reads=[tag + 'hf'], writes=[('ptf', q4)])
                        P.copy('dve', hTf[:, q4 * 4:(q4 + 1) * 4, :], ptf[q4][:], reads=[('ptf', q4)], writes=[('hTf', q4)])
                    P.mmg([(plg[:], hTf[:, dc, :], wr[:, dc, :], dc == 0, dc == 7) for dc in range(8)],
                          reads=[('hTf', 0), ('hTf', 1), 'wr'], writes=['plg'])
                    P.copy('dve', lg[:], plg[:], reads=['plg'], writes=['lg'])
                    P.reduce(m1[:], lg[:], ALU.max, reads=['lg'], writes=['m1'])
                    P.ts('dve', eq1[:], lg[:], m1[:, 0:1], None, ALU.is_equal, None, reads=['lg', 'm1'], writes=['eq1'])
                    P.stt(lg2[:], eq1[:], -1e30, lg[:], ALU.mult, ALU.add, reads=['eq1', 'lg'], writes=['lg2'])
                    P.reduce(m2[:], lg2[:], ALU.max, reads=['lg2'], writes=['m2'])
                    P.ts('dve', eq2[:], lg2[:], m2[:, 0:1], None, ALU.is_equal, None, reads=['lg2', 'm2'], writes=['eq2'])
                    P.tt('dve', w2[:], m2[:], m1[:], ALU.subtract, reads=['m1', 'm2'], writes=['w2'])
                    P.act(w2[:], w2[:], AF.Exp, reads=['w2'], writes=['w2'])
                    P.ts('dve', w1[:], w2[:], 1.0, None, ALU.add, None, reads=['w2'], writes=['w1'])
                    P.recip(w1[:], w1[:], reads=['w1'], writes=['w1'])
                    P.tt('dve', w2[:], w2[:], w1[:], ALU.mult, reads=['w1', 'w2'], writes=['w2'])
                    P.tt('dve', maskf[:, tl * 8:(tl + 1) * 8], eq1[:], eq2[:], ALU.add, reads=['eq1', 'eq2'], writes=[('mask', tl)])
                    P.ts('dve', eq1[:], eq1[:], w1[:, 0:1], None, ALU.mult, None, reads=['eq1', 'w1'], writes=['eq1'])
                    P.stt(comb[:, tl * 8:(tl + 1) * 8], eq2[:], w2[:, 0:1], eq1[:], ALU.mult, ALU.add, reads=['eq2', 'w2', 'eq1'], writes=[('comb', tl)])
                allm = [('mask', tl) for tl in range(NTQ)]
                P.mmg([(ppos[:], ltri[:], maskf[:], True, True)], reads=allm + ['ltri'], writes=['ppos'])
                P.mmg([(pcnt[:], ones[:], maskf[:], True, True)], reads=allm + ['ones'], writes=['pcnt'])
                P.copy('dve', pos[:], ppos[:], reads=['ppos'], writes=['pos'])
                P.copy('dve', cnt[:], pcnt[:], reads=['pcnt'], writes=['cnt'])
                cntw = P.sb(st, [128, NTQ * 4], F32, 'cntw')
                c4 = cnt[:].rearrange("p (w t e) -> p w t e", t=2, e=8)
                p4 = pos[:].rearrange("p (w t e) -> p w t e", t=2, e=8)
                pa4 = posA[:].rearrange("p (w t e) -> p w t e", t=2, e=8)
                P.tt('dve', cntw[:].rearrange("p (w e) -> p w e", e=8), c4[:, :, 0, :], c4[:, :, 1, :], ALU.add, reads=['cnt'], writes=['cntw'])
                P.copy('dve', pa4[:, :, 0, :], p4[:, :, 0, :], reads=['pos'], writes=['posA0'])
                P.tt('dve', pa4[:, :, 1, :], p4[:, :, 1, :], c4[:, :, 0, :], ALU.add, reads=['pos', 'cnt'], writes=['posA1'])
                P.reduce(mx[:], cntw[:], ALU.max, reads=['cntw'], writes=['mx'])
                P.ts('dve', flag[:], mx[:], thr, None, ALU.is_gt, None, reads=['mx'], writes=['flag'])
                P.flush()
            with ExitStack() as st:
                wg = [P.sb(st, [128, 8, 512], BF16, 'wg') for _ in range(2)]
                wu = [P.sb(st, [128, 8, 512], BF16, 'wu') for _ in range(2)]
                wd = [P.sb(st, [128, 4, D], BF16, 'wd') for _ in range(2)]
                sg = [P.sb(st, [128, 512], F32, 'sg') for _ in range(2)]
                aT = [P.sb(st, [128, 4, 512], BF16, 'aT') for _ in range(2)]
                hbt = [P.sb(st, [128, D], BF16, 'hbt') for _ in range(4)]
                sel2 = [P.sb(st, [128, NTQ, 128], BF16, 'sel') for _ in range(2)]
                selT2 = [P.sb(st, [128, NTQ, 128], BF16, 'selT') for _ in range(2)]
                hTg2 = [P.sb(st, [128, 8, NTQ * 128], BF16, 'hTg') for _ in range(2)]
                yacc = P.sb(st, [128, NTQ, D], F32, 'yacc')
                ybf = [P.sb(st, [128, D], BF16, 'ybf') for _ in range(2)]
                pg = [P.ps(st, [128, 512], F32, 'pg') for _ in range(2)]
                pu = [P.ps(st, [128, 512], F32, 'pu') for _ in range(2)]
                po = [P.ps(st, [128, 512], F32, 'po') for _ in range(3)]
                pts = P.ps(st, [128, NTQ, 128], BF16, 'pts')

                def body(cap, win):
                    NW = NTQ // win
                    NS = NW * cap
                    BS = min(512, NS)
                    nsb = NS // BS
                    upb = BS // cap
                    posX = posA if win == 2 else pos
                    ctr = {'it': 0, 'gi': 0, 'oi': 0, 'ai': 0, 'hb': 0, 'yb': 0}

                    def pre_units(e):
                        x = e % 2
                        sel, selT, hTg = sel2[x], selT2[x], hTg2[x]
                        units = []

                        def usel():
                            for tl in range(NTQ):
                                col = tl * 8 + e
                                P.ts('dve', sel[:, tl, 0:cap], iota[:, 0:cap], posX[:, col:col + 1], maskf[:, col:col + 1], ALU.is_equal, ALU.mult,
                                     reads=[], writes=[('sel', x, tl)])
                            P.trg([(pts[0:cap, tl, :], sel[:, tl, 0:cap], k.ident[:]) for tl in range(NTQ)],
                                  reads=[('sel', x, tl) for tl in range(NTQ)], writes=['pts'])
                            P.copy('act', selT[0:cap, :, :], pts[0:cap, :, :], reads=['pts'], writes=[('selT', x)])
                        units.append(usel)
                        for w in range(NW):
                            def ug(w=w):
                                tls = list(range(w * win, (w + 1) * win))
                                hbs = []
                                for tl in tls:
                                    tt = qt * NTQ + tl
                                    hbi = ctr['hb'] % 4
                                    ctr['hb'] += 1
                                    hbs.append(hbi)
                                    P.dma('sp', hbt[hbi][:], hbk[tt * 128:(tt + 1) * 128, :], writes=[('hbt', hbi)])
                                for g0 in range(0, 8, 4):
                                    o = ctr['oi'] % 3
                                    ctr['oi'] += 1
                                    pv = po[o][:, 0:4 * cap].rearrange("p (c s) -> p c s", s=cap)
                                    P.mmg([(pv[:, dc - g0, :], hbt[hbs[i]][:, dc * 128:(dc + 1) * 128], sel[:, tls[i], 0:cap], i == 0, i == win - 1)
                                           for dc in range(g0, g0 + 4) for i in range(win)],
                                          reads=[('hbt', h_) for h_ in hbs] + [('sel', x, tl) for tl in tls], writes=[('po', o)])
                                    P.copy('act' if (w % 2) else 'dve', hTg[:, g0:g0 + 4, w * cap:(w + 1) * cap], pv, reads=[('po', o)], writes=[('hTg', x, w, g0)])
                            units.append(ug)
                        return units

                    for u in pre_units(0):
                        u()
                    for e in range(NEXP):
                        gu = wview(w_gu_list[e])
                        w_dn_ap = w_dn_list[e]
                        x = e % 2
                        selT, hTg = selT2[x], hTg2[x]
                        nxt = pre_units(e + 1) if e + 1 < NEXP else []
                        allg = [('hTg', x, w, g0) for w in range(NW) for g0 in (0, 4)]
                        for fg in range(ngrp):
                            wb = ctr['it'] % 2
                            ctr['it'] += 1
                            P.dma('pool', wg[wb][:], gu[:, :, fg * 512:(fg + 1) * 512], writes=[('wg', wb)], cast=True)
                            P.dma('pool', wu[wb][:], gu[:, :, dff + fg * 512:dff + (fg + 1) * 512], writes=[('wu', wb)], cast=True)
                            P.dma('pool', wd[wb][:], wview(w_dn_ap[fg * 512:(fg + 1) * 512, :]), writes=[('wd', wb)], cast=True)
                            for sb_ in range(nsb):
                                blk = slice(sb_ * BS, (sb_ + 1) * BS)
                                ab = ctr['ai'] % 2
                                ctr['ai'] += 1
                                for fc in range(4):
                                    g = ctr['gi'] % 2
                                    ctr['gi'] += 1
                                    P.mmg([(pg[g][:, 0:BS], wg[wb][:, dc, fc * 128:(fc + 1) * 128], hTg[:, dc, blk], dc == 0, dc == 7) for dc in range(8)],
                                          reads=[('wg', wb)] + allg, writes=[('pg', g)])
                                    P.mmg([(pu[g][:, 0:BS], wu[wb][:, dc, fc * 128:(fc + 1) * 128], hTg[:, dc, blk], dc == 0, dc == 7) for dc in range(8)],
                                          reads=[('wu', wb)] + allg, writes=[('pu', g)])
                                    P.act(sg[g][:, 0:BS], pg[g][:, 0:BS], AF.Silu, reads=[('pg', g)], writes=[('sg', g)])
                                    P.tt('dve', aT[ab][:, fc, 0:BS], pu[g][:, 0:BS], sg[g][:, 0:BS], ALU.mult, reads=[('pu', g), ('sg', g)], writes=[('aT', ab, fc)])
                                for j in range(upb):
                                    u_ = sb_ * upb + j
                                    for hh in range(2):
                                        o = ctr['oi'] % 3
                                        ctr['oi'] += 1
                                        P.mmg([(po[o][0:cap, :], aT[ab][:, fc, j * cap:(j + 1) * cap], wd[wb][:, fc, hh * 512:(hh + 1) * 512], fc == 0, fc == 3) for fc in range(4)],
                                              reads=[('aT', ab, fc) for fc in range(4)] + [('wd', wb)], writes=[('po', o)])
                                        ydst = yacc[0:cap, u_, hh * 512:(hh + 1) * 512]
                                        if fg == 0:
                                            P.copy('dve', ydst, po[o][0:cap, :], reads=[('po', o)], writes=[('yacc', u_, hh)])
                                        else:
                                            P.tt('dve', ydst, po[o][0:cap, :], ydst, ALU.add, reads=[('po', o), ('yacc', u_, hh)], writes=[('yacc', u_, hh)])
                            for _ in range(2):
                                if nxt:
                                    nxt.pop(0)()
                        while nxt:
                            nxt.pop(0)()
                        for u_ in range(NW):
                            yb = ctr['yb'] % 2
                            ctr['yb'] += 1
                            P.copy('act', ybf[yb][0:cap, :], yacc[0:cap, u_, :], reads=[('yacc', u_, 0), ('yacc', u_, 1)], writes=[('ybf', yb)])
                            for tl in range(u_ * win, (u_ + 1) * win):
                                col = tl * 8 + e
                                for hh in range(2):
                                    o = ctr['oi'] % 3
                                    ctr['oi'] += 1
                                    P.mmg([(po[o][:], selT[0:cap, tl, :], ybf[yb][0:cap, hh * 512:(hh + 1) * 512], True, True)],
                                          reads=[('selT', x), ('ybf', yb)], writes=[('po', o)])
                                    P.stt(xacc[:, tl, hh * 512:(hh + 1) * 512], po[o][:], comb[:, col:col + 1], xacc[:, tl, hh * 512:(hh + 1) * 512], ALU.mult, ALU.add,
                                          reads=[('po', o), ('xacc', tl, hh)], writes=[('xacc', tl, hh)])
                    for tl in range(NTQ):
                        tt = qt * NTQ + tl
                        P.dma('sp', x_dst[tt * 128:(tt + 1) * 128, :], xacc[:, tl, :], reads=[('xacc', tl, 0), ('xacc', tl, 1)], writes=[('xd', tt)])

                P.flush_branch(flag[0:1, 0:1], lambda: body(96, 2), lambda: body(128, 1))


def phase_fnorm(P, k, x_src, gain_row, out_ap):
    with ExitStack() as st:
        gbc = P.sb(st, [128, D], F32, 'gbc')
        xt = [P.sb(st, [128, D], F32, 'xt') for _ in range(2)]
        yo = [P.sb(st, [128, D], F32, 'yo') for _ in range(2)]
        alloc_norm_tmps(P, k, st, ['n0', 'n1'])
        P.dma('sp', gbc[:], gain_row.partition_broadcast(128), writes=['gbc'])
        for tt in range(NT):
            b = tt % 2
            tag = 'n%d' % b
            t = k.nt[tag]
            P.dma('sp', xt[b][:], x_src[tt * 128:(tt + 1) * 128, :], writes=[tag + 'x'])
            P.act(t['junk'][:], xt[b][:], AF.Square, reads=[tag + 'x'], writes=[tag + 'junk', tag + 'ssq'], accum=t['ssq'][:])
            P.ts('dve', t['ms'][:], t['ssq'][:], 1.0 / D, EPS, ALU.mult, ALU.add, reads=[tag + 'ssq'], writes=[tag + 'ms'])
            P.tt('pool', t['rstd'][:], t['ms'][:], k.neghalf[:], ALU.pow, reads=[tag + 'ms'], writes=[tag + 'rstd'])
            P.stt(yo[b][:], xt[b][:], t['rstd'][:, 0:1], gbc[:], ALU.mult, ALU.mult, reads=[tag + 'x', tag + 'rstd', 'gbc'], writes=[('yo', b)])
            P.dma('sp', out_ap[tt * 128:(tt + 1) * 128, :], yo[b][:], reads=[('yo', b)], writes=[('out', tt)])
        P.flush()


CONST_SHAPES = {"c_ident": [128, 128], "c_kaug": [6, 6, S], "c_qaug": [6, 6, S], "c_dmask": [6, 128, 128], "c_bias": [128, 6 * 35],
                "c_cmask": [128, 128], "c_smask": [128, 512], "c_ltri": [128, 128], "c_iota": [128, 128]}
STEP_INPUTS = {
    'attn': {"x": [S, D], "mem": [MEM_LEN, D], "a_norm_mix": [1, D], "a_w_in": [1, D, 2560], "a_lam_q1": [1, 64], "a_lam_k1": [1, 64],
             "a_lam_q2": [1, 64], "a_lam_k2": [1, 64], "a_subln": [1, 128], "a_mem_norm": [1, D], "a_w_mem_kv": [1, D, 512],
             "a_w_out": [1, D, D], "c_lam": [1, 128, 2], "c_ident": 0, "c_kaug": 0, "c_qaug": 0, "c_dmask": 0, "c_bias": 0},
    'hgrn': {"x": [S, D], "mem": [MEM_LEN, D], "b_norm_mix": [1, D], "b_w_in": [1, D, 3328], "b_lb_logits": [2, 768], "b_out_norm": [1, 128],
             "b_mem_norm": [1, D], "b_w_mem_kv": [1, D, 512], "b_w_out": [1, D, D], "c_lbflag": [1, 128, 1], "c_ident": 0, "c_cmask": 0, "c_smask": 0},
    'dense': {"x": [S, D], "norm": [1, D], "w_gu": [D, 2 * DFF_D], "w_dn": [DFF_D, D], "c_ident": 0},
    'moe1': {"x": [S, D], "xacc": [S, D], "norm": [1, D], "router": [D, 8], "w_gu": [D, 2 * DFF_E], "w_dn": [DFF_E, D], "esel": [128, 8], "c_ident": 0},
    'fnorm': {"x": [S, D], "gain": [1, D], "c_ident": 0},
    'moes': {"x": [S, D], "norm": [1, D], "router": [D, 8], "w_gu": [8, D, 2 * DFF_E], "w_dn": [8, DFF_E, D], "c_ident": 0, "c_ltri": 0, "c_iota": 0},
}


def build_step(kind):
    nc = bass.Bass("TRN2", target_bir_lowering=False)
    inp = {}
    for n, shp in STEP_INPUTS[kind].items():
        if shp == 0:
            shp = CONST_SHAPES[n]
        inp[n] = nc.dram_tensor(n, shp, F32, kind="ExternalInput").ap()
    out = nc.dram_tensor("out", [S, D], F32, kind="ExternalOutput").ap()
    o_s = nc.dram_tensor("o_s", [S, D], BF16, kind="Internal").ap()
    with ExitStack() as st:
        P = Prog(nc, st)
        k = K()
        k.inp = inp
        k.ident = P.sb(st, [128, 128], BF16, 'ident')
        k.identf = P.sb(st, [128, 128], F32, 'identf')
        k.neghalf = P.sb(st, [128, 1], F32, 'neghalf')
        P.dma('pool', k.ident[:], inp['c_ident'], writes=['ident'], cast=True)
        P.dma('sp', k.identf[:], inp['c_ident'], writes=['identf'])
        P.memset('pool', k.neghalf[:], -0.5, writes=['neghalf'])
        P.flush()
        if kind in ('attn', 'hgrn'):
            pre = 'a_' if kind == 'attn' else 'b_'
            with ExitStack() as stm:
                hT = P.sb(stm, [128, 8, S], BF16, 'hT')
                kmT = P.sb(stm, [128, 2, MEM_LEN], BF16, 'kmT')
                vm = P.sb(stm, [128, 2, 4, 65], BF16, 'vm')
                phase_hT(P, k, inp['x'], inp[pre + 'norm_mix'][0:1, :], hT)
                phase_memkv(P, k, inp['mem'], inp[pre + 'mem_norm'][0:1, :], inp[pre + 'w_mem_kv'][0], kmT, vm)
                if kind == 'attn':
                    phase_diffattn(P, k, hT, 0, None, o_s)
                    phase_memattn(P, k, hT, inp['a_w_in'][0], 3 * DA_W, kmT, vm, o_s)
                else:
                    phase_hgrn(P, k, hT, 0, o_s)
                    phase_memattn(P, k, hT, inp['b_w_in'][0], 4 * HG_W, kmT, vm, o_s)
            phase_outproj(P, k, o_s, inp[pre + 'w_out'][0], inp['x'], out)
        elif kind == 'dense':
            phase_ffn(P, k, inp['x'], inp['x'], out, inp['norm'][0:1, :], [inp['w_gu']], [inp['w_dn']], DFF_D)
        elif kind == 'moe1':
            phase_ffn(P, k, inp['x'], inp['xacc'], out, inp['norm'][0:1, :], [inp['w_gu']], [inp['w_dn']], DFF_E,
                      router_ap=inp['router'], esel_ap=inp['esel'])
        elif kind == 'fnorm':
            phase_fnorm(P, k, inp['x'], inp['gain'][0:1, :], out)
        elif kind == 'moes':
            hbk = nc.dram_tensor("hbk", [S, D], BF16, kind="Internal").ap()
            phase_moe_sparse(P, k, inp['x'], out, inp['norm'][0:1, :], [inp['w_gu'][e] for e in range(NEXP)], [inp['w_dn'][e] for e in range(NEXP)], inp['router'], hbk)
        P.add('sp', lambda e: e.nop(), reads=[], writes=[])
        P.flush()
    return nc


def make_consts():
    slopes = 2.0 ** (-8.0 * np.arange(1, 7) / 6.0)
    import ml_dtypes
    bf = ml_dtypes.bfloat16

    def hi_lo(v):
        hi = np.float32(np.float32(v).astype(bf).astype(np.float32))
        lo = np.float32(np.float32(v - hi).astype(bf).astype(np.float32))
        return hi, lo
    c = {}
    c['c_ident'] = np.eye(128, dtype=np.float32)
    pos = np.arange(S)
    krel = (pos % 128).astype(np.float32)
    qrel = pos % 512
    qhi = (qrel & ~3).astype(np.float32)
    qlo = (qrel & 3).astype(np.float32)
    kaug = np.zeros((6, 6, S), np.float32)
    qaug = np.zeros((6, 6, S), np.float32)
    for h in range(6):
        hi, lo = hi_lo(slopes[h])
        kaug[h, 0] = krel
        kaug[h, 1] = krel
        kaug[h, 2] = hi
        kaug[h, 3] = lo
        kaug[h, 4] = hi
        kaug[h, 5] = lo
        qaug[h, 0] = hi
        qaug[h, 1] = lo
        qaug[h, 2] = -qhi
        qaug[h, 3] = -qhi
        qaug[h, 4] = -qlo
        qaug[h, 5] = -qlo
    c['c_kaug'] = kaug
    c['c_qaug'] = qaug
    kk = np.arange(128)[:, None]
    qq = np.arange(128)[None, :]
    dm = np.zeros((6, 128, 128), np.float32)
    for h in range(6):
        allowed = (kk // 64) <= (qq // 64)
        val = np.where(kk <= qq, 1.0, np.exp(-2.0 * slopes[h] * (kk - qq)))
        dm[h] = np.where(allowed, val, 0.0)
    c['c_dmask'] = dm
    cb = np.zeros((128, 6 * 35), np.float32)
    for h in range(6):
        for idx in range(35):
            d = idx - 3
            cb[:, h * 35 + idx] = -slopes[h] * 128.0 * d
    c['c_bias'] = cb
    s_ = np.arange(128)[:, None]
    t_ = np.arange(128)[None, :]
    c['c_cmask'] = ((s_ <= t_) & ((s_ // 64) == (t_ // 64))).astype(np.float32)
    sm = np.ones((128, 512), np.float32)
    sm[:, ::64] = 0.0
    c['c_smask'] = sm
    c['c_ltri'] = (s_ < t_).astype(np.float32)
    c['c_iota'] = np.broadcast_to(np.arange(128, dtype=np.float32)[None, :], (128, 128)).copy()
    return c


_CACHE = {}
_CONSTS = {}


def launch(kind, per_core, shared, n_cores):
    if kind not in _CACHE:
        _CACHE[kind] = build_step(kind)
    if not _CONSTS:
        _CONSTS.update(make_consts())
    nc = _CACHE[kind]
    sh = {}
    for n, shp in STEP_INPUTS[kind].items():
        if n in per_core:
            continue
        if shp == 0:
            sh[n] = _CONSTS[n]
        else:
            sh[n] = np.ascontiguousarray(np.asarray(shared[n], dtype=np.float32)).reshape(shp)
    in_maps = []
    for c in range(n_cores):
        m = dict(sh)
        for n, a in per_core.items():
            m[n] = np.ascontiguousarray(a[c])
        in_maps.append(m)
    import os
    tr = bool(os.environ.get('K_TRACE'))
    res = run_bass_kernel_spmd(nc, in_maps, core_ids=list(range(n_cores)), trace=tr)
    if tr:
        print('EXEC_NS', kind, res.exec_time_ns)
    return np.stack([np.asarray(r['out'], dtype=np.float32).reshape(S, D) for r in res.results], axis=0)


def run_step(i, which, x, inputs, n_cores):
    f = lambda n: np.asarray(inputs[n], dtype=np.float32)
    j = i // 2
    mem = f('mem')[:n_cores]
    if which == 'mix' and i % 2 == 0:
        lam_init = 0.8 - 0.6 * float(np.exp(-0.3 * i))
        clam = np.zeros((1, 128, 2), np.float32)
        clam[0, :, 0] = 1.0 - lam_init
        clam[0, :, 1] = -lam_init
        sh = {n: f(n)[j:j + 1] for n in ['a_norm_mix', 'a_w_in', 'a_lam_q1', 'a_lam_k1', 'a_lam_q2', 'a_lam_k2', 'a_subln', 'a_mem_norm', 'a_w_mem_kv', 'a_w_out']}
        sh['c_lam'] = clam
        return launch('attn', {'x': x, 'mem': mem}, sh, n_cores)
    if which == 'mix':
        sh = {n: f(n)[j:j + 1] for n in ['b_norm_mix', 'b_w_in', 'b_out_norm', 'b_mem_norm', 'b_w_mem_kv', 'b_w_out']}
        sh['b_lb_logits'] = f('b_lb_logits')
        sh['c_lbflag'] = np.full((1, 128, 1), -float(j), np.float32)
        return launch('hgrn', {'x': x, 'mem': mem}, sh, n_cores)
    if i % 2 == 0:
        sh = {'norm': f('dense_norm')[j:j + 1], 'w_gu': f('dense_w_gate_up')[j], 'w_dn': f('dense_w_down')[j]}
        return launch('dense', {'x': x}, sh, n_cores)
    xacc = x
    for e in range(NEXP):
        esel = np.zeros((128, 8), np.float32)
        esel[:, e] = 1.0
        sh = {'norm': f('moe_norm')[j:j + 1], 'router': f('moe_router')[j], 'w_gu': f('moe_w_gate_up')[j, e], 'w_dn': f('moe_w_down')[j, e], 'esel': esel}
        xacc = launch('moe1', {'x': x, 'xacc': xacc}, sh, n_cores)
    return xacc


FULL_SHAPES = {
    "x": [S, D], "mem": [MEM_LEN, D],
    "a_norm_mix": [2, D], "a_w_in": [2, D, 2560], "a_lam_q1": [2, 64], "a_lam_k1": [2, 64], "a_lam_q2": [2, 64], "a_lam_k2": [2, 64],
    "a_subln": [2, 128], "a_mem_norm": [2, D], "a_w_mem_kv": [2, D, 512], "a_w_out": [2, D, D],
    "b_norm_mix": [2, D], "b_w_in": [2, D, 3328], "b_lb_logits": [2, 768], "b_out_norm": [2, 128], "b_mem_norm": [2, D],
    "b_w_mem_kv": [2, D, 512], "b_w_out": [2, D, D],
    "dense_norm": [2, D], "dense_w_gate_up": [2, D, 2 * DFF_D], "dense_w_down": [2, DFF_D, D],
    "moe_norm": [2, D], "moe_router": [2, D, 8], "moe_w_gate_up": [2, 8, D, 2 * DFF_E], "moe_w_down": [2, 8, DFF_E, D],
    "final_norm": [1, D], "c_lam": [2, 128, 2], "c_lbflag": [2, 128, 1],
}
FULL_SHAPES.update(CONST_SHAPES)


def build_fused():
    nc = bass.Bass("TRN2", target_bir_lowering=False)
    inp = {n: nc.dram_tensor(n, shp, F32, kind="ExternalInput").ap() for n, shp in FULL_SHAPES.items()}
    out = nc.dram_tensor("out", [S, D], F32, kind="ExternalOutput").ap()
    xres = nc.dram_tensor("xres", [S, D], F32, kind="Internal").ap()
    o_s = nc.dram_tensor("o_s", [S, D], BF16, kind="Internal").ap()
    hbk = nc.dram_tensor("hbk", [S, D], BF16, kind="Internal").ap()
    with ExitStack() as st:
        P = Prog(nc, st)
        k = K()
        k.inp = inp
        k.ident = P.sb(st, [128, 128], BF16, 'ident')
        k.identf = P.sb(st, [128, 128], F32, 'identf')
        k.neghalf = P.sb(st, [128, 1], F32, 'neghalf')
        P.dma('pool', k.ident[:], inp['c_ident'], writes=['ident'], cast=True)
        P.dma('sp', k.identf[:], inp['c_ident'], writes=['identf'])
        P.memset('pool', k.neghalf[:], -0.5, writes=['neghalf'])
        P.flush()
        x_cur = inp['x']
        for i in range(DEPTH):
            j = i // 2
            pre = 'a_' if i % 2 == 0 else 'b_'
            with ExitStack() as stm:
                hT = P.sb(stm, [128, 8, S], BF16, 'hT')
                kmT = P.sb(stm, [128, 2, MEM_LEN], BF16, 'kmT')
                vm = P.sb(stm, [128, 2, 4, 65], BF16, 'vm')
                phase_hT(P, k, x_cur, inp[pre + 'norm_mix'][j:j + 1, :], hT)
                phase_memkv(P, k, inp['mem'], inp[pre + 'mem_norm'][j:j + 1, :], inp[pre + 'w_mem_kv'][j], kmT, vm)
                if i % 2 == 0:
                    phase_diffattn(P, k, hT, j, None, o_s)
                    phase_memattn(P, k, hT, inp['a_w_in'][j], 3 * DA_W, kmT, vm, o_s)
                else:
                    phase_hgrn(P, k, hT, j, o_s)
                    phase_memattn(P, k, hT, inp['b_w_in'][j], 4 * HG_W, kmT, vm, o_s)
            phase_outproj(P, k, o_s, inp[pre + 'w_out'][j], x_cur, xres)
            x_cur = xres
            if i % 2 == 0:
                phase_ffn(P, k, xres, xres, xres, inp['dense_norm'][j:j + 1, :], [inp['dense_w_gate_up'][j]], [inp['dense_w_down'][j]], DFF_D)
            else:
                phase_moe_sparse(P, k, xres, xres, inp['moe_norm'][j:j + 1, :],
                                 [inp['moe_w_gate_up'][j, e] for e in range(NEXP)], [inp['moe_w_down'][j, e] for e in range(NEXP)],
                                 inp['moe_router'][j], hbk)
        phase_fnorm(P, k, xres, inp['final_norm'][0:1, :], out)
        P.add('sp', lambda e: e.nop(), reads=[], writes=[])
        P.flush()
    return nc


def fused_consts():
    c = dict(make_consts())
    clam = np.zeros((2, 128, 2), np.float32)
    for j in range(2):
        lam_init = 0.8 - 0.6 * float(np.exp(-0.3 * (2 * j)))
        clam[j, :, 0] = 1.0 - lam_init
        clam[j, :, 1] = -lam_init
    c['c_lam'] = clam
    lbf = np.zeros((2, 128, 1), np.float32)
    lbf[1] = -1.0
    c['c_lbflag'] = lbf
    return c


def kernel_unfused(**inputs):
    n_cores = 8
    x = np.asarray(inputs['x'], dtype=np.float32)
    for i in range(DEPTH):
        x = run_step(i, 'mix', x, inputs, n_cores)
        x = run_step(i, 'ffn', x, inputs, n_cores)
    return launch('fnorm', {'x': x}, {'gain': np.asarray(inputs['final_norm'], dtype=np.float32).reshape(1, D)}, n_cores)


def kernel(**inputs):
    n_cores = 8
    if 'fused' not in _CACHE:
        _CACHE['fused'] = build_fused()
    nc = _CACHE['fused']
    consts = fused_consts()
    shared = {}
    for n, shp in FULL_SHAPES.items():
        if n in ('x', 'mem'):
            continue
        if n in consts:
            shared[n] = consts[n]
        else:
            shared[n] = np.ascontiguousarray(np.asarray(inputs[n], dtype=np.float32)).reshape(shp)
    x = np.asarray(inputs['x'], dtype=np.float32)
    mem = np.asarray(inputs['mem'], dtype=np.float32)
    in_maps = []
    for c in range(n_cores):
        m = dict(shared)
        m['x'] = np.ascontiguousarray(x[c])
        m['mem'] = np.ascontiguousarray(mem[c])
        in_maps.append(m)
    res = run_bass_kernel_spmd(nc, in_maps, core_ids=list(range(n_cores)))
    return np.stack([np.asarray(r['out'], dtype=np.float32).reshape(S, D) for r in res.results], axis=0)
```

```python
import numpy as np
import concourse.bass as bass
import concourse.mybir as mybir
from concourse.bass_utils import run_bass_kernel_spmd
from contextlib import ExitStack

F32 = mybir.dt.float32
BF16 = mybir.dt.bfloat16
ALU = mybir.AluOpType
AF = mybir.ActivationFunctionType
AX = mybir.AxisListType

S = 4096
D = 1024
NT = 32
EPS = 1e-6
DEPTH = 4
DA_W = 768
HG_W = 768
MEM_W = 256
DFF_D = 2816
DFF_E = 3584
NEXP = 8
MEM_LEN = 256

ENGS = ['pe', 'act', 'dve', 'pool', 'sp']
DMA_POOL = {'sp': 24, 'pool': 24, 'act': 4}


class Op:
    __slots__ = ('eng', 'fn', 'deps', 'need', 'sig', 'is_dma', 'dsem', 'dval', 'uid')


class Prog:
    def __init__(self, nc, stack, strict=True):
        self.nc = nc
        self.stack = stack
        self.strict = strict
        self.ops = {e: [] for e in ENGS}
        self.lastw = {}
        self.reads = {}
        self.uid = 0
        self.esem = {e: stack.enter_context(nc.semaphore('s_' + e)) for e in ENGS}
        self.sigc = {e: 0 for e in ENGS}
        self.dsems = {}
        self.dcur = {}
        self.dlast = {}
        self.dfence = {}
        for q, n in DMA_POOL.items():
            self.dsems[q] = [stack.enter_context(nc.semaphore('d_%s%d' % (q, i))) for i in range(n)]
            self.dcur[q] = 0
            self.dlast[q] = [None] * n
            self.dfence[q] = [0] * n
        self.efence = {e: 0 for e in ENGS}
        self.ntile = 0
        self.nflush = 0

    def sb(self, st, shape, dtype, name=None):
        self.ntile += 1
        name = (name or 't') + '_%d' % self.ntile
        return st.enter_context(self.nc.sbuf_tensor(name, list(shape), dtype))

    def ps(self, st, shape, dtype=F32, name=None):
        self.ntile += 1
        name = (name or 'p') + '_%d' % self.ntile
        return st.enter_context(self.nc.psum_tensor(name, list(shape), dtype))

    def add(self, eng, fn, reads=(), writes=(), dma=False):
        op = Op()
        op.eng = eng
        op.fn = fn
        op.need = False
        op.sig = None
        op.is_dma = dma
        op.uid = self.uid
        self.uid += 1
        deps = []
        for k in reads:
            w = self.lastw.get(k)
            if w is not None:
                deps.append((w, 'raw'))
        for k in writes:
            w = self.lastw.get(k)
            if w is not None:
                deps.append((w, 'waw'))
            rd = self.reads.get(k)
            if rd:
                for r in rd.values():
                    deps.append((r, 'war'))
        fdeps = []
        seen = set()
        for d, kind in deps:
            if d.uid in seen:
                continue
            if (not d.is_dma) and (not dma) and d.eng == eng:
                if eng == 'pe' or not self.strict:
                    continue
            seen.add(d.uid)
            fdeps.append(d)
        if dma:
            q = eng
            i = self.dcur[q]
            self.dcur[q] = (i + 1) % len(self.dsems[q])
            prev = self.dlast[q][i]
            op.dsem = self.dsems[q][i]
            op.dval = (prev.dval if prev is not None else 0) + 16
            if prev is not None and prev.uid not in seen and prev.dval > self.dfence[q][i]:
                fdeps.append(prev)
                seen.add(prev.uid)
            self.dlast[q][i] = op
        for d in fdeps:
            d.need = True
        op.deps = fdeps
        rk = ('dma', op.uid) if dma else eng
        for k in writes:
            self.lastw[k] = op
            self.reads[k] = {}
        for k in reads:
            self.reads.setdefault(k, {})[rk] = op
        self.ops[eng].append(op)
        return op

    def flush(self):
        for e in ENGS:
            comp = [op for op in self.ops[e] if not op.is_dma and op.fn is not None]
            if comp:
                comp[-1].need = True
            c = self.sigc[e]
            for op in self.ops[e]:
                if op.need and not op.is_dma:
                    c += 1
                    op.sig = c
            self.sigc[e] = c
        efence = dict(self.efence)
        dfence = {q: list(v) for q, v in self.dfence.items()}

        def run(e, engobj):
            waited = {}
            for e2 in ENGS:
                if efence[e2] > 0:
                    engobj.wait_ge(self.esem[e2], efence[e2])
                    waited[id(self.esem[e2])] = efence[e2]
            for q in dfence:
                for i, v in enumerate(dfence[q]):
                    if v > 0:
                        engobj.wait_ge(self.dsems[q][i], v)
                        waited[id(self.dsems[q][i])] = v
            for op in self.ops[e]:
                for d in op.deps:
                    if d.is_dma:
                        sem, val = d.dsem, d.dval
                    else:
                        sem, val = self.esem[d.eng], d.sig
                    key = id(sem)
                    if waited.get(key, 0) < val:
                        engobj.wait_ge(sem, val)
                        waited[key] = val
                ins = op.fn(engobj)
                if op.is_dma:
                    ins.then_inc(op.dsem, 16)
                elif op.need:
                    ins.then_inc(self.esem[e], 1)

        with self.nc.Block() as block:
            @block.tensor
            def _(t):
                run('pe', t)

            @block.scalar
            def _(t):
                run('act', t)

            @block.vector
            def _(t):
                run('dve', t)

            @block.gpsimd
            def _(t):
                run('pool', t)

            @block.sync
            def _(t):
                run('sp', t)

        for e in ENGS:
            self.efence[e] = self.sigc[e]
            self.ops[e] = []
        for q in self.dlast:
            for i, op in enumerate(self.dlast[q]):
                if op is not None:
                    self.dfence[q][i] = op.dval
        self.lastw = {}
        self.reads = {}
        self.nflush += 1

    def flush_branch(self, flag_ap, recA, recB):
        assert all(len(self.ops[e]) == 0 for e in ENGS)
        st0 = (dict(self.sigc), {q: list(v) for q, v in self.dlast.items()}, dict(self.dcur))

        def record(rec):
            self.sigc = dict(st0[0])
            self.dlast = {q: list(v) for q, v in st0[1].items()}
            self.dcur = dict(st0[2])
            self.lastw = {}
            self.reads = {}
            rec()
            for e in ENGS:
                comp = [op for op in self.ops[e] if not op.is_dma]
                if comp:
                    comp[-1].need = True
                c = self.sigc[e]
                for op in self.ops[e]:
                    if op.need and not op.is_dma:
                        c += 1
                        op.sig = c
                self.sigc[e] = c
            ops = self.ops
            self.ops = {e: [] for e in ENGS}
            dv = {q: [(o.dval if o is not None else 0) for o in self.dlast[q]] for q in self.dlast}
            return ops, dict(self.sigc), dv
        opsA, sigA, dvA = record(recA)
        opsB, sigB, dvB = record(recB)
        fin = {e: max(sigA[e], sigB[e]) for e in ENGS}
        dfin = {q: [max(a, b) for a, b in zip(dvA[q], dvB[q])] for q in dvA}
        efence = dict(self.efence)
        dfence = {q: list(v) for q, v in self.dfence.items()}

        def run_ops(e, engobj, ops, waited):
            for op in ops[e]:
                for d in op.deps:
                    if d.is_dma:
                        sem, val = d.dsem, d.dval
                    else:
                        sem, val = self.esem[d.eng], d.sig
                    key = id(sem)
                    if waited.get(key, 0) < val:
                        engobj.wait_ge(sem, val)
                        waited[key] = val
                ins = op.fn(engobj)
                if op.is_dma:
                    ins.then_inc(op.dsem, 16)
                elif op.need:
                    ins.then_inc(self.esem[e], 1)

        def catchup(e, engobj, sig, dv):
            if sig[e] > 0:
                engobj.wait_ge(self.esem[e], sig[e])
            if fin[e] > sig[e]:
                engobj.sem_inc(self.esem[e], fin[e] - sig[e])
            if e in dv:
                for i, v in enumerate(dv[e]):
                    if dfin[e][i] > v:
                        if v > 0:
                            engobj.wait_ge(self.dsems[e][i], v)
                        engobj.sem_inc(self.dsems[e][i], dfin[e][i] - v)

        def run(e, engobj):
            waited = {}
            for e2 in ENGS:
                if efence[e2] > 0:
                    engobj.wait_ge(self.esem[e2], efence[e2])
                    waited[id(self.esem[e2])] = efence[e2]
            for q in dfence:
                for i, v in enumerate(dfence[q]):
                    if v > 0:
                        engobj.wait_ge(self.dsems[q][i], v)
                        waited[id(self.dsems[q][i])] = v
            reg = engobj.alloc_register('flag_' + e + '_%d' % self.nflush)
            engobj.reg_load(reg, flag_ap)
            with engobj.If_eq(reg, 0):
                run_ops(e, engobj, opsA, dict(waited))
                catchup(e, engobj, sigA, dvA)
            with engobj.Else():
                run_ops(e, engobj, opsB, dict(waited))
                catchup(e, engobj, sigB, dvB)

        with self.nc.Block() as block:
            @block.tensor
            def _(t):
                run('pe', t)

            @block.scalar
            def _(t):
                run('act', t)

            @block.vector
            def _(t):
                run('dve', t)

            @block.gpsimd
            def _(t):
                run('pool', t)

            @block.sync
            def _(t):
                run('sp', t)

        self.sigc = dict(fin)
        for e in ENGS:
            self.efence[e] = fin[e]
        for q in dfin:
            for i, v in enumerate(dfin[q]):
                if v > 0:
                    o = Op()
                    o.dval = v
                    o.dsem = self.dsems[q][i]
                    o.is_dma = True
                    o.uid = -1
                    self.dlast[q][i] = o
                else:
                    self.dlast[q][i] = None
                self.dfence[q][i] = v
            self.dcur[q] = 0
        self.lastw = {}
        self.reads = {}
        self.nflush += 1

    def dma(self, q, out, in_, reads=(), writes=(), cast=False, slow=False):
        if slow:
            fn = lambda e, o=out, i=in_: e.dma_start(out=o, in_=i, allow_slow_non_contiguous=True)
        elif cast:
            fn = lambda e, o=out, i=in_: e.dma_start(out=o, in_=i, max_dma_last_dim=4096)
        else:
            fn = lambda e, o=out, i=in_: e.dma_start(out=o, in_=i)
        return self.add(q, fn, reads, writes, dma=True)

    def mmg(self, mms, reads, writes):
        mms = list(mms)

        def fn(e, mms=mms):
            ins = None
            for (o, l, r, s0, s1) in mms:
                ins = e.matmul(o, l, r, start=s0, stop=s1)
            return ins
        return self.add('pe', fn, reads, writes)

    def trg(self, trs, reads, writes):
        trs = list(trs)

        def fn(e, trs=trs):
            ins = None
            for (o, i, idn) in trs:
                ins = e.transpose(o, i, idn)
            return ins
        return self.add('pe', fn, reads, writes)

    def act(self, out, in_, func, reads, writes, bias=None, scale=None, accum=None):
        kw = {}
        if bias is not None:
            kw['bias'] = bias
        if scale is not None:
            kw['scale'] = scale
        if accum is not None:
            kw['accum_out'] = accum
        return self.add('act', lambda e, o=out, i=in_, f=func, kw=kw: e.activation(out=o, in_=i, func=f, **kw), reads, writes)

    def tt(self, eng, out, in0, in1, op, reads, writes):
        return self.add(eng, lambda e, o=out, a=in0, b=in1, p=op: e.tensor_tensor(out=o, in0=a, in1=b, op=p), reads, writes)

    def ts(self, eng, out, in0, s1, s2, op0, op1, reads, writes):
        if op1 is None:
            return self.add(eng, lambda e, o=out, a=in0, s1=s1, p0=op0: e.tensor_scalar(out=o, in0=a, scalar1=s1, scalar2=None, op0=p0), reads, writes)
        return self.add(eng, lambda e, o=out, a=in0, s1=s1, s2=s2, p0=op0, p1=op1: e.tensor_scalar(out=o, in0=a, scalar1=s1, scalar2=s2, op0=p0, op1=p1), reads, writes)

    def stt(self, out, in0, scalar, in1, op0, op1, reads, writes):
        return self.add('dve', lambda e, o=out, a=in0, s=scalar, b=in1, p0=op0, p1=op1: e.scalar_tensor_tensor(out=o, in0=a, scalar=s, in1=b, op0=p0, op1=p1), reads, writes)

    def copy(self, eng, out, in_, reads, writes):
        if eng == 'act':
            return self.add('act', lambda e, o=out, i=in_: e.copy(out=o, in_=i), reads, writes)
        return self.add(eng, lambda e, o=out, i=in_: e.tensor_copy(out=o, in_=i), reads, writes)

    def memset(self, eng, ap, val, writes):
        return self.add(eng, lambda e, a=ap, v=val: e.memset(a, v), (), writes)

    def recip(self, out, in_, reads, writes):
        return self.add('dve', lambda e, o=out, i=in_: e.reciprocal(out=o, in_=i), reads, writes)

    def reduce(self, out, in_, op, reads, writes):
        return self.add('dve', lambda e, o=out, i=in_, p=op: e.tensor_reduce(out=o, in_=i, axis=AX.X, op=p), reads, writes)

    def scan(self, out, d0, d1, reads, writes):
        return self.add('dve', lambda e, o=out, a=d0, b=d1: e.tensor_tensor_scan(out=o, data0=a, data1=b, initial=0.0, op0=ALU.mult, op1=ALU.add), reads, writes)


class K:
    pass


def wview(ap2d):
    return ap2d.rearrange("(c p) n -> p c n", p=128)


def norm_rows(P, k, st, x_ap, gbc, hb_out, tag, want_f32=None):
    t = k.nt[tag]
    P.act(t['junk'][:], x_ap, AF.Square, reads=[tag + 'x'], writes=[tag + 'junk', tag + 'ssq'], accum=t['ssq'][:])
    P.ts('dve', t['ms'][:], t['ssq'][:], 1.0 / D, EPS, ALU.mult, ALU.add, reads=[tag + 'ssq'], writes=[tag + 'ms'])
    P.tt('pool', t['rstd'][:], t['ms'][:], k.neghalf[:], ALU.pow, reads=[tag + 'ms'], writes=[tag + 'rstd'])
    if want_f32 is not None:
        P.stt(want_f32, x_ap, t['rstd'][:, 0:1], gbc, ALU.mult, ALU.mult, reads=[tag + 'x', tag + 'rstd', 'gbc'], writes=[tag + 'hf'])
        P.copy('act', hb_out, want_f32, reads=[tag + 'hf'], writes=[tag + 'hb'])
    else:
        P.stt(hb_out, x_ap, t['rstd'][:, 0:1], gbc, ALU.mult, ALU.mult, reads=[tag + 'x', tag + 'rstd', 'gbc'], writes=[tag + 'hb'])


def alloc_norm_tmps(P, k, st, tags):
    k.nt = {}
    for tag in tags:
        k.nt[tag] = {
            'junk': P.sb(st, [128, D], F32, 'junk'),
            'ssq': P.sb(st, [128, 1], F32, 'ssq'),
            'ms': P.sb(st, [128, 1], F32, 'ms'),
            'rstd': P.sb(st, [128, 1], F32, 'rstd'),
        }


def phase_hT(P, k, x_src, gain_row, hT):
    with ExitStack() as st:
        gbc = P.sb(st, [128, D], F32, 'gbc')
        xt = [P.sb(st, [128, D], F32, 'xt') for _ in range(3)]
        hb = [P.sb(st, [128, D], BF16, 'hb') for _ in range(3)]
        pt = [P.ps(st, [128, 8, 128], BF16, 'pt') for _ in range(3)]
        alloc_norm_tmps(P, k, st, ['n0', 'n1', 'n2'])
        P.dma('sp', gbc[:], gain_row.partition_broadcast(128), writes=['gbc'])
        for tt in range(NT):
            b = tt % 3
            tag = 'n%d' % b
            P.dma('sp' if tt % 2 else 'pool', xt[b][:], x_src[tt * 128:(tt + 1) * 128, :], writes=[tag + 'x'])
            norm_rows(P, k, st, xt[b][:], gbc[:], hb[b][:], tag)
            P.trg([(pt[b][:, c, :], hb[b][:, c * 128:(c + 1) * 128], k.ident[:]) for c in range(8)],
                  reads=[tag + 'hb'], writes=[tag + 'pt'])
            P.copy('act' if tt % 2 else 'dve', hT[:, :, tt * 128:(tt + 1) * 128], pt[b][:], reads=[tag + 'pt'], writes=[('hT', tt)])
        P.flush()


def phase_memkv(P, k, mem_ap, gain_row, wkv_ap, kmT, vm):
    with ExitStack() as st:
        gbc = P.sb(st, [128, D], F32, 'gbc')
        xt = [P.sb(st, [128, D], F32, 'xt') for _ in range(2)]
        hb = [P.sb(st, [128, D], BF16, 'hb') for _ in range(2)]
        pt = [P.ps(st, [128, 8, 128], BF16, 'pt') for _ in range(2)]
        memT = P.sb(st, [128, 8, MEM_LEN], BF16, 'memT')
        wkv = P.sb(st, [128, 8, 512], BF16, 'wkv')
        pk = P.ps(st, [128, 256], F32, 'pk')
        alloc_norm_tmps(P, k, st, ['n0', 'n1'])
        P.dma('sp', gbc[:], gain_row.partition_broadcast(128), writes=['gbc'])
        P.dma('pool', wkv[:], wview(wkv_ap), writes=['wkv'], cast=True)
        P.memset('pool', vm[:], 1.0, writes=['vm'])
        for mt in range(2):
            tag = 'n%d' % mt
            P.dma('sp', xt[mt][:], mem_ap[mt * 128:(mt + 1) * 128, :], writes=[tag + 'x'])
            norm_rows(P, k, st, xt[mt][:], gbc[:], hb[mt][:], tag)
            P.trg([(pt[mt][:, c, :], hb[mt][:, c * 128:(c + 1) * 128], k.ident[:]) for c in range(8)],
                  reads=[tag + 'hb'], writes=[tag + 'pt'])
            P.copy('dve', memT[:, :, mt * 128:(mt + 1) * 128], pt[mt][:], reads=[tag + 'pt'], writes=[('memT', mt)])
        for p in range(2):
            P.mmg([(pk[:], wkv[:, dc, p * 128:(p + 1) * 128], memT[:, dc, :], dc == 0, dc == 7) for dc in range(8)],
                  reads=['wkv', ('memT', 0), ('memT', 1)], writes=['pk'])
            P.copy('dve', kmT[:, p, :], pk[:], reads=['pk'], writes=[('kmT', p)])
        for mt in range(2):
            P.mmg([(pk[:], memT[:, dc, mt * 128:(mt + 1) * 128], wkv[:, dc, 256:512], dc == 0, dc == 7) for dc in range(8)],
                  reads=['wkv', ('memT', 0), ('memT', 1)], writes=['pk'])
            P.copy('dve', vm[:, mt, :, 0:64], pk[:].rearrange("p (h d) -> p h d", h=4), reads=['pk', 'vm'], writes=[('vm', mt)])
        P.flush()


def phase_memattn(P, k, hT, w_in_ap, col0, kmT, vm, o_s):
    with ExitStack() as st:
        wqm = P.sb(st, [128, 8, 256], BF16, 'wqm')
        qmT = P.sb(st, [128, 2, S], BF16, 'qmT')
        pq = [P.ps(st, [128, 512], F32, 'pq') for _ in range(2)]
        sc = [P.ps(st, [128, 512], F32, 'sc') for _ in range(2)]
        pacc = [P.ps(st, [128, 512], F32, 'pacc') for _ in range(4)]
        pT = [P.sb(st, [128, 512], BF16, 'pT') for _ in range(2)]
        rr = [P.sb(st, [128, 1], F32, 'rr') for _ in range(4)]
        omb = [P.sb(st, [128, 4, 256], BF16, 'omb') for _ in range(2)]
        P.dma('pool', wqm[:], wview(w_in_ap)[:, :, col0:col0 + 256], writes=['wqm'], cast=True)
        for tb in range(8):
            for p in range(2):
                b = (tb * 2 + p) % 2
                P.mmg([(pq[b][:], wqm[:, dc, p * 128:(p + 1) * 128], hT[:, dc, tb * 512:(tb + 1) * 512], dc == 0, dc == 7) for dc in range(8)],
                      reads=['wqm'], writes=[('pq', b)])
                P.act(qmT[:, p, tb * 512:(tb + 1) * 512], pq[b][:], AF.Identity, reads=[('pq', b)], writes=[('qmT', tb, p)], scale=0.125)
        cnt = 0
        for tb in range(8):
            ob = omb[tb % 2]
            for hm in range(4):
                p = hm // 2
                base = (hm % 2) * 64
                for mt in range(2):
                    b = cnt % 2
                    cnt += 1
                    P.mmg([(sc[b][:], kmT[base:base + 64, p, mt * 128:(mt + 1) * 128], qmT[base:base + 64, p, tb * 512:(tb + 1) * 512], True, True)],
                          reads=[('qmT', tb, p)], writes=[('sc', b)])
                    P.act(pT[b][:], sc[b][:], AF.Exp, reads=[('sc', b)], writes=[('pT', b)])
                    P.mmg([(pacc[qs][:, 0:65], pT[b][:, qs * 128:(qs + 1) * 128], vm[:, mt, hm, :], mt == 0, mt == 1) for qs in range(4)],
                          reads=[('pT', b)], writes=[('pacc', qs) for qs in range(4)])
                for qs in range(4):
                    P.recip(rr[qs][:], pacc[qs][:, 64:65], reads=[('pacc', qs)], writes=[('rr', qs)])
                    P.act(ob[:, qs, hm * 64:(hm + 1) * 64], pacc[qs][:, 0:64], AF.Identity, reads=[('pacc', qs), ('rr', qs)],
                          writes=[('omb', tb % 2, qs)], scale=rr[qs][:, 0:1])
            for qs in range(4):
                tt = tb * 4 + qs
                P.dma('sp', o_s[tt * 128:(tt + 1) * 128, 768:1024], ob[:, qs, :], reads=[('omb', tb % 2, qs)], writes=[('o_s', tt, 'm')])
        P.flush()


def phase_outproj(P, k, o_s, w_out_ap, x_src, x_dst):
    NB = 3
    with ExitStack() as st:
        wo = P.sb(st, [128, 8, D], BF16, 'wo')
        ot = [P.sb(st, [128, D], BF16, 'ot') for _ in range(NB)]
        oT = [P.sb(st, [128, 8, 128], BF16, 'oT') for _ in range(NB)]
        xt = [P.sb(st, [128, D], F32, 'xt') for _ in range(NB)]
        xn = [P.sb(st, [128, D], F32, 'xn') for _ in range(NB)]
        pt = [P.ps(st, [128, 8, 128], BF16, 'pt') for _ in range(2)]
        po = [P.ps(st, [128, 512], F32, 'po') for _ in range(4)]
        P.dma('pool', wo[:], wview(w_out_ap), writes=['wo'], cast=True)

        def stage_a(tt):
            b = tt % NB
            pb2 = tt % 2
            P.dma('sp', ot[b][:], o_s[tt * 128:(tt + 1) * 128, :], writes=[('ot', b)])
            P.dma('pool', xt[b][:], x_src[tt * 128:(tt + 1) * 128, :], writes=[('xt', b)])
            P.trg([(pt[pb2][:, c, :], ot[b][:, c * 128:(c + 1) * 128], k.ident[:]) for c in range(8)],
                  reads=[('ot', b)], writes=[('pt', pb2)])
            P.copy('act', oT[b][:], pt[pb2][:], reads=[('pt', pb2)], writes=[('oT', b)])

        def stage_b(tt):
            b = tt % NB
            pb2 = tt % 2
            for hh in range(2):
                pb = pb2 * 2 + hh
                P.mmg([(po[pb][:], oT[b][:, fc, :], wo[:, fc, hh * 512:(hh + 1) * 512], fc == 0, fc == 7) for fc in range(8)],
                      reads=[('oT', b), 'wo'], writes=[('po', pb)])
                P.tt('dve', xn[b][:, hh * 512:(hh + 1) * 512], po[pb][:], xt[b][:, hh * 512:(hh + 1) * 512], ALU.add,
                     reads=[('po', pb), ('xt', b)], writes=[('xn', b, hh)])
            P.dma('sp', x_dst[tt * 128:(tt + 1) * 128, :], xn[b][:], reads=[('xn', b, 0), ('xn', b, 1)], writes=[('xd', tt)])

        stage_a(0)
        for tt in range(NT):
            if tt + 1 < NT:
                stage_a(tt + 1)
            stage_b(tt)
        P.flush()


def phase_diffattn(P, k, hT, j, lam_init, o_s):
    inp = k.inp
    w_in = wview(inp['a_w_in'][j])
    with ExitStack() as st:
        wqkv = [P.sb(st, [128, 8, 384], BF16, 'wqkv') for _ in range(2)]
        qT = [P.sb(st, [128, 2, S], BF16, 'qT') for _ in range(2)]
        kT = [P.sb(st, [128, 2, S], BF16, 'kT') for _ in range(2)]
        va = [P.sb(st, [128, NT, 129], BF16, 'va') for _ in range(2)]
        dmask = P.sb(st, [128, 6, 128], BF16, 'dmask')
        cbias = P.sb(st, [128, 6 * 35], F32, 'cbias')
        gsub = P.sb(st, [128, 128], F32, 'gsub')
        lv = [P.sb(st, [128, 64], F32, 'lv') for _ in range(4)]
        lt = P.sb(st, [128, 64], F32, 'lt')
        ls = [P.sb(st, [128, 1], F32, 'ls') for _ in range(2)]
        neglam = P.sb(st, [128, 1], F32, 'neglam')
        clam = P.sb(st, [128, 2], F32, 'clam')
        pT = [P.sb(st, [128, 512], BF16, 'pT') for _ in range(3)]
        o0 = P.sb(st, [128, 4, 128], F32, 'o0')
        oo = [P.sb(st, [128, 128], F32, 'oo') for _ in range(2)]
        ob = [P.sb(st, [128, 128], BF16, 'ob') for _ in range(2)]
        junk = P.sb(st, [128, 128], F32, 'junk')
        sm = {n: [P.sb(st, [128, 1], F32, n) for _ in range(2)] for n in ('r0', 'r1', 'ssq', 'ms', 'lnm', 'rstd')}
        pp = [P.ps(st, [128, 512], F32, 'pp') for _ in range(2)]
        sc = [P.ps(st, [128, 512], F32, 'sc') for _ in range(2)]
        pacc = [P.ps(st, [128, 512], F32, 'pacc') for _ in range(4)]

        P.dma('pool', dmask[:], inp['c_dmask'].rearrange("h k q -> k h q"), writes=['dmask'], cast=True)
        P.dma('sp', cbias[:], inp['c_bias'], writes=['cbias'])
        P.dma('sp', gsub[:], inp['a_subln'][j:j + 1, :].partition_broadcast(128), writes=['gsub'])
        P.dma('sp', clam[:], inp['c_lam'][j], writes=['clam'])
        P.ts('dve', gsub[:], gsub[:], clam[:, 0:1], None, ALU.mult, None, reads=['gsub', 'clam'], writes=['gsub'])
        for i, nm in enumerate(['a_lam_q1', 'a_lam_k1', 'a_lam_q2', 'a_lam_k2']):
            P.dma('sp', lv[i][:], inp[nm][j:j + 1, :].partition_broadcast(128), writes=[('lv', i)])
        for i in range(2):
            P.tt('dve', lt[:], lv[2 * i][:], lv[2 * i + 1][:], ALU.mult, reads=[('lv', 2 * i), ('lv', 2 * i + 1)], writes=['lt'])
            P.reduce(ls[i][:], lt[:], ALU.add, reads=['lt'], writes=[('ls', i)])
            P.act(ls[i][:], ls[i][:], AF.Exp, reads=[('ls', i)], writes=[('ls', i)])
        P.tt('dve', neglam[:], ls[1][:], ls[0][:], ALU.subtract, reads=[('ls', 0), ('ls', 1)], writes=['neglam'])
        P.ts('dve', neglam[:], neglam[:], clam[:, 1:2], None, ALU.add, None, reads=['neglam', 'clam'], writes=['neglam'])
        for b in range(2):
            P.memset('pool', va[b][:, :, 128:129], 1.0, writes=[('va1', b)])

        scb = [sc[0], sc[1], pp[1]]
        pT4 = pT + [P.sb(st, [128, 512], BF16, 'pT')]

        def project_units(h):
            b = h % 2
            W = wqkv[b]
            units = []

            def u0():
                for i, c0 in enumerate([h * 128, DA_W + h * 128, 2 * DA_W + h * 128]):
                    P.dma('pool', W[:, :, i * 128:(i + 1) * 128], w_in[:, :, c0:c0 + 128], writes=[('w', b, i)], cast=True)
                for m in range(2):
                    P.dma('pool', qT[b][64:70, m, :], inp['c_qaug'][h], writes=[('qa', b, m)], cast=True)
                    P.dma('pool', kT[b][64:70, m, :], inp['c_kaug'][h], writes=[('ka', b, m)], cast=True)
            units.append(u0)
            for tb in range(8):
                for m in range(2):
                    for isk in range(2):
                        def u(tb=tb, m=m, isk=isk):
                            c0 = isk * 128 + m * 64
                            P.mmg([(pp[0][0:64, :], W[:, dc, c0:c0 + 64], hT[:, dc, tb * 512:(tb + 1) * 512], dc == 0, dc == 7) for dc in range(8)],
                                  reads=[('w', b, isk)], writes=[('pp', 0)])
                            if isk == 0:
                                P.ts('dve', qT[b][0:64, m, tb * 512:(tb + 1) * 512], pp[0][0:64, :], 0.125, None, ALU.mult, None,
                                     reads=[('pp', 0)], writes=[('q', b, m, tb)])
                            else:
                                P.copy('dve', kT[b][0:64, m, tb * 512:(tb + 1) * 512], pp[0][0:64, :], reads=[('pp', 0)], writes=[('k', b, m, tb)])
                        units.append(u)
                for tq in range(4):
                    def uv(tt=tb * 4 + tq):
                        P.mmg([(pp[0][:, 0:128], hT[:, dc, tt * 128:(tt + 1) * 128], W[:, dc, 256:384], dc == 0, dc == 7) for dc in range(8)],
                              reads=[('w', b, 2)], writes=[('pp', 0)])
                        P.copy('dve', va[b][:, tt, 0:128], pp[0][:, 0:128], reads=[('pp', 0), ('va1', b)], writes=[('v', b, tt)])
                    units.append(uv)
            return units

        def attend(h, nxt):
            b = h % 2
            blocks = [(jq, m, kt) for jq in range(8) for m in range(2) for kt in range(4 * jq + 4)]
            nb = len(blocks)
            evc = [0]

            def geom(i):
                jq, m, kt = blocks[i]
                r = kt - 4 * jq
                off = max(r, 0) * 128
                return jq, m, kt, r, off, 512 - off

            def emit_qk(i):
                jq, m, kt, r, off, N = geom(i)
                sb_ = i % 3
                P.mmg([(scb[sb_][:, 0:N], kT[b][0:70, m, kt * 128:(kt + 1) * 128], qT[b][0:70, m, jq * 512 + off:(jq + 1) * 512], True, True)],
                      reads=[('q', b, m, jq), ('qa', b, m), ('ka', b, m), ('k', b, m, kt // 4)], writes=[('sc', sb_)])

            def emit_exp(i):
                jq, m, kt, r, off, N = geom(i)
                sb_ = i % 3
                pb = i % 4
                bi = h * 35 + (4 * jq - kt + 3)
                P.act(pT4[pb][:, off:512], scb[sb_][:, 0:N], AF.Exp, reads=[('sc', sb_), 'cbias'], writes=[('pT', pb)], bias=cbias[:, bi:bi + 1])
                if r >= 0:
                    P.tt('dve', pT4[pb][:, off:off + 128], pT4[pb][:, off:off + 128], dmask[:, h, :], ALU.mult,
                         reads=[('pT', pb), 'dmask'], writes=[('pT', pb)])

            def emit_pv(i):
                jq, m, kt, r, off, N = geom(i)
                pb = i % 4
                qs0 = max(r, 0)
                P.mmg([(pacc[qs][:, 0:129], pT4[pb][:, qs * 128:(qs + 1) * 128], va[b][:, kt, :], kt == 0, kt == 4 * jq + qs) for qs in range(qs0, 4)],
                      reads=[('pT', pb), ('v', b, kt), ('va1', b)], writes=[('pacc', qs) for qs in range(qs0, 4)])
                if kt == 4 * jq + 3:
                    for qs in range(4):
                        e = evc[0] % 2
                        evc[0] += 1
                        if m == 0:
                            P.recip(sm['r0'][e][:], pacc[qs][:, 128:129], reads=[('pacc', qs)], writes=[('r0', e)])
                            P.ts('dve', o0[:, qs, :], pacc[qs][:, 0:128], sm['r0'][e][:, 0:1], None, ALU.mult, None,
                                 reads=[('pacc', qs), ('r0', e)], writes=[('o0', qs)])
                        else:
                            P.recip(sm['r1'][e][:], pacc[qs][:, 128:129], reads=[('pacc', qs)], writes=[('r1', e)])
                            P.tt('dve', sm['r1'][e][:], sm['r1'][e][:], neglam[:], ALU.mult, reads=[('r1', e), 'neglam'], writes=[('r1', e)])
                            P.stt(oo[e][:], pacc[qs][:, 0:128], sm['r1'][e][:, 0:1], o0[:, qs, :], ALU.mult, ALU.add,
                                  reads=[('pacc', qs), ('r1', e), ('o0', qs)], writes=[('oo', e)])
                            P.add('dve', lambda eng, o=junk[:], a=oo[e][:], acc=sm['ssq'][e][:]: eng.scalar_tensor_tensor(
                                out=o, in0=a, scalar=1.0, in1=a, op0=ALU.mult, op1=ALU.mult, accum_out=acc),
                                reads=[('oo', e)], writes=['junk', ('ssq', e)])
                            P.ts('dve', sm['ms'][e][:], sm['ssq'][e][:], 1.0 / 128, EPS, ALU.mult, ALU.add, reads=[('ssq', e)], writes=[('ms', e)])
                            P.tt('pool', sm['rstd'][e][:], sm['ms'][e][:], k.neghalf[:], ALU.pow, reads=[('ms', e)], writes=[('rstd', e)])
                            P.stt(ob[e][:], oo[e][:], sm['rstd'][e][:, 0:1], gsub[:], ALU.mult, ALU.mult,
                                  reads=[('oo', e), ('rstd', e), 'gsub'], writes=[('ob', e)])
                            tt = jq * 4 + qs
                            P.dma('sp', o_s[tt * 128:(tt + 1) * 128, h * 128:(h + 1) * 128], ob[e][:], reads=[('ob', e)], writes=[('o_s', tt, h)])

            emit_qk(0)
            emit_qk(1)
            for i in range(nb):
                emit_exp(i)
                if i + 2 < nb:
                    emit_qk(i + 2)
                emit_pv(i)
                if i % 4 == 3 and nxt:
                    nxt.pop(0)()
            while nxt:
                nxt.pop(0)()

        for u in project_units(0):
            u()
        for h in range(6):
            attend(h, project_units(h + 1) if h + 1 < 6 else [])
        P.flush()


def phase_hgrn(P, k, hT, j, o_s):
    inp = k.inp
    w_in = wview(inp['b_w_in'][j])
    with ExitStack() as st:
        W = [P.sb(st, [128, 8, 512], BF16, 'W') for _ in range(2)]
        qeT = [P.sb(st, [128, S], BF16, 'qeT') for _ in range(2)]
        keT = [P.sb(st, [128, S], BF16, 'keT') for _ in range(2)]
        ketok = [P.sb(st, [128, NT, 128], BF16, 'ketok') for _ in range(2)]
        vtok = [P.sb(st, [128, NT, 128], BF16, 'vtok') for _ in range(2)]
        sgtok = [P.sb(st, [128, NT, 128], BF16, 'sgtok') for _ in range(2)]
        decay = [P.sb(st, [128, 64], F32, 'decay') for _ in range(2)]
        oml = P.sb(st, [128, 6], F32, 'oml')
        gn = P.sb(st, [128, 128], F32, 'gn')
        cmask = P.sb(st, [128, 128], BF16, 'cmask')
        smask = P.sb(st, [128, 512], F32, 'smask')
        tmp = {n: [P.sb(st, [128, 512], F32, n) for _ in range(2)] for n in ('et', 'dt', 'kk', 'gt', 'bt', 'eb', 'enb', 'qraw')}
        Sst = P.sb(st, [128, 128], F32, 'Sst')
        Sbf = P.sb(st, [128, 128], BF16, 'Sbf')
        at = [P.sb(st, [128, 128], BF16, 'at') for _ in range(2)]
        o1 = [P.sb(st, [128, 128], F32, 'o1') for _ in range(2)]
        ob = [P.sb(st, [128, 128], BF16, 'ob') for _ in range(2)]
        junk = P.sb(st, [128, 128], F32, 'junk')
        sm = {n: [P.sb(st, [128, 1], F32, n) for _ in range(2)] for n in ('ssq', 'ms', 'rstd')}
        pq = P.ps(st, [128, 512], F32, 'pq')
        ptr = P.ps(st, [128, 4, 128], BF16, 'ptr')
        pvg = P.ps(st, [128, 128], F32, 'pvg')
        pat = P.ps(st, [128, 128], F32, 'pat')
        pkv2 = [P.ps(st, [128, 128], F32, 'pkv') for _ in range(2)]
        po = [P.ps(st, [128, 128], F32, 'po') for _ in range(2)]

        P.dma('pool', cmask[:], inp['c_cmask'], writes=['cmask'], cast=True)
        P.dma('sp', smask[:], inp['c_smask'], writes=['smask'])
        P.dma('sp', gn[:], inp['b_out_norm'][j:j + 1, :].partition_broadcast(128), writes=['gn'])
        lbf = P.sb(st, [128, 1], F32, 'lbf')
        lbl2 = P.sb(st, [2, 768], F32, 'lbl2')
        lbT = P.sb(st, [128, 6, 2], F32, 'lbT')
        P.dma('sp', lbf[:], inp['c_lbflag'][j], writes=['lbf'])
        P.dma('sp', lbl2[:], inp['b_lb_logits'], writes=['lbl2'])
        P.trg([(pvg[:, 2 * h:2 * h + 2], lbl2[0:2, h * 128:(h + 1) * 128], k.identf[0:2, 0:2]) for h in range(6)], reads=['lbl2'], writes=['pvg'])
        P.copy('dve', lbT[:], pvg[:, 0:12].rearrange("p (h l) -> p h l", l=2), reads=['pvg'], writes=['lbl'])
        P.tt('dve', oml[:], lbT[:, :, 0], lbT[:, :, 1], ALU.subtract, reads=['lbl'], writes=['oml'])
        P.act(oml[:], oml[:], AF.Exp, reads=['oml'], writes=['oml'])
        P.ts('dve', oml[:], oml[:], 1.0, None, ALU.add, None, reads=['oml'], writes=['oml'])
        P.recip(oml[:], oml[:], reads=['oml'], writes=['oml'])
        P.ts('dve', oml[:], oml[:], lbf[:, 0:1], 1.0, ALU.mult, ALU.add, reads=['oml', 'lbf'], writes=['oml'])

        def project_units(h):
            b = h % 2
            units = []

            def u0():
                for i, c0 in enumerate([h * 128, HG_W + h * 128, 2 * HG_W + h * 128, 3 * HG_W + h * 128]):
                    P.dma('pool', W[b][:, :, i * 128:(i + 1) * 128], w_in[:, :, c0:c0 + 128], writes=[('w', b, i)], cast=True)
            units.append(u0)
            for tb in range(8):
                def ua(tb=tb):
                    blk = slice(tb * 512, (tb + 1) * 512)
                    x = tb % 2
                    et, dt_, kk, gt, bt, eb, enb, qraw = (tmp[n][x] for n in ('et', 'dt', 'kk', 'gt', 'bt', 'eb', 'enb', 'qraw'))
                    T = lambda n: (n, x)
                    P.mmg([(pq[:], W[b][:, dc, 0:128], hT[:, dc, blk], dc == 0, dc == 7) for dc in range(8)], reads=[('w', b, 0)], writes=['pq'])
                    P.copy('act', qraw[:], pq[:], reads=['pq'], writes=[T('qraw')])
                    P.mmg([(pq[:], W[b][:, dc, 128:256], hT[:, dc, blk], dc == 0, dc == 7) for dc in range(8)], reads=[('w', b, 1)], writes=['pq'])
                    P.act(et[:], pq[:], AF.Exp, reads=['pq'], writes=[T('et')], scale=-1.0)
                    P.ts('dve', dt_[:], et[:], 1.0, None, ALU.add, None, reads=[T('et')], writes=[T('dt')])
                    P.recip(dt_[:], dt_[:], reads=[T('dt')], writes=[T('dt')])
                    P.stt(kk[:], et[:], oml[:, h:h + 1], dt_[:], ALU.mult, ALU.mult, reads=[T('et'), T('dt'), 'oml'], writes=[T('kk')])
                    P.act(gt[:], kk[:], AF.Ln, reads=[T('kk')], writes=[T('gt')], scale=-1.0, bias=1.0)
                    P.scan(bt[:], smask[:], gt[:], reads=['smask', T('gt')], writes=[T('bt')])
                    P.act(eb[:], bt[:], AF.Exp, reads=[T('bt')], writes=[T('eb')])
                    P.act(enb[:], bt[:], AF.Exp, reads=[T('bt')], writes=[T('enb')], scale=-1.0)
                    P.tt('dve', qeT[b][:, blk], qraw[:], eb[:], ALU.mult, reads=[T('qraw'), T('eb')], writes=[('qe', b, tb)])
                    P.tt('dve', keT[b][:, blk], kk[:], enb[:], ALU.mult, reads=[T('kk'), T('enb')], writes=[('ke', b, tb)])
                    P.copy('act', decay[b][:, tb * 8:(tb + 1) * 8], eb[:].rearrange("p (c t) -> p c t", t=64)[:, :, 63], reads=[T('eb')], writes=[('dec', b, tb)])
                    P.trg([(ptr[:, i, :], keT[b][:, tb * 512 + i * 128: tb * 512 + (i + 1) * 128], k.ident[:]) for i in range(4)],
                          reads=[('ke', b, tb)], writes=['ptr'])
                    P.copy('act', ketok[b][:, tb * 4:(tb + 1) * 4, :], ptr[:], reads=['ptr'], writes=[('ketok', b, tb)])
                units.append(ua)
                for tq in range(4):
                    def uv(tt=tb * 4 + tq):
                        tok = slice(tt * 128, (tt + 1) * 128)
                        P.mmg([(pvg[:], hT[:, dc, tok], W[b][:, dc, 256:384], dc == 0, dc == 7) for dc in range(8)], reads=[('w', b, 2)], writes=['pvg'])
                        P.copy('dve', vtok[b][:, tt, :], pvg[:], reads=['pvg'], writes=[('vtok', b, tt)])
                        P.mmg([(pvg[:], hT[:, dc, tok], W[b][:, dc, 384:512], dc == 0, dc == 7) for dc in range(8)], reads=[('w', b, 3)], writes=['pvg'])
                        P.act(sgtok[b][:, tt, :], pvg[:], AF.Silu, reads=['pvg'], writes=[('sgtok', b, tt)])
                    units.append(uv)
            return units

        def recur(h, nxt):
            b = h % 2
            P.memset('pool', Sst[:], 0.0, writes=['Sst'])
            P.memset('pool', Sbf[:], 0.0, writes=['Sbf'])
            for tt in range(NT):
                e = tt % 2
                tb = tt // 4
                tok = slice(tt * 128, (tt + 1) * 128)
                P.mmg([(pat[:], keT[b][:, tok], qeT[b][:, tok], True, True)], reads=[('ke', b, tb), ('qe', b, tb)], writes=['pat'])
                P.tt('dve', at[e][:], pat[:], cmask[:], ALU.mult, reads=['pat', 'cmask'], writes=[('at', e)])
                P.mmg([(pkv2[ci][:], ketok[b][ci * 64:(ci + 1) * 64, tt, :], vtok[b][ci * 64:(ci + 1) * 64, tt, :], True, True) for ci in range(2)],
                      reads=[('ketok', b, tb), ('vtok', b, tt)], writes=['pkv'])
                P.mmg([(po[e][:], at[e][:], vtok[b][:, tt, :], True, False),
                       (po[e][0:64, :], qeT[b][:, tt * 128:tt * 128 + 64], Sbf[:], False, False)],
                      reads=[('at', e), ('vtok', b, tt), ('qe', b, tb), 'Sbf'], writes=[('po', e)])
                for ci in range(2):
                    c = 2 * tt + ci
                    P.tt('dve', Sst[:], pkv2[ci][:], Sst[:], ALU.add, reads=['pkv', 'Sst'], writes=['Sst'])
                    P.ts('dve', Sst[:], Sst[:], decay[b][:, c:c + 1], None, ALU.mult, None, reads=['Sst', ('dec', b, tb)], writes=['Sst'])
                    P.copy('act', Sbf[:], Sst[:], reads=['Sst'], writes=['Sbf'])
                    if ci == 0:
                        P.mmg([(po[e][64:128, :], qeT[b][:, tt * 128 + 64:(tt + 1) * 128], Sbf[:], False, True)],
                              reads=[('qe', b, tb), 'Sbf'], writes=[('po', e)])
                P.act(junk[:], po[e][:], AF.Square, reads=[('po', e)], writes=['junk', ('ssq', e)], accum=sm['ssq'][e][:])
                P.ts('dve', sm['ms'][e][:], sm['ssq'][e][:], 1.0 / 128, EPS, ALU.mult, ALU.add, reads=[('ssq', e)], writes=[('ms', e)])
                P.tt('pool', sm['rstd'][e][:], sm['ms'][e][:], k.neghalf[:], ALU.pow, reads=[('ms', e)], writes=[('rstd', e)])
                P.stt(o1[e][:], po[e][:], sm['rstd'][e][:, 0:1], gn[:], ALU.mult, ALU.mult, reads=[('po', e), ('rstd', e), 'gn'], writes=[('o1', e)])
                P.tt('dve', ob[e][:], o1[e][:], sgtok[b][:, tt, :], ALU.mult, reads=[('o1', e), ('sgtok', b, tt)], writes=[('ob', e)])
                P.dma('sp', o_s[tt * 128:(tt + 1) * 128, h * 128:(h + 1) * 128], ob[e][:], reads=[('ob', e)], writes=[('o_s', tt, h)])
                for _ in range(2):
                    if nxt:
                        nxt.pop(0)()
            while nxt:
                nxt.pop(0)()

        for u in project_units(0):
            u()
        for h in range(6):
            recur(h, project_units(h + 1) if h + 1 < 6 else [])
        P.flush()


def phase_ffn(P, k, x_src, xacc_src, x_dst, gain_row, w_gu_list, w_dn_list, dff, router_ap=None, esel_ap=None):
    HT = S // 2
    NTH = NT // 2
    ngrp = (dff + 511) // 512
    moe = router_ap is not None
    same = xacc_src is x_src
    nexp = len(w_gu_list)
    for half in range(2):
        with ExitStack() as st0:
            xacc = P.sb(st0, [128, NTH, D], F32, 'xacc')
            hTh = P.sb(st0, [128, 8, HT], BF16, 'hTh')
            csel = P.sb(st0, [128, NTH], F32, 'csel')
            comb = P.sb(st0, [128, NTH, 8], F32, 'comb')
            with ExitStack() as st:
                gbc = P.sb(st, [128, D], F32, 'gbc')
                hb = [P.sb(st, [128, D], BF16, 'hb') for _ in range(2)]
                pt = [P.ps(st, [128, 8, 128], BF16, 'pt') for _ in range(2)]
                alloc_norm_tmps(P, k, st, ['n0', 'n1'])
                P.dma('sp', gbc[:], gain_row.partition_broadcast(128), writes=['gbc'])
                if moe:
                    xp = [P.sb(st, [128, D], F32, 'xp') for _ in range(2)]
                    hf2 = [P.sb(st, [128, D], F32, 'hf') for _ in range(2)]
                    hTf = P.sb(st, [128, 8, 128], F32, 'hTf')
                    wr = P.sb(st, [128, 8, 8], F32, 'wr')
                    esel = P.sb(st, [128, 8], F32, 'esel')
                    ptf = [P.ps(st, [128, 4, 128], F32, 'ptf') for _ in range(2)]
                    plg = P.ps(st, [128, 8], F32, 'plg')
                    lg = P.sb(st, [128, 8], F32, 'lg')
                    lg2 = P.sb(st, [128, 8], F32, 'lg2')
                    eq1 = P.sb(st, [128, 8], F32, 'eq1')
                    eq2 = P.sb(st, [128, 8], F32, 'eq2')
                    m1 = P.sb(st, [128, 1], F32, 'm1')
                    m2 = P.sb(st, [128, 1], F32, 'm2')
                    w1 = P.sb(st, [128, 1], F32, 'w1')
                    w2 = P.sb(st, [128, 1], F32, 'w2')
                    P.dma('sp', wr[:], wview(router_ap), writes=['wr'])
                    if esel_ap is not None:
                        P.dma('sp', esel[:], esel_ap, writes=['esel'])
                for tl in range(NTH):
                    tt = half * NTH + tl
                    b = tl % 2
                    tag = 'n%d' % b
                    rows = slice(tt * 128, (tt + 1) * 128)
                    if moe:
                        hf = hf2[b]
                        if same:
                            P.dma('sp', xacc[:, tl, :], x_src[rows, :], writes=[tag + 'x', ('xacc', tl)])
                            norm_rows(P, k, st, xacc[:, tl, :], gbc[:], hb[b][:], tag, want_f32=hf[:])
                        else:
                            P.dma('sp', xacc[:, tl, :], xacc_src[rows, :], writes=[('xacc', tl)])
                            P.dma('sp', xp[b][:], x_src[rows, :], writes=[tag + 'x'])
                            norm_rows(P, k, st, xp[b][:], gbc[:], hb[b][:], tag, want_f32=hf[:])
                        for q4 in range(2):
                            P.trg([(ptf[q4][:, c, :], hf[:, (q4 * 4 + c) * 128:(q4 * 4 + c + 1) * 128], k.identf[:]) for c in range(4)],
                                  reads=[tag + 'hf'], writes=[('ptf', q4)])
                            P.copy('dve', hTf[:, q4 * 4:(q4 + 1) * 4, :], ptf[q4][:], reads=[('ptf', q4)], writes=[('hTf', q4)])
                        P.mmg([(plg[:], hTf[:, dc, :], wr[:, dc, :], dc == 0, dc == 7) for dc in range(8)],
                              reads=[('hTf', 0), ('hTf', 1), 'wr'], writes=['plg'])
                        P.copy('dve', lg[:], plg[:], reads=['plg'], writes=['lg'])
                        P.reduce(m1[:], lg[:], ALU.max, reads=['lg'], writes=['m1'])
                        P.ts('dve', eq1[:], lg[:], m1[:, 0:1], None, ALU.is_equal, None, reads=['lg', 'm1'], writes=['eq1'])
                        P.stt(lg2[:], eq1[:], -1e30, lg[:], ALU.mult, ALU.add, reads=['eq1', 'lg'], writes=['lg2'])
                        P.reduce(m2[:], lg2[:], ALU.max, reads=['lg2'], writes=['m2'])
                        P.ts('dve', eq2[:], lg2[:], m2[:, 0:1], None, ALU.is_equal, None, reads=['lg2', 'm2'], writes=['eq2'])
                        P.tt('dve', w2[:], m2[:], m1[:], ALU.subtract, reads=['m1', 'm2'], writes=['w2'])
                        P.act(w2[:], w2[:], AF.Exp, reads=['w2'], writes=['w2'])
                        P.ts('dve', w1[:], w2[:], 1.0, None, ALU.add, None, reads=['w2'], writes=['w1'])
                        P.recip(w1[:], w1[:], reads=['w1'], writes=['w1'])
                        P.tt('dve', w2[:], w2[:], w1[:], ALU.mult, reads=['w1', 'w2'], writes=['w2'])
                        P.ts('dve', eq1[:], eq1[:], w1[:, 0:1], None, ALU.mult, None, reads=['eq1', 'w1'], writes=['eq1'])
                        if esel_ap is None:
                            P.stt(comb[:, tl, :], eq2[:], w2[:, 0:1], eq1[:], ALU.mult, ALU.add, reads=['eq2', 'w2', 'eq1'], writes=[('comb', tl)])
                        else:
                            P.stt(eq2[:], eq2[:], w2[:, 0:1], eq1[:], ALU.mult, ALU.add, reads=['eq2', 'w2', 'eq1'], writes=['eq2'])
                            P.tt('dve', eq2[:], eq2[:], esel[:], ALU.mult, reads=['eq2', 'esel'], writes=['eq2'])
                            P.reduce(csel[:, tl:tl + 1], eq2[:], ALU.add, reads=['eq2'], writes=[('csel', tl)])
                    else:
                        P.dma('sp', xacc[:, tl, :], x_src[rows, :], writes=[tag + 'x', ('xacc', tl)])
                        norm_rows(P, k, st, xacc[:, tl, :], gbc[:], hb[b][:], tag)
                    P.trg([(pt[b][:, c, :], hb[b][:, c * 128:(c + 1) * 128], k.ident[:]) for c in range(8)],
                          reads=[tag + 'hb'], writes=[tag + 'pt'])
                    P.copy('act' if tl % 2 else 'dve', hTh[:, :, tl * 128:(tl + 1) * 128], pt[b][:], reads=[tag + 'pt'], writes=[('hT', tl)])
                P.flush()
            with ExitStack() as st:
                wg = [P.sb(st, [128, 8, 512], BF16, 'wg') for _ in range(2)]
                wu = [P.sb(st, [128, 8, 512], BF16, 'wu') for _ in range(2)]
                wd = [P.sb(st, [128, 4, D], BF16, 'wd') for _ in range(2)]
                sg = [P.sb(st, [128, 512], F32, 'sg') for _ in range(2)]
                aT = [P.sb(st, [128, 4, 512], BF16, 'aT') for _ in range(2)]
                pg = [P.ps(st, [128, 512], F32, 'pg') for _ in range(2)]
                pu = [P.ps(st, [128, 512], F32, 'pu') for _ in range(2)]
                po = [P.ps(st, [128, 512], F32, 'po') for _ in range(4)]
                it = 0
                gi = 0
                oi = 0
                ai = 0
                for e in range(nexp):
                    gu = wview(w_gu_list[e])
                    w_dn_ap = w_dn_list[e]
                    for fg in range(ngrp):
                        F = min(512, dff - fg * 512)
                        nfc = F // 128
                        wb = it % 2
                        it += 1
                        P.dma('pool', wg[wb][:, :, 0:F], gu[:, :, fg * 512:fg * 512 + F], writes=[('wg', wb)], cast=True)
                        P.dma('pool', wu[wb][:, :, 0:F], gu[:, :, dff + fg * 512:dff + fg * 512 + F], writes=[('wu', wb)], cast=True)
                        P.dma('pool', wd[wb][:, 0:nfc, :], wview(w_dn_ap[fg * 512:fg * 512 + F, :]), writes=[('wd', wb)], cast=True)
                        for tb in range(HT // 512):
                            blk = slice(tb * 512, (tb + 1) * 512)
                            ab = ai % 2
                            ai += 1
                            for fc in range(nfc):
                                g = gi % 2
                                gi += 1
                                P.mmg([(pg[g][:], wg[wb][:, dc, fc * 128:(fc + 1) * 128], hTh[:, dc, blk], dc == 0, dc == 7) for dc in range(8)],
                                      reads=[('wg', wb)], writes=[('pg', g)])
                                P.mmg([(pu[g][:], wu[wb][:, dc, fc * 128:(fc + 1) * 128], hTh[:, dc, blk], dc == 0, dc == 7) for dc in range(8)],
                                      reads=[('wu', wb)], writes=[('pu', g)])
                                P.act(sg[g][:], pg[g][:], AF.Silu, reads=[('pg', g)], writes=[('sg', g)])
                                P.tt('dve', aT[ab][:, fc, :], pu[g][:], sg[g][:], ALU.mult, reads=[('pu', g), ('sg', g)], writes=[('aT', ab, fc)])
                            for tq in range(4):
                                tl = tb * 4 + tq
                                for hh in range(2):
                                    o = oi % 4
                                    oi += 1
                                    P.mmg([(po[o][:], aT[ab][:, fc, tq * 128:(tq + 1) * 128], wd[wb][:, fc, hh * 512:(hh + 1) * 512], fc == 0, fc == nfc - 1) for fc in range(nfc)],
                                          reads=[('aT', ab, fc) for fc in range(nfc)] + [('wd', wb)], writes=[('po', o)])
                                    sc_ = (comb[:, tl, e:e + 1] if esel_ap is None else csel[:, tl:tl + 1]) if moe else 1.0
                                    P.stt(xacc[:, tl, hh * 512:(hh + 1) * 512], po[o][:], sc_, xacc[:, tl, hh * 512:(hh + 1) * 512], ALU.mult, ALU.add,
                                          reads=[('po', o), ('xacc', tl, hh)], writes=[('xacc', tl, hh)])
                for tl in range(NTH):
                    tt = half * NTH + tl
                    P.dma('sp', x_dst[tt * 128:(tt + 1) * 128, :], xacc[:, tl, :], reads=[('xacc', tl, 0), ('xacc', tl, 1)], writes=[('xd', tt)])
                P.flush()


def phase_moe_sparse(P, k, x_src, x_dst, gain_row, w_gu_list, w_dn_list, router_ap, hbk):
    I32 = mybir.dt.int32
    NQ = 4
    NTQ = NT // NQ
    dff = DFF_E
    ngrp = dff // 512
    import os
    thr = float(os.environ.get('K_MOE_THR', '96'))
    for qt in range(NQ):
        with ExitStack() as st0:
            xacc = P.sb(st0, [128, NTQ, D], F32, 'xacc')
            comb = P.sb(st0, [128, NTQ * 8], F32, 'comb')
            maskf = P.sb(st0, [128, NTQ * 8], F32, 'maskf')
            pos = P.sb(st0, [128, NTQ * 8], F32, 'pos')
            flag = P.sb(st0, [128, 1], I32, 'flag')
            posA = P.sb(st0, [128, NTQ * 8], F32, 'posA')
            iota = P.sb(st0, [128, 128], F32, 'iota')
            with ExitStack() as st:
                gbc = P.sb(st, [128, D], F32, 'gbc')
                hb = [P.sb(st, [128, D], BF16, 'hb') for _ in range(2)]
                hf2 = [P.sb(st, [128, D], F32, 'hf') for _ in range(2)]
                alloc_norm_tmps(P, k, st, ['n0', 'n1'])
                hTf = P.sb(st, [128, 8, 128], F32, 'hTf')
                wr = P.sb(st, [128, 8, 8], F32, 'wr')
                ltri = P.sb(st, [128, 128], F32, 'ltri')
                ones = P.sb(st, [128, 128], F32, 'ones')
                cnt = P.sb(st, [128, NTQ * 8], F32, 'cnt')
                mx = P.sb(st, [128, 1], F32, 'mx')
                ptf = [P.ps(st, [128, 4, 128], F32, 'ptf') for _ in range(2)]
                plg = P.ps(st, [128, 8], F32, 'plg')
                ppos = P.ps(st, [128, NTQ * 8], F32, 'ppos')
                pcnt = P.ps(st, [128, NTQ * 8], F32, 'pcnt')
                lg = P.sb(st, [128, 8], F32, 'lg')
                lg2 = P.sb(st, [128, 8], F32, 'lg2')
                eq1 = P.sb(st, [128, 8], F32, 'eq1')
                eq2 = P.sb(st, [128, 8], F32, 'eq2')
                m1 = P.sb(st, [128, 1], F32, 'm1')
                m2 = P.sb(st, [128, 1], F32, 'm2')
                w1 = P.sb(st, [128, 1], F32, 'w1')
                w2 = P.sb(st, [128, 1], F32, 'w2')
                P.dma('sp', gbc[:], gain_row.partition_broadcast(128), writes=['gbc'])
                P.dma('sp', wr[:], wview(router_ap), writes=['wr'])
                P.dma('sp', ltri[:], k.inp['c_ltri'], writes=['ltri'])
                P.dma('sp', iota[:], k.inp['c_iota'], writes=['iota'])
                P.memset('pool', ones[:], 1.0, writes=['ones'])
                for tl in range(NTQ):
                    tt = qt * NTQ + tl
                    b = tl % 2
                    tag = 'n%d' % b
                    rows = slice(tt * 128, (tt + 1) * 128)
                    hf = hf2[b]
                    P.dma('sp', xacc[:, tl, :], x_src[rows, :], writes=[tag + 'x', ('xacc', tl)])
                    norm_rows(P, k, st, xacc[:, tl, :], gbc[:], hb[b][:], tag, want_f32=hf[:])
                    P.dma('sp', hbk[rows, :], hb[b][:], reads=[tag + 'hb'], writes=[('hbk', tt)])
                    for q4 in range(2):
                        P.trg([(ptf[q4][:, c, :], hf[:, (q4 * 4 + c) * 128:(q4 * 4 + c + 1) * 128], k.identf[:]) for c in range(4)],
                              reads=[tag + 'hf'], writes=[('ptf', q4)])
                        P.copy('dve', hTf[:, q4 * 4:(q4 + 1) * 4, :], ptf[q4][:], reads=[('ptf', q4)], writes=[('hTf', q4)])
                    P.mmg([(plg[:], hTf[:, dc, :], wr[:, dc, :], dc == 0, dc == 7) for dc in range(8)],
                          reads=[('hTf', 0), ('hTf', 1), 'wr'], writes=['plg'])
                    P.copy('dve', lg[:], plg[:], reads=['plg'], writes=['lg'])
                    P.reduce(m1[:], lg[:], ALU.max, reads=['lg'], writes=['m1'])
                    P.ts('dve', eq1[:], lg[:], m1[:, 0:1], None, ALU.is_equal, None, reads=['lg', 'm1'], writes=['eq1'])
                    P.stt(lg2[:], eq1[:], -1e30, lg[:], ALU.mult, ALU.add, reads=['eq1', 'lg'], writes=['lg2'])
                    P.reduce(m2[:], lg2[:], ALU.max, reads=['lg2'], writes=['m2'])
                    P.ts('dve', eq2[:], lg2[:], m2[:, 0:1], None, ALU.is_equal, None, reads=['lg2', 'm2'], writes=['eq2'])
                    P.tt('dve', w2[:], m2[:], m1[:], ALU.subtract, reads=['m1', 'm2'], writes=['w2'])
                    P.act(w2[:], w2[:], AF.Exp, reads=['w2'], writes=['w2'])
                    P.ts('dve', w1[:], w2[:], 1.0, None, ALU.add, None, reads=['w2'], writes=['w1'])
                    P.recip(w1[:], w1[:], reads=['w1'], writes=['w1'])
                    P.tt('dve', w2[:], w2[:], w1[:], ALU.mult, reads=['w1', 'w2'], writes=['w2'])
                    P.tt('dve', maskf[:, tl * 8:(tl + 1) * 8], eq1[:], eq2[:], ALU.add, reads=['eq1', 'eq2'], writes=[('mask', tl)])
                    P.ts('dve', eq1[:], eq1[:], w1[:, 0:1], None, ALU.mult, None, reads=['eq1', 'w1'], writes=['eq1'])
                    P.stt(comb[:, tl * 8:(tl + 1) * 8], eq2[:], w2[:, 0:1], eq1[:], ALU.mult, ALU.add, reads=['eq2', 'w2', 'eq1'], writes=[('comb', tl)])
                allm = [('mask', tl) for tl in range(NTQ)]
                P.mmg([(ppos[:], ltri[:], maskf[:], True, True)], reads=allm + ['ltri'], writes=['ppos'])
                P.mmg([(pcnt[:], ones[:], maskf[:], True, True)], reads=allm + ['ones'], writes=['pcnt'])
                P.copy('dve', pos[:], ppos[:], reads=['ppos'], writes=['pos'])
                P.copy('dve', cnt[:], pcnt[:], reads=['pcnt'], writes=['cnt'])
                cntw = P.sb(st, [128, NTQ * 4], F32, 'cntw')
                c4 = cnt[:].rearrange("p (w t e) -> p w t e", t=2, e=8)
                p4 = pos[:].rearrange("p (w t e) -> p w t e", t=2, e=8)
                pa4 = posA[:].rearrange("p (w t e) -> p w t e", t=2, e=8)
                P.tt('dve', cntw[:].rearrange("p (w e) -> p w e", e=8), c4[:, :, 0, :], c4[:, :, 1, :], ALU.add, reads=['cnt'], writes=['cntw'])
                P.copy('dve', pa4[:, :, 0, :], p4[:, :, 0, :], reads=['pos'], writes=['posA0'])
                P.tt('dve', pa4[:, :, 1, :], p4[:, :, 1, :], c4[:, :, 0, :], ALU.add, reads=['pos', 'cnt'], writes=['posA1'])
                P.reduce(mx[:], cntw[:], ALU.max, reads=['cntw'], writes=['mx'])
                P.ts('dve', flag[:], mx[:], thr, None, ALU.is_gt, None, reads=['mx'], writes=['flag'])
                P.flush()
            with ExitStack() as st:
                wg = [P.sb(st, [128, 8, 512], BF16, 'wg') for _ in range(2)]
                wu = [P.sb(st, [128, 8, 512], BF16, 'wu') for _ in range(2)]
                wd = [P.sb(st, [128, 4, D], BF16, 'wd') for _ in range(2)]
                sg = [P.sb(st, [128, 512], F32, 'sg') for _ in range(2)]
                aT = [P.sb(st, [128, 4, 512], BF16, 'aT') for _ in range(2)]
                hbt = [P.sb(st, [128, D], BF16, 'hbt') for _ in range(4)]
                sel2 = [P.sb(st, [128, NTQ, 128], BF16, 'sel') for _ in range(2)]
                selT2 = [P.sb(st, [128, NTQ, 128], BF16, 'selT') for _ in range(2)]
                hTg2 = [P.sb(st, [128, 8, NTQ * 128], BF16, 'hTg') for _ in range(2)]
                yacc = P.sb(st, [128, NTQ, D], F32, 'yacc')
                ybf = [P.sb(st, [128, D], BF16, 'ybf') for _ in range(2)]
                pg = [P.ps(st, [128, 512], F32, 'pg') for _ in range(2)]
                pu = [P.ps(st, [128, 512], F32, 'pu') for _ in range(2)]
                po = [P.ps(st, [128, 512], F32, 'po') for _ in range(3)]
                pts = P.ps(st, [128, NTQ, 128], BF16, 'pts')

                def body(cap, win):
                    NW = NTQ // win
                    NS = NW * cap
                    BS = min(512, NS)
                    nsb = NS // BS
                    upb = BS // cap
                    posX = posA if win == 2 else pos
                    defer = (2 * NW <= NTQ)
                    ybase = (lambda e: (e % 2) * NW) if defer else (lambda e: 0)
                    ctr = {'it': 0, 'gi': 0, 'oi': 0, 'ai': 0, 'hb': 0, 'yb': 0}

                    def pre_units(e):
                        x = e % 2
                        sel, selT, hTg = sel2[x], selT2[x], hTg2[x]
                        units = []

                        def usel():
                            for tl in range(NTQ):
                                col = tl * 8 + e
                                P.ts('dve', sel[:, tl, 0:cap], iota[:, 0:cap], posX[:, col:col + 1], maskf[:, col:col + 1], ALU.is_equal, ALU.mult,
                                     reads=[], writes=[('sel', x, tl)])
                            P.trg([(pts[0:cap, tl, :], sel[:, tl, 0:cap], k.ident[:]) for tl in range(NTQ)],
                                  reads=[('sel', x, tl) for tl in range(NTQ)], writes=['pts'])
                            P.copy('act', selT[0:cap, :, :], pts[0:cap, :, :], reads=['pts'], writes=[('selT', x)])
                        units.append(usel)
                        for w in range(NW):
                            def ug(w=w):
                                tls = list(range(w * win, (w + 1) * win))
                                hbs = []
                                for tl in tls:
                                    tt = qt * NTQ + tl
                                    hbi = ctr['hb'] % 4
                                    ctr['hb'] += 1
                                    hbs.append(hbi)
                                    P.dma('sp', hbt[hbi][:], hbk[tt * 128:(tt + 1) * 128, :], writes=[('hbt', hbi)])
                                for g0 in range(0, 8, 4):
                                    o = ctr['oi'] % 3
                                    ctr['oi'] += 1
                                    pv = po[o][:, 0:4 * cap].rearrange("p (c s) -> p c s", s=cap)
                                    P.mmg([(pv[:, dc - g0, :], hbt[hbs[i]][:, dc * 128:(dc + 1) * 128], sel[:, tls[i], 0:cap], i == 0, i == win - 1)
                                           for dc in range(g0, g0 + 4) for i in range(win)],
                                          reads=[('hbt', h_) for h_ in hbs] + [('sel', x, tl) for tl in tls], writes=[('po', o)])
                                    P.copy('act' if (w % 2) else 'dve', hTg[:, g0:g0 + 4, w * cap:(w + 1) * cap], pv, reads=[('po', o)], writes=[('hTg', x, w, g0)])
                            units.append(ug)
                        return units

                    def scatter_units(e):
                        x = e % 2
                        selT = selT2[x]
                        yb0 = ybase(e)
                        units = []
                        for u_ in range(NW):
                            def us(u_=u_):
                                yb = ctr['yb'] % 2
                                ctr['yb'] += 1
                                P.copy('act', ybf[yb][0:cap, :], yacc[0:cap, yb0 + u_, :], reads=[('yacc', yb0 + u_, 0), ('yacc', yb0 + u_, 1)], writes=[('ybf', yb)])
                                for tl in range(u_ * win, (u_ + 1) * win):
                                    col = tl * 8 + e
                                    for hh in range(2):
                                        o = ctr['oi'] % 3
                                        ctr['oi'] += 1
                                        P.mmg([(po[o][:], selT[0:cap, tl, :], ybf[yb][0:cap, hh * 512:(hh + 1) * 512], True, True)],
                                              reads=[('selT', x), ('ybf', yb)], writes=[('po', o)])
                                        P.stt(xacc[:, tl, hh * 512:(hh + 1) * 512], po[o][:], comb[:, col:col + 1], xacc[:, tl, hh * 512:(hh + 1) * 512], ALU.mult, ALU.add,
                                              reads=[('po', o), ('xacc', tl, hh)], writes=[('xacc', tl, hh)])
                            units.append(us)
                        return units

                    pend = []
                    for u in pre_units(0):
                        u()
                    for e in range(NEXP):
                        gu = wview(w_gu_list[e])
                        w_dn_ap = w_dn_list[e]
                        x = e % 2
                        selT, hTg = selT2[x], hTg2[x]
                        nxt = pre_units(e + 1) if e + 1 < NEXP else []
                        allg = [('hTg', x, w, g0) for w in range(NW) for g0 in (0, 4)]
                        yb0 = ybase(e)
                        for fg in range(ngrp):
                            wb = ctr['it'] % 2
                            ctr['it'] += 1
                            P.dma('pool', wg[wb][:], gu[:, :, fg * 512:(fg + 1) * 512], writes=[('wg', wb)], cast=True)
                            P.dma('pool', wu[wb][:], gu[:, :, dff + fg * 512:dff + (fg + 1) * 512], writes=[('wu', wb)], cast=True)
                            P.dma('pool', wd[wb][:], wview(w_dn_ap[fg * 512:(fg + 1) * 512, :]), writes=[('wd', wb)], cast=True)
                            for sb_ in range(nsb):
                                blk = slice(sb_ * BS, (sb_ + 1) * BS)
                                ab = ctr['ai'] % 2
                                ctr['ai'] += 1
                                for fc in range(4):
                                    g = ctr['gi'] % 2
                                    ctr['gi'] += 1
                                    P.mmg([(pg[g][:, 0:BS], wg[wb][:, dc, fc * 128:(fc + 1) * 128], hTg[:, dc, blk], dc == 0, dc == 7) for dc in range(8)],
                                          reads=[('wg', wb)] + allg, writes=[('pg', g)])
                                    P.mmg([(pu[g][:, 0:BS], wu[wb][:, dc, fc * 128:(fc + 1) * 128], hTg[:, dc, blk], dc == 0, dc == 7) for dc in range(8)],
                                          reads=[('wu', wb)] + allg, writes=[('pu', g)])
                                    P.act(sg[g][:, 0:BS], pg[g][:, 0:BS], AF.Silu, reads=[('pg', g)], writes=[('sg', g)])
                                    P.tt('dve', aT[ab][:, fc, 0:BS], pu[g][:, 0:BS], sg[g][:, 0:BS], ALU.mult, reads=[('pu', g), ('sg', g)], writes=[('aT', ab, fc)])
                                for j in range(upb):
                                    u_ = yb0 + sb_ * upb + j
                                    for hh in range(2):
                                        o = ctr['oi'] % 3
                                        ctr['oi'] += 1
                                        P.mmg([(po[o][0:cap, :], aT[ab][:, fc, j * cap:(j + 1) * cap], wd[wb][:, fc, hh * 512:(hh + 1) * 512], fc == 0, fc == 3) for fc in range(4)],
                                              reads=[('aT', ab, fc) for fc in range(4)] + [('wd', wb)], writes=[('po', o)])
                                        ydst = yacc[0:cap, u_, hh * 512:(hh + 1) * 512]
                                        if fg == 0:
                                            P.copy('dve', ydst, po[o][0:cap, :], reads=[('po', o)], writes=[('yacc', u_, hh)])
                                        else:
                                            P.tt('dve', ydst, po[o][0:cap, :], ydst, ALU.add, reads=[('po', o), ('yacc', u_, hh)], writes=[('yacc', u_, hh)])
                            while pend:
                                pend.pop(0)()
                            for _ in range(2):
                                if nxt:
                                    nxt.pop(0)()
                        while nxt:
                            nxt.pop(0)()
                        pend = scatter_units(e)
                        if not defer or e == NEXP - 1:
                            while pend:
                                pend.pop(0)()
                    for tl in range(NTQ):
                        tt = qt * NTQ + tl
                        P.dma('sp', x_dst[tt * 128:(tt + 1) * 128, :], xacc[:, tl, :], reads=[('xacc', tl, 0), ('xacc', tl, 1)], writes=[('xd', tt)])

                P.flush_branch(flag[0:1, 0:1], lambda: body(96, 2), lambda: body(128, 1))


def phase_fnorm(P, k, x_src, gain_row, out_ap):
    with ExitStack() as st:
        gbc = P.sb(st, [128, D], F32, 'gbc')
        xt = [P.sb(st, [128, D], F32, 'xt') for _ in range(2)]
        yo = [P.sb(st, [128, D], F32, 'yo') for _ in range(2)]
        alloc_norm_tmps(P, k, st, ['n0', 'n1'])
        P.dma('sp', gbc[:], gain_row.partition_broadcast(128), writes=['gbc'])
        for tt in range(NT):
            b = tt % 2
            tag = 'n%d' % b
            t = k.nt[tag]
            P.dma('sp', xt[b][:], x_src[tt * 128:(tt + 1) * 128, :], writes=[tag + 'x'])
            P.act(t['junk'][:], xt[b][:], AF.Square, reads=[tag + 'x'], writes=[tag + 'junk', tag + 'ssq'], accum=t['ssq'][:])
            P.ts('dve', t['ms'][:], t['ssq'][:], 1.0 / D, EPS, ALU.mult, ALU.add, reads=[tag + 'ssq'], writes=[tag + 'ms'])
            P.tt('pool', t['rstd'][:], t['ms'][:], k.neghalf[:], ALU.pow, reads=[tag + 'ms'], writes=[tag + 'rstd'])
            P.stt(yo[b][:], xt[b][:], t['rstd'][:, 0:1], gbc[:], ALU.mult, ALU.mult, reads=[tag + 'x', tag + 'rstd', 'gbc'], writes=[('yo', b)])
            P.dma('sp', out_ap[tt * 128:(tt + 1) * 128, :], yo[b][:], reads=[('yo', b)], writes=[('out', tt)])
        P.flush()


CONST_SHAPES = {"c_ident": [128, 128], "c_kaug": [6, 6, S], "c_qaug": [6, 6, S], "c_dmask": [6, 128, 128], "c_bias": [128, 6 * 35],
                "c_cmask": [128, 128], "c_smask": [128, 512], "c_ltri": [128, 128], "c_iota": [128, 128]}
STEP_INPUTS = {
    'attn': {"x": [S, D], "mem": [MEM_LEN, D], "a_norm_mix": [1, D], "a_w_in": [1, D, 2560], "a_lam_q1": [1, 64], "a_lam_k1": [1, 64],
             "a_lam_q2": [1, 64], "a_lam_k2": [1, 64], "a_subln": [1, 128], "a_mem_norm": [1, D], "a_w_mem_kv": [1, D, 512],
             "a_w_out": [1, D, D], "c_lam": [1, 128, 2], "c_ident": 0, "c_kaug": 0, "c_qaug": 0, "c_dmask": 0, "c_bias": 0},
    'hgrn': {"x": [S, D], "mem": [MEM_LEN, D], "b_norm_mix": [1, D], "b_w_in": [1, D, 3328], "b_lb_logits": [2, 768], "b_out_norm": [1, 128],
             "b_mem_norm": [1, D], "b_w_mem_kv": [1, D, 512], "b_w_out": [1, D, D], "c_lbflag": [1, 128, 1], "c_ident": 0, "c_cmask": 0, "c_smask": 0},
    'dense': {"x": [S, D], "norm": [1, D], "w_gu": [D, 2 * DFF_D], "w_dn": [DFF_D, D], "c_ident": 0},
    'moe1': {"x": [S, D], "xacc": [S, D], "norm": [1, D], "router": [D, 8], "w_gu": [D, 2 * DFF_E], "w_dn": [DFF_E, D], "esel": [128, 8], "c_ident": 0},
    'fnorm': {"x": [S, D], "gain": [1, D], "c_ident": 0},
    'moes': {"x": [S, D], "norm": [1, D], "router": [D, 8], "w_gu": [8, D, 2 * DFF_E], "w_dn": [8, DFF_E, D], "c_ident": 0, "c_ltri": 0, "c_iota": 0},
}


def build_step(kind):
    nc = bass.Bass("TRN2", target_bir_lowering=False)
    inp = {}
    for n, shp in STEP_INPUTS[kind].items():
        if shp == 0:
            shp = CONST_SHAPES[n]
        inp[n] = nc.dram_tensor(n, shp, F32, kind="ExternalInput").ap()
    out = nc.dram_tensor("out", [S, D], F32, kind="ExternalOutput").ap()
    o_s = nc.dram_tensor("o_s", [S, D], BF16, kind="Internal").ap()
    with ExitStack() as st:
        P = Prog(nc, st)
        k = K()
        k.inp = inp
        k.ident = P.sb(st, [128, 128], BF16, 'ident')
        k.identf = P.sb(st, [128, 128], F32, 'identf')
        k.neghalf = P.sb(st, [128, 1], F32, 'neghalf')
        P.dma('pool', k.ident[:], inp['c_ident'], writes=['ident'], cast=True)
        P.dma('sp', k.identf[:], inp['c_ident'], writes=['identf'])
        P.memset('pool', k.neghalf[:], -0.5, writes=['neghalf'])
        P.flush()
        if kind in ('attn', 'hgrn'):
            pre = 'a_' if kind == 'attn' else 'b_'
            with ExitStack() as stm:
                hT = P.sb(stm, [128, 8, S], BF16, 'hT')
                kmT = P.sb(stm, [128, 2, MEM_LEN], BF16, 'kmT')
                vm = P.sb(stm, [128, 2, 4, 65], BF16, 'vm')
                phase_hT(P, k, inp['x'], inp[pre + 'norm_mix'][0:1, :], hT)
                phase_memkv(P, k, inp['mem'], inp[pre + 'mem_norm'][0:1, :], inp[pre + 'w_mem_kv'][0], kmT, vm)
                if kind == 'attn':
                    phase_diffattn(P, k, hT, 0, None, o_s)
                    phase_memattn(P, k, hT, inp['a_w_in'][0], 3 * DA_W, kmT, vm, o_s)
                else:
                    phase_hgrn(P, k, hT, 0, o_s)
                    phase_memattn(P, k, hT, inp['b_w_in'][0], 4 * HG_W, kmT, vm, o_s)
            phase_outproj(P, k, o_s, inp[pre + 'w_out'][0], inp['x'], out)
        elif kind == 'dense':
            phase_ffn(P, k, inp['x'], inp['x'], out, inp['norm'][0:1, :], [inp['w_gu']], [inp['w_dn']], DFF_D)
        elif kind == 'moe1':
            phase_ffn(P, k, inp['x'], inp['xacc'], out, inp['norm'][0:1, :], [inp['w_gu']], [inp['w_dn']], DFF_E,
                      router_ap=inp['router'], esel_ap=inp['esel'])
        elif kind == 'fnorm':
            phase_fnorm(P, k, inp['x'], inp['gain'][0:1, :], out)
        elif kind == 'moes':
            hbk = nc.dram_tensor("hbk", [S, D], BF16, kind="Internal").ap()
            phase_moe_sparse(P, k, inp['x'], out, inp['norm'][0:1, :], [inp['w_gu'][e] for e in range(NEXP)], [inp['w_dn'][e] for e in range(NEXP)], inp['router'], hbk)
        P.add('sp', lambda e: e.nop(), reads=[], writes=[])
        P.flush()
    return nc


def make_consts():
    slopes = 2.0 ** (-8.0 * np.arange(1, 7) / 6.0)
    import ml_dtypes
    bf = ml_dtypes.bfloat16

    def hi_lo(v):
        hi = np.float32(np.float32(v).astype(bf).astype(np.float32))
        lo = np.float32(np.float32(v - hi).astype(bf).astype(np.float32))
        return hi, lo
    c = {}
    c['c_ident'] = np.eye(128, dtype=np.float32)
    pos = np.arange(S)
    krel = (pos % 128).astype(np.float32)
    qrel = pos % 512
    qhi = (qrel & ~3).astype(np.float32)
    qlo = (qrel & 3).astype(np.float32)
    kaug = np.zeros((6, 6, S), np.float32)
    qaug = np.zeros((6, 6, S), np.float32)
    for h in range(6):
        hi, lo = hi_lo(slopes[h])
        kaug[h, 0] = krel
        kaug[h, 1] = krel
        kaug[h, 2] = hi
        kaug[h, 3] = lo
        kaug[h, 4] = hi
        kaug[h, 5] = lo
        qaug[h, 0] = hi
        qaug[h, 1] = lo
        qaug[h, 2] = -qhi
        qaug[h, 3] = -qhi
        qaug[h, 4] = -qlo
        qaug[h, 5] = -qlo
    c['c_kaug'] = kaug
    c['c_qaug'] = qaug
    kk = np.arange(128)[:, None]
    qq = np.arange(128)[None, :]
    dm = np.zeros((6, 128, 128), np.float32)
    for h in range(6):
        allowed = (kk // 64) <= (qq // 64)
        val = np.where(kk <= qq, 1.0, np.exp(-2.0 * slopes[h] * (kk - qq)))
        dm[h] = np.where(allowed, val, 0.0)
    c['c_dmask'] = dm
    cb = np.zeros((128, 6 * 35), np.float32)
    for h in range(6):
        for idx in range(35):
            d = idx - 3
            cb[:, h * 35 + idx] = -slopes[h] * 128.0 * d
    c['c_bias'] = cb
    s_ = np.arange(128)[:, None]
    t_ = np.arange(128)[None, :]
    c['c_cmask'] = ((s_ <= t_) & ((s_ // 64) == (t_ // 64))).astype(np.float32)
    sm = np.ones((128, 512), np.float32)
    sm[:, ::64] = 0.0
    c['c_smask'] = sm
    c['c_ltri'] = (s_ < t_).astype(np.float32)
    c['c_iota'] = np.broadcast_to(np.arange(128, dtype=np.float32)[None, :], (128, 128)).copy()
    return c


_CACHE = {}
_CONSTS = {}


def launch(kind, per_core, shared, n_cores):
    if kind not in _CACHE:
        _CACHE[kind] = build_step(kind)
    if not _CONSTS:
        _CONSTS.update(make_consts())
    nc = _CACHE[kind]
    sh = {}
    for n, shp in STEP_INPUTS[kind].items():
        if n in per_core:
            continue
        if shp == 0:
            sh[n] = _CONSTS[n]
        else:
            sh[n] = np.ascontiguousarray(np.asarray(shared[n], dtype=np.float32)).reshape(shp)
    in_maps = []
    for c in range(n_cores):
        m = dict(sh)
        for n, a in per_core.items():
            m[n] = np.ascontiguousarray(a[c])
        in_maps.append(m)
    import os
    tr = bool(os.environ.get('K_TRACE'))
    res = run_bass_kernel_spmd(nc, in_maps, core_ids=list(range(n_cores)), trace=tr)
    if tr:
        print('EXEC_NS', kind, res.exec_time_ns)
    return np.stack([np.asarray(r['out'], dtype=np.float32).reshape(S, D) for r in res.results], axis=0)


def run_step(i, which, x, inputs, n_cores):
    f = lambda n: np.asarray(inputs[n], dtype=np.float32)
    j = i // 2
    mem = f('mem')[:n_cores]
    if which == 'mix' and i % 2 == 0:
        lam_init = 0.8 - 0.6 * float(np.exp(-0.3 * i))
        clam = np.zeros((1, 128, 2), np.float32)
        clam[0, :, 0] = 1.0 - lam_init
        clam[0, :, 1] = -lam_init
        sh = {n: f(n)[j:j + 1] for n in ['a_norm_mix', 'a_w_in', 'a_lam_q1', 'a_lam_k1', 'a_lam_q2', 'a_lam_k2', 'a_subln', 'a_mem_norm', 'a_w_mem_kv', 'a_w_out']}
        sh['c_lam'] = clam
        return launch('attn', {'x': x, 'mem': mem}, sh, n_cores)
    if which == 'mix':
        sh = {n: f(n)[j:j + 1] for n in ['b_norm_mix', 'b_w_in', 'b_out_norm', 'b_mem_norm', 'b_w_mem_kv', 'b_w_out']}
        sh['b_lb_logits'] = f('b_lb_logits')
        sh['c_lbflag'] = np.full((1, 128, 1), -float(j), np.float32)
        return launch('hgrn', {'x': x, 'mem': mem}, sh, n_cores)
    if i % 2 == 0:
        sh = {'norm': f('dense_norm')[j:j + 1], 'w_gu': f('dense_w_gate_up')[j], 'w_dn': f('dense_w_down')[j]}
        return launch('dense', {'x': x}, sh, n_cores)
    xacc = x
    for e in range(NEXP):
        esel = np.zeros((128, 8), np.float32)
        esel[:, e] = 1.0
        sh = {'norm': f('moe_norm')[j:j + 1], 'router': f('moe_router')[j], 'w_gu': f('moe_w_gate_up')[j, e], 'w_dn': f('moe_w_down')[j, e], 'esel': esel}
        xacc = launch('moe1', {'x': x, 'xacc': xacc}, sh, n_cores)
    return xacc


FULL_SHAPES = {
    "x": [S, D], "mem": [MEM_LEN, D],
    "a_norm_mix": [2, D], "a_w_in": [2, D, 2560], "a_lam_q1": [2, 64], "a_lam_k1": [2, 64], "a_lam_q2": [2, 64], "a_lam_k2": [2, 64],
    "a_subln": [2, 128], "a_mem_norm": [2, D], "a_w_mem_kv": [2, D, 512], "a_w_out": [2, D, D],
    "b_norm_mix": [2, D], "b_w_in": [2, D, 3328], "b_lb_logits": [2, 768], "b_out_norm": [2, 128], "b_mem_norm": [2, D],
    "b_w_mem_kv": [2, D, 512], "b_w_out": [2, D, D],
    "dense_norm": [2, D], "dense_w_gate_up": [2, D, 2 * DFF_D], "dense_w_down": [2, DFF_D, D],
    "moe_norm": [2, D], "moe_router": [2, D, 8], "moe_w_gate_up": [2, 8, D, 2 * DFF_E], "moe_w_down": [2, 8, DFF_E, D],
    "final_norm": [1, D], "c_lam": [2, 128, 2], "c_lbflag": [2, 128, 1],
}
FULL_SHAPES.update(CONST_SHAPES)


def build_fused():
    nc = bass.Bass("TRN2", target_bir_lowering=False)
    inp = {n: nc.dram_tensor(n, shp, F32, kind="ExternalInput").ap() for n, shp in FULL_SHAPES.items()}
    out = nc.dram_tensor("out", [S, D], F32, kind="ExternalOutput").ap()
    xres = nc.dram_tensor("xres", [S, D], F32, kind="Internal").ap()
    o_s = nc.dram_tensor("o_s", [S, D], BF16, kind="Internal").ap()
    hbk = nc.dram_tensor("hbk", [S, D], BF16, kind="Internal").ap()
    with ExitStack() as st:
        P = Prog(nc, st)
        k = K()
        k.inp = inp
        k.ident = P.sb(st, [128, 128], BF16, 'ident')
        k.identf = P.sb(st, [128, 128], F32, 'identf')
        k.neghalf = P.sb(st, [128, 1], F32, 'neghalf')
        P.dma('pool', k.ident[:], inp['c_ident'], writes=['ident'], cast=True)
        P.dma('sp', k.identf[:], inp['c_ident'], writes=['identf'])
        P.memset('pool', k.neghalf[:], -0.5, writes=['neghalf'])
        P.flush()
        x_cur = inp['x']
        for i in range(DEPTH):
            j = i // 2
            pre = 'a_' if i % 2 == 0 else 'b_'
            with ExitStack() as stm:
                hT = P.sb(stm, [128, 8, S], BF16, 'hT')
                kmT = P.sb(stm, [128, 2, MEM_LEN], BF16, 'kmT')
                vm = P.sb(stm, [128, 2, 4, 65], BF16, 'vm')
                phase_hT(P, k, x_cur, inp[pre + 'norm_mix'][j:j + 1, :], hT)
                phase_memkv(P, k, inp['mem'], inp[pre + 'mem_norm'][j:j + 1, :], inp[pre + 'w_mem_kv'][j], kmT, vm)
                if i % 2 == 0:
                    phase_diffattn(P, k, hT, j, None, o_s)
                    phase_memattn(P, k, hT, inp['a_w_in'][j], 3 * DA_W, kmT, vm, o_s)
                else:
                    phase_hgrn(P, k, hT, j, o_s)
                    phase_memattn(P, k, hT, inp['b_w_in'][j], 4 * HG_W, kmT, vm, o_s)
            phase_outproj(P, k, o_s, inp[pre + 'w_out'][j], x_cur, xres)
            x_cur = xres
            if i % 2 == 0:
                phase_ffn(P, k, xres, xres, xres, inp['dense_norm'][j:j + 1, :], [inp['dense_w_gate_up'][j]], [inp['dense_w_down'][j]], DFF_D)
            else:
                phase_moe_sparse(P, k, xres, xres, inp['moe_norm'][j:j + 1, :],
                                 [inp['moe_w_gate_up'][j, e] for e in range(NEXP)], [inp['moe_w_down'][j, e] for e in range(NEXP)],
                                 inp['moe_router'][j], hbk)
        phase_fnorm(P, k, xres, inp['final_norm'][0:1, :], out)
        P.add('sp', lambda e: e.nop(), reads=[], writes=[])
        P.flush()
    return nc


def fused_consts():
    c = dict(make_consts())
    clam = np.zeros((2, 128, 2), np.float32)
    for j in range(2):
        lam_init = 0.8 - 0.6 * float(np.exp(-0.3 * (2 * j)))
        clam[j, :, 0] = 1.0 - lam_init
        clam[j, :, 1] = -lam_init
    c['c_lam'] = clam
    lbf = np.zeros((2, 128, 1), np.float32)
    lbf[1] = -1.0
    c['c_lbflag'] = lbf
    return c


def kernel_unfused(**inputs):
    n_cores = 8
    x = np.asarray(inputs['x'], dtype=np.float32)
    for i in range(DEPTH):
        x = run_step(i, 'mix', x, inputs, n_cores)
        x = run_step(i, 'ffn', x, inputs, n_cores)
    return launch('fnorm', {'x': x}, {'gain': np.asarray(inputs['final_norm'], dtype=np.float32).reshape(1, D)}, n_cores)


def kernel(**inputs):
    n_cores = 8
    if 'fused' not in _CACHE:
        _CACHE['fused'] = build_fused()
    nc = _CACHE['fused']
    consts = fused_consts()
    shared = {}
    for n, shp in FULL_SHAPES.items():
        if n in ('x', 'mem'):
            continue
        if n in consts:
            shared[n] = consts[n]
        else:
            shared[n] = np.ascontiguousarray(np.asarray(inputs[n], dtype=np.float32)).reshape(shp)
    x = np.asarray(inputs['x'], dtype=np.float32)
    mem = np.asarray(inputs['mem'], dtype=np.float32)
    in_maps = []
    for c in range(n_cores):
        m = dict(shared)
        m['x'] = np.ascontiguousarray(x[c])
        m['mem'] = np.ascontiguousarray(mem[c])
        in_maps.append(m)
    res = run_bass_kernel_spmd(nc, in_maps, core_ids=list(range(n_cores)))
    return np.stack([np.asarray(r['out'], dtype=np.float32).reshape(S, D) for r in res.results], axis=0)
```
